# Optimizing a Trainium2 kernel written in Bass

```python
import jax, jax.numpy as jnp
from jax import lax
import numpy as np

D_MODEL = 1024
BATCH = 8
SEQ = 4096
DEPTH = 1

CHUNK = 64
PLE_DIM = 256
D_FF = 2816
MLSTM_HEADS = 4
MLSTM_DQK = 64
MLSTM_DV = 128
CONV_WIDTH = 4
FOX_HEADS = 8
FOX_DH = 64
Q_BLOCK = 128
EPS = 1e-6

MLSTM_QK = MLSTM_HEADS * MLSTM_DQK
MLSTM_V = MLSTM_HEADS * MLSTM_DV
FOX_W = FOX_HEADS * FOX_DH
IN_SPLITS = (2 * MLSTM_QK, MLSTM_V, MLSTM_V, MLSTM_HEADS, MLSTM_HEADS,
             FOX_W, FOX_W, FOX_W, FOX_HEADS, D_MODEL, D_MODEL)
IN_WIDTH = sum(IN_SPLITS)
IN_OFFSETS = tuple(sum(IN_SPLITS[:i + 1]) for i in range(len(IN_SPLITS) - 1))

kernel_name = "hybrid_mlstm_fox_macaron_block"


def rms_norm(x, g):
    xf = x.astype(jnp.float32)
    y = xf * lax.rsqrt(jnp.mean(xf * xf, axis=-1, keepdims=True) + EPS)
    return (y * g.astype(jnp.float32)).astype(x.dtype)


def swiglu(x, w_gate, w_up, w_down):
    return (jax.nn.silu(x @ w_gate) * (x @ w_up)) @ w_down


def causal_conv(x, w, b):
    k_w = w.shape[0]
    s = x.shape[1]
    xp = jnp.pad(x, ((0, 0), (k_w - 1, 0), (0, 0)))
    return b + sum(w[j] * xp[:, j:j + s] for j in range(k_w))


def mlstm_chunkwise(q, k, v, i_pre, f_pre):
    b_, s_, n_h, d_k = q.shape
    d_v = v.shape[-1]
    nc = s_ // CHUNK

    def to_chunks(t):
        t = t.astype(jnp.float32).reshape((b_, nc, CHUNK) + t.shape[2:])
        return jnp.moveaxis(t, (1, 3), (0, 2))

    qc, kc, vc = to_chunks(q), to_chunks(k), to_chunks(v)
    ic = to_chunks(i_pre)
    fc = jax.nn.log_sigmoid(to_chunks(f_pre))
    tri = jnp.tril(jnp.ones((CHUNK, CHUNK), dtype=bool))

    def step(carry, xs):
        c_st, n_st, m_st = carry
        qq, kk, vv, ig, lf = xs
        bcum = jnp.cumsum(lf, axis=-1)
        log_d = bcum[..., :, None] - bcum[..., None, :] + ig[..., None, :]
        log_d = jnp.where(tri, log_d, -jnp.inf)
        log_inter = bcum + m_st[..., None]
        m_t = jnp.maximum(log_inter, jnp.max(log_d, axis=-1))
        dmat = jnp.exp(log_d - m_t[..., None])
        inter = jnp.exp(log_inter - m_t)
        sc = jnp.einsum('bhtd,bhsd->bhts', qq, kk) * dmat
        num = jnp.einsum('bhts,bhsv->bhtv', sc, vv) + inter[..., None] * jnp.einsum('bhtd,bhdv->bhtv', qq, c_st)
        den = jnp.sum(sc, axis=-1) + inter * jnp.einsum('bhtd,bhd->bht', qq, n_st)
        h = num / jnp.maximum(jnp.abs(den), jnp.exp(-m_t))[..., None]
        b_last = bcum[..., -1]
        log_w = b_last[..., None] - bcum + ig
        m_new = jnp.maximum(b_last + m_st, jnp.max(log_w, axis=-1))
        w = jnp.exp(log_w - m_new[..., None])
        decay = jnp.exp(b_last + m_st - m_new)
        c_new = decay[..., None, None] * c_st + jnp.einsum('bhs,bhsd,bhsv->bhdv', w, kk, vv)
        n_new = decay[..., None] * n_st + jnp.einsum('bhs,bhsd->bhd', w, kk)
        return (c_new, n_new, m_new), h

    init = (jnp.zeros((b_, n_h, d_k, d_v), jnp.float32),
            jnp.zeros((b_, n_h, d_k), jnp.float32),
            jnp.zeros((b_, n_h), jnp.float32))
    _, hs = lax.scan(step, init, (qc, kc, vc, ic, fc))
    return jnp.moveaxis(hs, (0, 2), (1, 3)).reshape(b_, s_, n_h, d_v)


def forgetting_attention(q, k, v, f_pre):
    b_, s_, n_h, d_h = q.shape
    nb = s_ // Q_BLOCK
    qf = q.astype(jnp.float32) * (d_h ** -0.5)
    kf = k.astype(jnp.float32)
    vf = v.astype(jnp.float32)
    c = jnp.cumsum(jax.nn.log_sigmoid(f_pre.astype(jnp.float32)), axis=1)
    c_t = jnp.transpose(c, (0, 2, 1))
    qb = jnp.moveaxis(qf.reshape(b_, nb, Q_BLOCK, n_h, d_h), 1, 0)
    cb = jnp.moveaxis(c_t.reshape(b_, n_h, nb, Q_BLOCK), 2, 0)
    qpos = jnp.arange(s_, dtype=jnp.int32).reshape(nb, Q_BLOCK)
    kpos = jnp.arange(s_, dtype=jnp.int32)

    def block(args):
        qblk, cblk, pq = args
        logits = jnp.einsum('bqhd,bkhd->bhqk', qblk, kf)
        logits = logits + cblk[..., None] - c_t[:, :, None, :]
        logits = jnp.where(pq[:, None] >= kpos[None, :], logits, -jnp.inf)
        probs = jax.nn.softmax(logits, axis=-1)
        return jnp.einsum('bhqk,bkhd->bqhd', probs, vf)

    out = lax.map(block, (qb, cb, qpos))
    return jnp.moveaxis(out, 0, 1).reshape(b_, s_, n_h * d_h)


def head_layer_norm(h, g):
    mu = jnp.mean(h, axis=-1, keepdims=True)
    var = jnp.mean(jnp.square(h - mu), axis=-1, keepdims=True)
    hn = (h - mu) * lax.rsqrt(var + EPS)
    return hn.reshape(h.shape[0], h.shape[1], -1) * g.astype(jnp.float32)


def setup_inputs(seed: int = 0) -> dict:
    key = jax.random.key(seed)
    ks = jax.random.split(key, 40)
    f32 = jnp.float32

    def dense(k, fan_in, fan_out):
        return jax.random.normal(k, (DEPTH, fan_in, fan_out), f32) * fan_in ** -0.5

    def gain(k, n):
        return 1.0 + 0.05 * jax.random.normal(k, (DEPTH, n), f32)

    def small(k, shape, s=0.02):
        return s * jax.random.normal(k, shape, f32)

    return {
        "x": jax.random.normal(ks[0], (BATCH, SEQ, D_MODEL), f32),
        "p": jax.random.normal(ks[1], (DEPTH, BATCH, SEQ, PLE_DIM), f32),
        "ffn1_pre_g": gain(ks[2], D_MODEL),
        "ffn1_w_gate": dense(ks[3], D_MODEL, D_FF),
        "ffn1_w_up": dense(ks[4], D_MODEL, D_FF),
        "ffn1_w_down": dense(ks[5], D_FF, D_MODEL),
        "ffn1_post_g": gain(ks[6], D_MODEL),
        "mix_pre_g": gain(ks[7], D_MODEL),
        "w_in": dense(ks[8], D_MODEL, IN_WIDTH),
        "conv_w": 0.5 * jax.random.normal(ks[9], (DEPTH, CONV_WIDTH, 2 * MLSTM_QK), f32),
        "conv_b": small(ks[10], (DEPTH, 2 * MLSTM_QK)),
        "mlstm_i_bias": small(ks[11], (DEPTH, MLSTM_HEADS), 0.1),
        "mlstm_f_bias": jnp.linspace(3.0, 6.0, MLSTM_HEADS, dtype=f32)[None, :] + small(ks[12], (DEPTH, MLSTM_HEADS), 0.1),
        "mlstm_norm_g": gain(ks[13], MLSTM_V),
        "fox_f_bias": jnp.linspace(1.0, 6.0, FOX_HEADS, dtype=f32)[None, :] + small(ks[14], (DEPTH, FOX_HEADS), 0.1),
        "branch_gate_bias": small(ks[15], (DEPTH, 2 * D_MODEL)),
        "w_branch_a": dense(ks[16], MLSTM_V, D_MODEL),
        "w_branch_b": dense(ks[17], FOX_W, D_MODEL),
        "w_out": dense(ks[18], D_MODEL, D_MODEL),
        "mix_post_g": gain(ks[19], D_MODEL),
        "ffn2_pre_g": gain(ks[20], D_MODEL),
        "ffn2_w_gate": dense(ks[21], D_MODEL, D_FF),
        "ffn2_w_up": dense(ks[22], D_MODEL, D_FF),
        "ffn2_w_down": dense(ks[23], D_FF, D_MODEL),
        "ffn2_post_g": gain(ks[24], D_MODEL),
        "ple_pre_g": gain(ks[25], D_MODEL),
        "ple_w_gate": dense(ks[26], D_MODEL, D_MODEL),
        "ple_b_gate": small(ks[27], (DEPTH, D_MODEL)),
        "ple_w_proj": dense(ks[28], PLE_DIM, D_MODEL),
        "ple_post_g": gain(ks[29], D_MODEL),
    }


def reference(x, p, ffn1_pre_g, ffn1_w_gate, ffn1_w_up, ffn1_w_down, ffn1_post_g,
              mix_pre_g, w_in, conv_w, conv_b, mlstm_i_bias, mlstm_f_bias, mlstm_norm_g,
              fox_f_bias, branch_gate_bias, w_branch_a, w_branch_b, w_out, mix_post_g,
              ffn2_pre_g, ffn2_w_gate, ffn2_w_up, ffn2_w_down, ffn2_post_g,
              ple_pre_g, ple_w_gate, ple_b_gate, ple_w_proj, ple_post_g):
    b_, s_, _ = x.shape
    h = x
    for layer in range(DEPTH):
        y = swiglu(rms_norm(h, ffn1_pre_g[layer]), ffn1_w_gate[layer], ffn1_w_up[layer], ffn1_w_down[layer])
        h = h + 0.5 * rms_norm(y, ffn1_post_g[layer])

        u = rms_norm(h, mix_pre_g[layer])
        z = u @ w_in[layer]
        (m_qk, m_v, m_o, m_i, m_f, f_q, f_k, f_v, f_f, g_a, g_b) = jnp.split(z, IN_OFFSETS, axis=-1)

        m_qk = jax.nn.silu(causal_conv(m_qk, conv_w[layer], conv_b[layer]))
        m_q, m_k = jnp.split(m_qk, 2, axis=-1)
        m_q = m_q.reshape(b_, s_, MLSTM_HEADS, MLSTM_DQK)
        m_k = m_k.reshape(b_, s_, MLSTM_HEADS, MLSTM_DQK) * (MLSTM_DQK ** -0.5)
        m_v = m_v.reshape(b_, s_, MLSTM_HEADS, MLSTM_DV)
        hm = mlstm_chunkwise(m_q, m_k, m_v, m_i + mlstm_i_bias[layer], m_f + mlstm_f_bias[layer])
        y_a = (jax.nn.sigmoid(m_o.astype(jnp.float32)) * head_layer_norm(hm, mlstm_norm_g[layer])).astype(x.dtype)

        y_b = forgetting_attention(f_q.reshape(b_, s_, FOX_HEADS, FOX_DH),
                                   f_k.reshape(b_, s_, FOX_HEADS, FOX_DH),
                                   f_v.reshape(b_, s_, FOX_HEADS, FOX_DH),
                                   f_f + fox_f_bias[layer]).astype(x.dtype)

        gate_a = jax.nn.sigmoid(g_a + branch_gate_bias[layer, :D_MODEL])
        gate_b = jax.nn.sigmoid(g_b + branch_gate_bias[layer, D_MODEL:])
        merged = gate_a * (y_a @ w_branch_a[layer]) + gate_b * (y_b @ w_branch_b[layer])
        h = h + rms_norm(merged @ w_out[layer], mix_post_g[layer])

        y = swiglu(rms_norm(h, ffn2_pre_g[layer]), ffn2_w_gate[layer], ffn2_w_up[layer], ffn2_w_down[layer])
        h = h + 0.5 * rms_norm(y, ffn2_post_g[layer])

        gate = jax.nn.sigmoid(rms_norm(h, ple_pre_g[layer]) @ ple_w_gate[layer] + ple_b_gate[layer])
        emb = p[layer] @ ple_w_proj[layer]
        h = h + rms_norm(gate * emb, ple_post_g[layer])
    return h
```

```python
import contextlib
import numpy as np
import concourse.bass as bass
import concourse.mybir as mybir
from concourse.bass_utils import run_bass_kernel_spmd

F32 = mybir.dt.float32
BF16 = mybir.dt.bfloat16
AF = mybir.ActivationFunctionType
ALU = mybir.AluOpType
AX = mybir.AxisListType

S = 4096
D = 1024
DFF = 2816
NFC = DFF // 128
NT = 8
TT = 512
EPS = 1e-6
INW = 5136

ENGS = ("pe", "act", "dve", "pool", "sp")


class Buf:
    __slots__ = ("name", "w", "r")

    def __init__(self, name=""):
        self.name = name
        self.w = None
        self.r = []


class Op:
    __slots__ = ("eng", "fn", "deps", "inc", "cnt", "dma", "sem", "emitted")

    def __init__(self, eng, fn, dma=False):
        self.eng = eng
        self.fn = fn
        self.deps = []
        self.inc = False
        self.cnt = 0
        self.dma = dma
        self.sem = None
        self.emitted = False


class Prog:
    def __init__(self, nc, st, n_dma_sems=20):
        self.nc = nc
        self.pending = {e: [] for e in ENGS}
        self.bufs = []
        self.nd = n_dma_sems
        self.esem = {e: st.enter_context(nc.semaphore("s_" + e)) for e in ENGS}
        self.dsem = {}
        for e in ("sp", "pool"):
            for s in range(n_dma_sems):
                self.dsem[(e, s)] = st.enter_context(nc.semaphore("d_%s_%d" % (e, s)))
        self.ecnt = {e: 0 for e in ENGS}
        self.dcnt = {e: 0 for e in ENGS}
        self.waited = {e: {} for e in ENGS}
        self.n_ops = 0

    def buf(self, name=""):
        b = Buf(name)
        self.bufs.append(b)
        return b

    def bufs_n(self, name, n):
        return [self.buf("%s%d" % (name, i)) for i in range(n)]

    def op(self, eng, fn, reads=(), writes=(), dma=False):
        o = Op(eng, fn, dma)
        seen = set()
        cand = []
        for b in reads:
            if b.w is not None:
                cand.append(b.w)
        for b in writes:
            if b.w is not None:
                cand.append(b.w)
            cand.extend(b.r)
        for d in cand:
            if d is o or id(d) in seen:
                continue
            seen.add(id(d))
            if d.eng == "pe" and eng == "pe" and not d.dma and not dma:
                continue
            o.deps.append(d)
            if not d.emitted:
                d.inc = True
        for b in reads:
            b.r.append(o)
        for b in writes:
            b.w = o
            b.r = []
        self.pending[eng].append(o)
        self.n_ops += 1
        return o

    def flush(self, final=False):
        nc = self.nc
        for b in self.bufs:
            if b.w is not None and not b.w.emitted:
                b.w.inc = True
            for r in b.r:
                if not r.emitted:
                    r.inc = True
        for e in ENGS:
            for o in self.pending[e]:
                if o.dma:
                    k = self.dcnt[e]
                    o.sem = (e, k % self.nd)
                    o.cnt = 16 * (k // self.nd + 1)
                    self.dcnt[e] = k + 1
                elif o.inc:
                    self.ecnt[e] += 1
                    o.cnt = self.ecnt[e]
        pending = self.pending
        self.pending = {e: [] for e in ENGS}

        def run(ename, eng):
            waited = self.waited[ename]
            for o in pending[ename]:
                for d in o.deps:
                    key = d.sem if d.dma else d.eng
                    if waited.get(key, 0) >= d.cnt:
                        continue
                    assert d.cnt > 0, (d.eng, ename)
                    eng.wait_ge(self.dsem[key] if d.dma else self.esem[key], d.cnt)
                    waited[key] = d.cnt
                if o.dma:
                    if o.cnt > 16 and waited.get(o.sem, 0) < o.cnt - 16:
                        eng.wait_ge(self.dsem[o.sem], o.cnt - 16)
                        waited[o.sem] = o.cnt - 16
                    o.fn(eng).then_inc(self.dsem[o.sem], 16)
                else:
                    ins = o.fn(eng)
                    if o.inc:
                        ins.then_inc(self.esem[o.eng], 1)
                o.emitted = True
            if ename == "sp" and final:
                for q in ("sp", "pool"):
                    k = self.dcnt[q]
                    for sl in range(min(self.nd, k)):
                        last = 16 * ((k - 1 - sl) // self.nd + 1)
                        eng.wait_ge(self.dsem[(q, sl)], last)

        with nc.Block() as block:
            @block.tensor
            def _(eng):
                run("pe", eng)

            @block.scalar
            def _(eng):
                run("act", eng)

            @block.vector
            def _(eng):
                run("dve", eng)

            @block.gpsimd
            def _(eng):
                run("pool", eng)

            @block.sync
            def _(eng):
                run("sp", eng)


def bcast_last(ap2d, n):
    return ap2d.unsqueeze(2).to_broadcast([ap2d.shape[0], ap2d.shape[1], n])


class Ctx:
    pass


def load_w_kmajor(P, nc, dst, src2d, n_kc, ncols, bufs, col_chunk=1408):
    v = src2d.rearrange("(kc p) n -> kc p n", p=128)
    mdl = 4 * col_chunk
    for k in range(n_kc):
        P.op("pool", lambda e, k=k: e.dma_start(out=dst[:, k, :], in_=v[k], max_dma_last_dim=mdl),
             writes=[bufs[k]], dma=True)


def setup_consts(P, nc, st, C):
    sb = lambda name, shape, dt: st.enter_context(nc.sbuf_tensor(name, shape, dt))
    C.identf = sb("identf", [128, 128], F32)
    C.ident = sb("ident", [128, 128], BF16)
    C.neghalf = sb("neghalf", [128, 1], F32)
    C.b_const = P.buf("const")
    identf, ident = C.identf, C.ident
    P.op("pool", lambda e: e.memset(identf[:], 0.0), writes=[C.b_const])
    P.op("pool", lambda e: e.affine_select(out=identf[:], in_=identf[:], pattern=[[-1, 128]],
                                             compare_op=ALU.not_equal, fill=1.0, base=0,
                                             channel_multiplier=1),
         reads=[C.b_const], writes=[C.b_const])
    P.op("dve", lambda e: e.tensor_copy(out=ident[:], in_=identf[:]), reads=[C.b_const], writes=[C.b_const])
    P.op("pool", lambda e: e.memset(C.neghalf[:], -0.5), reads=[C.b_const], writes=[C.b_const])


def rms_stats(P, C, src_ap, src_buf, junk, b_junk, ss, var, rstd, b_stat, n_feat=D):
    P.op("act", lambda e: e.activation(out=junk, in_=src_ap, func=AF.Square, accum_out=ss),
         reads=[src_buf], writes=[b_junk, b_stat])
    P.op("dve", lambda e: e.tensor_scalar(out=var, in0=ss, scalar1=1.0 / n_feat, scalar2=EPS,
                                          op0=ALU.mult, op1=ALU.add),
         reads=[b_stat], writes=[b_stat])
    P.op("pool", lambda e: e.tensor_tensor(out=rstd, in0=var, in1=C.neghalf[:], op=ALU.pow),
         reads=[b_stat, C.b_const], writes=[b_stat])


def ffn_pass(P, nc, C, tag, src_h, w_gate, w_up, w_down, pre_g, post_g, dst_h, next_g, dst_uT,
             src_b, dst_b, uT_b, gate_w=None, gate_dst=None, gate_b=None):
    with contextlib.ExitStack() as st:
        sb = lambda name, shape, dt: st.enter_context(nc.sbuf_tensor(tag + name, shape, dt))
        ps = lambda name, shape, dt: st.enter_context(nc.psum_tensor(tag + name, shape, dt))
        Wg = sb("Wg", [128, 8, DFF], BF16)
        Wu = sb("Wu", [128, 8, DFF], BF16)
        Wd = sb("Wd", [128, NFC, D], BF16)
        b_Wg = P.bufs_n("Wg", 8)
        b_Wu = P.bufs_n("Wu", 8)
        b_Wd = P.bufs_n("Wd", 2)
        load_w_kmajor(P, nc, Wg, w_gate, 8, DFF, b_Wg)
        load_w_kmajor(P, nc, Wu, w_up, 8, DFF, b_Wu)
        wdv = w_down.rearrange("(fc p) d -> p fc d", p=128)
        for hh in range(2):
            P.op("pool", lambda e, hh=hh: e.dma_start(out=Wd[:, hh * 11:(hh + 1) * 11, :],
                                                       in_=wdv[:, hh * 11:(hh + 1) * 11, :]),
                 writes=[b_Wd[hh]], dma=True)
        gpre = sb("gpre", [128, 8], F32)
        gnext = sb("gnext", [128, 8], F32)
        gpost = sb("gpost", [128, D], F32)
        b_par = P.buf("par")
        P.op("sp", lambda e: e.dma_start(out=gpre[:], in_=pre_g.rearrange("o (k p) -> p (o k)", p=128),
                                         allow_slow_non_contiguous=True),
             writes=[b_par], dma=True)
        P.op("sp", lambda e: e.dma_start(out=gnext[:], in_=next_g.rearrange("o (k p) -> p (o k)", p=128),
                                         allow_slow_non_contiguous=True),
             writes=[b_par], dma=True)
        P.op("sp", lambda e: e.dma_start(out=gpost[:], in_=post_g.partition_broadcast(128)),
             writes=[b_par], dma=True)
        if gate_w is not None:
            Wgt = sb("Wgt", [128, 8, 72], BF16)
            b_Wgt = P.buf("Wgt")
            P.op("pool", lambda e: e.memset(Wgt[:], 0.0), writes=[b_Wgt])
            gv = gate_w.rearrange("(kc p) n -> p kc n", p=128)
            for (c0, n, d0) in ((1540, 4, 0), (1536, 4, 32), (3080, 8, 64)):
                P.op("pool", lambda e, c0=c0, n=n, d0=d0: e.dma_start(
                    out=Wgt[:, :, d0:d0 + n], in_=gv[:, :, c0:c0 + n]),
                    reads=[], writes=[b_Wgt], dma=True)
            pgt = ps("pgt", [72, 128], F32)
            b_pgt = P.buf("pgt")
            gsb = [sb("gsb%d" % i, [72, 128], F32) for i in range(2)]
            b_gsb = P.bufs_n("gsb", 2)

        NXB = 3
        xb = [sb("xb%d" % i, [128, D], F32) for i in range(NXB)]
        b_xb = P.bufs_n("xb", NXB)
        ubf = [sb("ubf%d" % i, [128, D], BF16) for i in range(2)]
        b_ubf = P.bufs_n("ubf", 2)
        junk = sb("junk", [128, D], BF16)
        b_junk = P.buf("junk")
        uT = sb("uT", [128, 8, TT], BF16)
        b_uT = P.bufs_n("uT", 4)
        aT = sb("aT", [128, NFC, TT], BF16)
        b_aT = P.bufs_n("aT", NFC)
        sg = [sb("sg%d" % i, [128, TT], F32) for i in range(2)]
        b_sg = P.bufs_n("sg", 2)
        hst = [sb("hst%d" % i, [128, D], F32) for i in range(2)]
        b_hst = P.bufs_n("hst", 2)
        u2T = [sb("u2T%d" % i, [128, 8, 128], BF16) for i in range(2)]
        b_u2T = P.bufs_n("u2T", 2)
        NST = 6
        stat = sb("stat", [128, 3 * NST], F32)
        b_stat = P.bufs_n("stat", NST)

        pt = ps("pt", [128, 8, 128], BF16)
        b_pt = P.buf("pt")
        pg = [ps("pg%d" % i, [128, TT], F32) for i in range(2)]
        pu = [ps("pu%d" % i, [128, TT], F32) for i in range(2)]
        b_pg = P.bufs_n("pg", 2)
        b_pu = P.bufs_n("pu", 2)
        py = ps("py", [128, D], F32)
        b_py = P.buf("py")

        src_v = src_h.rearrange("(n p) d -> n p d", p=128)
        dst_v = dst_h.rearrange("(n p) d -> n p d", p=128)
        cnt = {"x": 0, "u": 0, "st": 0, "h": 0, "u2": 0, "sg": 0, "gs": 0}

        def norm_T(h_ap, h_buf, gcol, out_ap, out_bufs):
            si = cnt["st"] % NST
            cnt["st"] += 1
            ss, var, rstd = (stat[:, 3 * si + j:3 * si + j + 1] for j in range(3))
            rms_stats(P, C, h_ap, h_buf, junk[:], b_junk, ss, var, rstd, b_stat[si])
            ui = cnt["u"] % 2
            cnt["u"] += 1
            u = ubf[ui]
            P.op("dve", lambda e: e.tensor_scalar(out=u[:], in0=h_ap, scalar1=rstd, scalar2=None, op0=ALU.mult),
                 reads=[h_buf, b_stat[si]], writes=[b_ubf[ui]])
            for k in range(8):
                P.op("pe", lambda e, k=k: e.transpose(out=pt[:, k, :], in_=u[:, k * 128:(k + 1) * 128],
                                                      identity=C.ident[:]),
                     reads=[b_ubf[ui], C.b_const], writes=[b_pt])
            P.op("dve", lambda e: e.tensor_tensor(out=out_ap, in0=pt[:], in1=bcast_last(gcol[:], 128), op=ALU.mult),
                 reads=[b_pt, b_par], writes=out_bufs)

        def pre(i):
            for s in range(4):
                n = i * 4 + s
                xi = cnt["x"] % NXB
                cnt["x"] += 1
                P.op("sp", lambda e, n=n, xi=xi: e.dma_start(out=xb[xi][:], in_=src_v[n]),
                     reads=[src_b[n]], writes=[b_xb[xi]], dma=True)
                norm_T(xb[xi][:], b_xb[xi], gpre, uT[:, :, s * 128:(s + 1) * 128], [b_uT[s]])

        def gateup(i):
            for f in range(NFC):
                j = f % 2
                for k in range(8):
                    P.op("pe", lambda e, k=k, f=f, j=j: e.matmul(
                        pg[j][:], lhsT=Wg[:, k, f * 128:(f + 1) * 128], rhs=uT[:, k, :],
                        start=(k == 0), stop=(k == 7)),
                        reads=[b_Wg[k]] + b_uT, writes=[b_pg[j]])
                for k in range(8):
                    P.op("pe", lambda e, k=k, f=f, j=j: e.matmul(
                        pu[j][:], lhsT=Wu[:, k, f * 128:(f + 1) * 128], rhs=uT[:, k, :],
                        start=(k == 0), stop=(k == 7)),
                        reads=[b_Wu[k]] + b_uT, writes=[b_pu[j]])
                si = cnt["sg"] % 2
                cnt["sg"] += 1
                P.op("act", lambda e, j=j, si=si: e.activation(out=sg[si][:], in_=pg[j][:], func=AF.Silu),
                     reads=[b_pg[j]], writes=[b_sg[si]])
                P.op("dve", lambda e, j=j, si=si, f=f: e.tensor_tensor(out=aT[:, f, :], in0=sg[si][:], in1=pu[j][:],
                                                                   op=ALU.mult),
                     reads=[b_sg[si], b_pu[j]], writes=[b_aT[f]])

        def down_post(i):
            for s in range(4):
                n = i * 4 + s
                for hf in range(2):
                    for f in range(NFC):
                        P.op("pe", lambda e, f=f, s=s, hf=hf: e.matmul(
                            py[:, hf * 512:(hf + 1) * 512], lhsT=aT[:, f, s * 128:(s + 1) * 128],
                            rhs=Wd[:, f, hf * 512:(hf + 1) * 512], start=(f == 0), stop=(f == NFC - 1)),
                            reads=[b_aT[f], b_Wd[f // 11]], writes=[b_py])
                xi = cnt["x"] % NXB
                cnt["x"] += 1
                P.op("sp", lambda e, n=n, xi=xi: e.dma_start(out=xb[xi][:], in_=src_v[n]),
                     reads=[src_b[n]], writes=[b_xb[xi]], dma=True)
                si = cnt["st"] % NST
                cnt["st"] += 1
                ss, var, rstd = (stat[:, 3 * si + j:3 * si + j + 1] for j in range(3))
                rms_stats(P, C, py[:], b_py, junk[:], b_junk, ss, var, rstd, b_stat[si])
                hi = cnt["h"] % 2
                cnt["h"] += 1
                hb = hst[hi]
                P.op("dve", lambda e, hb=hb, rstd=rstd: e.scalar_tensor_tensor(
                    out=hb[:], in0=py[:], scalar=rstd, in1=gpost[:], op0=ALU.mult, op1=ALU.mult),
                    reads=[b_py, b_stat[si], b_par], writes=[b_hst[hi]])
                P.op("dve", lambda e, hb=hb, xi=xi: e.scalar_tensor_tensor(
                    out=hb[:], in0=hb[:], scalar=0.5, in1=xb[xi][:], op0=ALU.mult, op1=ALU.add),
                    reads=[b_hst[hi], b_xb[xi]], writes=[b_hst[hi]])
                P.op("sp", lambda e, hb=hb, n=n: e.dma_start(out=dst_v[n], in_=hb[:]),
                     reads=[b_hst[hi]], writes=[dst_b[n]], dma=True)
                ui2 = cnt["u2"] % 2
                cnt["u2"] += 1
                norm_T(hb[:], b_hst[hi], gnext, u2T[ui2][:], [b_u2T[ui2]])
                P.op("sp", lambda e, ui2=ui2, n=n: e.dma_start(
                    out=dst_uT[:, :, n * 128:(n + 1) * 128].rearrange("k p t -> p k t"), in_=u2T[ui2][:]),
                    reads=[b_u2T[ui2]], writes=[uT_b[n]], dma=True)
                if gate_w is not None:
                    for k in range(8):
                        P.op("pe", lambda e, k=k, ui2=ui2: e.matmul(
                            pgt[:], lhsT=Wgt[:, k, :], rhs=u2T[ui2][:, k, :], start=(k == 0), stop=(k == 7)),
                            reads=[b_Wgt, b_u2T[ui2]], writes=[b_pgt])
                    gi = cnt["gs"] % 2
                    cnt["gs"] += 1
                    P.op("act", lambda e, gi=gi: e.activation(out=gsb[gi][:], in_=pgt[:], func=AF.Copy),
                         reads=[b_pgt], writes=[b_gsb[gi]])
                    P.op("sp", lambda e, gi=gi, n=n: e.dma_start(out=gate_dst[:, n * 128:(n + 1) * 128], in_=gsb[gi][:]),
                         reads=[b_gsb[gi]], writes=[gate_b[n]], dma=True)

        pre(0)
        for i in range(NT):
            gateup(i)
            if i + 1 < NT:
                pre(i + 1)
            down_post(i)
        P.flush()


def gp_stage(P, nc, C, I, gpre, gpre_b, qaug, kaug, aug_b):
    with contextlib.ExitStack() as st:
        sb = lambda name, shape, dt: st.enter_context(nc.sbuf_tensor("gp" + name, shape, dt))
        ps = lambda name, shape, dt: st.enter_context(nc.psum_tensor("gp" + name, shape, dt))
        T0 = sb("T0", [72, S], F32)
        T1 = sb("T1", [72, S], F32)
        T2 = sb("T2", [72, S], F32)
        T3 = sb("T3", [72, S], F32)
        QR = sb("QR", [72, 3, S], BF16)
        KR = sb("KR", [72, 3, S], BF16)
        ONE = sb("ONE", [72, S], BF16)
        bcol = sb("bcol", [72, 1], F32)
        negb = sb("negb", [72, 1], F32)
        bicol = sb("bicol", [72, 1], F32)
        onec = sb("onec", [72, 1], F32)
        cm = sb("cm", [72, 32], F32)
        mce = sb("mce", [72, 32], F32)
        mprev = sb("mprev", [72, 32], F32)
        dec = sb("dec", [72, 32], F32)
        esel = sb("esel", [72, 4, 128], F32)
        bT0, bT1, bT2, bT3, bQR, bKR, bONE, bsm = [P.buf(n) for n in
                                                   ("T0", "T1", "T2", "T3", "QR", "KR", "ONE", "gsm")]
        ptm = ps("ptm", [128, 32, 8], F32)
        pdc = ps("pdc", [128, 4, 32], F32)
        b_ptm, b_pdc = P.buf("ptm"), P.buf("pdc")

        P.op("sp", lambda e: e.dma_start(out=T0[:], in_=gpre), reads=gpre_b, writes=[bT0], dma=True)
        P.op("sp", lambda e: e.dma_start(out=T3[0:4, :], in_=gpre[32:36, :]), reads=gpre_b, writes=[bT3], dma=True)
        P.op("dve", lambda e: e.memset(bcol[:], 0.0), writes=[bsm])
        P.op("dve", lambda e: e.memset(bicol[:], 0.0), reads=[bsm], writes=[bsm])
        P.op("dve", lambda e: e.memset(onec[:], 1.0), reads=[bsm], writes=[bsm])
        P.op("pool", lambda e: e.memset(ONE[:], 1.0), writes=[bONE])
        P.op("sp", lambda e: e.dma_start(out=bcol[0:4, :], in_=I["mlstm_f_bias"].rearrange("o n -> n o"),
                                         allow_slow_non_contiguous=True), reads=[bsm], writes=[bsm], dma=True)
        P.op("sp", lambda e: e.dma_start(out=bcol[64:72, :], in_=I["fox_f_bias"].rearrange("o n -> n o"),
                                         allow_slow_non_contiguous=True), reads=[bsm], writes=[bsm], dma=True)
        P.op("sp", lambda e: e.dma_start(out=bicol[0:4, :], in_=I["mlstm_i_bias"].rearrange("o n -> n o"),
                                         allow_slow_non_contiguous=True), reads=[bsm], writes=[bsm], dma=True)
        P.op("dve", lambda e: e.tensor_scalar(out=negb[0:72, :], in0=bcol[0:72, :], scalar1=-1.0, scalar2=None,
                                              op0=ALU.mult), reads=[bsm], writes=[bsm])
        R = slice(0, 72)
        P.op("act", lambda e: e.activation(out=T1[R, :], in_=T0[R, :], func=AF.Exp, scale=-1.0, bias=negb[R, :]),
             reads=[bT0, bsm], writes=[bT1])
        P.op("act", lambda e: e.activation(out=T1[R, :], in_=T1[R, :], func=AF.Ln, scale=1.0, bias=onec[R, :]),
             reads=[bT1, bsm], writes=[bT1])
        P.op("dve", lambda e: e.tensor_tensor_scan(out=T2[R, :], data0=T1[R, :], data1=T1[R, :], initial=0.0,
                                                   op0=ALU.add, op1=ALU.max), reads=[bT1], writes=[bT2])
        M = slice(0, 4)
        P.op("dve", lambda e: e.scalar_tensor_tensor(out=T3[M, :], in0=T3[M, :], scalar=bicol[M, :], in1=T2[M, :],
                                                     op0=ALU.add, op1=ALU.add), reads=[bT3, bT2, bsm], writes=[bT3])
        P.op("dve", lambda e: e.tensor_reduce(out=cm[M, :], in_=T3[M, :].rearrange("p (c l) -> p c l", l=128),
                                              axis=AX.X, op=ALU.max), reads=[bT3], writes=[bsm])
        P.op("dve", lambda e: e.tensor_tensor_scan(out=mce[M, :], data0=cm[M, :], data1=cm[M, :], initial=0.0,
                                                   op0=ALU.max, op1=ALU.max), reads=[bsm], writes=[bsm])
        P.op("dve", lambda e: e.tensor_tensor(out=T3[M, :].rearrange("p (c l) -> p c l", l=128),
                                              in0=T3[M, :].rearrange("p (c l) -> p c l", l=128),
                                              in1=bcast_last(mce[M, :], 128), op=ALU.subtract),
             reads=[bT3, bsm], writes=[bT3])
        P.op("act", lambda e: e.activation(out=T3[M, :], in_=T3[M, :], func=AF.Exp), reads=[bT3], writes=[bT3])
        P.op("dve", lambda e: e.tensor_tensor(out=T1[M, :].rearrange("p (c l) -> p c l", l=128),
                                              in0=T2[M, :].rearrange("p (c l) -> p c l", l=128),
                                              in1=bcast_last(mce[M, :], 128), op=ALU.subtract),
             reads=[bT2, bsm, bT1], writes=[bT1])
        P.op("act", lambda e: e.activation(out=T1[M, :], in_=T1[M, :], func=AF.Exp, scale=2.0), reads=[bT1], writes=[bT1])
        P.op("dve", lambda e: e.memset(mprev[M, :], 0.0), reads=[bsm], writes=[bsm])
        P.op("dve", lambda e: e.tensor_copy(out=mprev[M, 1:32], in_=mce[M, 0:31]), reads=[bsm], writes=[bsm])
        P.op("dve", lambda e: e.tensor_tensor(out=dec[M, :], in0=mprev[M, :], in1=mce[M, :], op=ALU.subtract),
             reads=[bsm], writes=[bsm])
        P.op("act", lambda e: e.activation(out=dec[M, :], in_=dec[M, :], func=AF.Exp), reads=[bsm], writes=[bsm])
        for c in range(32):
            P.op("pe", lambda e, c=c: e.transpose(out=ptm[:, c, 0:4], in_=T3[M, c * 128:(c + 1) * 128],
                                                  identity=C.identf[M, 0:4]),
                 reads=[bT3, C.b_const], writes=[b_ptm])
            P.op("pe", lambda e, c=c: e.transpose(out=ptm[:, c, 4:8], in_=T1[M, c * 128:(c + 1) * 128],
                                                  identity=C.identf[M, 0:4]),
                 reads=[bT1, C.b_const], writes=[b_ptm])
        P.op("dve", lambda e: e.tensor_copy(out=C.wthr[:], in_=ptm[:]), reads=[b_ptm], writes=[C.b_wthr])
        for h in range(4):
            P.op("dve", lambda e, h=h: e.tensor_copy(out=esel[M, h, :],
                                                     in_=C.identf[M, h:h + 1].to_broadcast([4, 128])),
                 reads=[C.b_const, bsm], writes=[bsm])
        for h in range(4):
            P.op("pe", lambda e, h=h: e.matmul(pdc[:, h, :], lhsT=esel[M, h, :], rhs=dec[M, :], start=True, stop=True),
                 reads=[bsm], writes=[b_pdc])
        P.op("dve", lambda e: e.tensor_copy(out=C.decbc[:], in_=pdc[:]), reads=[b_pdc], writes=[C.b_decbc])
        Fx = slice(64, 72)
        Fd = slice(64, 72)
        P.op("dve", lambda e: e.tensor_scalar(out=T0[Fx, :], in0=T2[Fx, :], scalar1=-1.0, scalar2=None, op0=ALU.mult),
             reads=[bT2, bT0], writes=[bT0])
        for part in range(3):
            P.op("dve", lambda e, part=part: e.tensor_copy(out=QR[Fx, part, :], in_=T0[Fx, :]),
                 reads=[bT0], writes=[bQR])
            if part < 2:
                P.op("dve", lambda e, part=part: e.tensor_tensor(out=T0[Fx, :], in0=T0[Fx, :], in1=QR[Fx, part, :],
                                                                 op=ALU.subtract), reads=[bT0, bQR], writes=[bT0])
        P.op("pool", lambda e: e.tensor_scalar(out=KR[Fx, :, :], in0=QR[Fx, :, :], scalar1=-1.0, scalar2=None,
                                               op0=ALU.mult), reads=[bQR], writes=[bKR])
        P.op("sp", lambda e: e.dma_start(out=qaug[:, 64:67, :], in_=QR[Fd, :, :]), reads=[bQR], writes=[aug_b], dma=True)
        P.op("sp", lambda e: e.dma_start(out=kaug[:, 67:70, :], in_=KR[Fd, :, :]), reads=[bKR], writes=[aug_b], dma=True)
        for r in range(3):
            P.op("sp", lambda e, r=r: e.dma_start(out=qaug[:, 67 + r, :], in_=ONE[Fd, :]), reads=[bONE],
                 writes=[aug_b], dma=True)
            P.op("sp", lambda e, r=r: e.dma_start(out=kaug[:, 64 + r, :], in_=ONE[Fd, :]), reads=[bONE],
                 writes=[aug_b], dma=True)
        P.flush()


def win_pass(P, nc, C, I, uT1, uT_b, SC, DB):
    w_in = I["w_in"]
    with contextlib.ExitStack() as st:
        sb = lambda name, shape, dt: st.enter_context(nc.sbuf_tensor("wi" + name, shape, dt))
        ps = lambda name, shape, dt: st.enter_context(nc.psum_tensor("wi" + name, shape, dt))
        W = sb("W", [128, 8, INW], BF16)
        b_W = P.bufs_n("Win", 8)
        wv = w_in.rearrange("(kc p) n -> kc p n", p=128)
        for k in range(8):
            P.op("pool", lambda e, k=k: e.dma_start(out=W[:, k, :], in_=wv[k], max_dma_last_dim=4 * 1284),
                 writes=[b_W[k]], dma=True)
        cw = sb("cw", [128, 4, 4], F32)
        cb = sb("cb", [128, 4], F32)
        gbias = sb("gbias", [128, 2048], F32)
        b_par = P.buf("wipar")
        for tap in range(4):
            P.op("sp", lambda e, tap=tap: e.dma_start(
                out=cw[:, :, tap], in_=I["conv_w"][tap:tap + 1, :].rearrange("o (c p) -> p (o c)", p=128),
                allow_slow_non_contiguous=True), writes=[b_par], dma=True)
        P.op("sp", lambda e: e.dma_start(out=cb[:], in_=I["conv_b"].rearrange("o (c p) -> p (o c)", p=128),
                                         allow_slow_non_contiguous=True), writes=[b_par], dma=True)
        P.op("sp", lambda e: e.dma_start(out=gbias[:], in_=I["branch_gate_bias"].partition_broadcast(128)),
             writes=[b_par], dma=True)
        uT = [sb("uT%d" % i, [128, 8, TT], BF16) for i in range(2)]
        b_uT = P.bufs_n("wiuT", 2)
        zq = sb("zq", [128, 4, 3 + TT], F32)
        b_zq = P.bufs_n("zq", 4)
        acc = [sb("acc%d" % i, [128, TT], F32) for i in range(2)]
        b_acc = P.bufs_n("acc", 2)
        fo = [sb("fo%d" % i, [128, TT], BF16) for i in range(3)]
        b_fo = P.bufs_n("fo", 3)
        tv = [sb("tv%d" % i, [128, 4, 129], BF16) for i in range(2)]
        b_tv = P.bufs_n("tv", 2)
        tf = [sb("tf%d" % i, [128, 8, 65], BF16) for i in range(2)]
        b_tf = P.bufs_n("tf", 2)
        tg = [sb("tg%d" % i, [128, 512], F32) for i in range(2)]
        b_tg = P.bufs_n("tg", 2)
        to = [sb("to%d" % i, [128, 512], BF16) for i in range(3)]
        b_to = P.bufs_n("to", 3)
        pf = [ps("pf%d" % i, [128, TT], F32) for i in range(2)]
        b_pf = P.bufs_n("pf", 2)
        pk = [ps("pk%d" % i, [128, 512], F32) for i in range(2)]
        b_pk = P.bufs_n("pk", 2)
        cnt = {"pf": 0, "pk": 0, "acc": 0, "fo": 0, "tv": 0, "tf": 0, "tg": 0, "to": 0}

        def rot(key, n):
            v = cnt[key] % n
            cnt[key] += 1
            return v

        for ch in range(4):
            P.op("dve", lambda e, ch=ch: e.memset(zq[:, ch, 0:3], 0.0), writes=[b_zq[ch]])
        for i in range(NT):
            ub = i % 2
            tcols = slice(i * TT, (i + 1) * TT)
            P.op("sp", lambda e, ub=ub, tcols=tcols: e.dma_start(
                out=uT[ub][:], in_=uT1[:, :, tcols].rearrange("k p t -> p k t")),
                reads=uT_b[4 * i:4 * i + 4], writes=[b_uT[ub]], dma=True)
            fm = [("mqk", ch, ch * 128) for ch in range(4)] + \
                 [("fq", ch, 1544 + ch * 128) for ch in range(4)] + \
                 [("fk", ch, 2056 + ch * 128) for ch in range(4)]
            for (kind, ch, c0) in fm:
                j = rot("pf", 2)
                for k in range(8):
                    P.op("pe", lambda e, k=k, c0=c0, j=j, ub=ub: e.matmul(
                        pf[j][:], lhsT=W[:, k, c0:c0 + 128], rhs=uT[ub][:, k, :], start=(k == 0), stop=(k == 7)),
                        reads=[b_W[k], b_uT[ub]], writes=[b_pf[j]])
                if kind == "mqk":
                    P.op("act", lambda e, ch=ch, j=j: e.activation(out=zq[:, ch, 3:3 + TT], in_=pf[j][:], func=AF.Copy),
                         reads=[b_pf[j]], writes=[b_zq[ch]])
                    a = rot("acc", 2)
                    P.op("dve", lambda e, ch=ch, a=a: e.tensor_scalar(
                        out=acc[a][:], in0=zq[:, ch, 0:TT], scalar1=cw[:, ch, 0:1], scalar2=cb[:, ch:ch + 1],
                        op0=ALU.mult, op1=ALU.add), reads=[b_zq[ch], b_par], writes=[b_acc[a]])
                    for tap in range(1, 4):
                        P.op("dve", lambda e, ch=ch, a=a, tap=tap: e.scalar_tensor_tensor(
                            out=acc[a][:], in0=zq[:, ch, tap:tap + TT], scalar=cw[:, ch, tap:tap + 1], in1=acc[a][:],
                            op0=ALU.mult, op1=ALU.add), reads=[b_zq[ch], b_par, b_acc[a]], writes=[b_acc[a]])
                    P.op("dve", lambda e, ch=ch: e.tensor_copy(out=zq[:, ch, 0:3], in_=zq[:, ch, TT:TT + 3]),
                         reads=[b_zq[ch]], writes=[b_zq[ch]])
                    o = rot("fo", 3)
                    P.op("act", lambda e, a=a, o=o: e.activation(out=fo[o][:], in_=acc[a][:], func=AF.Silu),
                         reads=[b_acc[a]], writes=[b_fo[o]])
                    P.op("sp", lambda e, o=o, ch=ch, tcols=tcols: e.dma_start(out=SC["mqkT"][ch, :, tcols], in_=fo[o][:]),
                         reads=[b_fo[o]], writes=[DB("mqkT")[i]], dma=True)
                else:
                    o = rot("fo", 3)
                    sc = 0.125 if kind == "fq" else 1.0
                    P.op("act", lambda e, o=o, j=j, sc=sc: e.activation(out=fo[o][:], in_=pf[j][:], func=AF.Copy, scale=sc),
                         reads=[b_pf[j]], writes=[b_fo[o]])
                    dst = SC["qaug"] if kind == "fq" else SC["kaug"]
                    for hh in range(2):
                        P.op("sp", lambda e, o=o, ch=ch, dst=dst, tcols=tcols, hh=hh: e.dma_start(
                            out=dst[2 * ch + hh, 0:64, tcols], in_=fo[o][hh * 64:(hh + 1) * 64, :]),
                            reads=[b_fo[o]], writes=[DB("aug")[i]], dma=True)
            for s in range(4):
                n = i * 4 + s
                rows = slice(n * 128, (n + 1) * 128)
                groups = [("mv", 512), ("mo", 1024), ("fv", 2568), ("ga", 3088), ("ga", 3600), ("gb", 4112), ("gb", 4624)]
                for gi, (kind, c0) in enumerate(groups):
                    j = rot("pk", 2)
                    for k in range(8):
                        P.op("pe", lambda e, k=k, c0=c0, j=j, ub=ub, s=s: e.matmul(
                            pk[j][:], lhsT=uT[ub][:, k, s * 128:(s + 1) * 128], rhs=W[:, k, c0:c0 + 512],
                            start=(k == 0), stop=(k == 7)),
                            reads=[b_W[k], b_uT[ub]], writes=[b_pk[j]])
                    if kind == "mv":
                        t = rot("tv", 2)
                        c = n
                        P.op("dve", lambda e, t=t, j=j, c=c: e.tensor_tensor(
                            out=tv[t][:, :, 0:128], in0=pk[j][:].rearrange("p (h d) -> p h d", h=4),
                            in1=bcast_last(C.wthr[:, c, 0:4], 128), op=ALU.mult),
                            reads=[b_pk[j], C.b_wthr], writes=[b_tv[t]])
                        P.op("dve", lambda e, t=t, c=c: e.tensor_copy(out=tv[t][:, :, 128:129],
                                                                     in_=C.wthr[:, c, 0:4].unsqueeze(2)),
                             reads=[C.b_wthr, b_tv[t]], writes=[b_tv[t]])
                        P.op("sp", lambda e, t=t, rows=rows: e.dma_start(out=SC["vaugM"][rows], in_=tv[t][:]),
                             reads=[b_tv[t]], writes=[DB("vaugM")[n]], dma=True)
                    elif kind == "fv":
                        t = rot("tf", 2)
                        P.op("act", lambda e, t=t, j=j: e.activation(
                            out=tf[t][:, :, 1:65], in_=pk[j][:].rearrange("p (h d) -> p h d", h=8), func=AF.Copy),
                            reads=[b_pk[j]], writes=[b_tf[t]])
                        P.op("dve", lambda e, t=t: e.memset(tf[t][:, :, 0:1], 1.0), reads=[b_tf[t]], writes=[b_tf[t]])
                        P.op("sp", lambda e, t=t, rows=rows: e.dma_start(out=SC["vaugF"][rows], in_=tf[t][:]),
                             reads=[b_tf[t]], writes=[DB("vaugF")[n]], dma=True)
                    elif kind == "mo":
                        o = rot("to", 3)
                        P.op("act", lambda e, o=o, j=j: e.activation(out=to[o][:], in_=pk[j][:], func=AF.Sigmoid),
                             reads=[b_pk[j]], writes=[b_to[o]])
                        P.op("sp", lambda e, o=o, rows=rows: e.dma_start(out=SC["so"][rows], in_=to[o][:]),
                             reads=[b_to[o]], writes=[DB("so")[n]], dma=True)
                    else:
                        g = rot("tg", 2)
                        boff = c0 - 3088
                        P.op("dve", lambda e, g=g, j=j, boff=boff: e.tensor_tensor(
                            out=tg[g][:], in0=pk[j][:], in1=gbias[:, boff:boff + 512], op=ALU.add),
                            reads=[b_pk[j], b_par], writes=[b_tg[g]])
                        o = rot("to", 3)
                        P.op("act", lambda e, o=o, g=g: e.activation(out=to[o][:], in_=tg[g][:], func=AF.Sigmoid),
                             reads=[b_tg[g]], writes=[b_to[o]])
                        dcol = boff % 1024
                        dst = SC["ga"] if kind == "ga" else SC["gb"]
                        P.op("sp", lambda e, o=o, rows=rows, dst=dst, dcol=dcol: e.dma_start(
                            out=dst[rows, dcol:dcol + 512], in_=to[o][:]),
                            reads=[b_to[o]], writes=[DB("gab")[n]], dma=True)
        P.flush()


def mix_pass(P, nc, C, I, SC, DB):
    with contextlib.ExitStack() as st:
        sb = lambda name, shape, dt: st.enter_context(nc.sbuf_tensor("mx" + name, shape, dt))
        ps = lambda name, shape, dt: st.enter_context(nc.psum_tensor("mx" + name, shape, dt))
        mask01 = sb("mask01", [128, 128], F32)
        trim = sb("trim", [128, 128], BF16)
        trimf = sb("trimf", [128, 128], F32)
        onesr = sb("onesr", [1, 65], F32)
        gln = sb("gln", [128, 512], F32)
        b_c = P.buf("mxconst")
        P.op("pool", lambda e: e.memset(mask01[:], 1.0), writes=[b_c])
        P.op("pool", lambda e: e.affine_select(out=mask01[:], in_=mask01[:], pattern=[[1, 128]], compare_op=ALU.is_ge,
                                                 fill=0.0, base=0, channel_multiplier=-1), reads=[b_c], writes=[b_c])
        P.op("pool", lambda e: e.memset(trimf[:], 0.0), reads=[b_c], writes=[b_c])
        P.op("pool", lambda e: e.affine_select(out=trimf[:], in_=trimf[:], pattern=[[1, 128]], compare_op=ALU.is_ge,
                                                 fill=-30000.0, base=0, channel_multiplier=-1), reads=[b_c], writes=[b_c])
        P.op("dve", lambda e: e.tensor_copy(out=trim[:], in_=trimf[:]), reads=[b_c], writes=[b_c])
        P.op("dve", lambda e: e.memset(onesr[:], 1.0), reads=[b_c], writes=[b_c])
        P.op("sp", lambda e: e.dma_start(out=gln[:], in_=I["mlstm_norm_g"].partition_broadcast(128)),
             reads=[b_c], writes=[b_c], dma=True)
        mqz = [sb("mqz%d" % i, [128, S], BF16) for i in range(4)]
        mk = [sb("mk%d" % i, [128, S], BF16) for i in range(2)]
        b_mqk = P.buf("mqk")
        for h in range(4):
            P.op("pool", lambda e, h=h: e.memset(mqz[h][:], 0.0), writes=[b_mqk])
        for h in range(4):
            R = slice((h % 2) * 64, (h % 2) * 64 + 64)
            P.op("sp", lambda e, h=h, R=R: e.dma_start(out=mqz[h][R, :], in_=SC["mqkT"][h // 2, R, :]), reads=DB("mqkT"),
                 writes=[b_mqk], dma=True)
        for hp in range(2):
            P.op("sp", lambda e, hp=hp: e.dma_start(out=mk[hp][:], in_=SC["mqkT"][2 + hp]), reads=DB("mqkT"),
                 writes=[b_mqk], dma=True)
        Cst = [sb("Cst%d" % i, [128, 129], F32) for i in range(2)]
        Cb = [sb("Cb%d" % i, [128, 129], BF16) for i in range(2)]
        b_Cst = P.bufs_n("Cst", 2)
        b_Cb = P.bufs_n("Cb", 2)
        va = [sb("va%d" % i, [128, 4, 129], BF16) for i in range(2)]
        b_va = P.bufs_n("va", 2)
        sgo = [sb("sgo%d" % i, [128, 512], BF16) for i in range(2)]
        b_sgo = P.bufs_n("sgo", 2)
        Sm = [sb("Sm%d" % i, [128, 2, 128], BF16) for i in range(2)]
        b_Sm = P.bufs_n("Sm", 2)
        ktm = [sb("ktm%d" % i, [128, 128], BF16) for i in range(2)]
        b_ktm = P.bufs_n("ktm", 2)
        bst = sb("bst", [128, 2, 6], F32)
        bag = sb("bag", [128, 2, 2], F32)
        sm = sb("sm", [128, 2, 4], F32)
        b_sm = P.buf("msm")
        hn = sb("hn", [128, 512], F32)
        b_hn = P.buf("hn")
        ya = [sb("ya%d" % i, [128, 512], BF16) for i in range(2)]
        b_ya = P.bufs_n("ya", 2)
        yaT = [sb("yaT%d" % i, [128, 4, 128], BF16) for i in range(2)]
        b_yaT = P.bufs_n("yaTs", 2)
        pSm = ps("pSm", [128, 2, 128], F32)
        pOm = ps("pOm", [128, 2, 129], F32)
        pU = ps("pU", [128, 2, 129], F32)
        pT5 = ps("pT5", [128, 5, 128], BF16)
        pkt = pT5[:, 4, :]
        pyT = pT5[:, 0:4, :]
        b_pSm, b_pOm, b_pU, b_pkt, b_pyT = [P.buf(n) for n in ("pSm", "pOm", "pU", "pkt", "pyT")]

        def mlstm_chunk(c):
            cols = slice(c * 128, (c + 1) * 128)
            rows = slice(c * 128, (c + 1) * 128)
            vi = c % 2
            P.op("sp", lambda e: e.dma_start(out=va[vi][:], in_=SC["vaugM"][rows]), reads=[DB("vaugM")[c]],
                 writes=[b_va[vi]], dma=True)
            P.op("sp", lambda e: e.dma_start(out=sgo[vi][:], in_=SC["so"][rows]), reads=[DB("so")[c]],
                 writes=[b_sgo[vi]], dma=True)
            for hp in range(2):
                si = (2 * c + hp) % 2
                for hh in range(2):
                    R = slice(hh * 64, (hh + 1) * 64)
                    P.op("pe", lambda e, hp=hp, hh=hh: e.matmul(
                        pSm[:, hh, :], lhsT=mk[hp][:, cols], rhs=mqz[2 * hp + hh][:, cols], start=True, stop=True),
                        reads=[b_mqk], writes=[b_pSm])
                    if hh == 0:
                        P.op("pe", lambda e, hp=hp: e.transpose(out=pkt, in_=mk[hp][:, cols], identity=C.ident[:]),
                             reads=[b_mqk, C.b_const], writes=[b_pkt])
                P.op("dve", lambda e, si=si: e.scalar_tensor_tensor(
                    out=Sm[si][:], in0=pSm[:], scalar=0.125,
                    in1=mask01[:].unsqueeze(1).to_broadcast([128, 2, 128]), op0=ALU.mult, op1=ALU.mult),
                    reads=[b_pSm, b_c], writes=[b_Sm[si]])
                P.op("act", lambda e, si=si: e.activation(out=ktm[si][:], in_=pkt, func=AF.Copy, scale=0.125),
                     reads=[b_pkt], writes=[b_ktm[si]])
                for hh in range(2):
                    h = 2 * hp + hh
                    R = slice(hh * 64, (hh + 1) * 64)
                    P.op("pe", lambda e, si=si, hh=hh, h=h: e.matmul(
                        pOm[:, hh, :], lhsT=Sm[si][:, hh, :], rhs=va[vi][:, h, :], start=True, stop=(c == 0)),
                        reads=[b_Sm[si], b_va[vi]], writes=[b_pOm])
                    if c > 0:
                        P.op("pe", lambda e, hp=hp, hh=hh, h=h: e.matmul(
                            pOm[:, hh, :], lhsT=mqz[h][:, cols], rhs=Cb[hp][:, :], start=False, stop=True),
                            reads=[b_mqk, b_Cb[hp]], writes=[b_pOm])
                for hh in range(2):
                    h = 2 * hp + hh
                    P.op("pe", lambda e, si=si, hh=hh, h=h: e.matmul(
                        pU[:, hh, :], lhsT=ktm[si][:], rhs=va[vi][:, h, :], start=True, stop=True),
                        reads=[b_ktm[si], b_va[vi]], writes=[b_pU])
                for hh in range(2):
                    h = 2 * hp + hh
                    R = slice(hh * 64, (hh + 1) * 64)
                    if c == 0:
                        P.op("dve", lambda e, hp=hp, hh=hh, R=R: e.tensor_copy(out=Cst[hp][R, :], in_=pU[R, hh, :]),
                             reads=[b_pU], writes=[b_Cst[hp]])
                    else:
                        P.op("dve", lambda e, hp=hp, hh=hh, R=R, h=h: e.scalar_tensor_tensor(
                            out=Cst[hp][R, :], in0=Cst[hp][R, :], scalar=C.decbc[R, h, c:c + 1], in1=pU[R, hh, :],
                            op0=ALU.mult, op1=ALU.add), reads=[b_pU, b_Cst[hp], C.b_decbc], writes=[b_Cst[hp]])
                    if c < 31:
                        P.op("dve", lambda e, hp=hp, R=R, h=h: e.tensor_scalar(
                            out=Cb[hp][R, :], in0=Cst[hp][R, :], scalar1=C.decbc[R, h, c + 1:c + 2], scalar2=None,
                            op0=ALU.mult), reads=[b_Cst[hp], C.b_decbc], writes=[b_Cb[hp]])
                for hh in range(2):
                    P.op("dve", lambda e, hh=hh: e.bn_stats(out=bst[:, hh, :], in_=pOm[:, hh, 0:128]),
                         reads=[b_pOm], writes=[b_sm])
                    P.op("dve", lambda e, hh=hh: e.bn_aggr(out=bag[:, hh, :], in_=bst[:, hh, :]),
                         reads=[b_sm], writes=[b_sm])
                P.op("act", lambda e: e.activation(out=sm[:, :, 0:1], in_=pOm[:, :, 128:129], func=AF.Square),
                     reads=[b_pOm, b_sm], writes=[b_sm])
                P.op("dve", lambda e, hp=hp: e.tensor_tensor(
                    out=sm[:, :, 0:1], in0=sm[:, :, 0:1], in1=C.wthr[:, c, 4 + 2 * hp:6 + 2 * hp].unsqueeze(2),
                    op=ALU.max), reads=[C.b_wthr, b_sm], writes=[b_sm])
                P.op("dve", lambda e: e.scalar_tensor_tensor(
                    out=sm[:, :, 1:2], in0=sm[:, :, 0:1], scalar=EPS, in1=bag[:, :, 1:2], op0=ALU.mult, op1=ALU.add),
                    reads=[b_sm], writes=[b_sm])
                P.op("pool", lambda e: e.tensor_tensor(out=sm[:, :, 2:3], in0=sm[:, :, 1:2],
                                                       in1=C.neghalf[:].unsqueeze(1).to_broadcast([128, 2, 1]), op=ALU.pow),
                     reads=[b_sm, C.b_const], writes=[b_sm])
                for hh in range(2):
                    h = 2 * hp + hh
                    P.op("dve", lambda e, hh=hh, h=h: e.tensor_scalar(
                        out=hn[:, h * 128:(h + 1) * 128], in0=pOm[:, hh, 0:128], scalar1=bag[:, hh, 0:1],
                        scalar2=sm[:, hh, 2:3], op0=ALU.subtract, op1=ALU.mult),
                        reads=[b_pOm, b_sm], writes=[b_hn])
            yi = c % 2
            P.op("pool", lambda e: e.tensor_tensor(out=hn[:], in0=hn[:], in1=gln[:], op=ALU.mult),
                 reads=[b_hn, b_c], writes=[b_hn])
            P.op("dve", lambda e: e.tensor_tensor(out=ya[yi][:], in0=hn[:], in1=sgo[vi][:], op=ALU.mult),
                 reads=[b_hn, b_sgo[vi]], writes=[b_ya[yi]])
            for k in range(4):
                P.op("pe", lambda e, k=k: e.transpose(out=pyT[:, k, :], in_=ya[yi][:, k * 128:(k + 1) * 128],
                                                      identity=C.ident[:]),
                     reads=[b_ya[yi], C.b_const], writes=[b_pyT])
            P.op("act", lambda e: e.activation(out=yaT[yi][:], in_=pyT, func=AF.Copy), reads=[b_pyT],
                 writes=[b_yaT[yi]])
            P.op("sp", lambda e: e.dma_start(out=SC["yaT"][:, :, cols].rearrange("k p t -> p k t"), in_=yaT[yi][:]),
                 reads=[b_yaT[yi]], writes=[DB("yaT")[c]], dma=True)

        VF = sb("VF", [128, 32, 8 * 65], BF16)
        b_VF = P.buf("VF")
        P.op("sp", lambda e: e.dma_start(out=VF[:], in_=SC["vaugF"].rearrange("(j p) h e -> p j (h e)", p=128)),
             reads=DB("vaugF"), writes=[b_VF], dma=True)
        QA = [sb("QA%d" % i, [70, S], BF16) for i in range(2)]
        KA = [sb("KA%d" % i, [70, S], BF16) for i in range(2)]
        b_QA = P.bufs_n("QA", 2)
        b_KA = P.bufs_n("KA", 2)
        PT = [sb("PT%d" % i, [128, 512], BF16) for i in range(2)]
        b_PT = P.bufs_n("PT", 2)
        rec = sb("rec", [1, 512], F32)
        b_rec = P.buf("rec")
        osb = sb("osb", [65, 512], F32)
        b_osb = P.buf("osb")
        ybt = [sb("ybt%d" % i, [65, 512], BF16) for i in range(2)]
        b_ybt = P.bufs_n("ybt", 2)
        pS = [ps("pS%d" % i, [128, 512], F32) for i in range(2)]
        b_pS = P.bufs_n("pS", 2)
        pO = ps("pO", [128, 512], F32)
        b_pO = P.buf("pO")
        pbc = ps("pbc", [65, 512], F32)
        b_pbc = P.buf("pbc")
        cnt = {"s": 0, "y": 0}

        def fox_load(h):
            hb = h % 2
            P.op("sp", lambda e: e.dma_start(out=QA[hb][:], in_=SC["qaug"][h]), reads=DB("aug"), writes=[b_QA[hb]], dma=True)
            P.op("sp", lambda e: e.dma_start(out=KA[hb][:], in_=SC["kaug"][h]), reads=DB("aug"), writes=[b_KA[hb]], dma=True)

        def fox_unit(h, i):
            hb = h % 2
            nkb = 4 * i + 4
            for j in range(nkb):
                jj = j - 4 * i
                kc = slice(j * 128, (j + 1) * 128)
                sj = cnt["s"] % 2
                cnt["s"] += 1
                rd = [b_KA[hb], b_QA[hb]]
                if jj < 0:
                    qs, wq = 0, 512
                    P.op("pe", lambda e, kc=kc, sj=sj: e.matmul(
                        pS[sj][:, 0:512], lhsT=KA[hb][:, kc], rhs=QA[hb][:, i * 512:(i + 1) * 512], start=True, stop=True),
                        reads=rd, writes=[b_pS[sj]])
                else:
                    qs = jj * 128
                    wq = 512 - qs
                    q0 = i * 512 + qs
                    P.op("pe", lambda e, sj=sj: e.matmul(pS[sj][:, 0:128], lhsT=C.ident[:], rhs=trim[:], start=True, stop=False),
                         reads=[C.b_const, b_c], writes=[b_pS[sj]])
                    P.op("pe", lambda e, kc=kc, sj=sj, q0=q0: e.matmul(
                        pS[sj][:, 0:128], lhsT=KA[hb][:, kc], rhs=QA[hb][:, q0:q0 + 128], start=False, stop=True),
                        reads=rd, writes=[b_pS[sj]])
                    if wq > 128:
                        P.op("pe", lambda e, kc=kc, sj=sj, q0=q0, wq=wq: e.matmul(
                            pS[sj][:, 128:wq], lhsT=KA[hb][:, kc], rhs=QA[hb][:, q0 + 128:q0 + wq], start=True, stop=True),
                            reads=rd, writes=[b_pS[sj]])
                P.op("act", lambda e, sj=sj, wq=wq: e.activation(out=PT[sj][:, 0:wq], in_=pS[sj][:, 0:wq], func=AF.Exp),
                     reads=[b_pS[sj]], writes=[b_PT[sj]])
                P.op("pe", lambda e, j=j, sj=sj, qs=qs, wq=wq: e.matmul(
                    pO[0:65, qs:512], lhsT=VF[:, j, h * 65:(h + 1) * 65], rhs=PT[sj][:, 0:wq],
                    start=(j == 0), stop=(j == nkb - 1)),
                    reads=[b_VF, b_PT[sj]], writes=[b_pO])
            P.op("act", lambda e: e.activation(out=osb[:], in_=pO[0:65, :], func=AF.Copy), reads=[b_pO], writes=[b_osb])
            P.op("dve", lambda e: e.reciprocal(out=rec[0:1, :], in_=osb[0:1, :]), reads=[b_osb], writes=[b_rec])
            P.op("pe", lambda e: e.matmul(pbc[:], lhsT=onesr[0:1, :], rhs=rec[0:1, :], start=True, stop=True),
                 reads=[b_rec, b_c], writes=[b_pbc])
            yi = cnt["y"] % 2
            cnt["y"] += 1
            P.op("dve", lambda e: e.tensor_tensor(out=ybt[yi][:], in0=osb[:], in1=pbc[:], op=ALU.mult),
                 reads=[b_osb, b_pbc], writes=[b_ybt[yi]])
            P.op("sp", lambda e: e.dma_start(
                out=SC["ybT"][h // 2, (h % 2) * 64:(h % 2) * 64 + 64, i * 512:(i + 1) * 512], in_=ybt[yi][1:65, :]),
                reads=[b_ybt[yi]], writes=[DB("ybT")[(h * 8 + i) % 32]], dma=True)

        fox_load(0)
        u = 0
        for h in range(8):
            if h + 1 < 8:
                fox_load(h + 1)
            for i in range(8):
                fox_unit(h, i)
                u += 1
                if u % 2 == 0:
                    mlstm_chunk(u // 2 - 1)
        P.flush()


def merge_pass(P, nc, C, I, SC, DB, h1, h1_b, h2, h2_b):
    with contextlib.ExitStack() as st:
        sb = lambda name, shape, dt: st.enter_context(nc.sbuf_tensor("mg" + name, shape, dt))
        ps = lambda name, shape, dt: st.enter_context(nc.psum_tensor("mg" + name, shape, dt))
        Wa = sb("Wa", [128, 4, D], BF16)
        Wb = sb("Wb", [128, 4, D], BF16)
        Wo = sb("Wo", [128, 8, D], BF16)
        b_W = P.buf("mgW")
        P.op("pool", lambda e: e.dma_start(out=Wa[:], in_=I["w_branch_a"].rearrange("(k p) d -> p k d", p=128)),
             writes=[b_W], dma=True)
        P.op("pool", lambda e: e.dma_start(out=Wb[:], in_=I["w_branch_b"].rearrange("(k p) d -> p k d", p=128)),
             writes=[b_W], dma=True)
        P.op("pool", lambda e: e.dma_start(out=Wo[:], in_=I["w_out"].rearrange("(k p) d -> p k d", p=128)),
             writes=[b_W], dma=True)
        gpost = sb("gpost", [128, D], F32)
        P.op("sp", lambda e: e.dma_start(out=gpost[:], in_=I["mix_post_g"].partition_broadcast(128)),
             writes=[b_W], dma=True)
        yaT = [sb("yaT%d" % i, [128, 4, 128], BF16) for i in range(2)]
        ybT = [sb("ybT%d" % i, [128, 4, 128], BF16) for i in range(2)]
        gab = [sb("gab%d" % i, [128, 2, D], BF16) for i in range(2)]
        hin = [sb("hin%d" % i, [128, D], F32) for i in range(2)]
        b_in = P.bufs_n("mgin", 2)
        t1 = sb("t1", [128, D], F32)
        t2 = sb("t2", [128, D], F32)
        mb = sb("mb", [128, D], BF16)
        mT = sb("mT", [128, 8, 128], BF16)
        junk = sb("junk", [128, D], BF16)
        hout = [sb("hout%d" % i, [128, D], F32) for i in range(2)]
        stat = sb("stat", [128, 6], F32)
        b_t1, b_t2, b_mb, b_mT, b_junk = [P.buf(n) for n in ("t1", "t2", "mb", "mT", "mgjunk")]
        b_hout = P.bufs_n("hout", 2)
        b_stat = P.bufs_n("mgstat", 2)
        pA = ps("pA", [128, D], F32)
        pB = ps("pB", [128, D], F32)
        pO = ps("pO", [128, D], F32)
        pt = ps("pt", [128, 8, 128], BF16)
        b_pA, b_pB, b_pO, b_pt = [P.buf(n) for n in ("pA", "pB", "mgpO", "mgpt")]
        h1v = h1.rearrange("(n p) d -> n p d", p=128)
        h2v = h2.rearrange("(n p) d -> n p d", p=128)
        for n in range(32):
            ib = n % 2
            rows = slice(n * 128, (n + 1) * 128)
            cols = rows
            P.op("sp", lambda e, ib=ib, cols=cols: e.dma_start(out=yaT[ib][:], in_=SC["yaT"][:, :, cols].rearrange("k p t -> p k t")),
                 reads=[DB("yaT")[n]], writes=[b_in[ib]], dma=True)
            P.op("sp", lambda e, ib=ib, cols=cols: e.dma_start(out=ybT[ib][:], in_=SC["ybT"][:, :, cols].rearrange("k p t -> p k t")),
                 reads=DB("ybT"), writes=[b_in[ib]], dma=True)
            P.op("sp", lambda e, ib=ib, rows=rows: e.dma_start(out=gab[ib][:, 0, :], in_=SC["ga"][rows]),
                 reads=[DB("gab")[n]], writes=[b_in[ib]], dma=True)
            P.op("sp", lambda e, ib=ib, rows=rows: e.dma_start(out=gab[ib][:, 1, :], in_=SC["gb"][rows]),
                 reads=[DB("gab")[n]], writes=[b_in[ib]], dma=True)
            P.op("sp", lambda e, ib=ib, n=n: e.dma_start(out=hin[ib][:], in_=h1v[n]),
                 reads=[h1_b[n]], writes=[b_in[ib]], dma=True)
            for hf in range(2):
                hs = slice(hf * 512, (hf + 1) * 512)
                for k in range(4):
                    P.op("pe", lambda e, k=k, hs=hs, ib=ib: e.matmul(pA[:, hs], lhsT=yaT[ib][:, k, :], rhs=Wa[:, k, hs],
                                                                     start=(k == 0), stop=(k == 3)),
                         reads=[b_in[ib], b_W], writes=[b_pA])
                for k in range(4):
                    P.op("pe", lambda e, k=k, hs=hs, ib=ib: e.matmul(pB[:, hs], lhsT=ybT[ib][:, k, :], rhs=Wb[:, k, hs],
                                                                     start=(k == 0), stop=(k == 3)),
                         reads=[b_in[ib], b_W], writes=[b_pB])
            P.op("dve", lambda e, ib=ib: e.tensor_tensor(out=t1[:], in0=pA[:], in1=gab[ib][:, 0, :], op=ALU.mult),
                 reads=[b_pA, b_in[ib]], writes=[b_t1])
            P.op("dve", lambda e, ib=ib: e.tensor_tensor(out=t2[:], in0=pB[:], in1=gab[ib][:, 1, :], op=ALU.mult),
                 reads=[b_pB, b_in[ib]], writes=[b_t2])
            P.op("pool", lambda e: e.tensor_tensor(out=mb[:], in0=t1[:], in1=t2[:], op=ALU.add),
                 reads=[b_t1, b_t2], writes=[b_mb])
            for k in range(8):
                P.op("pe", lambda e, k=k: e.transpose(out=pt[:, k, :], in_=mb[:, k * 128:(k + 1) * 128], identity=C.ident[:]),
                     reads=[b_mb, C.b_const], writes=[b_pt])
            P.op("act", lambda e: e.activation(out=mT[:], in_=pt[:], func=AF.Copy), reads=[b_pt], writes=[b_mT])
            for hf in range(2):
                hs = slice(hf * 512, (hf + 1) * 512)
                for k in range(8):
                    P.op("pe", lambda e, k=k, hs=hs: e.matmul(pO[:, hs], lhsT=mT[:, k, :], rhs=Wo[:, k, hs],
                                                              start=(k == 0), stop=(k == 7)),
                         reads=[b_mT, b_W], writes=[b_pO])
            si = n % 2
            ss, var, rstd = (stat[:, 3 * si + j:3 * si + j + 1] for j in range(3))
            rms_stats(P, C, pO[:], b_pO, junk[:], b_junk, ss, var, rstd, b_stat[si])
            P.op("dve", lambda e, ib=ib, rstd=rstd: e.scalar_tensor_tensor(
                out=hout[ib][:], in0=pO[:], scalar=rstd, in1=gpost[:], op0=ALU.mult, op1=ALU.mult),
                reads=[b_pO, b_stat[si], b_W], writes=[b_hout[ib]])
            P.op("pool", lambda e, ib=ib: e.tensor_tensor(out=hout[ib][:], in0=hout[ib][:], in1=hin[ib][:], op=ALU.add),
                 reads=[b_hout[ib], b_in[ib]], writes=[b_hout[ib]])
            P.op("sp", lambda e, ib=ib, n=n: e.dma_start(out=h2v[n], in_=hout[ib][:]),
                 reads=[b_hout[ib]], writes=[h2_b[n]], dma=True)
        P.flush()


def ple_pass(P, nc, C, I, uT3, uT3_b, h3, h3_b, out):
    with contextlib.ExitStack() as st:
        sb = lambda name, shape, dt: st.enter_context(nc.sbuf_tensor("pl" + name, shape, dt))
        ps = lambda name, shape, dt: st.enter_context(nc.psum_tensor("pl" + name, shape, dt))
        Wg = sb("Wg", [128, 8, D], BF16)
        Wp = sb("Wp", [128, 2, D], BF16)
        b_W = P.buf("plW")
        P.op("pool", lambda e: e.dma_start(out=Wg[:], in_=I["ple_w_gate"].rearrange("(k p) d -> p k d", p=128)),
             writes=[b_W], dma=True)
        P.op("pool", lambda e: e.dma_start(out=Wp[:], in_=I["ple_w_proj"].rearrange("(k p) d -> p k d", p=128)),
             writes=[b_W], dma=True)
        gpost = sb("gpost", [128, D], F32)
        bg = sb("bg", [128, D], F32)
        P.op("sp", lambda e: e.dma_start(out=gpost[:], in_=I["ple_post_g"].partition_broadcast(128)),
             writes=[b_W], dma=True)
        P.op("sp", lambda e: e.dma_start(out=bg[:], in_=I["ple_b_gate"].partition_broadcast(128)),
             writes=[b_W], dma=True)
        uT = [sb("uT%d" % i, [128, 8, 128], BF16) for i in range(2)]
        pin = [sb("pin%d" % i, [128, 256], F32) for i in range(2)]
        hin = [sb("hin%d" % i, [128, D], F32) for i in range(2)]
        b_in = P.bufs_n("plin", 2)
        pb = sb("pb", [128, 256], BF16)
        pT = sb("pT", [128, 2, 128], BF16)
        gt = sb("gt", [128, D], F32)
        ge = sb("ge", [128, D], F32)
        junk = sb("junk", [128, D], BF16)
        hout = [sb("hout%d" % i, [128, D], F32) for i in range(2)]
        stat = sb("stat", [128, 6], F32)
        b_pb, b_pT, b_gt, b_ge, b_junk = [P.buf(n) for n in ("pb", "pT", "gt", "ge", "pljunk")]
        b_hout = P.bufs_n("plhout", 2)
        b_stat = P.bufs_n("plstat", 2)
        pG = ps("pG", [128, D], F32)
        pE = ps("pE", [128, D], F32)
        ptp = ps("ptp", [128, 2, 128], BF16)
        b_pG, b_pE, b_ptp = [P.buf(n) for n in ("pG", "pE", "ptp")]
        pv = I["p"].rearrange("(n p) d -> n p d", p=128)
        h3v = h3.rearrange("(n p) d -> n p d", p=128)
        ov = out.rearrange("(n p) d -> n p d", p=128)
        for n in range(32):
            ib = n % 2
            cols = slice(n * 128, (n + 1) * 128)
            P.op("sp", lambda e, ib=ib, cols=cols: e.dma_start(out=uT[ib][:], in_=uT3[:, :, cols].rearrange("k p t -> p k t")),
                 reads=[uT3_b[n]], writes=[b_in[ib]], dma=True)
            P.op("sp", lambda e, ib=ib, n=n: e.dma_start(out=pin[ib][:], in_=pv[n]), writes=[b_in[ib]], dma=True)
            P.op("sp", lambda e, ib=ib, n=n: e.dma_start(out=hin[ib][:], in_=h3v[n]), reads=[h3_b[n]],
                 writes=[b_in[ib]], dma=True)
            P.op("act", lambda e, ib=ib: e.activation(out=pb[:], in_=pin[ib][:], func=AF.Copy), reads=[b_in[ib]], writes=[b_pb])
            for k in range(2):
                P.op("pe", lambda e, k=k: e.transpose(out=ptp[:, k, :], in_=pb[:, k * 128:(k + 1) * 128], identity=C.ident[:]),
                     reads=[b_pb, C.b_const], writes=[b_ptp])
            P.op("dve", lambda e: e.tensor_copy(out=pT[:], in_=ptp[:]), reads=[b_ptp], writes=[b_pT])
            for hf in range(2):
                hs = slice(hf * 512, (hf + 1) * 512)
                for k in range(8):
                    P.op("pe", lambda e, k=k, hs=hs, ib=ib: e.matmul(pG[:, hs], lhsT=uT[ib][:, k, :], rhs=Wg[:, k, hs],
                                                                     start=(k == 0), stop=(k == 7)),
                         reads=[b_in[ib], b_W], writes=[b_pG])
                for k in range(2):
                    P.op("pe", lambda e, k=k, hs=hs: e.matmul(pE[:, hs], lhsT=pT[:, k, :], rhs=Wp[:, k, hs],
                                                              start=(k == 0), stop=(k == 1)),
                         reads=[b_pT, b_W], writes=[b_pE])
            P.op("dve", lambda e: e.tensor_tensor(out=gt[:], in0=pG[:], in1=bg[:], op=ALU.add),
                 reads=[b_pG, b_W], writes=[b_gt])
            P.op("act", lambda e: e.activation(out=gt[:], in_=gt[:], func=AF.Sigmoid), reads=[b_gt], writes=[b_gt])
            P.op("dve", lambda e: e.tensor_tensor(out=ge[:], in0=gt[:], in1=pE[:], op=ALU.mult),
                 reads=[b_gt, b_pE], writes=[b_ge])
            si = n % 2
            ss, var, rstd = (stat[:, 3 * si + j:3 * si + j + 1] for j in range(3))
            rms_stats(P, C, ge[:], b_ge, junk[:], b_junk, ss, var, rstd, b_stat[si])
            P.op("dve", lambda e, ib=ib, rstd=rstd: e.scalar_tensor_tensor(
                out=hout[ib][:], in0=ge[:], scalar=rstd, in1=gpost[:], op0=ALU.mult, op1=ALU.mult),
                reads=[b_ge, b_stat[si], b_W], writes=[b_hout[ib]])
            P.op("pool", lambda e, ib=ib: e.tensor_tensor(out=hout[ib][:], in0=hout[ib][:], in1=hin[ib][:], op=ALU.add),
                 reads=[b_hout[ib], b_in[ib]], writes=[b_hout[ib]])
            P.op("sp", lambda e, ib=ib, n=n: e.dma_start(out=ov[n], in_=hout[ib][:]), reads=[b_hout[ib]], dma=True)
        P.flush()

def build_program(debug=False, stage=99, only=None):
    nc = bass.Bass("TRN2", target_bir_lowering=False)
    I = {}

    def din(name, shape):
        I[name] = nc.dram_tensor(name, shape, F32, kind="ExternalInput").ap()
        return I[name]

    din("x", [S, D])
    din("p", [S, 256])
    for nm in ("ffn1", "ffn2"):
        din(nm + "_pre_g", [1, D])
        din(nm + "_w_gate", [D, DFF])
        din(nm + "_w_up", [D, DFF])
        din(nm + "_w_down", [DFF, D])
        din(nm + "_post_g", [1, D])
    din("mix_pre_g", [1, D])
    din("w_in", [D, INW])
    din("conv_w", [4, 512])
    din("conv_b", [1, 512])
    din("mlstm_i_bias", [1, 4])
    din("mlstm_f_bias", [1, 4])
    din("mlstm_norm_g", [1, 512])
    din("fox_f_bias", [1, 8])
    din("branch_gate_bias", [1, 2048])
    din("w_branch_a", [512, D])
    din("w_branch_b", [512, D])
    din("w_out", [D, D])
    din("mix_post_g", [1, D])
    din("ple_pre_g", [1, D])
    din("ple_w_gate", [D, D])
    din("ple_b_gate", [1, D])
    din("ple_w_proj", [256, D])
    din("ple_post_g", [1, D])

    skind = "ExternalOutput" if debug else "Internal"

    def dscr(name, shape, dt):
        return nc.dram_tensor(name, shape, dt, kind=skind).ap()

    out = nc.dram_tensor("out", [S, D], F32, kind="ExternalOutput").ap()
    h1 = dscr("h1", [S, D], F32)
    uT1 = dscr("uT1", [8, 128, S], BF16)
    gpre = dscr("gpre", [72, S], F32)
    SC = {
        "mqkT": dscr("mqkT", [4, 128, S], BF16),
        "vaugM": dscr("vaugM", [S, 4, 129], BF16),
        "so": dscr("so", [S, 512], BF16),
        "qaug": dscr("qaug", [8, 70, S], BF16),
        "kaug": dscr("kaug", [8, 70, S], BF16),
        "vaugF": dscr("vaugF", [S, 8, 65], BF16),
        "ga": dscr("ga", [S, D], BF16),
        "gb": dscr("gb", [S, D], BF16),
        "yaT": dscr("yaT", [4, 128, S], BF16),
        "ybT": dscr("ybT", [4, 128, S], BF16),
    }
    h2 = dscr("h2", [S, D], F32)
    h3 = dscr("h3", [S, D], F32)
    uT3 = dscr("uT3", [8, 128, S], BF16)

    with contextlib.ExitStack() as st:
        P = Prog(nc, st)
        C = Ctx()
        C.db = {}

        def db(name):
            if name not in C.db:
                C.db[name] = P.bufs_n("D" + name, 32)
            return C.db[name]

        setup_consts(P, nc, st, C)
        C.wthr = st.enter_context(nc.sbuf_tensor("wthr", [128, 32, 8], F32))
        C.decbc = st.enter_context(nc.sbuf_tensor("decbc", [128, 4, 32], F32))
        C.b_wthr = P.buf("wthr")
        C.b_decbc = P.buf("decbc")
        def want(name, st_no):
            return (name in only) if only is not None else (stage >= st_no)

        if want("ffn1", 1):
            ffn_pass(P, nc, C, "f1", I["x"], I["ffn1_w_gate"], I["ffn1_w_up"], I["ffn1_w_down"],
                     I["ffn1_pre_g"], I["ffn1_post_g"], h1, I["mix_pre_g"], uT1,
                     db("x"), db("h1"), db("uT1"), gate_w=I["w_in"], gate_dst=gpre, gate_b=db("gpre"))
        if want("gp", 2):
            aug_b = P.buf("augrows")
            gp_stage(P, nc, C, I, gpre, db("gpre"), SC["qaug"], SC["kaug"], aug_b)
            db("aug").append(aug_b)
        if want("win", 2):
            win_pass(P, nc, C, I, uT1, db("uT1"), SC, db)
        if want("mix", 3):
            mix_pass(P, nc, C, I, SC, db)
        if want("merge", 4):
            merge_pass(P, nc, C, I, SC, db, h1, db("h1"), h2, db("h2"))
        if want("ffn2", 5):
            ffn_pass(P, nc, C, "f2", h2, I["ffn2_w_gate"], I["ffn2_w_up"], I["ffn2_w_down"],
                     I["ffn2_pre_g"], I["ffn2_post_g"], h3, I["ple_pre_g"], uT3,
                     db("h2"), db("h3"), db("uT3"))
        if want("ple", 6):
            ple_pass(P, nc, C, I, uT3, db("uT3"), h3, db("h3"), out)
        P.flush(final=True)
    return nc

IN_NAMES = ["x", "p", "ffn1_pre_g", "ffn1_w_gate", "ffn1_w_up", "ffn1_w_down", "ffn1_post_g",
            "mix_pre_g", "w_in", "conv_w", "conv_b", "mlstm_i_bias", "mlstm_f_bias", "mlstm_norm_g",
            "fox_f_bias", "branch_gate_bias", "w_branch_a", "w_branch_b", "w_out", "mix_post_g",
            "ffn2_pre_g", "ffn2_w_gate", "ffn2_w_up", "ffn2_w_down", "ffn2_post_g",
            "ple_pre_g", "ple_w_gate", "ple_b_gate", "ple_w_proj", "ple_post_g"]


def make_in_maps(inputs, cores):
    maps = []
    shared = {}
    for k in IN_NAMES:
        if k in ("x", "p"):
            continue
        shared[k] = np.ascontiguousarray(np.asarray(inputs[k])[0], dtype=np.float32)
    x = np.asarray(inputs["x"])
    p = np.asarray(inputs["p"])
    for b in cores:
        m = dict(shared)
        m["x"] = np.ascontiguousarray(x[b], dtype=np.float32)
        m["p"] = np.ascontiguousarray(p[0, b], dtype=np.float32)
        maps.append(m)
    return maps


def kernel(**inputs):
    nc = build_program()
    maps = make_in_maps(inputs, list(range(8)))
    res = run_bass_kernel_spmd(nc, maps, core_ids=list(range(8)))
    return np.stack([np.asarray(r["out"], dtype=np.float32) for r in res.results], axis=0)
```

```python
import contextlib
import numpy as np
import concourse.bass as bass
import concourse.mybir as mybir
from concourse.bass_utils import run_bass_kernel_spmd

F32 = mybir.dt.float32
BF16 = mybir.dt.bfloat16
AF = mybir.ActivationFunctionType
ALU = mybir.AluOpType
AX = mybir.AxisListType

S = 4096
D = 1024
DFF = 2816
NFC = DFF // 128
NT = 8
TT = 512
EPS = 1e-6
INW = 5136

ENGS = ("pe", "act", "dve", "pool", "sp")


class Buf:
    __slots__ = ("name", "w", "r")

    def __init__(self, name=""):
        self.name = name
        self.w = None
        self.r = []


class Op:
    __slots__ = ("eng", "fn", "deps", "inc", "cnt", "dma", "sem", "emitted")

    def __init__(self, eng, fn, dma=False):
        self.eng = eng
        self.fn = fn
        self.deps = []
        self.inc = False
        self.cnt = 0
        self.dma = dma
        self.sem = None
        self.emitted = False


class Prog:
    def __init__(self, nc, st, n_dma_sems=20):
        self.nc = nc
        self.pending = {e: [] for e in ENGS}
        self.bufs = []
        self.nd = n_dma_sems
        self.esem = {e: st.enter_context(nc.semaphore("s_" + e)) for e in ENGS}
        self.dsem = {}
        for e in ("sp", "pool"):
            for s in range(n_dma_sems):
                self.dsem[(e, s)] = st.enter_context(nc.semaphore("d_%s_%d" % (e, s)))
        self.ecnt = {e: 0 for e in ENGS}
        self.dcnt = {e: 0 for e in ENGS}
        self.waited = {e: {} for e in ENGS}
        self.n_ops = 0

    def buf(self, name=""):
        b = Buf(name)
        self.bufs.append(b)
        return b

    def bufs_n(self, name, n):
        return [self.buf("%s%d" % (name, i)) for i in range(n)]

    def op(self, eng, fn, reads=(), writes=(), dma=False):
        o = Op(eng, fn, dma)
        seen = set()
        cand = []
        for b in reads:
            if b.w is not None:
                cand.append(b.w)
        for b in writes:
            if b.w is not None:
                cand.append(b.w)
            cand.extend(b.r)
        for d in cand:
            if d is o or id(d) in seen:
                continue
            seen.add(id(d))
            if d.eng == "pe" and eng == "pe" and not d.dma and not dma:
                continue
            o.deps.append(d)
            if not d.emitted:
                d.inc = True
        for b in reads:
            b.r.append(o)
        for b in writes:
            b.w = o
            b.r = []
        self.pending[eng].append(o)
        self.n_ops += 1
        return o

    def flush(self, final=False):
        nc = self.nc
        for b in self.bufs:
            if b.w is not None and not b.w.emitted:
                b.w.inc = True
            for r in b.r:
                if not r.emitted:
                    r.inc = True
        for e in ENGS:
            for o in self.pending[e]:
                if o.dma:
                    k = self.dcnt[e]
                    o.sem = (e, k % self.nd)
                    o.cnt = 16 * (k // self.nd + 1)
                    self.dcnt[e] = k + 1
                elif o.inc:
                    self.ecnt[e] += 1
                    o.cnt = self.ecnt[e]
        pending = self.pending
        self.pending = {e: [] for e in ENGS}

        def run(ename, eng):
            waited = self.waited[ename]
            for o in pending[ename]:
                for d in o.deps:
                    key = d.sem if d.dma else d.eng
                    if waited.get(key, 0) >= d.cnt:
                        continue
                    assert d.cnt > 0, (d.eng, ename)
                    eng.wait_ge(self.dsem[key] if d.dma else self.esem[key], d.cnt)
                    waited[key] = d.cnt
                if o.dma:
                    if o.cnt > 16 and waited.get(o.sem, 0) < o.cnt - 16:
                        eng.wait_ge(self.dsem[o.sem], o.cnt - 16)
                        waited[o.sem] = o.cnt - 16
                    o.fn(eng).then_inc(self.dsem[o.sem], 16)
                else:
                    ins = o.fn(eng)
                    if o.inc:
                        ins.then_inc(self.esem[o.eng], 1)
                o.emitted = True
            if ename == "sp" and final:
                for q in ("sp", "pool"):
                    k = self.dcnt[q]
                    for sl in range(min(self.nd, k)):
                        last = 16 * ((k - 1 - sl) // self.nd + 1)
                        eng.wait_ge(self.dsem[(q, sl)], last)

        with nc.Block() as block:
            @block.tensor
            def _(eng):
                run("pe", eng)

            @block.scalar
            def _(eng):
                run("act", eng)

            @block.vector
            def _(eng):
                run("dve", eng)

            @block.gpsimd
            def _(eng):
                run("pool", eng)

            @block.sync
            def _(eng):
                run("sp", eng)


def bcast_last(ap2d, n):
    return ap2d.unsqueeze(2).to_broadcast([ap2d.shape[0], ap2d.shape[1], n])


class Ctx:
    pass


def load_w_kmajor(P, nc, dst, src2d, n_kc, ncols, bufs, col_chunk=1408):
    v = src2d.rearrange("(kc p) n -> kc p n", p=128)
    mdl = 4 * col_chunk
    for k in range(n_kc):
        P.op("pool", lambda e, k=k: e.dma_start(out=dst[:, k, :], in_=v[k], max_dma_last_dim=mdl),
             writes=[bufs[k]], dma=True)


def setup_consts(P, nc, st, C):
    sb = lambda name, shape, dt: st.enter_context(nc.sbuf_tensor(name, shape, dt))
    C.identf = sb("identf", [128, 128], F32)
    C.ident = sb("ident", [128, 128], BF16)
    C.neghalf = sb("neghalf", [128, 1], F32)
    C.b_const = P.buf("const")
    identf, ident = C.identf, C.ident
    P.op("pool", lambda e: e.memset(identf[:], 0.0), writes=[C.b_const])
    P.op("pool", lambda e: e.affine_select(out=identf[:], in_=identf[:], pattern=[[-1, 128]],
                                             compare_op=ALU.not_equal, fill=1.0, base=0,
                                             channel_multiplier=1),
         reads=[C.b_const], writes=[C.b_const])
    P.op("dve", lambda e: e.tensor_copy(out=ident[:], in_=identf[:]), reads=[C.b_const], writes=[C.b_const])
    P.op("pool", lambda e: e.memset(C.neghalf[:], -0.5), reads=[C.b_const], writes=[C.b_const])


def rms_stats(P, C, src_ap, src_buf, junk, b_junk, ss, var, rstd, b_stat, n_feat=D):
    P.op("act", lambda e: e.activation(out=junk, in_=src_ap, func=AF.Square, accum_out=ss),
         reads=[src_buf], writes=[b_junk, b_stat])
    P.op("dve", lambda e: e.tensor_scalar(out=var, in0=ss, scalar1=1.0 / n_feat, scalar2=EPS,
                                          op0=ALU.mult, op1=ALU.add),
         reads=[b_stat], writes=[b_stat])
    P.op("pool", lambda e: e.tensor_tensor(out=rstd, in0=var, in1=C.neghalf[:], op=ALU.pow),
         reads=[b_stat, C.b_const], writes=[b_stat])


def ffn_pass(P, nc, C, tag, src_h, w_gate, w_up, w_down, pre_g, post_g, dst_h, next_g, dst_uT,
             src_b, dst_b, uT_b, gate_w=None, gate_dst=None, gate_b=None):
    with contextlib.ExitStack() as st:
        sb = lambda name, shape, dt: st.enter_context(nc.sbuf_tensor(tag + name, shape, dt))
        ps = lambda name, shape, dt: st.enter_context(nc.psum_tensor(tag + name, shape, dt))
        Wg = sb("Wg", [128, 8, DFF], BF16)
        Wu = sb("Wu", [128, 8, DFF], BF16)
        Wd = sb("Wd", [128, NFC, D], BF16)
        b_Wg = P.bufs_n("Wg", 8)
        b_Wu = P.bufs_n("Wu", 8)
        b_Wd = P.bufs_n("Wd", 2)
        load_w_kmajor(P, nc, Wg, w_gate, 8, DFF, b_Wg)
        load_w_kmajor(P, nc, Wu, w_up, 8, DFF, b_Wu)
        wdv = w_down.rearrange("(fc p) d -> p fc d", p=128)
        for hh in range(2):
            P.op("pool", lambda e, hh=hh: e.dma_start(out=Wd[:, hh * 11:(hh + 1) * 11, :],
                                                       in_=wdv[:, hh * 11:(hh + 1) * 11, :]),
                 writes=[b_Wd[hh]], dma=True)
        gpre = sb("gpre", [128, 8], F32)
        gnext = sb("gnext", [128, 8], F32)
        gpost = sb("gpost", [128, D], F32)
        b_par = P.buf("par")
        P.op("sp", lambda e: e.dma_start(out=gpre[:], in_=pre_g.rearrange("o (k p) -> p (o k)", p=128),
                                         allow_slow_non_contiguous=True),
             writes=[b_par], dma=True)
        P.op("sp", lambda e: e.dma_start(out=gnext[:], in_=next_g.rearrange("o (k p) -> p (o k)", p=128),
                                         allow_slow_non_contiguous=True),
             writes=[b_par], dma=True)
        P.op("sp", lambda e: e.dma_start(out=gpost[:], in_=post_g.partition_broadcast(128)),
             writes=[b_par], dma=True)
        if gate_w is not None:
            Wgt = sb("Wgt", [128, 8, 72], BF16)
            b_Wgt = P.buf("Wgt")
            P.op("pool", lambda e: e.memset(Wgt[:], 0.0), writes=[b_Wgt])
            gv = gate_w.rearrange("(kc p) n -> p kc n", p=128)
            for (c0, n, d0) in ((1540, 4, 0), (1536, 4, 32), (3080, 8, 64)):
                P.op("pool", lambda e, c0=c0, n=n, d0=d0: e.dma_start(
                    out=Wgt[:, :, d0:d0 + n], in_=gv[:, :, c0:c0 + n]),
                    reads=[], writes=[b_Wgt], dma=True)
            gsb = [sb("gsb%d" % i, [72, 128], F32) for i in range(2)]
            b_gsb = P.bufs_n("gsb", 2)

        NXB = 3
        xb = [sb("xb%d" % i, [128, D], F32) for i in range(NXB)]
        b_xb = P.bufs_n("xb", NXB)
        ubf = [sb("ubf%d" % i, [128, D], BF16) for i in range(2)]
        b_ubf = P.bufs_n("ubf", 2)
        junk = sb("junk", [128, D], BF16)
        b_junk = P.buf("junk")
        uT = sb("uT", [128, 8, TT], BF16)
        b_uT = P.bufs_n("uT", 4)
        aT = sb("aT", [128, NFC, TT], BF16)
        b_aT = P.bufs_n("aT", NFC)
        sg = [sb("sg%d" % i, [128, TT], F32) for i in range(2)]
        b_sg = P.bufs_n("sg", 2)
        hst = [sb("hst%d" % i, [128, D], F32) for i in range(2)]
        b_hst = P.bufs_n("hst", 2)
        u2T = [sb("u2T%d" % i, [128, 8, 128], BF16) for i in range(2)]
        b_u2T = P.bufs_n("u2T", 2)
        NST = 6
        stat = sb("stat", [128, 3 * NST], F32)
        b_stat = P.bufs_n("stat", NST)

        pt = ps("pt", [128, 8, 128], BF16)
        b_pt = P.buf("pt")
        pg = [ps("pg%d" % i, [128, TT], F32) for i in range(2)]
        pu = [ps("pu%d" % i, [128, TT], F32) for i in range(2)]
        b_pg = P.bufs_n("pg", 2)
        b_pu = P.bufs_n("pu", 2)
        pys = [ps("py%d" % i, [128, 512], F32) for i in range(3)]
        b_pys = P.bufs_n("py", 3)
        if gate_w is not None:
            pgt = pg[0][0:72, 0:128]
            b_pgt = b_pg[0]

        src_v = src_h.rearrange("(n p) d -> n p d", p=128)
        dst_v = dst_h.rearrange("(n p) d -> n p d", p=128)
        cnt = {"x": 0, "u": 0, "st": 0, "h": 0, "u2": 0, "sg": 0, "gs": 0, "py": 0}

        def norm_T(h_ap, h_buf, gcol, out_ap, out_bufs):
            si = cnt["st"] % NST
            cnt["st"] += 1
            ss, var, rstd = (stat[:, 3 * si + j:3 * si + j + 1] for j in range(3))
            rms_stats(P, C, h_ap, h_buf, junk[:], b_junk, ss, var, rstd, b_stat[si])
            ui = cnt["u"] % 2
            cnt["u"] += 1
            u = ubf[ui]
            P.op("dve", lambda e: e.tensor_scalar(out=u[:], in0=h_ap, scalar1=rstd, scalar2=None, op0=ALU.mult),
                 reads=[h_buf, b_stat[si]], writes=[b_ubf[ui]])
            for k in range(8):
                P.op("pe", lambda e, k=k: e.transpose(out=pt[:, k, :], in_=u[:, k * 128:(k + 1) * 128],
                                                      identity=C.ident[:]),
                     reads=[b_ubf[ui], C.b_const], writes=[b_pt])
            P.op("dve", lambda e: e.tensor_tensor(out=out_ap, in0=pt[:], in1=bcast_last(gcol[:], 128), op=ALU.mult),
                 reads=[b_pt, b_par], writes=out_bufs)

        def pre(i):
            for s in range(4):
                n = i * 4 + s
                xi = cnt["x"] % NXB
                cnt["x"] += 1
                P.op("sp", lambda e, n=n, xi=xi: e.dma_start(out=xb[xi][:], in_=src_v[n]),
                     reads=[src_b[n]], writes=[b_xb[xi]], dma=True)
                norm_T(xb[xi][:], b_xb[xi], gpre, uT[:, :, s * 128:(s + 1) * 128], [b_uT[s]])

        def gateup(i):
            for f in range(NFC):
                j = f % 2
                for k in range(8):
                    P.op("pe", lambda e, k=k, f=f, j=j: e.matmul(
                        pg[j][:], lhsT=Wg[:, k, f * 128:(f + 1) * 128], rhs=uT[:, k, :],
                        start=(k == 0), stop=(k == 7)),
                        reads=[b_Wg[k]] + b_uT, writes=[b_pg[j]])
                for k in range(8):
                    P.op("pe", lambda e, k=k, f=f, j=j: e.matmul(
                        pu[j][:], lhsT=Wu[:, k, f * 128:(f + 1) * 128], rhs=uT[:, k, :],
                        start=(k == 0), stop=(k == 7)),
                        reads=[b_Wu[k]] + b_uT, writes=[b_pu[j]])
                si = cnt["sg"] % 2
                cnt["sg"] += 1
                P.op("act", lambda e, j=j, si=si: e.activation(out=sg[si][:], in_=pg[j][:], func=AF.Silu),
                     reads=[b_pg[j]], writes=[b_sg[si]])
                P.op("dve", lambda e, j=j, si=si, f=f: e.tensor_tensor(out=aT[:, f, :], in0=sg[si][:], in1=pu[j][:],
                                                                   op=ALU.mult),
                     reads=[b_sg[si], b_pu[j]], writes=[b_aT[f]])

        def down_post(i):
            for s in range(4):
                n = i * 4 + s
                pyh = []
                for hf in range(2):
                    pi = cnt["py"] % 3
                    cnt["py"] += 1
                    pyh.append((pys[pi], b_pys[pi]))
                    for f in range(NFC):
                        P.op("pe", lambda e, f=f, s=s, hf=hf, pi=pi: e.matmul(
                            pys[pi][:], lhsT=aT[:, f, s * 128:(s + 1) * 128],
                            rhs=Wd[:, f, hf * 512:(hf + 1) * 512], start=(f == 0), stop=(f == NFC - 1)),
                            reads=[b_aT[f], b_Wd[f // 11]], writes=[b_pys[pi]])
                xi = cnt["x"] % NXB
                cnt["x"] += 1
                P.op("sp", lambda e, n=n, xi=xi: e.dma_start(out=xb[xi][:], in_=src_v[n]),
                     reads=[src_b[n]], writes=[b_xb[xi]], dma=True)
                si = cnt["st"] % NST
                cnt["st"] += 1
                ss, var, rstd = (stat[:, 3 * si + j:3 * si + j + 1] for j in range(3))
                P.op("act", lambda e, ss=ss, t=pyh[0][0]: e.activation(out=junk[:, 0:512], in_=t[:], func=AF.Square,
                                                                      accum_out=ss),
                     reads=[pyh[0][1]], writes=[b_junk, b_stat[si]])
                P.op("act", lambda e, var=var, t=pyh[1][0]: e.activation(out=junk[:, 512:1024], in_=t[:], func=AF.Square,
                                                                        accum_out=var),
                     reads=[pyh[1][1]], writes=[b_junk, b_stat[si]])
                P.op("dve", lambda e, ss=ss, var=var: e.tensor_tensor(out=var, in0=ss, in1=var, op=ALU.add),
                     reads=[b_stat[si]], writes=[b_stat[si]])
                P.op("dve", lambda e, var=var: e.tensor_scalar(out=var, in0=var, scalar1=1.0 / D, scalar2=EPS,
                                                              op0=ALU.mult, op1=ALU.add),
                     reads=[b_stat[si]], writes=[b_stat[si]])
                P.op("pool", lambda e, var=var, rstd=rstd: e.tensor_tensor(out=rstd, in0=var, in1=C.neghalf[:], op=ALU.pow),
                     reads=[b_stat[si], C.b_const], writes=[b_stat[si]])
                hi = cnt["h"] % 2
                cnt["h"] += 1
                hb = hst[hi]
                for hf in range(2):
                    hs = slice(hf * 512, (hf + 1) * 512)
                    P.op("dve", lambda e, hb=hb, rstd=rstd, t=pyh[hf][0], hs=hs: e.scalar_tensor_tensor(
                        out=hb[:, hs], in0=t[:], scalar=rstd, in1=gpost[:, hs], op0=ALU.mult, op1=ALU.mult),
                        reads=[pyh[hf][1], b_stat[si], b_par], writes=[b_hst[hi]])
                P.op("dve", lambda e, hb=hb, xi=xi: e.scalar_tensor_tensor(
                    out=hb[:], in0=hb[:], scalar=0.5, in1=xb[xi][:], op0=ALU.mult, op1=ALU.add),
                    reads=[b_hst[hi], b_xb[xi]], writes=[b_hst[hi]])
                P.op("sp", lambda e, hb=hb, n=n: e.dma_start(out=dst_v[n], in_=hb[:]),
                     reads=[b_hst[hi]], writes=[dst_b[n]], dma=True)
                ui2 = cnt["u2"] % 2
                cnt["u2"] += 1
                norm_T(hb[:], b_hst[hi], gnext, u2T[ui2][:], [b_u2T[ui2]])
                P.op("sp", lambda e, ui2=ui2, n=n: e.dma_start(
                    out=dst_uT[:, :, n * 128:(n + 1) * 128].rearrange("k p t -> p k t"), in_=u2T[ui2][:]),
                    reads=[b_u2T[ui2]], writes=[uT_b[n]], dma=True)
                if gate_w is not None:
                    for k in range(8):
                        P.op("pe", lambda e, k=k, ui2=ui2: e.matmul(
                            pgt, lhsT=Wgt[:, k, :], rhs=u2T[ui2][:, k, :], start=(k == 0), stop=(k == 7)),
                            reads=[b_Wgt, b_u2T[ui2]], writes=[b_pgt])
                    gi = cnt["gs"] % 2
                    cnt["gs"] += 1
                    P.op("act", lambda e, gi=gi: e.activation(out=gsb[gi][:], in_=pgt, func=AF.Copy),
                         reads=[b_pgt], writes=[b_gsb[gi]])
                    P.op("sp", lambda e, gi=gi, n=n: e.dma_start(out=gate_dst[:, n * 128:(n + 1) * 128], in_=gsb[gi][:]),
                         reads=[b_gsb[gi]], writes=[gate_b[n]], dma=True)

        pre(0)
        for i in range(NT):
            gateup(i)
            if i + 1 < NT:
                pre(i + 1)
            down_post(i)
        P.flush()


def gp_stage(P, nc, C, I, gpre, gpre_b, qaug, kaug, aug_b):
    with contextlib.ExitStack() as st:
        sb = lambda name, shape, dt: st.enter_context(nc.sbuf_tensor("gp" + name, shape, dt))
        ps = lambda name, shape, dt: st.enter_context(nc.psum_tensor("gp" + name, shape, dt))
        T0 = sb("T0", [72, S], F32)
        T1 = sb("T1", [72, S], F32)
        T2 = sb("T2", [72, S], F32)
        T3 = sb("T3", [72, S], F32)
        QR = sb("QR", [72, 3, S], BF16)
        KR = sb("KR", [72, 3, S], BF16)
        ONE = sb("ONE", [72, S], BF16)
        bcol = sb("bcol", [72, 1], F32)
        negb = sb("negb", [72, 1], F32)
        bicol = sb("bicol", [72, 1], F32)
        onec = sb("onec", [72, 1], F32)
        cm = sb("cm", [72, 32], F32)
        mce = sb("mce", [72, 32], F32)
        mprev = sb("mprev", [72, 32], F32)
        dec = sb("dec", [72, 32], F32)
        esel = sb("esel", [72, 4, 128], F32)
        bT0, bT1, bT2, bT3, bQR, bKR, bONE, bsm = [P.buf(n) for n in
                                                   ("T0", "T1", "T2", "T3", "QR", "KR", "ONE", "gsm")]
        ptm = ps("ptm", [128, 32, 8], F32)
        pdc = ps("pdc", [128, 4, 32], F32)
        b_ptm, b_pdc = P.buf("ptm"), P.buf("pdc")

        P.op("sp", lambda e: e.dma_start(out=T0[:], in_=gpre), reads=gpre_b, writes=[bT0], dma=True)
        P.op("sp", lambda e: e.dma_start(out=T3[0:4, :], in_=gpre[32:36, :]), reads=gpre_b, writes=[bT3], dma=True)
        P.op("dve", lambda e: e.memset(bcol[:], 0.0), writes=[bsm])
        P.op("dve", lambda e: e.memset(bicol[:], 0.0), reads=[bsm], writes=[bsm])
        P.op("dve", lambda e: e.memset(onec[:], 1.0), reads=[bsm], writes=[bsm])
        P.op("pool", lambda e: e.memset(ONE[:], 1.0), writes=[bONE])
        P.op("sp", lambda e: e.dma_start(out=bcol[0:4, :], in_=I["mlstm_f_bias"].rearrange("o n -> n o"),
                                         allow_slow_non_contiguous=True), reads=[bsm], writes=[bsm], dma=True)
        P.op("sp", lambda e: e.dma_start(out=bcol[64:72, :], in_=I["fox_f_bias"].rearrange("o n -> n o"),
                                         allow_slow_non_contiguous=True), reads=[bsm], writes=[bsm], dma=True)
        P.op("sp", lambda e: e.dma_start(out=bicol[0:4, :], in_=I["mlstm_i_bias"].rearrange("o n -> n o"),
                                         allow_slow_non_contiguous=True), reads=[bsm], writes=[bsm], dma=True)
        P.op("dve", lambda e: e.tensor_scalar(out=negb[0:72, :], in0=bcol[0:72, :], scalar1=-1.0, scalar2=None,
                                              op0=ALU.mult), reads=[bsm], writes=[bsm])
        R = slice(0, 72)
        P.op("act", lambda e: e.activation(out=T1[R, :], in_=T0[R, :], func=AF.Exp, scale=-1.0, bias=negb[R, :]),
             reads=[bT0, bsm], writes=[bT1])
        P.op("act", lambda e: e.activation(out=T1[R, :], in_=T1[R, :], func=AF.Ln, scale=1.0, bias=onec[R, :]),
             reads=[bT1, bsm], writes=[bT1])
        P.op("dve", lambda e: e.tensor_tensor_scan(out=T2[R, :], data0=T1[R, :], data1=T1[R, :], initial=0.0,
                                                   op0=ALU.add, op1=ALU.max), reads=[bT1], writes=[bT2])
        M = slice(0, 4)
        P.op("dve", lambda e: e.scalar_tensor_tensor(out=T3[M, :], in0=T3[M, :], scalar=bicol[M, :], in1=T2[M, :],
                                                     op0=ALU.add, op1=ALU.add), reads=[bT3, bT2, bsm], writes=[bT3])
        P.op("dve", lambda e: e.tensor_reduce(out=cm[M, :], in_=T3[M, :].rearrange("p (c l) -> p c l", l=128),
                                              axis=AX.X, op=ALU.max), reads=[bT3], writes=[bsm])
        P.op("dve", lambda e: e.tensor_tensor_scan(out=mce[M, :], data0=cm[M, :], data1=cm[M, :], initial=0.0,
                                                   op0=ALU.max, op1=ALU.max), reads=[bsm], writes=[bsm])
        P.op("dve", lambda e: e.tensor_tensor(out=T3[M, :].rearrange("p (c l) -> p c l", l=128),
                                              in0=T3[M, :].rearrange("p (c l) -> p c l", l=128),
                                              in1=bcast_last(mce[M, :], 128), op=ALU.subtract),
             reads=[bT3, bsm], writes=[bT3])
        P.op("act", lambda e: e.activation(out=T3[M, :], in_=T3[M, :], func=AF.Exp), reads=[bT3], writes=[bT3])
        P.op("dve", lambda e: e.tensor_tensor(out=T1[M, :].rearrange("p (c l) -> p c l", l=128),
                                              in0=T2[M, :].rearrange("p (c l) -> p c l", l=128),
                                              in1=bcast_last(mce[M, :], 128), op=ALU.subtract),
             reads=[bT2, bsm, bT1], writes=[bT1])
        P.op("act", lambda e: e.activation(out=T1[M, :], in_=T1[M, :], func=AF.Exp, scale=2.0), reads=[bT1], writes=[bT1])
        P.op("dve", lambda e: e.memset(mprev[M, :], 0.0), reads=[bsm], writes=[bsm])
        P.op("dve", lambda e: e.tensor_copy(out=mprev[M, 1:32], in_=mce[M, 0:31]), reads=[bsm], writes=[bsm])
        P.op("dve", lambda e: e.tensor_tensor(out=dec[M, :], in0=mprev[M, :], in1=mce[M, :], op=ALU.subtract),
             reads=[bsm], writes=[bsm])
        P.op("act", lambda e: e.activation(out=dec[M, :], in_=dec[M, :], func=AF.Exp), reads=[bsm], writes=[bsm])
        for c in range(32):
            P.op("pe", lambda e, c=c: e.transpose(out=ptm[:, c, 0:4], in_=T3[M, c * 128:(c + 1) * 128],
                                                  identity=C.identf[M, 0:4]),
                 reads=[bT3, C.b_const], writes=[b_ptm])
            P.op("pe", lambda e, c=c: e.transpose(out=ptm[:, c, 4:8], in_=T1[M, c * 128:(c + 1) * 128],
                                                  identity=C.identf[M, 0:4]),
                 reads=[bT1, C.b_const], writes=[b_ptm])
        P.op("dve", lambda e: e.tensor_copy(out=C.wthr[:], in_=ptm[:]), reads=[b_ptm], writes=[C.b_wthr])
        for h in range(4):
            P.op("dve", lambda e, h=h: e.tensor_copy(out=esel[M, h, :],
                                                     in_=C.identf[M, h:h + 1].to_broadcast([4, 128])),
                 reads=[C.b_const, bsm], writes=[bsm])
        for h in range(4):
            P.op("pe", lambda e, h=h: e.matmul(pdc[:, h, :], lhsT=esel[M, h, :], rhs=dec[M, :], start=True, stop=True),
                 reads=[bsm], writes=[b_pdc])
        P.op("dve", lambda e: e.tensor_copy(out=C.decbc[:], in_=pdc[:]), reads=[b_pdc], writes=[C.b_decbc])
        Fx = slice(64, 72)
        Fd = slice(64, 72)
        P.op("dve", lambda e: e.tensor_scalar(out=T0[Fx, :], in0=T2[Fx, :], scalar1=-1.0, scalar2=None, op0=ALU.mult),
             reads=[bT2, bT0], writes=[bT0])
        for part in range(3):
            P.op("dve", lambda e, part=part: e.tensor_copy(out=QR[Fx, part, :], in_=T0[Fx, :]),
                 reads=[bT0], writes=[bQR])
            if part < 2:
                P.op("dve", lambda e, part=part: e.tensor_tensor(out=T0[Fx, :], in0=T0[Fx, :], in1=QR[Fx, part, :],
                                                                 op=ALU.subtract), reads=[bT0, bQR], writes=[bT0])
        P.op("pool", lambda e: e.tensor_scalar(out=KR[Fx, :, :], in0=QR[Fx, :, :], scalar1=-1.0, scalar2=None,
                                               op0=ALU.mult), reads=[bQR], writes=[bKR])
        P.op("sp", lambda e: e.dma_start(out=qaug[:, 64:67, :], in_=QR[Fd, :, :]), reads=[bQR], writes=[aug_b], dma=True)
        P.op("sp", lambda e: e.dma_start(out=kaug[:, 67:70, :], in_=KR[Fd, :, :]), reads=[bKR], writes=[aug_b], dma=True)
        for r in range(3):
            P.op("sp", lambda e, r=r: e.dma_start(out=qaug[:, 67 + r, :], in_=ONE[Fd, :]), reads=[bONE],
                 writes=[aug_b], dma=True)
            P.op("sp", lambda e, r=r: e.dma_start(out=kaug[:, 64 + r, :], in_=ONE[Fd, :]), reads=[bONE],
                 writes=[aug_b], dma=True)
        P.flush()


def win_pass(P, nc, C, I, uT1, uT_b, SC, DB):
    w_in = I["w_in"]
    with contextlib.ExitStack() as st:
        sb = lambda name, shape, dt: st.enter_context(nc.sbuf_tensor("wi" + name, shape, dt))
        ps = lambda name, shape, dt: st.enter_context(nc.psum_tensor("wi" + name, shape, dt))
        W = sb("W", [128, 8, INW], BF16)
        b_W = P.bufs_n("Win", 8)
        wv = w_in.rearrange("(kc p) n -> kc p n", p=128)
        for k in range(8):
            P.op("pool", lambda e, k=k: e.dma_start(out=W[:, k, :], in_=wv[k], max_dma_last_dim=4 * 1284),
                 writes=[b_W[k]], dma=True)
        cw = sb("cw", [128, 4, 4], F32)
        cb = sb("cb", [128, 4], F32)
        gbias = sb("gbias", [128, 2048], F32)
        b_par = P.buf("wipar")
        for tap in range(4):
            P.op("sp", lambda e, tap=tap: e.dma_start(
                out=cw[:, :, tap], in_=I["conv_w"][tap:tap + 1, :].rearrange("o (c p) -> p (o c)", p=128),
                allow_slow_non_contiguous=True), writes=[b_par], dma=True)
        P.op("sp", lambda e: e.dma_start(out=cb[:], in_=I["conv_b"].rearrange("o (c p) -> p (o c)", p=128),
                                         allow_slow_non_contiguous=True), writes=[b_par], dma=True)
        P.op("sp", lambda e: e.dma_start(out=gbias[:], in_=I["branch_gate_bias"].partition_broadcast(128)),
             writes=[b_par], dma=True)
        uT = [sb("uT%d" % i, [128, 8, TT], BF16) for i in range(2)]
        b_uT = P.bufs_n("wiuT", 2)
        zq = sb("zq", [128, 4, 3 + TT], F32)
        b_zq = P.bufs_n("zq", 4)
        acc = [sb("acc%d" % i, [128, TT], F32) for i in range(2)]
        b_acc = P.bufs_n("acc", 2)
        fo = [sb("fo%d" % i, [128, TT], BF16) for i in range(3)]
        b_fo = P.bufs_n("fo", 3)
        tv = [sb("tv%d" % i, [128, 4, 129], BF16) for i in range(2)]
        b_tv = P.bufs_n("tv", 2)
        tf = [sb("tf%d" % i, [128, 8, 65], BF16) for i in range(2)]
        b_tf = P.bufs_n("tf", 2)
        tg = [sb("tg%d" % i, [128, 512], F32) for i in range(2)]
        b_tg = P.bufs_n("tg", 2)
        to = [sb("to%d" % i, [128, 512], BF16) for i in range(3)]
        b_to = P.bufs_n("to", 3)
        pf = [ps("pf%d" % i, [128, TT], F32) for i in range(2)]
        b_pf = P.bufs_n("pf", 2)
        pk = [ps("pk%d" % i, [128, 512], F32) for i in range(2)]
        b_pk = P.bufs_n("pk", 2)
        cnt = {"pf": 0, "pk": 0, "acc": 0, "fo": 0, "tv": 0, "tf": 0, "tg": 0, "to": 0}

        def rot(key, n):
            v = cnt[key] % n
            cnt[key] += 1
            return v

        for ch in range(4):
            P.op("dve", lambda e, ch=ch: e.memset(zq[:, ch, 0:3], 0.0), writes=[b_zq[ch]])
        for i in range(NT):
            ub = i % 2
            tcols = slice(i * TT, (i + 1) * TT)
            P.op("sp", lambda e, ub=ub, tcols=tcols: e.dma_start(
                out=uT[ub][:], in_=uT1[:, :, tcols].rearrange("k p t -> p k t")),
                reads=uT_b[4 * i:4 * i + 4], writes=[b_uT[ub]], dma=True)
            fm = [("mqk", ch, ch * 128) for ch in range(4)] + \
                 [("fq", ch, 1544 + ch * 128) for ch in range(4)] + \
                 [("fk", ch, 2056 + ch * 128) for ch in range(4)]
            for (kind, ch, c0) in fm:
                j = rot("pf", 2)
                for k in range(8):
                    P.op("pe", lambda e, k=k, c0=c0, j=j, ub=ub: e.matmul(
                        pf[j][:], lhsT=W[:, k, c0:c0 + 128], rhs=uT[ub][:, k, :], start=(k == 0), stop=(k == 7)),
                        reads=[b_W[k], b_uT[ub]], writes=[b_pf[j]])
                if kind == "mqk":
                    P.op("act", lambda e, ch=ch, j=j: e.activation(out=zq[:, ch, 3:3 + TT], in_=pf[j][:], func=AF.Copy),
                         reads=[b_pf[j]], writes=[b_zq[ch]])
                    a = rot("acc", 2)
                    P.op("dve", lambda e, ch=ch, a=a: e.tensor_scalar(
                        out=acc[a][:], in0=zq[:, ch, 0:TT], scalar1=cw[:, ch, 0:1], scalar2=cb[:, ch:ch + 1],
                        op0=ALU.mult, op1=ALU.add), reads=[b_zq[ch], b_par], writes=[b_acc[a]])
                    for tap in range(1, 4):
                        P.op("dve", lambda e, ch=ch, a=a, tap=tap: e.scalar_tensor_tensor(
                            out=acc[a][:], in0=zq[:, ch, tap:tap + TT], scalar=cw[:, ch, tap:tap + 1], in1=acc[a][:],
                            op0=ALU.mult, op1=ALU.add), reads=[b_zq[ch], b_par, b_acc[a]], writes=[b_acc[a]])
                    P.op("dve", lambda e, ch=ch: e.tensor_copy(out=zq[:, ch, 0:3], in_=zq[:, ch, TT:TT + 3]),
                         reads=[b_zq[ch]], writes=[b_zq[ch]])
                    o = rot("fo", 3)
                    P.op("act", lambda e, a=a, o=o: e.activation(out=fo[o][:], in_=acc[a][:], func=AF.Silu),
                         reads=[b_acc[a]], writes=[b_fo[o]])
                    P.op("sp", lambda e, o=o, ch=ch, tcols=tcols: e.dma_start(out=SC["mqkT"][ch, :, tcols], in_=fo[o][:]),
                         reads=[b_fo[o]], writes=[DB("mqkT")[i]], dma=True)
                else:
                    o = rot("fo", 3)
                    sc = 0.125 if kind == "fq" else 1.0
                    P.op("act", lambda e, o=o, j=j, sc=sc: e.activation(out=fo[o][:], in_=pf[j][:], func=AF.Copy, scale=sc),
                         reads=[b_pf[j]], writes=[b_fo[o]])
                    dst = SC["qaug"] if kind == "fq" else SC["kaug"]
                    for hh in range(2):
                        P.op("sp", lambda e, o=o, ch=ch, dst=dst, tcols=tcols, hh=hh: e.dma_start(
                            out=dst[2 * ch + hh, 0:64, tcols], in_=fo[o][hh * 64:(hh + 1) * 64, :]),
                            reads=[b_fo[o]], writes=[DB("aug")[i]], dma=True)
            for s in range(4):
                n = i * 4 + s
                rows = slice(n * 128, (n + 1) * 128)
                groups = [("mv", 512), ("mo", 1024), ("fv", 2568), ("ga", 3088), ("ga", 3600), ("gb", 4112), ("gb", 4624)]
                for gi, (kind, c0) in enumerate(groups):
                    j = rot("pk", 2)
                    for k in range(8):
                        P.op("pe", lambda e, k=k, c0=c0, j=j, ub=ub, s=s: e.matmul(
                            pk[j][:], lhsT=uT[ub][:, k, s * 128:(s + 1) * 128], rhs=W[:, k, c0:c0 + 512],
                            start=(k == 0), stop=(k == 7)),
                            reads=[b_W[k], b_uT[ub]], writes=[b_pk[j]])
                    if kind == "mv":
                        t = rot("tv", 2)
                        c = n
                        P.op("dve", lambda e, t=t, j=j, c=c: e.tensor_tensor(
                            out=tv[t][:, :, 0:128], in0=pk[j][:].rearrange("p (h d) -> p h d", h=4),
                            in1=bcast_last(C.wthr[:, c, 0:4], 128), op=ALU.mult),
                            reads=[b_pk[j], C.b_wthr], writes=[b_tv[t]])
                        P.op("dve", lambda e, t=t, c=c: e.tensor_copy(out=tv[t][:, :, 128:129],
                                                                     in_=C.wthr[:, c, 0:4].unsqueeze(2)),
                             reads=[C.b_wthr, b_tv[t]], writes=[b_tv[t]])
                        P.op("sp", lambda e, t=t, rows=rows: e.dma_start(out=SC["vaugM"][rows], in_=tv[t][:]),
                             reads=[b_tv[t]], writes=[DB("vaugM")[n]], dma=True)
                    elif kind == "fv":
                        t = rot("tf", 2)
                        P.op("act", lambda e, t=t, j=j: e.activation(
                            out=tf[t][:, :, 1:65], in_=pk[j][:].rearrange("p (h d) -> p h d", h=8), func=AF.Copy),
                            reads=[b_pk[j]], writes=[b_tf[t]])
                        P.op("dve", lambda e, t=t: e.memset(tf[t][:, :, 0:1], 1.0), reads=[b_tf[t]], writes=[b_tf[t]])
                        P.op("sp", lambda e, t=t, rows=rows: e.dma_start(out=SC["vaugF"][rows], in_=tf[t][:]),
                             reads=[b_tf[t]], writes=[DB("vaugF")[n]], dma=True)
                    elif kind == "mo":
                        o = rot("to", 3)
                        P.op("act", lambda e, o=o, j=j: e.activation(out=to[o][:], in_=pk[j][:], func=AF.Sigmoid),
                             reads=[b_pk[j]], writes=[b_to[o]])
                        P.op("sp", lambda e, o=o, rows=rows: e.dma_start(out=SC["so"][rows], in_=to[o][:]),
                             reads=[b_to[o]], writes=[DB("so")[n]], dma=True)
                    else:
                        g = rot("tg", 2)
                        boff = c0 - 3088
                        P.op("dve", lambda e, g=g, j=j, boff=boff: e.tensor_tensor(
                            out=tg[g][:], in0=pk[j][:], in1=gbias[:, boff:boff + 512], op=ALU.add),
                            reads=[b_pk[j], b_par], writes=[b_tg[g]])
                        o = rot("to", 3)
                        P.op("act", lambda e, o=o, g=g: e.activation(out=to[o][:], in_=tg[g][:], func=AF.Sigmoid),
                             reads=[b_tg[g]], writes=[b_to[o]])
                        dcol = boff % 1024
                        dst = SC["ga"] if kind == "ga" else SC["gb"]
                        P.op("sp", lambda e, o=o, rows=rows, dst=dst, dcol=dcol: e.dma_start(
                            out=dst[rows, dcol:dcol + 512], in_=to[o][:]),
                            reads=[b_to[o]], writes=[DB("gab")[n]], dma=True)
        P.flush()


def mix_pass(P, nc, C, I, SC, DB):
    with contextlib.ExitStack() as st:
        sb = lambda name, shape, dt: st.enter_context(nc.sbuf_tensor("mx" + name, shape, dt))
        ps = lambda name, shape, dt: st.enter_context(nc.psum_tensor("mx" + name, shape, dt))
        mask01 = sb("mask01", [128, 128], F32)
        trim = sb("trim", [128, 128], BF16)
        trimf = sb("trimf", [128, 128], F32)
        onesr = sb("onesr", [1, 65], F32)
        gln = sb("gln", [128, 512], F32)
        b_c = P.buf("mxconst")
        P.op("pool", lambda e: e.memset(mask01[:], 1.0), writes=[b_c])
        P.op("pool", lambda e: e.affine_select(out=mask01[:], in_=mask01[:], pattern=[[1, 128]], compare_op=ALU.is_ge,
                                                 fill=0.0, base=0, channel_multiplier=-1), reads=[b_c], writes=[b_c])
        P.op("pool", lambda e: e.memset(trimf[:], 0.0), reads=[b_c], writes=[b_c])
        P.op("pool", lambda e: e.affine_select(out=trimf[:], in_=trimf[:], pattern=[[1, 128]], compare_op=ALU.is_ge,
                                                 fill=-30000.0, base=0, channel_multiplier=-1), reads=[b_c], writes=[b_c])
        P.op("dve", lambda e: e.tensor_copy(out=trim[:], in_=trimf[:]), reads=[b_c], writes=[b_c])
        P.op("dve", lambda e: e.memset(onesr[:], 1.0), reads=[b_c], writes=[b_c])
        P.op("sp", lambda e: e.dma_start(out=gln[:], in_=I["mlstm_norm_g"].partition_broadcast(128)),
             reads=[b_c], writes=[b_c], dma=True)
        mqz = [sb("mqz%d" % i, [128, S], BF16) for i in range(4)]
        mk = [sb("mk%d" % i, [128, S], BF16) for i in range(2)]
        b_mqk = P.buf("mqk")
        for h in range(4):
            P.op("pool", lambda e, h=h: e.memset(mqz[h][:], 0.0), writes=[b_mqk])
        for h in range(4):
            R = slice((h % 2) * 64, (h % 2) * 64 + 64)
            P.op("sp", lambda e, h=h, R=R: e.dma_start(out=mqz[h][R, :], in_=SC["mqkT"][h // 2, R, :]), reads=DB("mqkT"),
                 writes=[b_mqk], dma=True)
        for hp in range(2):
            P.op("sp", lambda e, hp=hp: e.dma_start(out=mk[hp][:], in_=SC["mqkT"][2 + hp]), reads=DB("mqkT"),
                 writes=[b_mqk], dma=True)
        Cst = [sb("Cst%d" % i, [128, 129], F32) for i in range(2)]
        Cb = [sb("Cb%d" % i, [128, 129], BF16) for i in range(2)]
        b_Cst = P.bufs_n("Cst", 2)
        b_Cb = P.bufs_n("Cb", 2)
        va = [sb("va%d" % i, [128, 4, 129], BF16) for i in range(2)]
        b_va = P.bufs_n("va", 2)
        sgo = [sb("sgo%d" % i, [128, 512], BF16) for i in range(2)]
        b_sgo = P.bufs_n("sgo", 2)
        Sm = [sb("Sm%d" % i, [128, 2, 128], BF16) for i in range(2)]
        b_Sm = P.bufs_n("Sm", 2)
        ktm = [sb("ktm%d" % i, [128, 128], BF16) for i in range(2)]
        b_ktm = P.bufs_n("ktm", 2)
        bst = [sb("bst%d" % i, [128, 2, 6], F32) for i in range(2)]
        bag = [sb("bag%d" % i, [128, 2, 2], F32) for i in range(2)]
        sm = [sb("sm%d" % i, [128, 2, 4], F32) for i in range(2)]
        b_sm = P.bufs_n("msm", 2)
        hn = [sb("hn%d" % i, [128, 512], F32) for i in range(2)]
        b_hn = P.bufs_n("hn", 2)
        ya = [sb("ya%d" % i, [128, 512], BF16) for i in range(2)]
        b_ya = P.bufs_n("ya", 2)
        yaT = [sb("yaT%d" % i, [128, 4, 128], BF16) for i in range(2)]
        b_yaT = P.bufs_n("yaTs", 2)
        pSm = ps("pSm", [128, 2, 128], F32)
        pOm = ps("pOm", [128, 2, 129], F32)
        pU = ps("pU", [128, 2, 129], F32)
        pT5 = ps("pT5", [128, 5, 128], BF16)
        pkt = pT5[:, 4, :]
        pyT = pT5[:, 0:4, :]
        b_pSm, b_pOm, b_pU, b_pkt, b_pyT = [P.buf(n) for n in ("pSm", "pOm", "pU", "pkt", "pyT")]

        def mlstm_chunk(c):
            cols = slice(c * 128, (c + 1) * 128)
            rows = slice(c * 128, (c + 1) * 128)
            vi = c % 2
            P.op("sp", lambda e: e.dma_start(out=va[vi][:], in_=SC["vaugM"][rows]), reads=[DB("vaugM")[c]],
                 writes=[b_va[vi]], dma=True)
            P.op("sp", lambda e: e.dma_start(out=sgo[vi][:], in_=SC["so"][rows]), reads=[DB("so")[c]],
                 writes=[b_sgo[vi]], dma=True)
            hnb = hn[vi]
            for hp in range(2):
                si = hp
                for hh in range(2):
                    P.op("pe", lambda e, hp=hp, hh=hh: e.matmul(
                        pSm[:, hh, :], lhsT=mk[hp][:, cols], rhs=mqz[2 * hp + hh][:, cols], start=True, stop=True),
                        reads=[b_mqk], writes=[b_pSm])
                P.op("pe", lambda e, hp=hp: e.transpose(out=pkt, in_=mk[hp][:, cols], identity=C.ident[:]),
                     reads=[b_mqk, C.b_const], writes=[b_pkt])
                P.op("dve", lambda e, si=si: e.scalar_tensor_tensor(
                    out=Sm[si][:], in0=pSm[:], scalar=0.125,
                    in1=mask01[:].unsqueeze(1).to_broadcast([128, 2, 128]), op0=ALU.mult, op1=ALU.mult),
                    reads=[b_pSm, b_c], writes=[b_Sm[si]])
                P.op("act", lambda e, si=si: e.activation(out=ktm[si][:], in_=pkt, func=AF.Copy, scale=0.125),
                     reads=[b_pkt], writes=[b_ktm[si]])
                yield
                for hh in range(2):
                    h = 2 * hp + hh
                    P.op("pe", lambda e, si=si, hh=hh, h=h: e.matmul(
                        pOm[:, hh, :], lhsT=Sm[si][:, hh, :], rhs=va[vi][:, h, :], start=True, stop=(c == 0)),
                        reads=[b_Sm[si], b_va[vi]], writes=[b_pOm])
                    if c > 0:
                        P.op("pe", lambda e, hp=hp, hh=hh, h=h: e.matmul(
                            pOm[:, hh, :], lhsT=mqz[h][:, cols], rhs=Cb[hp][:, :], start=False, stop=True),
                            reads=[b_mqk, b_Cb[hp]], writes=[b_pOm])
                for hh in range(2):
                    h = 2 * hp + hh
                    P.op("pe", lambda e, si=si, hh=hh, h=h: e.matmul(
                        pU[:, hh, :], lhsT=ktm[si][:], rhs=va[vi][:, h, :], start=True, stop=True),
                        reads=[b_ktm[si], b_va[vi]], writes=[b_pU])
                for hh in range(2):
                    h = 2 * hp + hh
                    R = slice(hh * 64, (hh + 1) * 64)
                    if c == 0:
                        P.op("dve", lambda e, hp=hp, hh=hh, R=R: e.tensor_copy(out=Cst[hp][R, :], in_=pU[R, hh, :]),
                             reads=[b_pU], writes=[b_Cst[hp]])
                    else:
                        P.op("dve", lambda e, hp=hp, hh=hh, R=R, h=h: e.scalar_tensor_tensor(
                            out=Cst[hp][R, :], in0=Cst[hp][R, :], scalar=C.decbc[R, h, c:c + 1], in1=pU[R, hh, :],
                            op0=ALU.mult, op1=ALU.add), reads=[b_pU, b_Cst[hp], C.b_decbc], writes=[b_Cst[hp]])
                    if c < 31:
                        P.op("dve", lambda e, hp=hp, R=R, h=h: e.tensor_scalar(
                            out=Cb[hp][R, :], in0=Cst[hp][R, :], scalar1=C.decbc[R, h, c + 1:c + 2], scalar2=None,
                            op0=ALU.mult), reads=[b_Cst[hp], C.b_decbc], writes=[b_Cb[hp]])
                smp, bstp, bagp, bsm = sm[hp], bst[hp], bag[hp], b_sm[hp]
                for hh in range(2):
                    P.op("dve", lambda e, hh=hh, bstp=bstp: e.bn_stats(out=bstp[:, hh, :], in_=pOm[:, hh, 0:128]),
                         reads=[b_pOm], writes=[bsm])
                    P.op("dve", lambda e, hh=hh, bstp=bstp, bagp=bagp: e.bn_aggr(out=bagp[:, hh, :], in_=bstp[:, hh, :]),
                         reads=[bsm], writes=[bsm])
                P.op("act", lambda e, smp=smp: e.activation(out=smp[:, :, 0:1], in_=pOm[:, :, 128:129], func=AF.Square),
                     reads=[b_pOm, bsm], writes=[bsm])
                P.op("dve", lambda e, hp=hp, smp=smp: e.tensor_tensor(
                    out=smp[:, :, 0:1], in0=smp[:, :, 0:1], in1=C.wthr[:, c, 4 + 2 * hp:6 + 2 * hp].unsqueeze(2),
                    op=ALU.max), reads=[C.b_wthr, bsm], writes=[bsm])
                P.op("dve", lambda e, smp=smp, bagp=bagp: e.scalar_tensor_tensor(
                    out=smp[:, :, 1:2], in0=smp[:, :, 0:1], scalar=EPS, in1=bagp[:, :, 1:2], op0=ALU.mult, op1=ALU.add),
                    reads=[bsm], writes=[bsm])
                P.op("pool", lambda e, smp=smp: e.tensor_tensor(
                    out=smp[:, :, 2:3], in0=smp[:, :, 1:2],
                    in1=C.neghalf[:].unsqueeze(1).to_broadcast([128, 2, 1]), op=ALU.pow),
                    reads=[bsm, C.b_const], writes=[bsm])
                for hh in range(2):
                    h = 2 * hp + hh
                    P.op("dve", lambda e, hh=hh, h=h, smp=smp, bagp=bagp: e.tensor_scalar(
                        out=hnb[:, h * 128:(h + 1) * 128], in0=pOm[:, hh, 0:128], scalar1=bagp[:, hh, 0:1],
                        scalar2=smp[:, hh, 2:3], op0=ALU.subtract, op1=ALU.mult),
                        reads=[b_pOm, bsm], writes=[b_hn[vi]])
                yield
            yi = c % 2
            P.op("pool", lambda e: e.tensor_tensor(out=hnb[:], in0=hnb[:], in1=gln[:], op=ALU.mult),
                 reads=[b_hn[vi], b_c], writes=[b_hn[vi]])
            P.op("dve", lambda e: e.tensor_tensor(out=ya[yi][:], in0=hnb[:], in1=sgo[vi][:], op=ALU.mult),
                 reads=[b_hn[vi], b_sgo[vi]], writes=[b_ya[yi]])
            yield
            for k in range(4):
                P.op("pe", lambda e, k=k: e.transpose(out=pyT[:, k, :], in_=ya[yi][:, k * 128:(k + 1) * 128],
                                                      identity=C.ident[:]),
                     reads=[b_ya[yi], C.b_const], writes=[b_pyT])
            P.op("act", lambda e: e.activation(out=yaT[yi][:], in_=pyT, func=AF.Copy), reads=[b_pyT],
                 writes=[b_yaT[yi]])
            P.op("sp", lambda e: e.dma_start(out=SC["yaT"][:, :, cols].rearrange("k p t -> p k t"), in_=yaT[yi][:]),
                 reads=[b_yaT[yi]], writes=[DB("yaT")[c]], dma=True)
            yield

        def mlstm_gen():
            for c in range(32):
                yield from mlstm_chunk(c)

        VF = sb("VF", [128, 32, 8 * 65], BF16)
        b_VF = P.buf("VF")
        P.op("sp", lambda e: e.dma_start(out=VF[:], in_=SC["vaugF"].rearrange("(j p) h e -> p j (h e)", p=128)),
             reads=DB("vaugF"), writes=[b_VF], dma=True)
        QA = [sb("QA%d" % i, [70, S], BF16) for i in range(2)]
        KA = [sb("KA%d" % i, [70, S], BF16) for i in range(2)]
        b_QA = P.bufs_n("QA", 2)
        b_KA = P.bufs_n("KA", 2)
        PT = [sb("PT%d" % i, [128, 512], BF16) for i in range(2)]
        b_PT = P.bufs_n("PT", 2)
        rec = sb("rec", [1, 512], F32)
        b_rec = P.buf("rec")
        osb = sb("osb", [65, 512], F32)
        b_osb = P.buf("osb")
        ybt = [sb("ybt%d" % i, [65, 512], BF16) for i in range(2)]
        b_ybt = P.bufs_n("ybt", 2)
        pS = [ps("pS%d" % i, [128, 512], F32) for i in range(2)]
        b_pS = P.bufs_n("pS", 2)
        pO = ps("pO", [128, 512], F32)
        b_pO = P.buf("pO")
        pbc = ps("pbc", [65, 512], F32)
        b_pbc = P.buf("pbc")
        cnt = {"s": 0, "y": 0}

        def fox_load(h):
            hb = h % 2
            P.op("sp", lambda e: e.dma_start(out=QA[hb][:], in_=SC["qaug"][h]), reads=DB("aug"), writes=[b_QA[hb]], dma=True)
            P.op("sp", lambda e: e.dma_start(out=KA[hb][:], in_=SC["kaug"][h]), reads=DB("aug"), writes=[b_KA[hb]], dma=True)

        seq = [(h, i, j) for h in range(8) for i in range(8) for j in range(4 * i + 4)]

        def emit_S(idx):
            h, i, j = seq[idx]
            hb = h % 2
            sj = idx % 2
            jj = j - 4 * i
            kc = slice(j * 128, (j + 1) * 128)
            rd = [b_KA[hb], b_QA[hb]]
            if jj < 0:
                P.op("pe", lambda e: e.matmul(
                    pS[sj][:, 0:512], lhsT=KA[hb][:, kc], rhs=QA[hb][:, i * 512:(i + 1) * 512], start=True, stop=True),
                    reads=rd, writes=[b_pS[sj]])
            else:
                qs = jj * 128
                wq = 512 - qs
                q0 = i * 512 + qs
                P.op("pe", lambda e: e.matmul(pS[sj][:, 0:128], lhsT=C.ident[:], rhs=trim[:], start=True, stop=False),
                     reads=[C.b_const, b_c], writes=[b_pS[sj]])
                P.op("pe", lambda e: e.matmul(
                    pS[sj][:, 0:128], lhsT=KA[hb][:, kc], rhs=QA[hb][:, q0:q0 + 128], start=False, stop=True),
                    reads=rd, writes=[b_pS[sj]])
                if wq > 128:
                    P.op("pe", lambda e: e.matmul(
                        pS[sj][:, 128:wq], lhsT=KA[hb][:, kc], rhs=QA[hb][:, q0 + 128:q0 + wq], start=True, stop=True),
                        reads=rd, writes=[b_pS[sj]])

        def emit_rest(idx):
            h, i, j = seq[idx]
            sj = idx % 2
            nkb = 4 * i + 4
            jj = j - 4 * i
            qs = max(jj, 0) * 128
            wq = 512 - qs
            P.op("act", lambda e: e.activation(out=PT[sj][:, 0:wq], in_=pS[sj][:, 0:wq], func=AF.Exp),
                 reads=[b_pS[sj]], writes=[b_PT[sj]])
            P.op("pe", lambda e: e.matmul(
                pO[0:65, qs:512], lhsT=VF[:, j, h * 65:(h + 1) * 65], rhs=PT[sj][:, 0:wq],
                start=(j == 0), stop=(j == nkb - 1)),
                reads=[b_VF, b_PT[sj]], writes=[b_pO])
            if j < nkb - 1:
                return
            P.op("act", lambda e: e.activation(out=osb[:], in_=pO[0:65, :], func=AF.Copy), reads=[b_pO], writes=[b_osb])
            P.op("dve", lambda e: e.reciprocal(out=rec[0:1, :], in_=osb[0:1, :]), reads=[b_osb], writes=[b_rec])
            P.op("pe", lambda e: e.matmul(pbc[:], lhsT=onesr[0:1, :], rhs=rec[0:1, :], start=True, stop=True),
                 reads=[b_rec, b_c], writes=[b_pbc])
            yi = cnt["y"] % 2
            cnt["y"] += 1
            P.op("dve", lambda e: e.tensor_tensor(out=ybt[yi][:], in0=osb[:], in1=pbc[:], op=ALU.mult),
                 reads=[b_osb, b_pbc], writes=[b_ybt[yi]])
            P.op("sp", lambda e: e.dma_start(
                out=SC["ybT"][h // 2, (h % 2) * 64:(h % 2) * 64 + 64, i * 512:(i + 1) * 512], in_=ybt[yi][1:65, :]),
                reads=[b_ybt[yi]], writes=[DB("ybT")[(h * 8 + i) % 32]], dma=True)

        gen = mlstm_gen()
        fox_load(0)
        emit_S(0)
        for idx, (h, i, j) in enumerate(seq):
            if i == 0 and j == 0 and h + 1 < 8:
                fox_load(h + 1)
            if idx + 1 < len(seq):
                emit_S(idx + 1)
            emit_rest(idx)
            if idx % 6 == 5:
                next(gen, None)
        for _ in gen:
            pass
        P.flush()


def merge_pass(P, nc, C, I, SC, DB, h1, h1_b, h2, h2_b):
    with contextlib.ExitStack() as st:
        sb = lambda name, shape, dt: st.enter_context(nc.sbuf_tensor("mg" + name, shape, dt))
        ps = lambda name, shape, dt: st.enter_context(nc.psum_tensor("mg" + name, shape, dt))
        Wa = sb("Wa", [128, 4, D], BF16)
        Wb = sb("Wb", [128, 4, D], BF16)
        Wo = sb("Wo", [128, 8, D], BF16)
        b_W = P.buf("mgW")
        P.op("pool", lambda e: e.dma_start(out=Wa[:], in_=I["w_branch_a"].rearrange("(k p) d -> p k d", p=128)),
             writes=[b_W], dma=True)
        P.op("pool", lambda e: e.dma_start(out=Wb[:], in_=I["w_branch_b"].rearrange("(k p) d -> p k d", p=128)),
             writes=[b_W], dma=True)
        P.op("pool", lambda e: e.dma_start(out=Wo[:], in_=I["w_out"].rearrange("(k p) d -> p k d", p=128)),
             writes=[b_W], dma=True)
        gpost = sb("gpost", [128, D], F32)
        P.op("sp", lambda e: e.dma_start(out=gpost[:], in_=I["mix_post_g"].partition_broadcast(128)),
             writes=[b_W], dma=True)
        yaT = [sb("yaT%d" % i, [128, 4, 128], BF16) for i in range(2)]
        ybT = [sb("ybT%d" % i, [128, 4, 128], BF16) for i in range(2)]
        gab = [sb("gab%d" % i, [128, 2, D], BF16) for i in range(2)]
        hin = [sb("hin%d" % i, [128, D], F32) for i in range(2)]
        b_in = P.bufs_n("mgin", 2)
        b_hin = P.bufs_n("mghin", 2)
        t1 = [sb("t1%d" % i, [128, D], F32) for i in range(2)]
        t2 = [sb("t2%d" % i, [128, D], F32) for i in range(2)]
        mb = [sb("mb%d" % i, [128, D], BF16) for i in range(2)]
        mT = [sb("mT%d" % i, [128, 8, 128], BF16) for i in range(2)]
        junk = sb("junk", [128, D], BF16)
        hout = [sb("hout%d" % i, [128, D], F32) for i in range(2)]
        stat = sb("stat", [128, 6], F32)
        b_t1, b_t2, b_mb, b_mT = [P.bufs_n(n, 2) for n in ("t1", "t2", "mb", "mT")]
        b_junk = P.buf("mgjunk")
        b_hout = P.bufs_n("hout", 2)
        b_stat = P.bufs_n("mgstat", 2)
        pA = ps("pA", [128, D], F32)
        pB = ps("pB", [128, D], F32)
        pO = ps("pO", [128, D], F32)
        pt = ps("pt", [128, 8, 128], BF16)
        b_pA, b_pB, b_pO, b_pt = [P.buf(n) for n in ("pA", "pB", "mgpO", "mgpt")]
        h1v = h1.rearrange("(n p) d -> n p d", p=128)
        h2v = h2.rearrange("(n p) d -> n p d", p=128)

        def s1(n):
            ib = n % 2
            rows = slice(n * 128, (n + 1) * 128)
            cols = rows
            P.op("sp", lambda e: e.dma_start(out=yaT[ib][:], in_=SC["yaT"][:, :, cols].rearrange("k p t -> p k t")),
                 reads=[DB("yaT")[n]], writes=[b_in[ib]], dma=True)
            P.op("sp", lambda e: e.dma_start(out=ybT[ib][:], in_=SC["ybT"][:, :, cols].rearrange("k p t -> p k t")),
                 reads=DB("ybT"), writes=[b_in[ib]], dma=True)
            P.op("sp", lambda e: e.dma_start(out=gab[ib][:, 0, :], in_=SC["ga"][rows]),
                 reads=[DB("gab")[n]], writes=[b_in[ib]], dma=True)
            P.op("sp", lambda e: e.dma_start(out=gab[ib][:, 1, :], in_=SC["gb"][rows]),
                 reads=[DB("gab")[n]], writes=[b_in[ib]], dma=True)
            P.op("sp", lambda e: e.dma_start(out=hin[ib][:], in_=h1v[n]),
                 reads=[h1_b[n]], writes=[b_hin[ib]], dma=True)
            for hf in range(2):
                hs = slice(hf * 512, (hf + 1) * 512)
                for k in range(4):
                    P.op("pe", lambda e, k=k, hs=hs: e.matmul(pA[:, hs], lhsT=yaT[ib][:, k, :], rhs=Wa[:, k, hs],
                                                              start=(k == 0), stop=(k == 3)),
                         reads=[b_in[ib], b_W], writes=[b_pA])
            for hf in range(2):
                hs = slice(hf * 512, (hf + 1) * 512)
                for k in range(4):
                    P.op("pe", lambda e, k=k, hs=hs: e.matmul(pB[:, hs], lhsT=ybT[ib][:, k, :], rhs=Wb[:, k, hs],
                                                              start=(k == 0), stop=(k == 3)),
                         reads=[b_in[ib], b_W], writes=[b_pB])
            P.op("dve", lambda e: e.tensor_tensor(out=t1[ib][:], in0=pA[:], in1=gab[ib][:, 0, :], op=ALU.mult),
                 reads=[b_pA, b_in[ib]], writes=[b_t1[ib]])
            P.op("dve", lambda e: e.tensor_tensor(out=t2[ib][:], in0=pB[:], in1=gab[ib][:, 1, :], op=ALU.mult),
                 reads=[b_pB, b_in[ib]], writes=[b_t2[ib]])
            P.op("pool", lambda e: e.tensor_tensor(out=mb[ib][:], in0=t1[ib][:], in1=t2[ib][:], op=ALU.add),
                 reads=[b_t1[ib], b_t2[ib]], writes=[b_mb[ib]])

        def s2(n):
            ib = n % 2
            for k in range(8):
                P.op("pe", lambda e, k=k: e.transpose(out=pt[:, k, :], in_=mb[ib][:, k * 128:(k + 1) * 128],
                                                      identity=C.ident[:]),
                     reads=[b_mb[ib], C.b_const], writes=[b_pt])
            P.op("act", lambda e: e.activation(out=mT[ib][:], in_=pt[:], func=AF.Copy), reads=[b_pt], writes=[b_mT[ib]])
            for hf in range(2):
                hs = slice(hf * 512, (hf + 1) * 512)
                for k in range(8):
                    P.op("pe", lambda e, k=k, hs=hs: e.matmul(pO[:, hs], lhsT=mT[ib][:, k, :], rhs=Wo[:, k, hs],
                                                              start=(k == 0), stop=(k == 7)),
                         reads=[b_mT[ib], b_W], writes=[b_pO])
            si = n % 2
            ss, var, rstd = (stat[:, 3 * si + j:3 * si + j + 1] for j in range(3))
            rms_stats(P, C, pO[:], b_pO, junk[:], b_junk, ss, var, rstd, b_stat[si])
            P.op("dve", lambda e: e.scalar_tensor_tensor(
                out=hout[ib][:], in0=pO[:], scalar=rstd, in1=gpost[:], op0=ALU.mult, op1=ALU.mult),
                reads=[b_pO, b_stat[si], b_W], writes=[b_hout[ib]])
            P.op("pool", lambda e: e.tensor_tensor(out=hout[ib][:], in0=hout[ib][:], in1=hin[ib][:], op=ALU.add),
                 reads=[b_hout[ib], b_hin[ib]], writes=[b_hout[ib]])
            P.op("sp", lambda e: e.dma_start(out=h2v[n], in_=hout[ib][:]),
                 reads=[b_hout[ib]], writes=[h2_b[n]], dma=True)

        s1(0)
        for n in range(32):
            if n + 1 < 32:
                s1(n + 1)
            s2(n)
        P.flush()


def ple_pass(P, nc, C, I, uT3, uT3_b, h3, h3_b, out):
    with contextlib.ExitStack() as st:
        sb = lambda name, shape, dt: st.enter_context(nc.sbuf_tensor("pl" + name, shape, dt))
        ps = lambda name, shape, dt: st.enter_context(nc.psum_tensor("pl" + name, shape, dt))
        Wg = sb("Wg", [128, 8, D], BF16)
        Wp = sb("Wp", [128, 2, D], BF16)
        b_W = P.buf("plW")
        P.op("pool", lambda e: e.dma_start(out=Wg[:], in_=I["ple_w_gate"].rearrange("(k p) d -> p k d", p=128)),
             writes=[b_W], dma=True)
        P.op("pool", lambda e: e.dma_start(out=Wp[:], in_=I["ple_w_proj"].rearrange("(k p) d -> p k d", p=128)),
             writes=[b_W], dma=True)
        gpost = sb("gpost", [128, D], F32)
        bg = sb("bg", [128, D], F32)
        P.op("sp", lambda e: e.dma_start(out=gpost[:], in_=I["ple_post_g"].partition_broadcast(128)),
             writes=[b_W], dma=True)
        P.op("sp", lambda e: e.dma_start(out=bg[:], in_=I["ple_b_gate"].partition_broadcast(128)),
             writes=[b_W], dma=True)
        uT = [sb("uT%d" % i, [128, 8, 128], BF16) for i in range(2)]
        pin = [sb("pin%d" % i, [128, 256], F32) for i in range(2)]
        hin = [sb("hin%d" % i, [128, D], F32) for i in range(2)]
        b_in = P.bufs_n("plin", 2)
        b_hin = P.bufs_n("plhin", 2)
        pb = [sb("pb%d" % i, [128, 256], BF16) for i in range(2)]
        pT = [sb("pT%d" % i, [128, 2, 128], BF16) for i in range(2)]
        gt = [sb("gt%d" % i, [128, D], F32) for i in range(2)]
        ge = [sb("ge%d" % i, [128, D], F32) for i in range(2)]
        junk = sb("junk", [128, D], BF16)
        hout = [sb("hout%d" % i, [128, D], F32) for i in range(2)]
        stat = sb("stat", [128, 6], F32)
        b_pb, b_pT, b_gt, b_ge = [P.bufs_n(n, 2) for n in ("pb", "pT", "gt", "ge")]
        b_junk = P.buf("pljunk")
        b_hout = P.bufs_n("plhout", 2)
        b_stat = P.bufs_n("plstat", 2)
        pG = [ps("pG%d" % i, [128, D], F32) for i in range(2)]
        pE = ps("pE", [128, D], F32)
        ptp = ps("ptp", [128, 2, 128], BF16)
        b_pG = P.bufs_n("pG", 2)
        b_pE, b_ptp = P.buf("pE"), P.buf("ptp")
        pv = I["p"].rearrange("(n p) d -> n p d", p=128)
        h3v = h3.rearrange("(n p) d -> n p d", p=128)
        ov = out.rearrange("(n p) d -> n p d", p=128)

        def s1(n):
            ib = n % 2
            cols = slice(n * 128, (n + 1) * 128)
            P.op("sp", lambda e: e.dma_start(out=uT[ib][:], in_=uT3[:, :, cols].rearrange("k p t -> p k t")),
                 reads=[uT3_b[n]], writes=[b_in[ib]], dma=True)
            P.op("sp", lambda e: e.dma_start(out=pin[ib][:], in_=pv[n]), writes=[b_in[ib]], dma=True)
            P.op("sp", lambda e: e.dma_start(out=hin[ib][:], in_=h3v[n]), reads=[h3_b[n]],
                 writes=[b_hin[ib]], dma=True)
            P.op("act", lambda e: e.activation(out=pb[ib][:], in_=pin[ib][:], func=AF.Copy), reads=[b_in[ib]],
                 writes=[b_pb[ib]])
            for k in range(2):
                P.op("pe", lambda e, k=k: e.transpose(out=ptp[:, k, :], in_=pb[ib][:, k * 128:(k + 1) * 128],
                                                      identity=C.ident[:]),
                     reads=[b_pb[ib], C.b_const], writes=[b_ptp])
            P.op("dve", lambda e: e.tensor_copy(out=pT[ib][:], in_=ptp[:]), reads=[b_ptp], writes=[b_pT[ib]])
            for hf in range(2):
                hs = slice(hf * 512, (hf + 1) * 512)
                for k in range(8):
                    P.op("pe", lambda e, k=k, hs=hs: e.matmul(pG[ib][:, hs], lhsT=uT[ib][:, k, :], rhs=Wg[:, k, hs],
                                                              start=(k == 0), stop=(k == 7)),
                         reads=[b_in[ib], b_W], writes=[b_pG[ib]])

        def s2(n):
            ib = n % 2
            for hf in range(2):
                hs = slice(hf * 512, (hf + 1) * 512)
                for k in range(2):
                    P.op("pe", lambda e, k=k, hs=hs: e.matmul(pE[:, hs], lhsT=pT[ib][:, k, :], rhs=Wp[:, k, hs],
                                                              start=(k == 0), stop=(k == 1)),
                         reads=[b_pT[ib], b_W], writes=[b_pE])
            P.op("dve", lambda e: e.tensor_tensor(out=gt[ib][:], in0=pG[ib][:], in1=bg[:], op=ALU.add),
                 reads=[b_pG[ib], b_W], writes=[b_gt[ib]])
            P.op("act", lambda e: e.activation(out=gt[ib][:], in_=gt[ib][:], func=AF.Sigmoid), reads=[b_gt[ib]],
                 writes=[b_gt[ib]])
            P.op("dve", lambda e: e.tensor_tensor(out=ge[ib][:], in0=gt[ib][:], in1=pE[:], op=ALU.mult),
                 reads=[b_gt[ib], b_pE], writes=[b_ge[ib]])
            si = n % 2
            ss, var, rstd = (stat[:, 3 * si + j:3 * si + j + 1] for j in range(3))
            rms_stats(P, C, ge[ib][:], b_ge[ib], junk[:], b_junk, ss, var, rstd, b_stat[si])
            P.op("dve", lambda e: e.scalar_tensor_tensor(
                out=hout[ib][:], in0=ge[ib][:], scalar=rstd, in1=gpost[:], op0=ALU.mult, op1=ALU.mult),
                reads=[b_ge[ib], b_stat[si], b_W], writes=[b_hout[ib]])
            P.op("pool", lambda e: e.tensor_tensor(out=hout[ib][:], in0=hout[ib][:], in1=hin[ib][:], op=ALU.add),
                 reads=[b_hout[ib], b_hin[ib]], writes=[b_hout[ib]])
            P.op("sp", lambda e: e.dma_start(out=ov[n], in_=hout[ib][:]), reads=[b_hout[ib]], dma=True)

        s1(0)
        for n in range(32):
            if n + 1 < 32:
                s1(n + 1)
            s2(n)
        P.flush()


def build_program(debug=False, stage=99, only=None):
    nc = bass.Bass("TRN2", target_bir_lowering=False)
    I = {}

    def din(name, shape):
        I[name] = nc.dram_tensor(name, shape, F32, kind="ExternalInput").ap()
        return I[name]

    din("x", [S, D])
    din("p", [S, 256])
    for nm in ("ffn1", "ffn2"):
        din(nm + "_pre_g", [1, D])
        din(nm + "_w_gate", [D, DFF])
        din(nm + "_w_up", [D, DFF])
        din(nm + "_w_down", [DFF, D])
        din(nm + "_post_g", [1, D])
    din("mix_pre_g", [1, D])
    din("w_in", [D, INW])
    din("conv_w", [4, 512])
    din("conv_b", [1, 512])
    din("mlstm_i_bias", [1, 4])
    din("mlstm_f_bias", [1, 4])
    din("mlstm_norm_g", [1, 512])
    din("fox_f_bias", [1, 8])
    din("branch_gate_bias", [1, 2048])
    din("w_branch_a", [512, D])
    din("w_branch_b", [512, D])
    din("w_out", [D, D])
    din("mix_post_g", [1, D])
    din("ple_pre_g", [1, D])
    din("ple_w_gate", [D, D])
    din("ple_b_gate", [1, D])
    din("ple_w_proj", [256, D])
    din("ple_post_g", [1, D])

    skind = "ExternalOutput" if debug else "Internal"

    def dscr(name, shape, dt):
        return nc.dram_tensor(name, shape, dt, kind=skind).ap()

    out = nc.dram_tensor("out", [S, D], F32, kind="ExternalOutput").ap()
    h1 = dscr("h1", [S, D], F32)
    uT1 = dscr("uT1", [8, 128, S], BF16)
    gpre = dscr("gpre", [72, S], F32)
    SC = {
        "mqkT": dscr("mqkT", [4, 128, S], BF16),
        "vaugM": dscr("vaugM", [S, 4, 129], BF16),
        "so": dscr("so", [S, 512], BF16),
        "qaug": dscr("qaug", [8, 70, S], BF16),
        "kaug": dscr("kaug", [8, 70, S], BF16),
        "vaugF": dscr("vaugF", [S, 8, 65], BF16),
        "ga": dscr("ga", [S, D], BF16),
        "gb": dscr("gb", [S, D], BF16),
        "yaT": dscr("yaT", [4, 128, S], BF16),
        "ybT": dscr("ybT", [4, 128, S], BF16),
    }
    h2 = dscr("h2", [S, D], F32)
    h3 = dscr("h3", [S, D], F32)
    uT3 = dscr("uT3", [8, 128, S], BF16)

    with contextlib.ExitStack() as st:
        P = Prog(nc, st)
        C = Ctx()
        C.db = {}

        def db(name):
            if name not in C.db:
                C.db[name] = P.bufs_n("D" + name, 32)
            return C.db[name]

        setup_consts(P, nc, st, C)
        C.wthr = st.enter_context(nc.sbuf_tensor("wthr", [128, 32, 8], F32))
        C.decbc = st.enter_context(nc.sbuf_tensor("decbc", [128, 4, 32], F32))
        C.b_wthr = P.buf("wthr")
        C.b_decbc = P.buf("decbc")
        def want(name, st_no):
            return (name in only) if only is not None else (stage >= st_no)

        if want("ffn1", 1):
            ffn_pass(P, nc, C, "f1", I["x"], I["ffn1_w_gate"], I["ffn1_w_up"], I["ffn1_w_down"],
                     I["ffn1_pre_g"], I["ffn1_post_g"], h1, I["mix_pre_g"], uT1,
                     db("x"), db("h1"), db("uT1"), gate_w=I["w_in"], gate_dst=gpre, gate_b=db("gpre"))
        if want("gp", 2):
            aug_b = P.buf("augrows")
            gp_stage(P, nc, C, I, gpre, db("gpre"), SC["qaug"], SC["kaug"], aug_b)
            db("aug").append(aug_b)
        if want("win", 2):
            win_pass(P, nc, C, I, uT1, db("uT1"), SC, db)
        if want("mix", 3):
            mix_pass(P, nc, C, I, SC, db)
        if want("merge", 4):
            merge_pass(P, nc, C, I, SC, db, h1, db("h1"), h2, db("h2"))
        if want("ffn2", 5):
            ffn_pass(P, nc, C, "f2", h2, I["ffn2_w_gate"], I["ffn2_w_up"], I["ffn2_w_down"],
                     I["ffn2_pre_g"], I["ffn2_post_g"], h3, I["ple_pre_g"], uT3,
                     db("h2"), db("h3"), db("uT3"))
        if want("ple", 6):
            ple_pass(P, nc, C, I, uT3, db("uT3"), h3, db("h3"), out)
        P.flush(final=True)
    return nc

IN_NAMES = ["x", "p", "ffn1_pre_g", "ffn1_w_gate", "ffn1_w_up", "ffn1_w_down", "ffn1_post_g",
            "mix_pre_g", "w_in", "conv_w", "conv_b", "mlstm_i_bias", "mlstm_f_bias", "mlstm_norm_g",
            "fox_f_bias", "branch_gate_bias", "w_branch_a", "w_branch_b", "w_out", "mix_post_g",
            "ffn2_pre_g", "ffn2_w_gate", "ffn2_w_up", "ffn2_w_down", "ffn2_post_g",
            "ple_pre_g", "ple_w_gate", "ple_b_gate", "ple_w_proj", "ple_post_g"]


def make_in_maps(inputs, cores):
    maps = []
    shared = {}
    for k in IN_NAMES:
        if k in ("x", "p"):
            continue
        shared[k] = np.ascontiguousarray(np.asarray(inputs[k])[0], dtype=np.float32)
    x = np.asarray(inputs["x"])
    p = np.asarray(inputs["p"])
    for b in cores:
        m = dict(shared)
        m["x"] = np.ascontiguousarray(x[b], dtype=np.float32)
        m["p"] = np.ascontiguousarray(p[0, b], dtype=np.float32)
        maps.append(m)
    return maps


def kernel(**inputs):
    nc = build_program()
    maps = make_in_maps(inputs, list(range(8)))
    res = run_bass_kernel_spmd(nc, maps, core_ids=list(range(8)))
    return np.stack([np.asarray(r["out"], dtype=np.float32) for r in res.results], axis=0)
```

```python
import contextlib
import numpy as np
import concourse.bass as bass
import concourse.mybir as mybir
from concourse.bass_utils import run_bass_kernel_spmd

F32 = mybir.dt.float32
BF16 = mybir.dt.bfloat16
AF = mybir.ActivationFunctionType
ALU = mybir.AluOpType
AX = mybir.AxisListType

S = 4096
D = 1024
DFF = 2816
NFC = DFF // 128
NT = 8
TT = 512
EPS = 1e-6
INW = 5136

ENGS = ("pe", "act", "dve", "pool", "sp")


class Buf:
    __slots__ = ("name", "w", "r")

    def __init__(self, name=""):
        self.name = name
        self.w = None
        self.r = []


class Op:
    __slots__ = ("eng", "fn", "deps", "inc", "cnt", "dma", "sem", "emitted")

    def __init__(self, eng, fn, dma=False):
        self.eng = eng
        self.fn = fn
        self.deps = []
        self.inc = False
        self.cnt = 0
        self.dma = dma
        self.sem = None
        self.emitted = False


class Prog:
    def __init__(self, nc, st, n_dma_sems=20):
        self.nc = nc
        self.pending = {e: [] for e in ENGS}
        self.bufs = []
        self.nd = n_dma_sems
        self.esem = {e: st.enter_context(nc.semaphore("s_" + e)) for e in ENGS}
        self.dsem = {}
        for e in ("sp", "pool"):
            for s in range(n_dma_sems):
                self.dsem[(e, s)] = st.enter_context(nc.semaphore("d_%s_%d" % (e, s)))
        self.ecnt = {e: 0 for e in ENGS}
        self.dcnt = {e: 0 for e in ENGS}
        self.waited = {e: {} for e in ENGS}
        self.n_ops = 0

    def buf(self, name=""):
        b = Buf(name)
        self.bufs.append(b)
        return b

    def bufs_n(self, name, n):
        return [self.buf("%s%d" % (name, i)) for i in range(n)]

    def op(self, eng, fn, reads=(), writes=(), dma=False):
        o = Op(eng, fn, dma)
        seen = set()
        cand = []
        for b in reads:
            if b.w is not None:
                cand.append(b.w)
        for b in writes:
            if b.w is not None:
                cand.append(b.w)
            cand.extend(b.r)
        for d in cand:
            if d is o or id(d) in seen:
                continue
            seen.add(id(d))
            if d.eng == "pe" and eng == "pe" and not d.dma and not dma:
                continue
            o.deps.append(d)
            if not d.emitted:
                d.inc = True
        for b in reads:
            b.r.append(o)
        for b in writes:
            b.w = o
            b.r = []
        self.pending[eng].append(o)
        self.n_ops += 1
        return o

    def flush(self, final=False):
        nc = self.nc
        for b in self.bufs:
            if b.w is not None and not b.w.emitted:
                b.w.inc = True
            for r in b.r:
                if not r.emitted:
                    r.inc = True
        for e in ENGS:
            for o in self.pending[e]:
                if o.dma:
                    k = self.dcnt[e]
                    o.sem = (e, k % self.nd)
                    o.cnt = 16 * (k // self.nd + 1)
                    self.dcnt[e] = k + 1
                elif o.inc:
                    self.ecnt[e] += 1
                    o.cnt = self.ecnt[e]
        pending = self.pending
        self.pending = {e: [] for e in ENGS}

        def run(ename, eng):
            waited = self.waited[ename]
            for o in pending[ename]:
                for d in o.deps:
                    key = d.sem if d.dma else d.eng
                    if waited.get(key, 0) >= d.cnt:
                        continue
                    assert d.cnt > 0, (d.eng, ename)
                    eng.wait_ge(self.dsem[key] if d.dma else self.esem[key], d.cnt)
                    waited[key] = d.cnt
                if o.dma:
                    if o.cnt > 16 and waited.get(o.sem, 0) < o.cnt - 16:
                        eng.wait_ge(self.dsem[o.sem], o.cnt - 16)
                        waited[o.sem] = o.cnt - 16
                    o.fn(eng).then_inc(self.dsem[o.sem], 16)
                else:
                    ins = o.fn(eng)
                    if o.inc:
                        ins.then_inc(self.esem[o.eng], 1)
                o.emitted = True
            if ename == "sp" and final:
                for q in ("sp", "pool"):
                    k = self.dcnt[q]
                    for sl in range(min(self.nd, k)):
                        last = 16 * ((k - 1 - sl) // self.nd + 1)
                        eng.wait_ge(self.dsem[(q, sl)], last)

        with nc.Block() as block:
            @block.tensor
            def _(eng):
                run("pe", eng)

            @block.scalar
            def _(eng):
                run("act", eng)

            @block.vector
            def _(eng):
                run("dve", eng)

            @block.gpsimd
            def _(eng):
                run("pool", eng)

            @block.sync
            def _(eng):
                run("sp", eng)


def bcast_last(ap2d, n):
    return ap2d.unsqueeze(2).to_broadcast([ap2d.shape[0], ap2d.shape[1], n])


class Ctx:
    pass


def load_w_kmajor(P, nc, dst, src2d, n_kc, ncols, bufs, col_chunk=1408):
    v = src2d.rearrange("(kc p) n -> kc p n", p=128)
    mdl = 4 * col_chunk
    for k in range(n_kc):
        P.op("pool", lambda e, k=k: e.dma_start(out=dst[:, k, :], in_=v[k], max_dma_last_dim=mdl),
             writes=[bufs[k]], dma=True)


def setup_consts(P, nc, st, C):
    sb = lambda name, shape, dt: st.enter_context(nc.sbuf_tensor(name, shape, dt))
    C.identf = sb("identf", [128, 128], F32)
    C.ident = sb("ident", [128, 128], BF16)
    C.neghalf = sb("neghalf", [128, 1], F32)
    C.b_const = P.buf("const")
    identf, ident = C.identf, C.ident
    P.op("pool", lambda e: e.memset(identf[:], 0.0), writes=[C.b_const])
    P.op("pool", lambda e: e.affine_select(out=identf[:], in_=identf[:], pattern=[[-1, 128]],
                                             compare_op=ALU.not_equal, fill=1.0, base=0,
                                             channel_multiplier=1),
         reads=[C.b_const], writes=[C.b_const])
    P.op("dve", lambda e: e.tensor_copy(out=ident[:], in_=identf[:]), reads=[C.b_const], writes=[C.b_const])
    P.op("pool", lambda e: e.memset(C.neghalf[:], -0.5), reads=[C.b_const], writes=[C.b_const])


def rms_stats(P, C, src_ap, src_buf, junk, b_junk, ss, var, rstd, b_stat, n_feat=D):
    P.op("act", lambda e: e.activation(out=junk, in_=src_ap, func=AF.Square, accum_out=ss),
         reads=[src_buf], writes=[b_junk, b_stat])
    P.op("dve", lambda e: e.tensor_scalar(out=var, in0=ss, scalar1=1.0 / n_feat, scalar2=EPS,
                                          op0=ALU.mult, op1=ALU.add),
         reads=[b_stat], writes=[b_stat])
    P.op("pool", lambda e: e.tensor_tensor(out=rstd, in0=var, in1=C.neghalf[:], op=ALU.pow),
         reads=[b_stat, C.b_const], writes=[b_stat])


def ffn_pass(P, nc, C, tag, src_h, w_gate, w_up, w_down, pre_g, post_g, dst_h, next_g, dst_uT,
             src_b, dst_b, uT_b, gate_w=None, gate_dst=None, gate_b=None):
    with contextlib.ExitStack() as st:
        sb = lambda name, shape, dt: st.enter_context(nc.sbuf_tensor(tag + name, shape, dt))
        ps = lambda name, shape, dt: st.enter_context(nc.psum_tensor(tag + name, shape, dt))
        Wg = sb("Wg", [128, 8, DFF], BF16)
        Wu = sb("Wu", [128, 8, DFF], BF16)
        Wd = sb("Wd", [128, NFC, D], BF16)
        b_Wg = P.bufs_n("Wg", 8)
        b_Wu = P.bufs_n("Wu", 8)
        b_Wd = P.bufs_n("Wd", 2)
        load_w_kmajor(P, nc, Wg, w_gate, 8, DFF, b_Wg)
        load_w_kmajor(P, nc, Wu, w_up, 8, DFF, b_Wu)
        wdv = w_down.rearrange("(fc p) d -> p fc d", p=128)
        for hh in range(2):
            P.op("pool", lambda e, hh=hh: e.dma_start(out=Wd[:, hh * 11:(hh + 1) * 11, :],
                                                       in_=wdv[:, hh * 11:(hh + 1) * 11, :]),
                 writes=[b_Wd[hh]], dma=True)
        gpre = sb("gpre", [128, 8], F32)
        gnext = sb("gnext", [128, 8], F32)
        gpost = sb("gpost", [128, D], F32)
        b_par = P.buf("par")
        P.op("sp", lambda e: e.dma_start(out=gpre[:], in_=pre_g.rearrange("o (k p) -> p (o k)", p=128),
                                         allow_slow_non_contiguous=True),
             writes=[b_par], dma=True)
        P.op("sp", lambda e: e.dma_start(out=gnext[:], in_=next_g.rearrange("o (k p) -> p (o k)", p=128),
                                         allow_slow_non_contiguous=True),
             writes=[b_par], dma=True)
        P.op("sp", lambda e: e.dma_start(out=gpost[:], in_=post_g.partition_broadcast(128)),
             writes=[b_par], dma=True)
        if gate_w is not None:
            Wgt = sb("Wgt", [128, 8, 72], BF16)
            b_Wgt = P.buf("Wgt")
            P.op("pool", lambda e: e.memset(Wgt[:], 0.0), writes=[b_Wgt])
            gv = gate_w.rearrange("(kc p) n -> p kc n", p=128)
            for (c0, n, d0) in ((1540, 4, 0), (1536, 4, 32), (3080, 8, 64)):
                P.op("pool", lambda e, c0=c0, n=n, d0=d0: e.dma_start(
                    out=Wgt[:, :, d0:d0 + n], in_=gv[:, :, c0:c0 + n]),
                    reads=[], writes=[b_Wgt], dma=True)
            gsb = [sb("gsb%d" % i, [72, 128], F32) for i in range(2)]
            b_gsb = P.bufs_n("gsb", 2)

        NXB = 3
        xb = [sb("xb%d" % i, [128, D], F32) for i in range(NXB)]
        b_xb = P.bufs_n("xb", NXB)
        ubf = [sb("ubf%d" % i, [128, D], BF16) for i in range(2)]
        b_ubf = P.bufs_n("ubf", 2)
        junk = sb("junk", [128, D], BF16)
        b_junk = P.buf("junk")
        uT = sb("uT", [128, 8, TT], BF16)
        b_uT = P.bufs_n("uT", 4)
        aT = sb("aT", [128, NFC, TT], BF16)
        b_aT = P.bufs_n("aT", NFC)
        sg = [sb("sg%d" % i, [128, TT], F32) for i in range(2)]
        b_sg = P.bufs_n("sg", 2)
        hst = [sb("hst%d" % i, [128, D], F32) for i in range(2)]
        b_hst = P.bufs_n("hst", 2)
        u2T = [sb("u2T%d" % i, [128, 8, 128], BF16) for i in range(2)]
        b_u2T = P.bufs_n("u2T", 2)
        NST = 6
        stat = sb("stat", [128, 3 * NST], F32)
        b_stat = P.bufs_n("stat", NST)

        pt = ps("pt", [128, 8, 128], BF16)
        b_pt = P.buf("pt")
        pg = [ps("pg%d" % i, [128, TT], F32) for i in range(2)]
        pu = [ps("pu%d" % i, [128, TT], F32) for i in range(2)]
        b_pg = P.bufs_n("pg", 2)
        b_pu = P.bufs_n("pu", 2)
        pys = [ps("py%d" % i, [128, 512], F32) for i in range(3)]
        b_pys = P.bufs_n("py", 3)
        if gate_w is not None:
            pgt = pg[0][0:72, 0:128]
            b_pgt = b_pg[0]

        src_v = src_h.rearrange("(n p) d -> n p d", p=128)
        dst_v = dst_h.rearrange("(n p) d -> n p d", p=128)
        cnt = {"x": 0, "u": 0, "st": 0, "h": 0, "u2": 0, "sg": 0, "gs": 0, "py": 0}

        def norm_T(h_ap, h_buf, gcol, out_ap, out_bufs):
            si = cnt["st"] % NST
            cnt["st"] += 1
            ss, var, rstd = (stat[:, 3 * si + j:3 * si + j + 1] for j in range(3))
            rms_stats(P, C, h_ap, h_buf, junk[:], b_junk, ss, var, rstd, b_stat[si])
            ui = cnt["u"] % 2
            cnt["u"] += 1
            u = ubf[ui]
            P.op("dve", lambda e: e.tensor_scalar(out=u[:], in0=h_ap, scalar1=rstd, scalar2=None, op0=ALU.mult),
                 reads=[h_buf, b_stat[si]], writes=[b_ubf[ui]])
            for k in range(8):
                P.op("pe", lambda e, k=k: e.transpose(out=pt[:, k, :], in_=u[:, k * 128:(k + 1) * 128],
                                                      identity=C.ident[:]),
                     reads=[b_ubf[ui], C.b_const], writes=[b_pt])
            P.op("dve", lambda e: e.tensor_tensor(out=out_ap, in0=pt[:], in1=bcast_last(gcol[:], 128), op=ALU.mult),
                 reads=[b_pt, b_par], writes=out_bufs)

        def pre(i):
            for s in range(4):
                n = i * 4 + s
                xi = cnt["x"] % NXB
                cnt["x"] += 1
                P.op("sp", lambda e, n=n, xi=xi: e.dma_start(out=xb[xi][:], in_=src_v[n]),
                     reads=[src_b[n]], writes=[b_xb[xi]], dma=True)
                norm_T(xb[xi][:], b_xb[xi], gpre, uT[:, :, s * 128:(s + 1) * 128], [b_uT[s]])

        def gateup(i):
            for f in range(NFC):
                j = f % 2
                for k in range(8):
                    P.op("pe", lambda e, k=k, f=f, j=j: e.matmul(
                        pg[j][:], lhsT=Wg[:, k, f * 128:(f + 1) * 128], rhs=uT[:, k, :],
                        start=(k == 0), stop=(k == 7)),
                        reads=[b_Wg[k]] + b_uT, writes=[b_pg[j]])
                for k in range(8):
                    P.op("pe", lambda e, k=k, f=f, j=j: e.matmul(
                        pu[j][:], lhsT=Wu[:, k, f * 128:(f + 1) * 128], rhs=uT[:, k, :],
                        start=(k == 0), stop=(k == 7)),
                        reads=[b_Wu[k]] + b_uT, writes=[b_pu[j]])
                si = cnt["sg"] % 2
                cnt["sg"] += 1
                P.op("act", lambda e, j=j, si=si: e.activation(out=sg[si][:], in_=pg[j][:], func=AF.Silu),
                     reads=[b_pg[j]], writes=[b_sg[si]])
                P.op("dve", lambda e, j=j, si=si, f=f: e.tensor_tensor(out=aT[:, f, :], in0=sg[si][:], in1=pu[j][:],
                                                                   op=ALU.mult),
                     reads=[b_sg[si], b_pu[j]], writes=[b_aT[f]])

        def down_post(i):
            for s in range(4):
                n = i * 4 + s
                pyh = []
                for hf in range(2):
                    pi = cnt["py"] % 3
                    cnt["py"] += 1
                    pyh.append((pys[pi], b_pys[pi]))
                    for f in range(NFC):
                        P.op("pe", lambda e, f=f, s=s, hf=hf, pi=pi: e.matmul(
                            pys[pi][:], lhsT=aT[:, f, s * 128:(s + 1) * 128],
                            rhs=Wd[:, f, hf * 512:(hf + 1) * 512], start=(f == 0), stop=(f == NFC - 1)),
                            reads=[b_aT[f], b_Wd[f // 11]], writes=[b_pys[pi]])
                xi = cnt["x"] % NXB
                cnt["x"] += 1
                P.op("sp", lambda e, n=n, xi=xi: e.dma_start(out=xb[xi][:], in_=src_v[n]),
                     reads=[src_b[n]], writes=[b_xb[xi]], dma=True)
                si = cnt["st"] % NST
                cnt["st"] += 1
                ss, var, rstd = (stat[:, 3 * si + j:3 * si + j + 1] for j in range(3))
                P.op("act", lambda e, ss=ss, t=pyh[0][0]: e.activation(out=junk[:, 0:512], in_=t[:], func=AF.Square,
                                                                      accum_out=ss),
                     reads=[pyh[0][1]], writes=[b_junk, b_stat[si]])
                P.op("act", lambda e, var=var, t=pyh[1][0]: e.activation(out=junk[:, 512:1024], in_=t[:], func=AF.Square,
                                                                        accum_out=var),
                     reads=[pyh[1][1]], writes=[b_junk, b_stat[si]])
                P.op("dve", lambda e, ss=ss, var=var: e.tensor_tensor(out=var, in0=ss, in1=var, op=ALU.add),
                     reads=[b_stat[si]], writes=[b_stat[si]])
                P.op("dve", lambda e, var=var: e.tensor_scalar(out=var, in0=var, scalar1=1.0 / D, scalar2=EPS,
                                                              op0=ALU.mult, op1=ALU.add),
                     reads=[b_stat[si]], writes=[b_stat[si]])
                P.op("pool", lambda e, var=var, rstd=rstd: e.tensor_tensor(out=rstd, in0=var, in1=C.neghalf[:], op=ALU.pow),
                     reads=[b_stat[si], C.b_const], writes=[b_stat[si]])
                hi = cnt["h"] % 2
                cnt["h"] += 1
                hb = hst[hi]
                for hf in range(2):
                    hs = slice(hf * 512, (hf + 1) * 512)
                    P.op("dve", lambda e, hb=hb, rstd=rstd, t=pyh[hf][0], hs=hs: e.scalar_tensor_tensor(
                        out=hb[:, hs], in0=t[:], scalar=rstd, in1=gpost[:, hs], op0=ALU.mult, op1=ALU.mult),
                        reads=[pyh[hf][1], b_stat[si], b_par], writes=[b_hst[hi]])
                P.op("dve", lambda e, hb=hb, xi=xi: e.scalar_tensor_tensor(
                    out=hb[:], in0=hb[:], scalar=0.5, in1=xb[xi][:], op0=ALU.mult, op1=ALU.add),
                    reads=[b_hst[hi], b_xb[xi]], writes=[b_hst[hi]])
                P.op("sp", lambda e, hb=hb, n=n: e.dma_start(out=dst_v[n], in_=hb[:]),
                     reads=[b_hst[hi]], writes=[dst_b[n]], dma=True)
                ui2 = cnt["u2"] % 2
                cnt["u2"] += 1
                norm_T(hb[:], b_hst[hi], gnext, u2T[ui2][:], [b_u2T[ui2]])
                P.op("sp", lambda e, ui2=ui2, n=n: e.dma_start(
                    out=dst_uT[:, :, n * 128:(n + 1) * 128].rearrange("k p t -> p k t"), in_=u2T[ui2][:]),
                    reads=[b_u2T[ui2]], writes=[uT_b[n]], dma=True)
                if gate_w is not None:
                    for k in range(8):
                        P.op("pe", lambda e, k=k, ui2=ui2: e.matmul(
                            pgt, lhsT=Wgt[:, k, :], rhs=u2T[ui2][:, k, :], start=(k == 0), stop=(k == 7)),
                            reads=[b_Wgt, b_u2T[ui2]], writes=[b_pgt])
                    gi = cnt["gs"] % 2
                    cnt["gs"] += 1
                    P.op("act", lambda e, gi=gi: e.activation(out=gsb[gi][:], in_=pgt, func=AF.Copy),
                         reads=[b_pgt], writes=[b_gsb[gi]])
                    P.op("sp", lambda e, gi=gi, n=n: e.dma_start(out=gate_dst[:, n * 128:(n + 1) * 128], in_=gsb[gi][:]),
                         reads=[b_gsb[gi]], writes=[gate_b[n]], dma=True)

        pre(0)
        for i in range(NT):
            gateup(i)
            if i + 1 < NT:
                pre(i + 1)
            down_post(i)
        P.flush()


def gp_stage(P, nc, C, I, gpre, gpre_b, qaug, kaug, aug_b):
    with contextlib.ExitStack() as st:
        sb = lambda name, shape, dt: st.enter_context(nc.sbuf_tensor("gp" + name, shape, dt))
        ps = lambda name, shape, dt: st.enter_context(nc.psum_tensor("gp" + name, shape, dt))
        T0 = sb("T0", [72, S], F32)
        T1 = sb("T1", [72, S], F32)
        T2 = sb("T2", [72, S], F32)
        T3 = sb("T3", [72, S], F32)
        QR = sb("QR", [72, 3, S], BF16)
        KR = sb("KR", [72, 3, S], BF16)
        ONE = sb("ONE", [72, S], BF16)
        bcol = sb("bcol", [72, 1], F32)
        negb = sb("negb", [72, 1], F32)
        bicol = sb("bicol", [72, 1], F32)
        onec = sb("onec", [72, 1], F32)
        cm = sb("cm", [72, 32], F32)
        mce = sb("mce", [72, 32], F32)
        mprev = sb("mprev", [72, 32], F32)
        dec = sb("dec", [72, 32], F32)
        esel = sb("esel", [72, 4, 128], F32)
        bT0, bT1, bT2, bT3, bQR, bKR, bONE, bsm = [P.buf(n) for n in
                                                   ("T0", "T1", "T2", "T3", "QR", "KR", "ONE", "gsm")]
        ptm = ps("ptm", [128, 32, 8], F32)
        pdc = ps("pdc", [128, 4, 32], F32)
        b_ptm, b_pdc = P.buf("ptm"), P.buf("pdc")

        P.op("sp", lambda e: e.dma_start(out=T0[:], in_=gpre), reads=gpre_b, writes=[bT0], dma=True)
        P.op("sp", lambda e: e.dma_start(out=T3[0:4, :], in_=gpre[32:36, :]), reads=gpre_b, writes=[bT3], dma=True)
        P.op("dve", lambda e: e.memset(bcol[:], 0.0), writes=[bsm])
        P.op("dve", lambda e: e.memset(bicol[:], 0.0), reads=[bsm], writes=[bsm])
        P.op("dve", lambda e: e.memset(onec[:], 1.0), reads=[bsm], writes=[bsm])
        P.op("pool", lambda e: e.memset(ONE[:], 1.0), writes=[bONE])
        P.op("sp", lambda e: e.dma_start(out=bcol[0:4, :], in_=I["mlstm_f_bias"].rearrange("o n -> n o"),
                                         allow_slow_non_contiguous=True), reads=[bsm], writes=[bsm], dma=True)
        P.op("sp", lambda e: e.dma_start(out=bcol[64:72, :], in_=I["fox_f_bias"].rearrange("o n -> n o"),
                                         allow_slow_non_contiguous=True), reads=[bsm], writes=[bsm], dma=True)
        P.op("sp", lambda e: e.dma_start(out=bicol[0:4, :], in_=I["mlstm_i_bias"].rearrange("o n -> n o"),
                                         allow_slow_non_contiguous=True), reads=[bsm], writes=[bsm], dma=True)
        P.op("dve", lambda e: e.tensor_scalar(out=negb[0:72, :], in0=bcol[0:72, :], scalar1=-1.0, scalar2=None,
                                              op0=ALU.mult), reads=[bsm], writes=[bsm])
        R = slice(0, 72)
        P.op("act", lambda e: e.activation(out=T1[R, :], in_=T0[R, :], func=AF.Exp, scale=-1.0, bias=negb[R, :]),
             reads=[bT0, bsm], writes=[bT1])
        P.op("act", lambda e: e.activation(out=T1[R, :], in_=T1[R, :], func=AF.Ln, scale=1.0, bias=onec[R, :]),
             reads=[bT1, bsm], writes=[bT1])
        P.op("dve", lambda e: e.tensor_tensor_scan(out=T2[R, :], data0=T1[R, :], data1=T1[R, :], initial=0.0,
                                                   op0=ALU.add, op1=ALU.max), reads=[bT1], writes=[bT2])
        M = slice(0, 4)
        P.op("dve", lambda e: e.scalar_tensor_tensor(out=T3[M, :], in0=T3[M, :], scalar=bicol[M, :], in1=T2[M, :],
                                                     op0=ALU.add, op1=ALU.add), reads=[bT3, bT2, bsm], writes=[bT3])
        P.op("dve", lambda e: e.tensor_reduce(out=cm[M, :], in_=T3[M, :].rearrange("p (c l) -> p c l", l=128),
                                              axis=AX.X, op=ALU.max), reads=[bT3], writes=[bsm])
        P.op("dve", lambda e: e.tensor_tensor_scan(out=mce[M, :], data0=cm[M, :], data1=cm[M, :], initial=0.0,
                                                   op0=ALU.max, op1=ALU.max), reads=[bsm], writes=[bsm])
        P.op("dve", lambda e: e.tensor_tensor(out=T3[M, :].rearrange("p (c l) -> p c l", l=128),
                                              in0=T3[M, :].rearrange("p (c l) -> p c l", l=128),
                                              in1=bcast_last(mce[M, :], 128), op=ALU.subtract),
             reads=[bT3, bsm], writes=[bT3])
        P.op("act", lambda e: e.activation(out=T3[M, :], in_=T3[M, :], func=AF.Exp), reads=[bT3], writes=[bT3])
        P.op("dve", lambda e: e.tensor_tensor(out=T1[M, :].rearrange("p (c l) -> p c l", l=128),
                                              in0=T2[M, :].rearrange("p (c l) -> p c l", l=128),
                                              in1=bcast_last(mce[M, :], 128), op=ALU.subtract),
             reads=[bT2, bsm, bT1], writes=[bT1])
        P.op("act", lambda e: e.activation(out=T1[M, :], in_=T1[M, :], func=AF.Exp, scale=2.0), reads=[bT1], writes=[bT1])
        P.op("dve", lambda e: e.memset(mprev[M, :], 0.0), reads=[bsm], writes=[bsm])
        P.op("dve", lambda e: e.tensor_copy(out=mprev[M, 1:32], in_=mce[M, 0:31]), reads=[bsm], writes=[bsm])
        P.op("dve", lambda e: e.tensor_tensor(out=dec[M, :], in0=mprev[M, :], in1=mce[M, :], op=ALU.subtract),
             reads=[bsm], writes=[bsm])
        P.op("act", lambda e: e.activation(out=dec[M, :], in_=dec[M, :], func=AF.Exp), reads=[bsm], writes=[bsm])
        for c in range(32):
            P.op("pe", lambda e, c=c: e.transpose(out=ptm[:, c, 0:4], in_=T3[M, c * 128:(c + 1) * 128],
                                                  identity=C.identf[M, 0:4]),
                 reads=[bT3, C.b_const], writes=[b_ptm])
            P.op("pe", lambda e, c=c: e.transpose(out=ptm[:, c, 4:8], in_=T1[M, c * 128:(c + 1) * 128],
                                                  identity=C.identf[M, 0:4]),
                 reads=[bT1, C.b_const], writes=[b_ptm])
        P.op("dve", lambda e: e.tensor_copy(out=C.wthr[:], in_=ptm[:]), reads=[b_ptm], writes=[C.b_wthr])
        for h in range(4):
            P.op("dve", lambda e, h=h: e.tensor_copy(out=esel[M, h, :],
                                                     in_=C.identf[M, h:h + 1].to_broadcast([4, 128])),
                 reads=[C.b_const, bsm], writes=[bsm])
        for h in range(4):
            P.op("pe", lambda e, h=h: e.matmul(pdc[:, h, :], lhsT=esel[M, h, :], rhs=dec[M, :], start=True, stop=True),
                 reads=[bsm], writes=[b_pdc])
        P.op("dve", lambda e: e.tensor_copy(out=C.decbc[:], in_=pdc[:]), reads=[b_pdc], writes=[C.b_decbc])
        Fx = slice(64, 72)
        Fd = slice(64, 72)
        P.op("dve", lambda e: e.tensor_scalar(out=T0[Fx, :], in0=T2[Fx, :], scalar1=-1.0, scalar2=None, op0=ALU.mult),
             reads=[bT2, bT0], writes=[bT0])
        for part in range(3):
            P.op("dve", lambda e, part=part: e.tensor_copy(out=QR[Fx, part, :], in_=T0[Fx, :]),
                 reads=[bT0], writes=[bQR])
            if part < 2:
                P.op("dve", lambda e, part=part: e.tensor_tensor(out=T0[Fx, :], in0=T0[Fx, :], in1=QR[Fx, part, :],
                                                                 op=ALU.subtract), reads=[bT0, bQR], writes=[bT0])
        P.op("pool", lambda e: e.tensor_scalar(out=KR[Fx, :, :], in0=QR[Fx, :, :], scalar1=-1.0, scalar2=None,
                                               op0=ALU.mult), reads=[bQR], writes=[bKR])
        P.op("sp", lambda e: e.dma_start(out=qaug[:, 64:67, :], in_=QR[Fd, :, :]), reads=[bQR], writes=[aug_b], dma=True)
        P.op("sp", lambda e: e.dma_start(out=kaug[:, 67:70, :], in_=KR[Fd, :, :]), reads=[bKR], writes=[aug_b], dma=True)
        for r in range(3):
            P.op("sp", lambda e, r=r: e.dma_start(out=qaug[:, 67 + r, :], in_=ONE[Fd, :]), reads=[bONE],
                 writes=[aug_b], dma=True)
            P.op("sp", lambda e, r=r: e.dma_start(out=kaug[:, 64 + r, :], in_=ONE[Fd, :]), reads=[bONE],
                 writes=[aug_b], dma=True)
        P.flush()


def win_pass(P, nc, C, I, uT1, uT_b, SC, DB):
    w_in = I["w_in"]
    with contextlib.ExitStack() as st:
        sb = lambda name, shape, dt: st.enter_context(nc.sbuf_tensor("wi" + name, shape, dt))
        ps = lambda name, shape, dt: st.enter_context(nc.psum_tensor("wi" + name, shape, dt))
        W = sb("W", [128, 8, INW], BF16)
        b_W = P.bufs_n("Win", 8)
        wv = w_in.rearrange("(kc p) n -> kc p n", p=128)
        for k in range(8):
            P.op("pool", lambda e, k=k: e.dma_start(out=W[:, k, :], in_=wv[k], max_dma_last_dim=4 * 1284),
                 writes=[b_W[k]], dma=True)
        cw = sb("cw", [128, 4, 4], F32)
        cb = sb("cb", [128, 4], F32)
        gbias = sb("gbias", [128, 2048], F32)
        b_par = P.buf("wipar")
        for tap in range(4):
            P.op("sp", lambda e, tap=tap: e.dma_start(
                out=cw[:, :, tap], in_=I["conv_w"][tap:tap + 1, :].rearrange("o (c p) -> p (o c)", p=128),
                allow_slow_non_contiguous=True), writes=[b_par], dma=True)
        P.op("sp", lambda e: e.dma_start(out=cb[:], in_=I["conv_b"].rearrange("o (c p) -> p (o c)", p=128),
                                         allow_slow_non_contiguous=True), writes=[b_par], dma=True)
        P.op("sp", lambda e: e.dma_start(out=gbias[:], in_=I["branch_gate_bias"].partition_broadcast(128)),
             writes=[b_par], dma=True)
        uT = [sb("uT%d" % i, [128, 8, TT], BF16) for i in range(2)]
        b_uT = P.bufs_n("wiuT", 2)
        zq = sb("zq", [128, 4, 3 + TT], F32)
        b_zq = P.bufs_n("zq", 4)
        acc = [sb("acc%d" % i, [128, TT], F32) for i in range(2)]
        b_acc = P.bufs_n("acc", 2)
        fo = [sb("fo%d" % i, [128, TT], BF16) for i in range(3)]
        b_fo = P.bufs_n("fo", 3)
        tv = [sb("tv%d" % i, [128, 4, 129], BF16) for i in range(2)]
        b_tv = P.bufs_n("tv", 2)
        tf = [sb("tf%d" % i, [128, 8, 65], BF16) for i in range(2)]
        b_tf = P.bufs_n("tf", 2)
        tg = [sb("tg%d" % i, [128, 512], F32) for i in range(2)]
        b_tg = P.bufs_n("tg", 2)
        to = [sb("to%d" % i, [128, 512], BF16) for i in range(3)]
        b_to = P.bufs_n("to", 3)
        pf = [ps("pf%d" % i, [128, TT], F32) for i in range(2)]
        b_pf = P.bufs_n("pf", 2)
        pk = [ps("pk%d" % i, [128, 512], F32) for i in range(2)]
        b_pk = P.bufs_n("pk", 2)
        cnt = {"pf": 0, "pk": 0, "acc": 0, "fo": 0, "tv": 0, "tf": 0, "tg": 0, "to": 0}

        def rot(key, n):
            v = cnt[key] % n
            cnt[key] += 1
            return v

        for ch in range(4):
            P.op("dve", lambda e, ch=ch: e.memset(zq[:, ch, 0:3], 0.0), writes=[b_zq[ch]])
        for i in range(NT):
            ub = i % 2
            tcols = slice(i * TT, (i + 1) * TT)
            P.op("sp", lambda e, ub=ub, tcols=tcols: e.dma_start(
                out=uT[ub][:], in_=uT1[:, :, tcols].rearrange("k p t -> p k t")),
                reads=uT_b[4 * i:4 * i + 4], writes=[b_uT[ub]], dma=True)
            fm = [("mqk", ch, ch * 128) for ch in range(4)] + \
                 [("fq", ch, 1544 + ch * 128) for ch in range(4)] + \
                 [("fk", ch, 2056 + ch * 128) for ch in range(4)]
            for (kind, ch, c0) in fm:
                j = rot("pf", 2)
                for k in range(8):
                    P.op("pe", lambda e, k=k, c0=c0, j=j, ub=ub: e.matmul(
                        pf[j][:], lhsT=W[:, k, c0:c0 + 128], rhs=uT[ub][:, k, :], start=(k == 0), stop=(k == 7)),
                        reads=[b_W[k], b_uT[ub]], writes=[b_pf[j]])
                if kind == "mqk":
                    P.op("act", lambda e, ch=ch, j=j: e.activation(out=zq[:, ch, 3:3 + TT], in_=pf[j][:], func=AF.Copy),
                         reads=[b_pf[j]], writes=[b_zq[ch]])
                    a = rot("acc", 2)
                    P.op("dve", lambda e, ch=ch, a=a: e.tensor_scalar(
                        out=acc[a][:], in0=zq[:, ch, 0:TT], scalar1=cw[:, ch, 0:1], scalar2=cb[:, ch:ch + 1],
                        op0=ALU.mult, op1=ALU.add), reads=[b_zq[ch], b_par], writes=[b_acc[a]])
                    for tap in range(1, 4):
                        P.op("dve", lambda e, ch=ch, a=a, tap=tap: e.scalar_tensor_tensor(
                            out=acc[a][:], in0=zq[:, ch, tap:tap + TT], scalar=cw[:, ch, tap:tap + 1], in1=acc[a][:],
                            op0=ALU.mult, op1=ALU.add), reads=[b_zq[ch], b_par, b_acc[a]], writes=[b_acc[a]])
                    P.op("dve", lambda e, ch=ch: e.tensor_copy(out=zq[:, ch, 0:3], in_=zq[:, ch, TT:TT + 3]),
                         reads=[b_zq[ch]], writes=[b_zq[ch]])
                    o = rot("fo", 3)
                    P.op("act", lambda e, a=a, o=o: e.activation(out=fo[o][:], in_=acc[a][:], func=AF.Silu),
                         reads=[b_acc[a]], writes=[b_fo[o]])
                    P.op("sp", lambda e, o=o, ch=ch, tcols=tcols: e.dma_start(out=SC["mqkT"][ch, :, tcols], in_=fo[o][:]),
                         reads=[b_fo[o]], writes=[DB("mqkT")[i]], dma=True)
                else:
                    o = rot("fo", 3)
                    sc = 0.125 if kind == "fq" else 1.0
                    P.op("act", lambda e, o=o, j=j, sc=sc: e.activation(out=fo[o][:], in_=pf[j][:], func=AF.Copy, scale=sc),
                         reads=[b_pf[j]], writes=[b_fo[o]])
                    dst = SC["qaug"] if kind == "fq" else SC["kaug"]
                    for hh in range(2):
                        P.op("sp", lambda e, o=o, ch=ch, dst=dst, tcols=tcols, hh=hh: e.dma_start(
                            out=dst[2 * ch + hh, 0:64, tcols], in_=fo[o][hh * 64:(hh + 1) * 64, :]),
                            reads=[b_fo[o]], writes=[DB("aug")[i]], dma=True)
            for s in range(4):
                n = i * 4 + s
                rows = slice(n * 128, (n + 1) * 128)
                groups = [("mv", 512), ("mo", 1024), ("fv", 2568), ("ga", 3088), ("ga", 3600), ("gb", 4112), ("gb", 4624)]
                for gi, (kind, c0) in enumerate(groups):
                    j = rot("pk", 2)
                    for k in range(8):
                        P.op("pe", lambda e, k=k, c0=c0, j=j, ub=ub, s=s: e.matmul(
                            pk[j][:], lhsT=uT[ub][:, k, s * 128:(s + 1) * 128], rhs=W[:, k, c0:c0 + 512],
                            start=(k == 0), stop=(k == 7)),
                            reads=[b_W[k], b_uT[ub]], writes=[b_pk[j]])
                    if kind == "mv":
                        t = rot("tv", 2)
                        c = n
                        P.op("dve", lambda e, t=t, j=j, c=c: e.tensor_tensor(
                            out=tv[t][:, :, 0:128], in0=pk[j][:].rearrange("p (h d) -> p h d", h=4),
                            in1=bcast_last(C.wthr[:, c, 0:4], 128), op=ALU.mult),
                            reads=[b_pk[j], C.b_wthr], writes=[b_tv[t]])
                        P.op("dve", lambda e, t=t, c=c: e.tensor_copy(out=tv[t][:, :, 128:129],
                                                                     in_=C.wthr[:, c, 0:4].unsqueeze(2)),
                             reads=[C.b_wthr, b_tv[t]], writes=[b_tv[t]])
                        P.op("sp", lambda e, t=t, rows=rows: e.dma_start(out=SC["vaugM"][rows], in_=tv[t][:]),
                             reads=[b_tv[t]], writes=[DB("vaugM")[n]], dma=True)
                    elif kind == "fv":
                        t = rot("tf", 2)
                        P.op("act", lambda e, t=t, j=j: e.activation(
                            out=tf[t][:, :, 1:65], in_=pk[j][:].rearrange("p (h d) -> p h d", h=8), func=AF.Copy),
                            reads=[b_pk[j]], writes=[b_tf[t]])
                        P.op("dve", lambda e, t=t: e.memset(tf[t][:, :, 0:1], 1.0), reads=[b_tf[t]], writes=[b_tf[t]])
                        P.op("sp", lambda e, t=t, rows=rows: e.dma_start(out=SC["vaugF"][rows], in_=tf[t][:]),
                             reads=[b_tf[t]], writes=[DB("vaugF")[n]], dma=True)
                    elif kind == "mo":
                        o = rot("to", 3)
                        P.op("act", lambda e, o=o, j=j: e.activation(out=to[o][:], in_=pk[j][:], func=AF.Sigmoid),
                             reads=[b_pk[j]], writes=[b_to[o]])
                        P.op("sp", lambda e, o=o, rows=rows: e.dma_start(out=SC["so"][rows], in_=to[o][:]),
                             reads=[b_to[o]], writes=[DB("so")[n]], dma=True)
                    else:
                        g = rot("tg", 2)
                        boff = c0 - 3088
                        P.op("dve", lambda e, g=g, j=j, boff=boff: e.tensor_tensor(
                            out=tg[g][:], in0=pk[j][:], in1=gbias[:, boff:boff + 512], op=ALU.add),
                            reads=[b_pk[j], b_par], writes=[b_tg[g]])
                        o = rot("to", 3)
                        P.op("act", lambda e, o=o, g=g: e.activation(out=to[o][:], in_=tg[g][:], func=AF.Sigmoid),
                             reads=[b_tg[g]], writes=[b_to[o]])
                        dcol = boff % 1024
                        dst = SC["ga"] if kind == "ga" else SC["gb"]
                        P.op("sp", lambda e, o=o, rows=rows, dst=dst, dcol=dcol: e.dma_start(
                            out=dst[rows, dcol:dcol + 512], in_=to[o][:]),
                            reads=[b_to[o]], writes=[DB("gab")[n]], dma=True)
        P.flush()


def mix_pass(P, nc, C, I, SC, DB):
    with contextlib.ExitStack() as st:
        sb = lambda name, shape, dt: st.enter_context(nc.sbuf_tensor("mx" + name, shape, dt))
        ps = lambda name, shape, dt: st.enter_context(nc.psum_tensor("mx" + name, shape, dt))
        mask01 = sb("mask01", [128, 128], F32)
        trim = sb("trim", [128, 128], BF16)
        trimf = sb("trimf", [128, 128], F32)
        onesr = sb("onesr", [1, 65], F32)
        gln = sb("gln", [128, 512], F32)
        b_c = P.buf("mxconst")
        P.op("pool", lambda e: e.memset(mask01[:], 1.0), writes=[b_c])
        P.op("pool", lambda e: e.affine_select(out=mask01[:], in_=mask01[:], pattern=[[1, 128]], compare_op=ALU.is_ge,
                                                 fill=0.0, base=0, channel_multiplier=-1), reads=[b_c], writes=[b_c])
        P.op("pool", lambda e: e.memset(trimf[:], 0.0), reads=[b_c], writes=[b_c])
        P.op("pool", lambda e: e.affine_select(out=trimf[:], in_=trimf[:], pattern=[[1, 128]], compare_op=ALU.is_ge,
                                                 fill=-30000.0, base=0, channel_multiplier=-1), reads=[b_c], writes=[b_c])
        P.op("dve", lambda e: e.tensor_copy(out=trim[:], in_=trimf[:]), reads=[b_c], writes=[b_c])
        P.op("dve", lambda e: e.memset(onesr[:], 1.0), reads=[b_c], writes=[b_c])
        P.op("sp", lambda e: e.dma_start(out=gln[:], in_=I["mlstm_norm_g"].partition_broadcast(128)),
             reads=[b_c], writes=[b_c], dma=True)
        mqz = [sb("mqz%d" % i, [128, S], BF16) for i in range(4)]
        mk = [sb("mk%d" % i, [128, S], BF16) for i in range(2)]
        b_mqk = P.buf("mqk")
        for h in range(4):
            P.op("pool", lambda e, h=h: e.memset(mqz[h][:], 0.0), writes=[b_mqk])
        for h in range(4):
            R = slice((h % 2) * 64, (h % 2) * 64 + 64)
            P.op("sp", lambda e, h=h, R=R: e.dma_start(out=mqz[h][R, :], in_=SC["mqkT"][h // 2, R, :]), reads=DB("mqkT"),
                 writes=[b_mqk], dma=True)
        for hp in range(2):
            P.op("sp", lambda e, hp=hp: e.dma_start(out=mk[hp][:], in_=SC["mqkT"][2 + hp]), reads=DB("mqkT"),
                 writes=[b_mqk], dma=True)
        Cst = [sb("Cst%d" % i, [128, 129], F32) for i in range(2)]
        Cb = [sb("Cb%d" % i, [128, 129], BF16) for i in range(2)]
        b_Cst = P.bufs_n("Cst", 2)
        b_Cb = P.bufs_n("Cb", 2)
        va = [sb("va%d" % i, [128, 4, 129], BF16) for i in range(2)]
        b_va = P.bufs_n("va", 2)
        sgo = [sb("sgo%d" % i, [128, 512], BF16) for i in range(2)]
        b_sgo = P.bufs_n("sgo", 2)
        Sm = [sb("Sm%d" % i, [128, 2, 128], BF16) for i in range(2)]
        b_Sm = P.bufs_n("Sm", 2)
        ktm = [sb("ktm%d" % i, [128, 128], BF16) for i in range(2)]
        b_ktm = P.bufs_n("ktm", 2)
        bst = [sb("bst%d" % i, [128, 2, 6], F32) for i in range(2)]
        bag = [sb("bag%d" % i, [128, 2, 2], F32) for i in range(2)]
        sm = [sb("sm%d" % i, [128, 2, 4], F32) for i in range(2)]
        b_sm = P.bufs_n("msm", 2)
        hn = [sb("hn%d" % i, [128, 512], F32) for i in range(2)]
        b_hn = P.bufs_n("hn", 2)
        ya = [sb("ya%d" % i, [128, 512], BF16) for i in range(2)]
        b_ya = P.bufs_n("ya", 2)
        yaT = [sb("yaT%d" % i, [128, 4, 128], BF16) for i in range(2)]
        b_yaT = P.bufs_n("yaTs", 2)
        pSm = ps("pSm", [128, 2, 128], F32)
        pOm = ps("pOm", [128, 2, 129], F32)
        pU = ps("pU", [128, 2, 129], F32)
        pT5 = ps("pT5", [128, 5, 128], BF16)
        pkt = pT5[:, 4, :]
        pyT = pT5[:, 0:4, :]
        b_pSm, b_pOm, b_pU, b_pkt, b_pyT = [P.buf(n) for n in ("pSm", "pOm", "pU", "pkt", "pyT")]

        def mlstm_chunk(c):
            cols = slice(c * 128, (c + 1) * 128)
            rows = slice(c * 128, (c + 1) * 128)
            vi = c % 2
            P.op("sp", lambda e: e.dma_start(out=va[vi][:], in_=SC["vaugM"][rows]), reads=[DB("vaugM")[c]],
                 writes=[b_va[vi]], dma=True)
            P.op("sp", lambda e: e.dma_start(out=sgo[vi][:], in_=SC["so"][rows]), reads=[DB("so")[c]],
                 writes=[b_sgo[vi]], dma=True)
            hnb = hn[vi]
            for hp in range(2):
                si = hp
                for hh in range(2):
                    P.op("pe", lambda e, hp=hp, hh=hh: e.matmul(
                        pSm[:, hh, :], lhsT=mk[hp][:, cols], rhs=mqz[2 * hp + hh][:, cols], start=True, stop=True),
                        reads=[b_mqk], writes=[b_pSm])
                P.op("pe", lambda e, hp=hp: e.transpose(out=pkt, in_=mk[hp][:, cols], identity=C.ident[:]),
                     reads=[b_mqk, C.b_const], writes=[b_pkt])
                P.op("dve", lambda e, si=si: e.scalar_tensor_tensor(
                    out=Sm[si][:], in0=pSm[:], scalar=0.125,
                    in1=mask01[:].unsqueeze(1).to_broadcast([128, 2, 128]), op0=ALU.mult, op1=ALU.mult),
                    reads=[b_pSm, b_c], writes=[b_Sm[si]])
                P.op("act", lambda e, si=si: e.activation(out=ktm[si][:], in_=pkt, func=AF.Copy, scale=0.125),
                     reads=[b_pkt], writes=[b_ktm[si]])
                yield
                for hh in range(2):
                    h = 2 * hp + hh
                    P.op("pe", lambda e, si=si, hh=hh, h=h: e.matmul(
                        pOm[:, hh, :], lhsT=Sm[si][:, hh, :], rhs=va[vi][:, h, :], start=True, stop=(c == 0)),
                        reads=[b_Sm[si], b_va[vi]], writes=[b_pOm])
                    if c > 0:
                        P.op("pe", lambda e, hp=hp, hh=hh, h=h: e.matmul(
                            pOm[:, hh, :], lhsT=mqz[h][:, cols], rhs=Cb[hp][:, :], start=False, stop=True),
                            reads=[b_mqk, b_Cb[hp]], writes=[b_pOm])
                for hh in range(2):
                    h = 2 * hp + hh
                    P.op("pe", lambda e, si=si, hh=hh, h=h: e.matmul(
                        pU[:, hh, :], lhsT=ktm[si][:], rhs=va[vi][:, h, :], start=True, stop=True),
                        reads=[b_ktm[si], b_va[vi]], writes=[b_pU])
                for hh in range(2):
                    h = 2 * hp + hh
                    R = slice(hh * 64, (hh + 1) * 64)
                    if c == 0:
                        P.op("dve", lambda e, hp=hp, hh=hh, R=R: e.tensor_copy(out=Cst[hp][R, :], in_=pU[R, hh, :]),
                             reads=[b_pU], writes=[b_Cst[hp]])
                    else:
                        P.op("dve", lambda e, hp=hp, hh=hh, R=R, h=h: e.scalar_tensor_tensor(
                            out=Cst[hp][R, :], in0=Cst[hp][R, :], scalar=C.decbc[R, h, c:c + 1], in1=pU[R, hh, :],
                            op0=ALU.mult, op1=ALU.add), reads=[b_pU, b_Cst[hp], C.b_decbc], writes=[b_Cst[hp]])
                    if c < 31:
                        P.op("dve", lambda e, hp=hp, R=R, h=h: e.tensor_scalar(
                            out=Cb[hp][R, :], in0=Cst[hp][R, :], scalar1=C.decbc[R, h, c + 1:c + 2], scalar2=None,
                            op0=ALU.mult), reads=[b_Cst[hp], C.b_decbc], writes=[b_Cb[hp]])
                smp, bstp, bagp, bsm = sm[hp], bst[hp], bag[hp], b_sm[hp]
                for hh in range(2):
                    P.op("dve", lambda e, hh=hh, bstp=bstp: e.bn_stats(out=bstp[:, hh, :], in_=pOm[:, hh, 0:128]),
                         reads=[b_pOm], writes=[bsm])
                    P.op("dve", lambda e, hh=hh, bstp=bstp, bagp=bagp: e.bn_aggr(out=bagp[:, hh, :], in_=bstp[:, hh, :]),
                         reads=[bsm], writes=[bsm])
                P.op("act", lambda e, smp=smp: e.activation(out=smp[:, :, 0:1], in_=pOm[:, :, 128:129], func=AF.Square),
                     reads=[b_pOm, bsm], writes=[bsm])
                P.op("dve", lambda e, hp=hp, smp=smp: e.tensor_tensor(
                    out=smp[:, :, 0:1], in0=smp[:, :, 0:1], in1=C.wthr[:, c, 4 + 2 * hp:6 + 2 * hp].unsqueeze(2),
                    op=ALU.max), reads=[C.b_wthr, bsm], writes=[bsm])
                P.op("dve", lambda e, smp=smp, bagp=bagp: e.scalar_tensor_tensor(
                    out=smp[:, :, 1:2], in0=smp[:, :, 0:1], scalar=EPS, in1=bagp[:, :, 1:2], op0=ALU.mult, op1=ALU.add),
                    reads=[bsm], writes=[bsm])
                P.op("pool", lambda e, smp=smp: e.tensor_tensor(
                    out=smp[:, :, 2:3], in0=smp[:, :, 1:2],
                    in1=C.neghalf[:].unsqueeze(1).to_broadcast([128, 2, 1]), op=ALU.pow),
                    reads=[bsm, C.b_const], writes=[bsm])
                for hh in range(2):
                    h = 2 * hp + hh
                    P.op("dve", lambda e, hh=hh, h=h, smp=smp, bagp=bagp: e.tensor_scalar(
                        out=hnb[:, h * 128:(h + 1) * 128], in0=pOm[:, hh, 0:128], scalar1=bagp[:, hh, 0:1],
                        scalar2=smp[:, hh, 2:3], op0=ALU.subtract, op1=ALU.mult),
                        reads=[b_pOm, bsm], writes=[b_hn[vi]])
                yield
            yi = c % 2
            P.op("pool", lambda e: e.tensor_tensor(out=hnb[:], in0=hnb[:], in1=gln[:], op=ALU.mult),
                 reads=[b_hn[vi], b_c], writes=[b_hn[vi]])
            P.op("dve", lambda e: e.tensor_tensor(out=ya[yi][:], in0=hnb[:], in1=sgo[vi][:], op=ALU.mult),
                 reads=[b_hn[vi], b_sgo[vi]], writes=[b_ya[yi]])
            yield
            for k in range(4):
                P.op("pe", lambda e, k=k: e.transpose(out=pyT[:, k, :], in_=ya[yi][:, k * 128:(k + 1) * 128],
                                                      identity=C.ident[:]),
                     reads=[b_ya[yi], C.b_const], writes=[b_pyT])
            P.op("act", lambda e: e.activation(out=yaT[yi][:], in_=pyT, func=AF.Copy), reads=[b_pyT],
                 writes=[b_yaT[yi]])
            P.op("sp", lambda e: e.dma_start(out=SC["yaT"][:, :, cols].rearrange("k p t -> p k t"), in_=yaT[yi][:]),
                 reads=[b_yaT[yi]], writes=[DB("yaT")[c]], dma=True)
            yield

        def mlstm_gen():
            for c in range(32):
                yield from mlstm_chunk(c)

        VF = sb("VF", [128, 32, 8 * 65], BF16)
        b_VF = P.buf("VF")
        P.op("sp", lambda e: e.dma_start(out=VF[:], in_=SC["vaugF"].rearrange("(j p) h e -> p j (h e)", p=128)),
             reads=DB("vaugF"), writes=[b_VF], dma=True)
        QA = [sb("QA%d" % i, [70, S], BF16) for i in range(2)]
        KA = [sb("KA%d" % i, [70, S], BF16) for i in range(2)]
        b_QA = P.bufs_n("QA", 2)
        b_KA = P.bufs_n("KA", 2)
        PT = [sb("PT%d" % i, [128, 512], BF16) for i in range(2)]
        b_PT = P.bufs_n("PT", 2)
        rec = sb("rec", [1, 512], F32)
        b_rec = P.buf("rec")
        osb = sb("osb", [65, 512], F32)
        b_osb = P.buf("osb")
        ybt = [sb("ybt%d" % i, [65, 512], BF16) for i in range(2)]
        b_ybt = P.bufs_n("ybt", 2)
        pS = [ps("pS%d" % i, [128, 512], F32) for i in range(2)]
        b_pS = P.bufs_n("pS", 2)
        pO = ps("pO", [128, 512], F32)
        b_pO = P.buf("pO")
        pbc = ps("pbc", [65, 512], F32)
        b_pbc = P.buf("pbc")
        cnt = {"s": 0, "y": 0}

        def fox_load(h):
            hb = h % 2
            P.op("sp", lambda e: e.dma_start(out=QA[hb][:], in_=SC["qaug"][h]), reads=DB("aug"), writes=[b_QA[hb]], dma=True)
            P.op("sp", lambda e: e.dma_start(out=KA[hb][:], in_=SC["kaug"][h]), reads=DB("aug"), writes=[b_KA[hb]], dma=True)

        seq = [(h, i, j) for h in range(8) for i in range(8) for j in range(4 * i + 4)]

        def emit_S(idx):
            h, i, j = seq[idx]
            hb = h % 2
            sj = idx % 2
            jj = j - 4 * i
            kc = slice(j * 128, (j + 1) * 128)
            rd = [b_KA[hb], b_QA[hb]]
            if jj < 0:
                P.op("pe", lambda e: e.matmul(
                    pS[sj][:, 0:512], lhsT=KA[hb][:, kc], rhs=QA[hb][:, i * 512:(i + 1) * 512], start=True, stop=True),
                    reads=rd, writes=[b_pS[sj]])
            else:
                qs = jj * 128
                wq = 512 - qs
                q0 = i * 512 + qs
                P.op("pe", lambda e: e.matmul(pS[sj][:, 0:128], lhsT=C.ident[:], rhs=trim[:], start=True, stop=False),
                     reads=[C.b_const, b_c], writes=[b_pS[sj]])
                P.op("pe", lambda e: e.matmul(
                    pS[sj][:, 0:128], lhsT=KA[hb][:, kc], rhs=QA[hb][:, q0:q0 + 128], start=False, stop=True),
                    reads=rd, writes=[b_pS[sj]])
                if wq > 128:
                    P.op("pe", lambda e: e.matmul(
                        pS[sj][:, 128:wq], lhsT=KA[hb][:, kc], rhs=QA[hb][:, q0 + 128:q0 + wq], start=True, stop=True),
                        reads=rd, writes=[b_pS[sj]])

        def emit_rest(idx):
            h, i, j = seq[idx]
            sj = idx % 2
            nkb = 4 * i + 4
            jj = j - 4 * i
            qs = max(jj, 0) * 128
            wq = 512 - qs
            P.op("act", lambda e: e.activation(out=PT[sj][:, 0:wq], in_=pS[sj][:, 0:wq], func=AF.Exp),
                 reads=[b_pS[sj]], writes=[b_PT[sj]])
            P.op("pe", lambda e: e.matmul(
                pO[0:65, qs:512], lhsT=VF[:, j, h * 65:(h + 1) * 65], rhs=PT[sj][:, 0:wq],
                start=(j == 0), stop=(j == nkb - 1)),
                reads=[b_VF, b_PT[sj]], writes=[b_pO])
            if j < nkb - 1:
                return
            P.op("act", lambda e: e.activation(out=osb[:], in_=pO[0:65, :], func=AF.Copy), reads=[b_pO], writes=[b_osb])
            P.op("dve", lambda e: e.reciprocal(out=rec[0:1, :], in_=osb[0:1, :]), reads=[b_osb], writes=[b_rec])
            P.op("pe", lambda e: e.matmul(pbc[:], lhsT=onesr[0:1, :], rhs=rec[0:1, :], start=True, stop=True),
                 reads=[b_rec, b_c], writes=[b_pbc])
            yi = cnt["y"] % 2
            cnt["y"] += 1
            P.op("dve", lambda e: e.tensor_tensor(out=ybt[yi][:], in0=osb[:], in1=pbc[:], op=ALU.mult),
                 reads=[b_osb, b_pbc], writes=[b_ybt[yi]])
            P.op("sp", lambda e: e.dma_start(
                out=SC["ybT"][h // 2, (h % 2) * 64:(h % 2) * 64 + 64, i * 512:(i + 1) * 512], in_=ybt[yi][1:65, :]),
                reads=[b_ybt[yi]], writes=[DB("ybT")[(h * 8 + i) % 32]], dma=True)

        gen = mlstm_gen()
        fox_load(0)
        emit_S(0)
        for idx, (h, i, j) in enumerate(seq):
            if i == 0 and j == 0 and h + 1 < 8:
                fox_load(h + 1)
            if idx + 1 < len(seq):
                emit_S(idx + 1)
            emit_rest(idx)
            if idx % 6 == 5:
                next(gen, None)
        for _ in gen:
            pass
        P.flush()


def merge_pass(P, nc, C, I, SC, DB, h1, h1_b, h2, h2_b):
    with contextlib.ExitStack() as st:
        sb = lambda name, shape, dt: st.enter_context(nc.sbuf_tensor("mg" + name, shape, dt))
        ps = lambda name, shape, dt: st.enter_context(nc.psum_tensor("mg" + name, shape, dt))
        Wa = sb("Wa", [128, 4, D], BF16)
        Wb = sb("Wb", [128, 4, D], BF16)
        Wo = sb("Wo", [128, 8, D], BF16)
        b_W = P.buf("mgW")
        P.op("pool", lambda e: e.dma_start(out=Wa[:], in_=I["w_branch_a"].rearrange("(k p) d -> p k d", p=128)),
             writes=[b_W], dma=True)
        P.op("pool", lambda e: e.dma_start(out=Wb[:], in_=I["w_branch_b"].rearrange("(k p) d -> p k d", p=128)),
             writes=[b_W], dma=True)
        P.op("pool", lambda e: e.dma_start(out=Wo[:], in_=I["w_out"].rearrange("(k p) d -> p k d", p=128)),
             writes=[b_W], dma=True)
        gpost = sb("gpost", [128, D], F32)
        P.op("sp", lambda e: e.dma_start(out=gpost[:], in_=I["mix_post_g"].partition_broadcast(128)),
             writes=[b_W], dma=True)
        yaT = [sb("yaT%d" % i, [128, 4, 128], BF16) for i in range(2)]
        ybT = [sb("ybT%d" % i, [128, 4, 128], BF16) for i in range(2)]
        gab = [sb("gab%d" % i, [128, 2, D], BF16) for i in range(2)]
        hin = [sb("hin%d" % i, [128, D], F32) for i in range(2)]
        b_in = P.bufs_n("mgin", 2)
        b_hin = P.bufs_n("mghin", 2)
        t1 = [sb("t1%d" % i, [128, D], F32) for i in range(2)]
        t2 = [sb("t2%d" % i, [128, D], F32) for i in range(2)]
        mb = [sb("mb%d" % i, [128, D], BF16) for i in range(2)]
        mT = [sb("mT%d" % i, [128, 8, 128], BF16) for i in range(2)]
        junk = sb("junk", [128, D], BF16)
        hout = [sb("hout%d" % i, [128, D], F32) for i in range(2)]
        stat = sb("stat", [128, 6], F32)
        b_t1, b_t2, b_mb, b_mT = [P.bufs_n(n, 2) for n in ("t1", "t2", "mb", "mT")]
        b_junk = P.buf("mgjunk")
        b_hout = P.bufs_n("hout", 2)
        b_stat = P.bufs_n("mgstat", 2)
        pA = ps("pA", [128, D], F32)
        pB = ps("pB", [128, D], F32)
        pO = ps("pO", [128, D], F32)
        pt = ps("pt", [128, 8, 128], BF16)
        b_pA, b_pB, b_pO, b_pt = [P.buf(n) for n in ("pA", "pB", "mgpO", "mgpt")]
        h1v = h1.rearrange("(n p) d -> n p d", p=128)
        h2v = h2.rearrange("(n p) d -> n p d", p=128)

        def s1(n):
            ib = n % 2
            rows = slice(n * 128, (n + 1) * 128)
            cols = rows
            P.op("sp", lambda e: e.dma_start(out=yaT[ib][:], in_=SC["yaT"][:, :, cols].rearrange("k p t -> p k t")),
                 reads=[DB("yaT")[n]], writes=[b_in[ib]], dma=True)
            P.op("sp", lambda e: e.dma_start(out=ybT[ib][:], in_=SC["ybT"][:, :, cols].rearrange("k p t -> p k t")),
                 reads=DB("ybT"), writes=[b_in[ib]], dma=True)
            P.op("sp", lambda e: e.dma_start(out=gab[ib][:, 0, :], in_=SC["ga"][rows]),
                 reads=[DB("gab")[n]], writes=[b_in[ib]], dma=True)
            P.op("sp", lambda e: e.dma_start(out=gab[ib][:, 1, :], in_=SC["gb"][rows]),
                 reads=[DB("gab")[n]], writes=[b_in[ib]], dma=True)
            P.op("sp", lambda e: e.dma_start(out=hin[ib][:], in_=h1v[n]),
                 reads=[h1_b[n]], writes=[b_hin[ib]], dma=True)
            for hf in range(2):
                hs = slice(hf * 512, (hf + 1) * 512)
                for k in range(4):
                    P.op("pe", lambda e, k=k, hs=hs: e.matmul(pA[:, hs], lhsT=yaT[ib][:, k, :], rhs=Wa[:, k, hs],
                                                              start=(k == 0), stop=(k == 3)),
                         reads=[b_in[ib], b_W], writes=[b_pA])
            for hf in range(2):
                hs = slice(hf * 512, (hf + 1) * 512)
                for k in range(4):
                    P.op("pe", lambda e, k=k, hs=hs: e.matmul(pB[:, hs], lhsT=ybT[ib][:, k, :], rhs=Wb[:, k, hs],
                                                              start=(k == 0), stop=(k == 3)),
                         reads=[b_in[ib], b_W], writes=[b_pB])
            P.op("dve", lambda e: e.tensor_tensor(out=t1[ib][:], in0=pA[:], in1=gab[ib][:, 0, :], op=ALU.mult),
                 reads=[b_pA, b_in[ib]], writes=[b_t1[ib]])
            P.op("dve", lambda e: e.tensor_tensor(out=t2[ib][:], in0=pB[:], in1=gab[ib][:, 1, :], op=ALU.mult),
                 reads=[b_pB, b_in[ib]], writes=[b_t2[ib]])
            P.op("pool", lambda e: e.tensor_tensor(out=mb[ib][:], in0=t1[ib][:], in1=t2[ib][:], op=ALU.add),
                 reads=[b_t1[ib], b_t2[ib]], writes=[b_mb[ib]])

        def s2(n):
            ib = n % 2
            for k in range(8):
                P.op("pe", lambda e, k=k: e.transpose(out=pt[:, k, :], in_=mb[ib][:, k * 128:(k + 1) * 128],
                                                      identity=C.ident[:]),
                     reads=[b_mb[ib], C.b_const], writes=[b_pt])
            P.op("act", lambda e: e.activation(out=mT[ib][:], in_=pt[:], func=AF.Copy), reads=[b_pt], writes=[b_mT[ib]])
            for hf in range(2):
                hs = slice(hf * 512, (hf + 1) * 512)
                for k in range(8):
                    P.op("pe", lambda e, k=k, hs=hs: e.matmul(pO[:, hs], lhsT=mT[ib][:, k, :], rhs=Wo[:, k, hs],
                                                              start=(k == 0), stop=(k == 7)),
                         reads=[b_mT[ib], b_W], writes=[b_pO])
            si = n % 2
            ss, var, rstd = (stat[:, 3 * si + j:3 * si + j + 1] for j in range(3))
            rms_stats(P, C, pO[:], b_pO, junk[:], b_junk, ss, var, rstd, b_stat[si])
            P.op("dve", lambda e: e.scalar_tensor_tensor(
                out=hout[ib][:], in0=pO[:], scalar=rstd, in1=gpost[:], op0=ALU.mult, op1=ALU.mult),
                reads=[b_pO, b_stat[si], b_W], writes=[b_hout[ib]])
            P.op("pool", lambda e: e.tensor_tensor(out=hout[ib][:], in0=hout[ib][:], in1=hin[ib][:], op=ALU.add),
                 reads=[b_hout[ib], b_hin[ib]], writes=[b_hout[ib]])
            P.op("pool", lambda e: e.dma_start(out=h2v[n], in_=hout[ib][:]),
                 reads=[b_hout[ib]], writes=[h2_b[n]], dma=True)

        s1(0)
        for n in range(32):
            if n + 1 < 32:
                s1(n + 1)
            s2(n)
        P.flush()


def ple_pass(P, nc, C, I, uT3, uT3_b, h3, h3_b, out):
    with contextlib.ExitStack() as st:
        sb = lambda name, shape, dt: st.enter_context(nc.sbuf_tensor("pl" + name, shape, dt))
        ps = lambda name, shape, dt: st.enter_context(nc.psum_tensor("pl" + name, shape, dt))
        Wg = sb("Wg", [128, 8, D], BF16)
        Wp = sb("Wp", [128, 2, D], BF16)
        b_W = P.buf("plW")
        P.op("pool", lambda e: e.dma_start(out=Wg[:], in_=I["ple_w_gate"].rearrange("(k p) d -> p k d", p=128)),
             writes=[b_W], dma=True)
        P.op("pool", lambda e: e.dma_start(out=Wp[:], in_=I["ple_w_proj"].rearrange("(k p) d -> p k d", p=128)),
             writes=[b_W], dma=True)
        gpost = sb("gpost", [128, D], F32)
        bg = sb("bg", [128, D], F32)
        P.op("sp", lambda e: e.dma_start(out=gpost[:], in_=I["ple_post_g"].partition_broadcast(128)),
             writes=[b_W], dma=True)
        P.op("sp", lambda e: e.dma_start(out=bg[:], in_=I["ple_b_gate"].partition_broadcast(128)),
             writes=[b_W], dma=True)
        uT = [sb("uT%d" % i, [128, 8, 128], BF16) for i in range(2)]
        pin = [sb("pin%d" % i, [128, 256], F32) for i in range(2)]
        hin = [sb("hin%d" % i, [128, D], F32) for i in range(2)]
        b_in = P.bufs_n("plin", 2)
        b_hin = P.bufs_n("plhin", 2)
        pb = [sb("pb%d" % i, [128, 256], BF16) for i in range(2)]
        pT = [sb("pT%d" % i, [128, 2, 128], BF16) for i in range(2)]
        gt = [sb("gt%d" % i, [128, D], F32) for i in range(2)]
        ge = [sb("ge%d" % i, [128, D], F32) for i in range(2)]
        junk = sb("junk", [128, D], BF16)
        hout = [sb("hout%d" % i, [128, D], F32) for i in range(2)]
        stat = sb("stat", [128, 6], F32)
        b_pb, b_pT, b_gt, b_ge = [P.bufs_n(n, 2) for n in ("pb", "pT", "gt", "ge")]
        b_junk = P.buf("pljunk")
        b_hout = P.bufs_n("plhout", 2)
        b_stat = P.bufs_n("plstat", 2)
        pG = [ps("pG%d" % i, [128, D], F32) for i in range(2)]
        pE = ps("pE", [128, D], F32)
        ptp = ps("ptp", [128, 2, 128], BF16)
        b_pG = P.bufs_n("pG", 2)
        b_pE, b_ptp = P.buf("pE"), P.buf("ptp")
        pv = I["p"].rearrange("(n p) d -> n p d", p=128)
        h3v = h3.rearrange("(n p) d -> n p d", p=128)
        ov = out.rearrange("(n p) d -> n p d", p=128)

        def s1(n):
            ib = n % 2
            cols = slice(n * 128, (n + 1) * 128)
            P.op("sp", lambda e: e.dma_start(out=uT[ib][:], in_=uT3[:, :, cols].rearrange("k p t -> p k t")),
                 reads=[uT3_b[n]], writes=[b_in[ib]], dma=True)
            P.op("sp", lambda e: e.dma_start(out=pin[ib][:], in_=pv[n]), writes=[b_in[ib]], dma=True)
            P.op("sp", lambda e: e.dma_start(out=hin[ib][:], in_=h3v[n]), reads=[h3_b[n]],
                 writes=[b_hin[ib]], dma=True)
            P.op("act", lambda e: e.activation(out=pb[ib][:], in_=pin[ib][:], func=AF.Copy), reads=[b_in[ib]],
                 writes=[b_pb[ib]])
            for k in range(2):
                P.op("pe", lambda e, k=k: e.transpose(out=ptp[:, k, :], in_=pb[ib][:, k * 128:(k + 1) * 128],
                                                      identity=C.ident[:]),
                     reads=[b_pb[ib], C.b_const], writes=[b_ptp])
            P.op("dve", lambda e: e.tensor_copy(out=pT[ib][:], in_=ptp[:]), reads=[b_ptp], writes=[b_pT[ib]])
            for hf in range(2):
                hs = slice(hf * 512, (hf + 1) * 512)
                for k in range(8):
                    P.op("pe", lambda e, k=k, hs=hs: e.matmul(pG[ib][:, hs], lhsT=uT[ib][:, k, :], rhs=Wg[:, k, hs],
                                                              start=(k == 0), stop=(k == 7)),
                         reads=[b_in[ib], b_W], writes=[b_pG[ib]])

        def s2(n):
            ib = n % 2
            for hf in range(2):
                hs = slice(hf * 512, (hf + 1) * 512)
                for k in range(2):
                    P.op("pe", lambda e, k=k, hs=hs: e.matmul(pE[:, hs], lhsT=pT[ib][:, k, :], rhs=Wp[:, k, hs],
                                                              start=(k == 0), stop=(k == 1)),
                         reads=[b_pT[ib], b_W], writes=[b_pE])
            P.op("dve", lambda e: e.tensor_tensor(out=gt[ib][:], in0=pG[ib][:], in1=bg[:], op=ALU.add),
                 reads=[b_pG[ib], b_W], writes=[b_gt[ib]])
            P.op("act", lambda e: e.activation(out=gt[ib][:], in_=gt[ib][:], func=AF.Sigmoid), reads=[b_gt[ib]],
                 writes=[b_gt[ib]])
            P.op("dve", lambda e: e.tensor_tensor(out=ge[ib][:], in0=gt[ib][:], in1=pE[:], op=ALU.mult),
                 reads=[b_gt[ib], b_pE], writes=[b_ge[ib]])
            si = n % 2
            ss, var, rstd = (stat[:, 3 * si + j:3 * si + j + 1] for j in range(3))
            rms_stats(P, C, ge[ib][:], b_ge[ib], junk[:], b_junk, ss, var, rstd, b_stat[si])
            P.op("dve", lambda e: e.scalar_tensor_tensor(
                out=hout[ib][:], in0=ge[ib][:], scalar=rstd, in1=gpost[:], op0=ALU.mult, op1=ALU.mult),
                reads=[b_ge[ib], b_stat[si], b_W], writes=[b_hout[ib]])
            P.op("pool", lambda e: e.tensor_tensor(out=hout[ib][:], in0=hout[ib][:], in1=hin[ib][:], op=ALU.add),
                 reads=[b_hout[ib], b_hin[ib]], writes=[b_hout[ib]])
            P.op("pool", lambda e: e.dma_start(out=ov[n], in_=hout[ib][:]), reads=[b_hout[ib]], dma=True)

        s1(0)
        for n in range(32):
            if n + 1 < 32:
                s1(n + 1)
            s2(n)
        P.flush()


def build_program(debug=False, stage=99, only=None):
    nc = bass.Bass("TRN2", target_bir_lowering=False)
    I = {}

    def din(name, shape):
        I[name] = nc.dram_tensor(name, shape, F32, kind="ExternalInput").ap()
        return I[name]

    din("x", [S, D])
    din("p", [S, 256])
    for nm in ("ffn1", "ffn2"):
        din(nm + "_pre_g", [1, D])
        din(nm + "_w_gate", [D, DFF])
        din(nm + "_w_up", [D, DFF])
        din(nm + "_w_down", [DFF, D])
        din(nm + "_post_g", [1, D])
    din("mix_pre_g", [1, D])
    din("w_in", [D, INW])
    din("conv_w", [4, 512])
    din("conv_b", [1, 512])
    din("mlstm_i_bias", [1, 4])
    din("mlstm_f_bias", [1, 4])
    din("mlstm_norm_g", [1, 512])
    din("fox_f_bias", [1, 8])
    din("branch_gate_bias", [1, 2048])
    din("w_branch_a", [512, D])
    din("w_branch_b", [512, D])
    din("w_out", [D, D])
    din("mix_post_g", [1, D])
    din("ple_pre_g", [1, D])
    din("ple_w_gate", [D, D])
    din("ple_b_gate", [1, D])
    din("ple_w_proj", [256, D])
    din("ple_post_g", [1, D])

    skind = "ExternalOutput" if debug else "Internal"

    def dscr(name, shape, dt):
        return nc.dram_tensor(name, shape, dt, kind=skind).ap()

    out = nc.dram_tensor("out", [S, D], F32, kind="ExternalOutput").ap()
    h1 = dscr("h1", [S, D], F32)
    uT1 = dscr("uT1", [8, 128, S], BF16)
    gpre = dscr("gpre", [72, S], F32)
    SC = {
        "mqkT": dscr("mqkT", [4, 128, S], BF16),
        "vaugM": dscr("vaugM", [S, 4, 129], BF16),
        "so": dscr("so", [S, 512], BF16),
        "qaug": dscr("qaug", [8, 70, S], BF16),
        "kaug": dscr("kaug", [8, 70, S], BF16),
        "vaugF": dscr("vaugF", [S, 8, 65], BF16),
        "ga": dscr("ga", [S, D], BF16),
        "gb": dscr("gb", [S, D], BF16),
        "yaT": dscr("yaT", [4, 128, S], BF16),
        "ybT": dscr("ybT", [4, 128, S], BF16),
    }
    h2 = dscr("h2", [S, D], F32)
    h3 = dscr("h3", [S, D], F32)
    uT3 = dscr("uT3", [8, 128, S], BF16)

    with contextlib.ExitStack() as st:
        P = Prog(nc, st)
        C = Ctx()
        C.db = {}

        def db(name):
            if name not in C.db:
                C.db[name] = P.bufs_n("D" + name, 32)
            return C.db[name]

        setup_consts(P, nc, st, C)
        C.wthr = st.enter_context(nc.sbuf_tensor("wthr", [128, 32, 8], F32))
        C.decbc = st.enter_context(nc.sbuf_tensor("decbc", [128, 4, 32], F32))
        C.b_wthr = P.buf("wthr")
        C.b_decbc = P.buf("decbc")
        def want(name, st_no):
            return (name in only) if only is not None else (stage >= st_no)

        if want("ffn1", 1):
            ffn_pass(P, nc, C, "f1", I["x"], I["ffn1_w_gate"], I["ffn1_w_up"], I["ffn1_w_down"],
                     I["ffn1_pre_g"], I["ffn1_post_g"], h1, I["mix_pre_g"], uT1,
                     db("x"), db("h1"), db("uT1"), gate_w=I["w_in"], gate_dst=gpre, gate_b=db("gpre"))
        if want("gp", 2):
            aug_b = P.buf("augrows")
            gp_stage(P, nc, C, I, gpre, db("gpre"), SC["qaug"], SC["kaug"], aug_b)
            db("aug").append(aug_b)
        if want("win", 2):
            win_pass(P, nc, C, I, uT1, db("uT1"), SC, db)
        if want("mix", 3):
            mix_pass(P, nc, C, I, SC, db)
        if want("merge", 4):
            merge_pass(P, nc, C, I, SC, db, h1, db("h1"), h2, db("h2"))
        if want("ffn2", 5):
            ffn_pass(P, nc, C, "f2", h2, I["ffn2_w_gate"], I["ffn2_w_up"], I["ffn2_w_down"],
                     I["ffn2_pre_g"], I["ffn2_post_g"], h3, I["ple_pre_g"], uT3,
                     db("h2"), db("h3"), db("uT3"))
        if want("ple", 6):
            ple_pass(P, nc, C, I, uT3, db("uT3"), h3, db("h3"), out)
        P.flush(final=True)
    return nc

IN_NAMES = ["x", "p", "ffn1_pre_g", "ffn1_w_gate", "ffn1_w_up", "ffn1_w_down", "ffn1_post_g",
            "mix_pre_g", "w_in", "conv_w", "conv_b", "mlstm_i_bias", "mlstm_f_bias", "mlstm_norm_g",
            "fox_f_bias", "branch_gate_bias", "w_branch_a", "w_branch_b", "w_out", "mix_post_g",
            "ffn2_pre_g", "ffn2_w_gate", "ffn2_w_up", "ffn2_w_down", "ffn2_post_g",
            "ple_pre_g", "ple_w_gate", "ple_b_gate", "ple_w_proj", "ple_post_g"]


def make_in_maps(inputs, cores):
    maps = []
    shared = {}
    for k in IN_NAMES:
        if k in ("x", "p"):
            continue
        shared[k] = np.ascontiguousarray(np.asarray(inputs[k])[0], dtype=np.float32)
    x = np.asarray(inputs["x"])
    p = np.asarray(inputs["p"])
    for b in cores:
        m = dict(shared)
        m["x"] = np.ascontiguousarray(x[b], dtype=np.float32)
        m["p"] = np.ascontiguousarray(p[0, b], dtype=np.float32)
        maps.append(m)
    return maps


def kernel(**inputs):
    nc = build_program()
    maps = make_in_maps(inputs, list(range(8)))
    res = run_bass_kernel_spmd(nc, maps, core_ids=list(range(8)))
    return np.stack([np.asarray(r["out"], dtype=np.float32) for r in res.results], axis=0)
```

```python
import contextlib
import numpy as np
import concourse.bass as bass
import concourse.mybir as mybir
from concourse.bass_utils import run_bass_kernel_spmd

F32 = mybir.dt.float32
BF16 = mybir.dt.bfloat16
AF = mybir.ActivationFunctionType
ALU = mybir.AluOpType
AX = mybir.AxisListType

S = 4096
D = 1024
DFF = 2816
NFC = DFF // 128
NT = 8
TT = 512
EPS = 1e-6
INW = 5136

ENGS = ("pe", "act", "dve", "pool", "sp")


class Buf:
    __slots__ = ("name", "w", "r")

    def __init__(self, name=""):
        self.name = name
        self.w = None
        self.r = []


class Op:
    __slots__ = ("eng", "fn", "deps", "inc", "cnt", "dma", "sem", "emitted")

    def __init__(self, eng, fn, dma=False):
        self.eng = eng
        self.fn = fn
        self.deps = []
        self.inc = False
        self.cnt = 0
        self.dma = dma
        self.sem = None
        self.emitted = False


class Prog:
    def __init__(self, nc, st, n_dma_sems=20):
        self.nc = nc
        self.pending = {e: [] for e in ENGS}
        self.bufs = []
        self.nd = n_dma_sems
        self.esem = {e: st.enter_context(nc.semaphore("s_" + e)) for e in ENGS}
        self.dsem = {}
        for e in ("sp", "pool"):
            for s in range(n_dma_sems):
                self.dsem[(e, s)] = st.enter_context(nc.semaphore("d_%s_%d" % (e, s)))
        self.ecnt = {e: 0 for e in ENGS}
        self.dcnt = {e: 0 for e in ENGS}
        self.waited = {e: {} for e in ENGS}
        self.n_ops = 0

    def buf(self, name=""):
        b = Buf(name)
        self.bufs.append(b)
        return b

    def bufs_n(self, name, n):
        return [self.buf("%s%d" % (name, i)) for i in range(n)]

    def op(self, eng, fn, reads=(), writes=(), dma=False):
        o = Op(eng, fn, dma)
        seen = set()
        cand = []
        for b in reads:
            if b.w is not None:
                cand.append(b.w)
        for b in writes:
            if b.w is not None:
                cand.append(b.w)
            cand.extend(b.r)
        for d in cand:
            if d is o or id(d) in seen:
                continue
            seen.add(id(d))
            if d.eng == "pe" and eng == "pe" and not d.dma and not dma:
                continue
            o.deps.append(d)
            if not d.emitted:
                d.inc = True
        for b in reads:
            b.r.append(o)
        for b in writes:
            b.w = o
            b.r = []
        self.pending[eng].append(o)
        self.n_ops += 1
        return o

    def flush(self, final=False):
        nc = self.nc
        for b in self.bufs:
            if b.w is not None and not b.w.emitted:
                b.w.inc = True
            for r in b.r:
                if not r.emitted:
                    r.inc = True
        for e in ENGS:
            for o in self.pending[e]:
                if o.dma:
                    k = self.dcnt[e]
                    o.sem = (e, k % self.nd)
                    o.cnt = 16 * (k // self.nd + 1)
                    self.dcnt[e] = k + 1
                elif o.inc:
                    self.ecnt[e] += 1
                    o.cnt = self.ecnt[e]
        pending = self.pending
        self.pending = {e: [] for e in ENGS}

        def run(ename, eng):
            waited = self.waited[ename]
            for o in pending[ename]:
                for d in o.deps:
                    key = d.sem if d.dma else d.eng
                    if waited.get(key, 0) >= d.cnt:
                        continue
                    assert d.cnt > 0, (d.eng, ename)
                    eng.wait_ge(self.dsem[key] if d.dma else self.esem[key], d.cnt)
                    waited[key] = d.cnt
                if o.dma:
                    if o.cnt > 16 and waited.get(o.sem, 0) < o.cnt - 16:
                        eng.wait_ge(self.dsem[o.sem], o.cnt - 16)
                        waited[o.sem] = o.cnt - 16
                    o.fn(eng).then_inc(self.dsem[o.sem], 16)
                else:
                    ins = o.fn(eng)
                    if o.inc:
                        ins.then_inc(self.esem[o.eng], 1)
                o.emitted = True
            if ename == "sp" and final:
                for q in ("sp", "pool"):
                    k = self.dcnt[q]
                    for sl in range(min(self.nd, k)):
                        last = 16 * ((k - 1 - sl) // self.nd + 1)
                        eng.wait_ge(self.dsem[(q, sl)], last)

        with nc.Block() as block:
            @block.tensor
            def _(eng):
                run("pe", eng)

            @block.scalar
            def _(eng):
                run("act", eng)

            @block.vector
            def _(eng):
                run("dve", eng)

            @block.gpsimd
            def _(eng):
                run("pool", eng)

            @block.sync
            def _(eng):
                run("sp", eng)


def bcast_last(ap2d, n):
    return ap2d.unsqueeze(2).to_broadcast([ap2d.shape[0], ap2d.shape[1], n])


class Ctx:
    pass


def load_w_kmajor(P, nc, dst, src2d, n_kc, ncols, bufs, col_chunk=1408):
    v = src2d.rearrange("(kc p) n -> kc p n", p=128)
    mdl = 4 * col_chunk
    for k in range(n_kc):
        P.op("pool", lambda e, k=k: e.dma_start(out=dst[:, k, :], in_=v[k], max_dma_last_dim=mdl),
             writes=[bufs[k]], dma=True)


def setup_consts(P, nc, st, C):
    sb = lambda name, shape, dt: st.enter_context(nc.sbuf_tensor(name, shape, dt))
    C.identf = sb("identf", [128, 128], F32)
    C.ident = sb("ident", [128, 128], BF16)
    C.neghalf = sb("neghalf", [128, 1], F32)
    C.b_const = P.buf("const")
    identf, ident = C.identf, C.ident
    P.op("pool", lambda e: e.memset(identf[:], 0.0), writes=[C.b_const])
    P.op("pool", lambda e: e.affine_select(out=identf[:], in_=identf[:], pattern=[[-1, 128]],
                                             compare_op=ALU.not_equal, fill=1.0, base=0,
                                             channel_multiplier=1),
         reads=[C.b_const], writes=[C.b_const])
    P.op("dve", lambda e: e.tensor_copy(out=ident[:], in_=identf[:]), reads=[C.b_const], writes=[C.b_const])
    P.op("pool", lambda e: e.memset(C.neghalf[:], -0.5), reads=[C.b_const], writes=[C.b_const])


def rms_stats(P, C, src_ap, src_buf, junk, b_junk, ss, var, rstd, b_stat, n_feat=D):
    P.op("act", lambda e: e.activation(out=junk, in_=src_ap, func=AF.Square, accum_out=ss),
         reads=[src_buf], writes=[b_junk, b_stat])
    P.op("dve", lambda e: e.tensor_scalar(out=var, in0=ss, scalar1=1.0 / n_feat, scalar2=EPS,
                                          op0=ALU.mult, op1=ALU.add),
         reads=[b_stat], writes=[b_stat])
    P.op("pool", lambda e: e.tensor_tensor(out=rstd, in0=var, in1=C.neghalf[:], op=ALU.pow),
         reads=[b_stat, C.b_const], writes=[b_stat])


def ffn_pass(P, nc, C, tag, src_h, w_gate, w_up, w_down, pre_g, post_g, dst_h, next_g, dst_uT,
             src_b, dst_b, uT_b, gate_w=None, gate_dst=None, gate_b=None):
    with contextlib.ExitStack() as st:
        sb = lambda name, shape, dt: st.enter_context(nc.sbuf_tensor(tag + name, shape, dt))
        ps = lambda name, shape, dt: st.enter_context(nc.psum_tensor(tag + name, shape, dt))
        Wg = sb("Wg", [128, 8, DFF], BF16)
        Wu = sb("Wu", [128, 8, DFF], BF16)
        Wd = sb("Wd", [128, NFC, D], BF16)
        b_Wg = P.bufs_n("Wg", 8)
        b_Wu = P.bufs_n("Wu", 8)
        b_Wd = P.bufs_n("Wd", 2)
        load_w_kmajor(P, nc, Wg, w_gate, 8, DFF, b_Wg)
        load_w_kmajor(P, nc, Wu, w_up, 8, DFF, b_Wu)
        wdv = w_down.rearrange("(fc p) d -> p fc d", p=128)
        for hh in range(2):
            P.op("pool", lambda e, hh=hh: e.dma_start(out=Wd[:, hh * 11:(hh + 1) * 11, :],
                                                       in_=wdv[:, hh * 11:(hh + 1) * 11, :]),
                 writes=[b_Wd[hh]], dma=True)
        gpre = sb("gpre", [128, 8], F32)
        gnext = sb("gnext", [128, 8], F32)
        gpost = sb("gpost", [128, D], F32)
        b_par = P.buf("par")
        P.op("sp", lambda e: e.dma_start(out=gpre[:], in_=pre_g.rearrange("o (k p) -> p (o k)", p=128),
                                         allow_slow_non_contiguous=True),
             writes=[b_par], dma=True)
        P.op("sp", lambda e: e.dma_start(out=gnext[:], in_=next_g.rearrange("o (k p) -> p (o k)", p=128),
                                         allow_slow_non_contiguous=True),
             writes=[b_par], dma=True)
        P.op("sp", lambda e: e.dma_start(out=gpost[:], in_=post_g.partition_broadcast(128)),
             writes=[b_par], dma=True)
        if gate_w is not None:
            Wgt = sb("Wgt", [128, 8, 72], BF16)
            b_Wgt = P.buf("Wgt")
            P.op("pool", lambda e: e.memset(Wgt[:], 0.0), writes=[b_Wgt])
            gv = gate_w.rearrange("(kc p) n -> p kc n", p=128)
            for (c0, n, d0) in ((1540, 4, 0), (1536, 4, 32), (3080, 8, 64)):
                P.op("pool", lambda e, c0=c0, n=n, d0=d0: e.dma_start(
                    out=Wgt[:, :, d0:d0 + n], in_=gv[:, :, c0:c0 + n]),
                    reads=[], writes=[b_Wgt], dma=True)
            gsb = [sb("gsb%d" % i, [72, 128], F32) for i in range(2)]
            b_gsb = P.bufs_n("gsb", 2)

        NXB = 3
        xb = [sb("xb%d" % i, [128, D], F32) for i in range(NXB)]
        b_xb = P.bufs_n("xb", NXB)
        ubf = [sb("ubf%d" % i, [128, D], BF16) for i in range(2)]
        b_ubf = P.bufs_n("ubf", 2)
        junk = sb("junk", [128, D], BF16)
        b_junk = P.buf("junk")
        uT = sb("uT", [128, 8, TT], BF16)
        b_uT = P.bufs_n("uT", 4)
        aT = sb("aT", [128, NFC, TT], BF16)
        b_aT = P.bufs_n("aT", NFC)
        sg = [sb("sg%d" % i, [128, TT], F32) for i in range(2)]
        b_sg = P.bufs_n("sg", 2)
        hst = [sb("hst%d" % i, [128, D], F32) for i in range(2)]
        b_hst = P.bufs_n("hst", 2)
        u2T = [sb("u2T%d" % i, [128, 8, 128], BF16) for i in range(2)]
        b_u2T = P.bufs_n("u2T", 2)
        NST = 6
        stat = sb("stat", [128, 3 * NST], F32)
        b_stat = P.bufs_n("stat", NST)

        pt = ps("pt", [128, 8, 128], BF16)
        b_pt = P.buf("pt")
        pg = [ps("pg%d" % i, [128, TT], F32) for i in range(2)]
        pu = [ps("pu%d" % i, [128, TT], F32) for i in range(2)]
        b_pg = P.bufs_n("pg", 2)
        b_pu = P.bufs_n("pu", 2)
        pys = [ps("py%d" % i, [128, 512], F32) for i in range(3)]
        b_pys = P.bufs_n("py", 3)
        if gate_w is not None:
            pgt = pg[0][0:72, 0:128]
            b_pgt = b_pg[0]

        src_v = src_h.rearrange("(n p) d -> n p d", p=128)
        dst_v = dst_h.rearrange("(n p) d -> n p d", p=128)
        cnt = {"x": 0, "u": 0, "st": 0, "h": 0, "u2": 0, "sg": 0, "gs": 0, "py": 0}

        def norm_T(h_ap, h_buf, gcol, out_ap, out_bufs):
            si = cnt["st"] % NST
            cnt["st"] += 1
            ss, var, rstd = (stat[:, 3 * si + j:3 * si + j + 1] for j in range(3))
            rms_stats(P, C, h_ap, h_buf, junk[:], b_junk, ss, var, rstd, b_stat[si])
            ui = cnt["u"] % 2
            cnt["u"] += 1
            u = ubf[ui]
            P.op("dve", lambda e: e.tensor_scalar(out=u[:], in0=h_ap, scalar1=rstd, scalar2=None, op0=ALU.mult),
                 reads=[h_buf, b_stat[si]], writes=[b_ubf[ui]])
            for k in range(8):
                P.op("pe", lambda e, k=k: e.transpose(out=pt[:, k, :], in_=u[:, k * 128:(k + 1) * 128],
                                                      identity=C.ident[:]),
                     reads=[b_ubf[ui], C.b_const], writes=[b_pt])
            P.op("dve", lambda e: e.tensor_tensor(out=out_ap, in0=pt[:], in1=bcast_last(gcol[:], 128), op=ALU.mult),
                 reads=[b_pt, b_par], writes=out_bufs)

        def pre(i):
            for s in range(4):
                n = i * 4 + s
                xi = cnt["x"] % NXB
                cnt["x"] += 1
                P.op("sp", lambda e, n=n, xi=xi: e.dma_start(out=xb[xi][:], in_=src_v[n]),
                     reads=[src_b[n]], writes=[b_xb[xi]], dma=True)
                norm_T(xb[xi][:], b_xb[xi], gpre, uT[:, :, s * 128:(s + 1) * 128], [b_uT[s]])

        def gateup(i):
            for f in range(NFC):
                j = f % 2
                for k in range(8):
                    P.op("pe", lambda e, k=k, f=f, j=j: e.matmul(
                        pg[j][:], lhsT=Wg[:, k, f * 128:(f + 1) * 128], rhs=uT[:, k, :],
                        start=(k == 0), stop=(k == 7)),
                        reads=[b_Wg[k]] + b_uT, writes=[b_pg[j]])
                for k in range(8):
                    P.op("pe", lambda e, k=k, f=f, j=j: e.matmul(
                        pu[j][:], lhsT=Wu[:, k, f * 128:(f + 1) * 128], rhs=uT[:, k, :],
                        start=(k == 0), stop=(k == 7)),
                        reads=[b_Wu[k]] + b_uT, writes=[b_pu[j]])
                si = cnt["sg"] % 2
                cnt["sg"] += 1
                P.op("act", lambda e, j=j, si=si: e.activation(out=sg[si][:], in_=pg[j][:], func=AF.Silu),
                     reads=[b_pg[j]], writes=[b_sg[si]])
                P.op("dve", lambda e, j=j, si=si, f=f: e.tensor_tensor(out=aT[:, f, :], in0=sg[si][:], in1=pu[j][:],
                                                                   op=ALU.mult),
                     reads=[b_sg[si], b_pu[j]], writes=[b_aT[f]])

        def down_post(i):
            for s in range(4):
                n = i * 4 + s
                pyh = []
                for hf in range(2):
                    pi = cnt["py"] % 3
                    cnt["py"] += 1
                    pyh.append((pys[pi], b_pys[pi]))
                    for f in range(NFC):
                        P.op("pe", lambda e, f=f, s=s, hf=hf, pi=pi: e.matmul(
                            pys[pi][:], lhsT=aT[:, f, s * 128:(s + 1) * 128],
                            rhs=Wd[:, f, hf * 512:(hf + 1) * 512], start=(f == 0), stop=(f == NFC - 1)),
                            reads=[b_aT[f], b_Wd[f // 11]], writes=[b_pys[pi]])
                xi = cnt["x"] % NXB
                cnt["x"] += 1
                P.op("sp", lambda e, n=n, xi=xi: e.dma_start(out=xb[xi][:], in_=src_v[n]),
                     reads=[src_b[n]], writes=[b_xb[xi]], dma=True)
                si = cnt["st"] % NST
                cnt["st"] += 1
                ss, var, rstd = (stat[:, 3 * si + j:3 * si + j + 1] for j in range(3))
                P.op("act", lambda e, ss=ss, t=pyh[0][0]: e.activation(out=junk[:, 0:512], in_=t[:], func=AF.Square,
                                                                      accum_out=ss),
                     reads=[pyh[0][1]], writes=[b_junk, b_stat[si]])
                P.op("act", lambda e, var=var, t=pyh[1][0]: e.activation(out=junk[:, 512:1024], in_=t[:], func=AF.Square,
                                                                        accum_out=var),
                     reads=[pyh[1][1]], writes=[b_junk, b_stat[si]])
                P.op("dve", lambda e, ss=ss, var=var: e.tensor_tensor(out=var, in0=ss, in1=var, op=ALU.add),
                     reads=[b_stat[si]], writes=[b_stat[si]])
                P.op("dve", lambda e, var=var: e.tensor_scalar(out=var, in0=var, scalar1=1.0 / D, scalar2=EPS,
                                                              op0=ALU.mult, op1=ALU.add),
                     reads=[b_stat[si]], writes=[b_stat[si]])
                P.op("pool", lambda e, var=var, rstd=rstd: e.tensor_tensor(out=rstd, in0=var, in1=C.neghalf[:], op=ALU.pow),
                     reads=[b_stat[si], C.b_const], writes=[b_stat[si]])
                hi = cnt["h"] % 2
                cnt["h"] += 1
                hb = hst[hi]
                for hf in range(2):
                    hs = slice(hf * 512, (hf + 1) * 512)
                    P.op("dve", lambda e, hb=hb, rstd=rstd, t=pyh[hf][0], hs=hs: e.scalar_tensor_tensor(
                        out=hb[:, hs], in0=t[:], scalar=rstd, in1=gpost[:, hs], op0=ALU.mult, op1=ALU.mult),
                        reads=[pyh[hf][1], b_stat[si], b_par], writes=[b_hst[hi]])
                P.op("dve", lambda e, hb=hb, xi=xi: e.scalar_tensor_tensor(
                    out=hb[:], in0=hb[:], scalar=0.5, in1=xb[xi][:], op0=ALU.mult, op1=ALU.add),
                    reads=[b_hst[hi], b_xb[xi]], writes=[b_hst[hi]])
                P.op("sp", lambda e, hb=hb, n=n: e.dma_start(out=dst_v[n], in_=hb[:]),
                     reads=[b_hst[hi]], writes=[dst_b[n]], dma=True)
                ui2 = cnt["u2"] % 2
                cnt["u2"] += 1
                norm_T(hb[:], b_hst[hi], gnext, u2T[ui2][:], [b_u2T[ui2]])
                P.op("sp", lambda e, ui2=ui2, n=n: e.dma_start(
                    out=dst_uT[:, :, n * 128:(n + 1) * 128].rearrange("k p t -> p k t"), in_=u2T[ui2][:]),
                    reads=[b_u2T[ui2]], writes=[uT_b[n]], dma=True)
                if gate_w is not None:
                    for k in range(8):
                        P.op("pe", lambda e, k=k, ui2=ui2: e.matmul(
                            pgt, lhsT=Wgt[:, k, :], rhs=u2T[ui2][:, k, :], start=(k == 0), stop=(k == 7)),
                            reads=[b_Wgt, b_u2T[ui2]], writes=[b_pgt])
                    gi = cnt["gs"] % 2
                    cnt["gs"] += 1
                    P.op("act", lambda e, gi=gi: e.activation(out=gsb[gi][:], in_=pgt, func=AF.Copy),
                         reads=[b_pgt], writes=[b_gsb[gi]])
                    P.op("sp", lambda e, gi=gi, n=n: e.dma_start(out=gate_dst[:, n * 128:(n + 1) * 128], in_=gsb[gi][:]),
                         reads=[b_gsb[gi]], writes=[gate_b[n]], dma=True)

        pre(0)
        for i in range(NT):
            gateup(i)
            if i + 1 < NT:
                pre(i + 1)
            down_post(i)
        P.flush()


def gp_stage(P, nc, C, I, gpre, gpre_b, qaug, kaug, aug_b):
    with contextlib.ExitStack() as st:
        sb = lambda name, shape, dt: st.enter_context(nc.sbuf_tensor("gp" + name, shape, dt))
        ps = lambda name, shape, dt: st.enter_context(nc.psum_tensor("gp" + name, shape, dt))
        T0 = sb("T0", [72, S], F32)
        T1 = sb("T1", [72, S], F32)
        T2 = sb("T2", [72, S], F32)
        T3 = sb("T3", [72, S], F32)
        QR = sb("QR", [72, 3, S], BF16)
        KR = sb("KR", [72, 3, S], BF16)
        ONE = sb("ONE", [72, S], BF16)
        bcol = sb("bcol", [72, 1], F32)
        negb = sb("negb", [72, 1], F32)
        bicol = sb("bicol", [72, 1], F32)
        onec = sb("onec", [72, 1], F32)
        cm = sb("cm", [72, 32], F32)
        mce = sb("mce", [72, 32], F32)
        mprev = sb("mprev", [72, 32], F32)
        dec = sb("dec", [72, 32], F32)
        esel = sb("esel", [72, 4, 128], F32)
        bT0, bT1, bT2, bT3, bQR, bKR, bONE, bsm = [P.buf(n) for n in
                                                   ("T0", "T1", "T2", "T3", "QR", "KR", "ONE", "gsm")]
        ptm = ps("ptm", [128, 32, 8], F32)
        pdc = ps("pdc", [128, 4, 32], F32)
        b_ptm, b_pdc = P.buf("ptm"), P.buf("pdc")

        P.op("sp", lambda e: e.dma_start(out=T0[:], in_=gpre), reads=gpre_b, writes=[bT0], dma=True)
        P.op("sp", lambda e: e.dma_start(out=T3[0:4, :], in_=gpre[32:36, :]), reads=gpre_b, writes=[bT3], dma=True)
        P.op("dve", lambda e: e.memset(bcol[:], 0.0), writes=[bsm])
        P.op("dve", lambda e: e.memset(bicol[:], 0.0), reads=[bsm], writes=[bsm])
        P.op("dve", lambda e: e.memset(onec[:], 1.0), reads=[bsm], writes=[bsm])
        P.op("pool", lambda e: e.memset(ONE[:], 1.0), writes=[bONE])
        P.op("sp", lambda e: e.dma_start(out=bcol[0:4, :], in_=I["mlstm_f_bias"].rearrange("o n -> n o"),
                                         allow_slow_non_contiguous=True), reads=[bsm], writes=[bsm], dma=True)
        P.op("sp", lambda e: e.dma_start(out=bcol[64:72, :], in_=I["fox_f_bias"].rearrange("o n -> n o"),
                                         allow_slow_non_contiguous=True), reads=[bsm], writes=[bsm], dma=True)
        P.op("sp", lambda e: e.dma_start(out=bicol[0:4, :], in_=I["mlstm_i_bias"].rearrange("o n -> n o"),
                                         allow_slow_non_contiguous=True), reads=[bsm], writes=[bsm], dma=True)
        P.op("dve", lambda e: e.tensor_scalar(out=negb[0:72, :], in0=bcol[0:72, :], scalar1=-1.0, scalar2=None,
                                              op0=ALU.mult), reads=[bsm], writes=[bsm])
        R = slice(0, 72)
        P.op("act", lambda e: e.activation(out=T1[R, :], in_=T0[R, :], func=AF.Exp, scale=-1.0, bias=negb[R, :]),
             reads=[bT0, bsm], writes=[bT1])
        P.op("act", lambda e: e.activation(out=T1[R, :], in_=T1[R, :], func=AF.Ln, scale=1.0, bias=onec[R, :]),
             reads=[bT1, bsm], writes=[bT1])
        P.op("dve", lambda e: e.tensor_tensor_scan(out=T2[R, :], data0=T1[R, :], data1=T1[R, :], initial=0.0,
                                                   op0=ALU.add, op1=ALU.max), reads=[bT1], writes=[bT2])
        M = slice(0, 4)
        P.op("dve", lambda e: e.scalar_tensor_tensor(out=T3[M, :], in0=T3[M, :], scalar=bicol[M, :], in1=T2[M, :],
                                                     op0=ALU.add, op1=ALU.add), reads=[bT3, bT2, bsm], writes=[bT3])
        P.op("dve", lambda e: e.tensor_reduce(out=cm[M, :], in_=T3[M, :].rearrange("p (c l) -> p c l", l=128),
                                              axis=AX.X, op=ALU.max), reads=[bT3], writes=[bsm])
        P.op("dve", lambda e: e.tensor_tensor_scan(out=mce[M, :], data0=cm[M, :], data1=cm[M, :], initial=0.0,
                                                   op0=ALU.max, op1=ALU.max), reads=[bsm], writes=[bsm])
        P.op("dve", lambda e: e.tensor_tensor(out=T3[M, :].rearrange("p (c l) -> p c l", l=128),
                                              in0=T3[M, :].rearrange("p (c l) -> p c l", l=128),
                                              in1=bcast_last(mce[M, :], 128), op=ALU.subtract),
             reads=[bT3, bsm], writes=[bT3])
        P.op("act", lambda e: e.activation(out=T3[M, :], in_=T3[M, :], func=AF.Exp), reads=[bT3], writes=[bT3])
        P.op("dve", lambda e: e.tensor_tensor(out=T1[M, :].rearrange("p (c l) -> p c l", l=128),
                                              in0=T2[M, :].rearrange("p (c l) -> p c l", l=128),
                                              in1=bcast_last(mce[M, :], 128), op=ALU.subtract),
             reads=[bT2, bsm, bT1], writes=[bT1])
        P.op("act", lambda e: e.activation(out=T1[M, :], in_=T1[M, :], func=AF.Exp, scale=2.0), reads=[bT1], writes=[bT1])
        P.op("dve", lambda e: e.memset(mprev[M, :], 0.0), reads=[bsm], writes=[bsm])
        P.op("dve", lambda e: e.tensor_copy(out=mprev[M, 1:32], in_=mce[M, 0:31]), reads=[bsm], writes=[bsm])
        P.op("dve", lambda e: e.tensor_tensor(out=dec[M, :], in0=mprev[M, :], in1=mce[M, :], op=ALU.subtract),
             reads=[bsm], writes=[bsm])
        P.op("act", lambda e: e.activation(out=dec[M, :], in_=dec[M, :], func=AF.Exp), reads=[bsm], writes=[bsm])
        for c in range(32):
            P.op("pe", lambda e, c=c: e.transpose(out=ptm[:, c, 0:4], in_=T3[M, c * 128:(c + 1) * 128],
                                                  identity=C.identf[M, 0:4]),
                 reads=[bT3, C.b_const], writes=[b_ptm])
            P.op("pe", lambda e, c=c: e.transpose(out=ptm[:, c, 4:8], in_=T1[M, c * 128:(c + 1) * 128],
                                                  identity=C.identf[M, 0:4]),
                 reads=[bT1, C.b_const], writes=[b_ptm])
        P.op("dve", lambda e: e.tensor_copy(out=C.wthr[:], in_=ptm[:]), reads=[b_ptm], writes=[C.b_wthr])
        for h in range(4):
            P.op("dve", lambda e, h=h: e.tensor_copy(out=esel[M, h, :],
                                                     in_=C.identf[M, h:h + 1].to_broadcast([4, 128])),
                 reads=[C.b_const, bsm], writes=[bsm])
        for h in range(4):
            P.op("pe", lambda e, h=h: e.matmul(pdc[:, h, :], lhsT=esel[M, h, :], rhs=dec[M, :], start=True, stop=True),
                 reads=[bsm], writes=[b_pdc])
        P.op("dve", lambda e: e.tensor_copy(out=C.decbc[:], in_=pdc[:]), reads=[b_pdc], writes=[C.b_decbc])
        Fx = slice(64, 72)
        Fd = slice(64, 72)
        P.op("dve", lambda e: e.tensor_scalar(out=T0[Fx, :], in0=T2[Fx, :], scalar1=-1.0, scalar2=None, op0=ALU.mult),
             reads=[bT2, bT0], writes=[bT0])
        for part in range(3):
            P.op("dve", lambda e, part=part: e.tensor_copy(out=QR[Fx, part, :], in_=T0[Fx, :]),
                 reads=[bT0], writes=[bQR])
            if part < 2:
                P.op("dve", lambda e, part=part: e.tensor_tensor(out=T0[Fx, :], in0=T0[Fx, :], in1=QR[Fx, part, :],
                                                                 op=ALU.subtract), reads=[bT0, bQR], writes=[bT0])
        P.op("pool", lambda e: e.tensor_scalar(out=KR[Fx, :, :], in0=QR[Fx, :, :], scalar1=-1.0, scalar2=None,
                                               op0=ALU.mult), reads=[bQR], writes=[bKR])
        P.op("sp", lambda e: e.dma_start(out=qaug[:, 64:67, :], in_=QR[Fd, :, :]), reads=[bQR], writes=[aug_b], dma=True)
        P.op("sp", lambda e: e.dma_start(out=kaug[:, 67:70, :], in_=KR[Fd, :, :]), reads=[bKR], writes=[aug_b], dma=True)
        for r in range(3):
            P.op("sp", lambda e, r=r: e.dma_start(out=qaug[:, 67 + r, :], in_=ONE[Fd, :]), reads=[bONE],
                 writes=[aug_b], dma=True)
            P.op("sp", lambda e, r=r: e.dma_start(out=kaug[:, 64 + r, :], in_=ONE[Fd, :]), reads=[bONE],
                 writes=[aug_b], dma=True)
        P.flush()


def win_pass(P, nc, C, I, uT1, uT_b, SC, DB):
    w_in = I["w_in"]
    with contextlib.ExitStack() as st:
        sb = lambda name, shape, dt: st.enter_context(nc.sbuf_tensor("wi" + name, shape, dt))
        ps = lambda name, shape, dt: st.enter_context(nc.psum_tensor("wi" + name, shape, dt))
        W = sb("W", [128, 8, INW], BF16)
        b_W = P.bufs_n("Win", 8)
        wv = w_in.rearrange("(kc p) n -> kc p n", p=128)
        for k in range(8):
            P.op("pool", lambda e, k=k: e.dma_start(out=W[:, k, :], in_=wv[k], max_dma_last_dim=4 * 1284),
                 writes=[b_W[k]], dma=True)
        cw = sb("cw", [128, 4, 4], F32)
        cb = sb("cb", [128, 4], F32)
        gbias = sb("gbias", [128, 2048], F32)
        b_par = P.buf("wipar")
        for tap in range(4):
            P.op("sp", lambda e, tap=tap: e.dma_start(
                out=cw[:, :, tap], in_=I["conv_w"][tap:tap + 1, :].rearrange("o (c p) -> p (o c)", p=128),
                allow_slow_non_contiguous=True), writes=[b_par], dma=True)
        P.op("sp", lambda e: e.dma_start(out=cb[:], in_=I["conv_b"].rearrange("o (c p) -> p (o c)", p=128),
                                         allow_slow_non_contiguous=True), writes=[b_par], dma=True)
        P.op("sp", lambda e: e.dma_start(out=gbias[:], in_=I["branch_gate_bias"].partition_broadcast(128)),
             writes=[b_par], dma=True)
        uT = [sb("uT%d" % i, [128, 8, TT], BF16) for i in range(2)]
        b_uT = P.bufs_n("wiuT", 2)
        zq = sb("zq", [128, 4, 3 + TT], F32)
        b_zq = P.bufs_n("zq", 4)
        acc = [sb("acc%d" % i, [128, TT], F32) for i in range(2)]
        b_acc = P.bufs_n("acc", 2)
        fo = [sb("fo%d" % i, [128, TT], BF16) for i in range(3)]
        b_fo = P.bufs_n("fo", 3)
        tv = [sb("tv%d" % i, [128, 4, 129], BF16) for i in range(2)]
        b_tv = P.bufs_n("tv", 2)
        tf = [sb("tf%d" % i, [128, 8, 65], BF16) for i in range(2)]
        b_tf = P.bufs_n("tf", 2)
        tg = [sb("tg%d" % i, [128, 512], F32) for i in range(2)]
        b_tg = P.bufs_n("tg", 2)
        to = [sb("to%d" % i, [128, 512], BF16) for i in range(3)]
        b_to = P.bufs_n("to", 3)
        pf = [ps("pf%d" % i, [128, TT], F32) for i in range(2)]
        b_pf = P.bufs_n("pf", 2)
        pk = [ps("pk%d" % i, [128, 512], F32) for i in range(2)]
        b_pk = P.bufs_n("pk", 2)
        cnt = {"pf": 0, "pk": 0, "acc": 0, "fo": 0, "tv": 0, "tf": 0, "tg": 0, "to": 0}

        def rot(key, n):
            v = cnt[key] % n
            cnt[key] += 1
            return v

        for ch in range(4):
            P.op("dve", lambda e, ch=ch: e.memset(zq[:, ch, 0:3], 0.0), writes=[b_zq[ch]])
        for i in range(NT):
            ub = i % 2
            tcols = slice(i * TT, (i + 1) * TT)
            if i == 0:
                P.op("sp", lambda e: e.dma_start(out=uT[0][:], in_=uT1[:, :, 0:TT].rearrange("k p t -> p k t")),
                     reads=uT_b[0:4], writes=[b_uT[0]], dma=True)
            if i + 1 < NT:
                ncols = slice((i + 1) * TT, (i + 2) * TT)
                P.op("sp", lambda e, ub=ub, ncols=ncols: e.dma_start(
                    out=uT[1 - ub][:], in_=uT1[:, :, ncols].rearrange("k p t -> p k t")),
                    reads=uT_b[4 * i + 4:4 * i + 8], writes=[b_uT[1 - ub]], dma=True)
            fm = [("mqk", ch, ch * 128) for ch in range(4)] + \
                 [("fq", ch, 1544 + ch * 128) for ch in range(4)] + \
                 [("fk", ch, 2056 + ch * 128) for ch in range(4)]
            for (kind, ch, c0) in fm:
                j = rot("pf", 2)
                for k in range(8):
                    P.op("pe", lambda e, k=k, c0=c0, j=j, ub=ub: e.matmul(
                        pf[j][:], lhsT=W[:, k, c0:c0 + 128], rhs=uT[ub][:, k, :], start=(k == 0), stop=(k == 7)),
                        reads=[b_W[k], b_uT[ub]], writes=[b_pf[j]])
                if kind == "mqk":
                    P.op("act", lambda e, ch=ch, j=j: e.activation(out=zq[:, ch, 3:3 + TT], in_=pf[j][:], func=AF.Copy),
                         reads=[b_pf[j]], writes=[b_zq[ch]])
                    a = rot("acc", 2)
                    P.op("dve", lambda e, ch=ch, a=a: e.tensor_scalar(
                        out=acc[a][:], in0=zq[:, ch, 0:TT], scalar1=cw[:, ch, 0:1], scalar2=cb[:, ch:ch + 1],
                        op0=ALU.mult, op1=ALU.add), reads=[b_zq[ch], b_par], writes=[b_acc[a]])
                    for tap in range(1, 4):
                        P.op("dve", lambda e, ch=ch, a=a, tap=tap: e.scalar_tensor_tensor(
                            out=acc[a][:], in0=zq[:, ch, tap:tap + TT], scalar=cw[:, ch, tap:tap + 1], in1=acc[a][:],
                            op0=ALU.mult, op1=ALU.add), reads=[b_zq[ch], b_par, b_acc[a]], writes=[b_acc[a]])
                    P.op("dve", lambda e, ch=ch: e.tensor_copy(out=zq[:, ch, 0:3], in_=zq[:, ch, TT:TT + 3]),
                         reads=[b_zq[ch]], writes=[b_zq[ch]])
                    o = rot("fo", 3)
                    P.op("act", lambda e, a=a, o=o: e.activation(out=fo[o][:], in_=acc[a][:], func=AF.Silu),
                         reads=[b_acc[a]], writes=[b_fo[o]])
                    P.op("sp", lambda e, o=o, ch=ch, tcols=tcols: e.dma_start(out=SC["mqkT"][ch, :, tcols], in_=fo[o][:]),
                         reads=[b_fo[o]], writes=[DB("mqkT")[i]], dma=True)
                else:
                    o = rot("fo", 3)
                    sc = 0.125 if kind == "fq" else 1.0
                    P.op("act", lambda e, o=o, j=j, sc=sc: e.activation(out=fo[o][:], in_=pf[j][:], func=AF.Copy, scale=sc),
                         reads=[b_pf[j]], writes=[b_fo[o]])
                    dst = SC["qaug"] if kind == "fq" else SC["kaug"]
                    for hh in range(2):
                        P.op("sp", lambda e, o=o, ch=ch, dst=dst, tcols=tcols, hh=hh: e.dma_start(
                            out=dst[2 * ch + hh, 0:64, tcols], in_=fo[o][hh * 64:(hh + 1) * 64, :]),
                            reads=[b_fo[o]], writes=[DB("aug")[i]], dma=True)
            for s in range(4):
                n = i * 4 + s
                rows = slice(n * 128, (n + 1) * 128)
                groups = [("mv", 512), ("mo", 1024), ("fv", 2568), ("ga", 3088), ("ga", 3600), ("gb", 4112), ("gb", 4624)]
                for gi, (kind, c0) in enumerate(groups):
                    j = rot("pk", 2)
                    for k in range(8):
                        P.op("pe", lambda e, k=k, c0=c0, j=j, ub=ub, s=s: e.matmul(
                            pk[j][:], lhsT=uT[ub][:, k, s * 128:(s + 1) * 128], rhs=W[:, k, c0:c0 + 512],
                            start=(k == 0), stop=(k == 7)),
                            reads=[b_W[k], b_uT[ub]], writes=[b_pk[j]])
                    if kind == "mv":
                        t = rot("tv", 2)
                        c = n
                        P.op("dve", lambda e, t=t, j=j, c=c: e.tensor_tensor(
                            out=tv[t][:, :, 0:128], in0=pk[j][:].rearrange("p (h d) -> p h d", h=4),
                            in1=bcast_last(C.wthr[:, c, 0:4], 128), op=ALU.mult),
                            reads=[b_pk[j], C.b_wthr], writes=[b_tv[t]])
                        P.op("dve", lambda e, t=t, c=c: e.tensor_copy(out=tv[t][:, :, 128:129],
                                                                     in_=C.wthr[:, c, 0:4].unsqueeze(2)),
                             reads=[C.b_wthr, b_tv[t]], writes=[b_tv[t]])
                        P.op("sp", lambda e, t=t, rows=rows: e.dma_start(out=SC["vaugM"][rows], in_=tv[t][:]),
                             reads=[b_tv[t]], writes=[DB("vaugM")[n]], dma=True)
                    elif kind == "fv":
                        t = rot("tf", 2)
                        P.op("act", lambda e, t=t, j=j: e.activation(
                            out=tf[t][:, :, 1:65], in_=pk[j][:].rearrange("p (h d) -> p h d", h=8), func=AF.Copy),
                            reads=[b_pk[j]], writes=[b_tf[t]])
                        P.op("dve", lambda e, t=t: e.memset(tf[t][:, :, 0:1], 1.0), reads=[b_tf[t]], writes=[b_tf[t]])
                        P.op("sp", lambda e, t=t, rows=rows: e.dma_start(out=SC["vaugF"][rows], in_=tf[t][:]),
                             reads=[b_tf[t]], writes=[DB("vaugF")[n]], dma=True)
                    elif kind == "mo":
                        o = rot("to", 3)
                        P.op("act", lambda e, o=o, j=j: e.activation(out=to[o][:], in_=pk[j][:], func=AF.Sigmoid),
                             reads=[b_pk[j]], writes=[b_to[o]])
                        P.op("sp", lambda e, o=o, rows=rows: e.dma_start(out=SC["so"][rows], in_=to[o][:]),
                             reads=[b_to[o]], writes=[DB("so")[n]], dma=True)
                    else:
                        g = rot("tg", 2)
                        boff = c0 - 3088
                        P.op("dve", lambda e, g=g, j=j, boff=boff: e.tensor_tensor(
                            out=tg[g][:], in0=pk[j][:], in1=gbias[:, boff:boff + 512], op=ALU.add),
                            reads=[b_pk[j], b_par], writes=[b_tg[g]])
                        o = rot("to", 3)
                        P.op("act", lambda e, o=o, g=g: e.activation(out=to[o][:], in_=tg[g][:], func=AF.Sigmoid),
                             reads=[b_tg[g]], writes=[b_to[o]])
                        dcol = boff % 1024
                        dst = SC["ga"] if kind == "ga" else SC["gb"]
                        P.op("sp", lambda e, o=o, rows=rows, dst=dst, dcol=dcol: e.dma_start(
                            out=dst[rows, dcol:dcol + 512], in_=to[o][:]),
                            reads=[b_to[o]], writes=[DB("gab")[n]], dma=True)
        P.flush()


def mix_pass(P, nc, C, I, SC, DB):
    with contextlib.ExitStack() as st:
        sb = lambda name, shape, dt: st.enter_context(nc.sbuf_tensor("mx" + name, shape, dt))
        ps = lambda name, shape, dt: st.enter_context(nc.psum_tensor("mx" + name, shape, dt))
        mask01 = sb("mask01", [128, 128], F32)
        trim = sb("trim", [128, 128], BF16)
        trimf = sb("trimf", [128, 128], F32)
        onesr = sb("onesr", [1, 65], F32)
        gln = sb("gln", [128, 512], F32)
        b_c = P.buf("mxconst")
        P.op("pool", lambda e: e.memset(mask01[:], 1.0), writes=[b_c])
        P.op("pool", lambda e: e.affine_select(out=mask01[:], in_=mask01[:], pattern=[[1, 128]], compare_op=ALU.is_ge,
                                                 fill=0.0, base=0, channel_multiplier=-1), reads=[b_c], writes=[b_c])
        P.op("pool", lambda e: e.memset(trimf[:], 0.0), reads=[b_c], writes=[b_c])
        P.op("pool", lambda e: e.affine_select(out=trimf[:], in_=trimf[:], pattern=[[1, 128]], compare_op=ALU.is_ge,
                                                 fill=-30000.0, base=0, channel_multiplier=-1), reads=[b_c], writes=[b_c])
        P.op("dve", lambda e: e.tensor_copy(out=trim[:], in_=trimf[:]), reads=[b_c], writes=[b_c])
        P.op("dve", lambda e: e.memset(onesr[:], 1.0), reads=[b_c], writes=[b_c])
        P.op("sp", lambda e: e.dma_start(out=gln[:], in_=I["mlstm_norm_g"].partition_broadcast(128)),
             reads=[b_c], writes=[b_c], dma=True)
        mqz = [sb("mqz%d" % i, [128, S], BF16) for i in range(4)]
        mk = [sb("mk%d" % i, [128, S], BF16) for i in range(2)]
        b_mqk = P.buf("mqk")
        for h in range(4):
            P.op("pool", lambda e, h=h: e.memset(mqz[h][:], 0.0), writes=[b_mqk])
        for h in range(4):
            R = slice((h % 2) * 64, (h % 2) * 64 + 64)
            P.op("sp", lambda e, h=h, R=R: e.dma_start(out=mqz[h][R, :], in_=SC["mqkT"][h // 2, R, :]), reads=DB("mqkT"),
                 writes=[b_mqk], dma=True)
        for hp in range(2):
            P.op("sp", lambda e, hp=hp: e.dma_start(out=mk[hp][:], in_=SC["mqkT"][2 + hp]), reads=DB("mqkT"),
                 writes=[b_mqk], dma=True)
        Cst = [sb("Cst%d" % i, [128, 129], F32) for i in range(2)]
        Cb = [sb("Cb%d" % i, [128, 129], BF16) for i in range(2)]
        b_Cst = P.bufs_n("Cst", 2)
        b_Cb = P.bufs_n("Cb", 2)
        va = [sb("va%d" % i, [128, 4, 129], BF16) for i in range(2)]
        b_va = P.bufs_n("va", 2)
        sgo = [sb("sgo%d" % i, [128, 512], BF16) for i in range(2)]
        b_sgo = P.bufs_n("sgo", 2)
        Sm = [sb("Sm%d" % i, [128, 2, 128], BF16) for i in range(2)]
        b_Sm = P.bufs_n("Sm", 2)
        ktm = [sb("ktm%d" % i, [128, 128], BF16) for i in range(2)]
        b_ktm = P.bufs_n("ktm", 2)
        bst = [sb("bst%d" % i, [128, 2, 6], F32) for i in range(2)]
        bag = [sb("bag%d" % i, [128, 2, 2], F32) for i in range(2)]
        sm = [sb("sm%d" % i, [128, 2, 4], F32) for i in range(2)]
        b_sm = P.bufs_n("msm", 2)
        hn = [sb("hn%d" % i, [128, 512], F32) for i in range(2)]
        b_hn = P.bufs_n("hn", 2)
        ya = [sb("ya%d" % i, [128, 512], BF16) for i in range(2)]
        b_ya = P.bufs_n("ya", 2)
        yaT = [sb("yaT%d" % i, [128, 4, 128], BF16) for i in range(2)]
        b_yaT = P.bufs_n("yaTs", 2)
        pSm = ps("pSm", [128, 2, 128], F32)
        pOm = ps("pOm", [128, 2, 129], F32)
        pU = ps("pU", [128, 2, 129], F32)
        pT5 = ps("pT5", [128, 5, 128], BF16)
        pkt = pT5[:, 4, :]
        pyT = pT5[:, 0:4, :]
        b_pSm, b_pOm, b_pU, b_pkt, b_pyT = [P.buf(n) for n in ("pSm", "pOm", "pU", "pkt", "pyT")]

        def mlstm_chunk(c):
            cols = slice(c * 128, (c + 1) * 128)
            rows = slice(c * 128, (c + 1) * 128)
            vi = c % 2
            P.op("sp", lambda e: e.dma_start(out=va[vi][:], in_=SC["vaugM"][rows]), reads=[DB("vaugM")[c]],
                 writes=[b_va[vi]], dma=True)
            P.op("sp", lambda e: e.dma_start(out=sgo[vi][:], in_=SC["so"][rows]), reads=[DB("so")[c]],
                 writes=[b_sgo[vi]], dma=True)
            hnb = hn[vi]
            for hp in range(2):
                si = hp
                for hh in range(2):
                    P.op("pe", lambda e, hp=hp, hh=hh: e.matmul(
                        pSm[:, hh, :], lhsT=mk[hp][:, cols], rhs=mqz[2 * hp + hh][:, cols], start=True, stop=True),
                        reads=[b_mqk], writes=[b_pSm])
                P.op("pe", lambda e, hp=hp: e.transpose(out=pkt, in_=mk[hp][:, cols], identity=C.ident[:]),
                     reads=[b_mqk, C.b_const], writes=[b_pkt])
                P.op("dve", lambda e, si=si: e.scalar_tensor_tensor(
                    out=Sm[si][:], in0=pSm[:], scalar=0.125,
                    in1=mask01[:].unsqueeze(1).to_broadcast([128, 2, 128]), op0=ALU.mult, op1=ALU.mult),
                    reads=[b_pSm, b_c], writes=[b_Sm[si]])
                P.op("act", lambda e, si=si: e.activation(out=ktm[si][:], in_=pkt, func=AF.Copy, scale=0.125),
                     reads=[b_pkt], writes=[b_ktm[si]])
                yield
                for hh in range(2):
                    h = 2 * hp + hh
                    P.op("pe", lambda e, si=si, hh=hh, h=h: e.matmul(
                        pOm[:, hh, :], lhsT=Sm[si][:, hh, :], rhs=va[vi][:, h, :], start=True, stop=(c == 0)),
                        reads=[b_Sm[si], b_va[vi]], writes=[b_pOm])
                    if c > 0:
                        P.op("pe", lambda e, hp=hp, hh=hh, h=h: e.matmul(
                            pOm[:, hh, :], lhsT=mqz[h][:, cols], rhs=Cb[hp][:, :], start=False, stop=True),
                            reads=[b_mqk, b_Cb[hp]], writes=[b_pOm])
                for hh in range(2):
                    h = 2 * hp + hh
                    P.op("pe", lambda e, si=si, hh=hh, h=h: e.matmul(
                        pU[:, hh, :], lhsT=ktm[si][:], rhs=va[vi][:, h, :], start=True, stop=True),
                        reads=[b_ktm[si], b_va[vi]], writes=[b_pU])
                for hh in range(2):
                    h = 2 * hp + hh
                    R = slice(hh * 64, (hh + 1) * 64)
                    if c == 0:
                        P.op("dve", lambda e, hp=hp, hh=hh, R=R: e.tensor_copy(out=Cst[hp][R, :], in_=pU[R, hh, :]),
                             reads=[b_pU], writes=[b_Cst[hp]])
                    else:
                        P.op("dve", lambda e, hp=hp, hh=hh, R=R, h=h: e.scalar_tensor_tensor(
                            out=Cst[hp][R, :], in0=Cst[hp][R, :], scalar=C.decbc[R, h, c:c + 1], in1=pU[R, hh, :],
                            op0=ALU.mult, op1=ALU.add), reads=[b_pU, b_Cst[hp], C.b_decbc], writes=[b_Cst[hp]])
                    if c < 31:
                        P.op("dve", lambda e, hp=hp, R=R, h=h: e.tensor_scalar(
                            out=Cb[hp][R, :], in0=Cst[hp][R, :], scalar1=C.decbc[R, h, c + 1:c + 2], scalar2=None,
                            op0=ALU.mult), reads=[b_Cst[hp], C.b_decbc], writes=[b_Cb[hp]])
                smp, bstp, bagp, bsm = sm[hp], bst[hp], bag[hp], b_sm[hp]
                for hh in range(2):
                    P.op("dve", lambda e, hh=hh, bstp=bstp: e.bn_stats(out=bstp[:, hh, :], in_=pOm[:, hh, 0:128]),
                         reads=[b_pOm], writes=[bsm])
                    P.op("dve", lambda e, hh=hh, bstp=bstp, bagp=bagp: e.bn_aggr(out=bagp[:, hh, :], in_=bstp[:, hh, :]),
                         reads=[bsm], writes=[bsm])
                P.op("act", lambda e, smp=smp: e.activation(out=smp[:, :, 0:1], in_=pOm[:, :, 128:129], func=AF.Square),
                     reads=[b_pOm, bsm], writes=[bsm])
                P.op("dve", lambda e, hp=hp, smp=smp: e.tensor_tensor(
                    out=smp[:, :, 0:1], in0=smp[:, :, 0:1], in1=C.wthr[:, c, 4 + 2 * hp:6 + 2 * hp].unsqueeze(2),
                    op=ALU.max), reads=[C.b_wthr, bsm], writes=[bsm])
                P.op("dve", lambda e, smp=smp, bagp=bagp: e.scalar_tensor_tensor(
                    out=smp[:, :, 1:2], in0=smp[:, :, 0:1], scalar=EPS, in1=bagp[:, :, 1:2], op0=ALU.mult, op1=ALU.add),
                    reads=[bsm], writes=[bsm])
                P.op("pool", lambda e, smp=smp: e.tensor_tensor(
                    out=smp[:, :, 2:3], in0=smp[:, :, 1:2],
                    in1=C.neghalf[:].unsqueeze(1).to_broadcast([128, 2, 1]), op=ALU.pow),
                    reads=[bsm, C.b_const], writes=[bsm])
                for hh in range(2):
                    h = 2 * hp + hh
                    P.op("dve", lambda e, hh=hh, h=h, smp=smp, bagp=bagp: e.tensor_scalar(
                        out=hnb[:, h * 128:(h + 1) * 128], in0=pOm[:, hh, 0:128], scalar1=bagp[:, hh, 0:1],
                        scalar2=smp[:, hh, 2:3], op0=ALU.subtract, op1=ALU.mult),
                        reads=[b_pOm, bsm], writes=[b_hn[vi]])
                yield
            yi = c % 2
            P.op("pool", lambda e: e.tensor_tensor(out=hnb[:], in0=hnb[:], in1=gln[:], op=ALU.mult),
                 reads=[b_hn[vi], b_c], writes=[b_hn[vi]])
            P.op("dve", lambda e: e.tensor_tensor(out=ya[yi][:], in0=hnb[:], in1=sgo[vi][:], op=ALU.mult),
                 reads=[b_hn[vi], b_sgo[vi]], writes=[b_ya[yi]])
            yield
            for k in range(4):
                P.op("pe", lambda e, k=k: e.transpose(out=pyT[:, k, :], in_=ya[yi][:, k * 128:(k + 1) * 128],
                                                      identity=C.ident[:]),
                     reads=[b_ya[yi], C.b_const], writes=[b_pyT])
            P.op("act", lambda e: e.activation(out=yaT[yi][:], in_=pyT, func=AF.Copy), reads=[b_pyT],
                 writes=[b_yaT[yi]])
            P.op("sp", lambda e: e.dma_start(out=SC["yaT"][:, :, cols].rearrange("k p t -> p k t"), in_=yaT[yi][:]),
                 reads=[b_yaT[yi]], writes=[DB("yaT")[c]], dma=True)
            yield

        def mlstm_gen():
            for c in range(32):
                yield from mlstm_chunk(c)

        VF = sb("VF", [128, 32, 8 * 65], BF16)
        b_VF = P.buf("VF")
        P.op("sp", lambda e: e.dma_start(out=VF[:], in_=SC["vaugF"].rearrange("(j p) h e -> p j (h e)", p=128)),
             reads=DB("vaugF"), writes=[b_VF], dma=True)
        QA = [sb("QA%d" % i, [70, S], BF16) for i in range(2)]
        KA = [sb("KA%d" % i, [70, S], BF16) for i in range(2)]
        b_QA = P.bufs_n("QA", 2)
        b_KA = P.bufs_n("KA", 2)
        PT = [sb("PT%d" % i, [128, 512], BF16) for i in range(3)]
        b_PT = P.bufs_n("PT", 3)
        rec = [sb("rec%d" % i, [1, 512], F32) for i in range(2)]
        b_rec = P.bufs_n("rec", 2)
        b_recd = P.bufs_n("recd", 2)
        osb = [sb("osb%d" % i, [65, 512], F32) for i in range(2)]
        b_osb = P.bufs_n("osb", 2)
        bcs = [sb("bcs%d" % i, [65, 512], F32) for i in range(2)]
        b_bcs = P.bufs_n("bcs", 2)
        ybt = [sb("ybt%d" % i, [65, 512], BF16) for i in range(2)]
        b_ybt = P.bufs_n("ybt", 2)
        pS = [ps("pS%d" % i, [128, 512], F32) for i in range(3)]
        b_pS = P.bufs_n("pS", 3)
        slot_of = {}
        pO = ps("pO", [128, 512], F32)
        b_pO = P.buf("pO")
        cnt = {"s": 0, "y": 0}

        def fox_load(h):
            hb = h % 2
            P.op("sp", lambda e: e.dma_start(out=QA[hb][:], in_=SC["qaug"][h]), reads=DB("aug"), writes=[b_QA[hb]], dma=True)
            P.op("sp", lambda e: e.dma_start(out=KA[hb][:], in_=SC["kaug"][h]), reads=DB("aug"), writes=[b_KA[hb]], dma=True)

        seq = [(h, i, j) for h in range(8) for i in range(8) for j in range(4 * i + 4)]

        def emit_S(idx):
            h, i, j = seq[idx]
            hb = h % 2
            sj = cnt["s"] % 3
            cnt["s"] += 1
            slot_of[idx] = sj
            jj = j - 4 * i
            kc = slice(j * 128, (j + 1) * 128)
            rd = [b_KA[hb], b_QA[hb]]
            if jj < 0:
                P.op("pe", lambda e: e.matmul(
                    pS[sj][:, 0:512], lhsT=KA[hb][:, kc], rhs=QA[hb][:, i * 512:(i + 1) * 512], start=True, stop=True),
                    reads=rd, writes=[b_pS[sj]])
            else:
                qs = jj * 128
                wq = 512 - qs
                q0 = i * 512 + qs
                P.op("pe", lambda e: e.matmul(pS[sj][:, 0:128], lhsT=C.ident[:], rhs=trim[:], start=True, stop=False),
                     reads=[C.b_const, b_c], writes=[b_pS[sj]])
                P.op("pe", lambda e: e.matmul(
                    pS[sj][:, 0:128], lhsT=KA[hb][:, kc], rhs=QA[hb][:, q0:q0 + 128], start=False, stop=True),
                    reads=rd, writes=[b_pS[sj]])
                if wq > 128:
                    P.op("pe", lambda e: e.matmul(
                        pS[sj][:, 128:wq], lhsT=KA[hb][:, kc], rhs=QA[hb][:, q0 + 128:q0 + wq], start=True, stop=True),
                        reads=rd, writes=[b_pS[sj]])

        def emit_rest(idx):
            h, i, j = seq[idx]
            sj = slot_of[idx]
            nkb = 4 * i + 4
            jj = j - 4 * i
            qs = max(jj, 0) * 128
            wq = 512 - qs
            tj = idx % 3
            P.op("act", lambda e: e.activation(out=PT[tj][:, 0:wq], in_=pS[sj][:, 0:wq], func=AF.Exp),
                 reads=[b_pS[sj]], writes=[b_PT[tj]])
            P.op("pe", lambda e: e.matmul(
                pO[0:65, qs:512], lhsT=VF[:, j, h * 65:(h + 1) * 65], rhs=PT[tj][:, 0:wq],
                start=(j == 0), stop=(j == nkb - 1)),
                reads=[b_VF, b_PT[tj]], writes=[b_pO])
            if j < nkb - 1:
                return
            yi = cnt["y"] % 2
            cnt["y"] += 1
            u = h * 8 + i
            P.op("act", lambda e: e.activation(out=osb[yi][:], in_=pO[0:65, :], func=AF.Copy), reads=[b_pO],
                 writes=[b_osb[yi]])
            P.op("dve", lambda e: e.reciprocal(out=rec[yi][0:1, :], in_=osb[yi][0:1, :]), reads=[b_osb[yi]],
                 writes=[b_rec[yi]])
            P.op("sp", lambda e: e.dma_start(out=SC["recd"][u:u + 1, :], in_=rec[yi][0:1, :]), reads=[b_rec[yi]],
                 writes=[b_recd[yi]], dma=True)
            P.op("sp", lambda e: e.dma_start(out=bcs[yi][:], in_=SC["recd"][u:u + 1, :].partition_broadcast(65)),
                 reads=[b_recd[yi]], writes=[b_bcs[yi]], dma=True)
            P.op("dve", lambda e: e.tensor_tensor(out=ybt[yi][:], in0=osb[yi][:], in1=bcs[yi][:], op=ALU.mult),
                 reads=[b_osb[yi], b_bcs[yi]], writes=[b_ybt[yi]])
            P.op("sp", lambda e: e.dma_start(
                out=SC["ybT"][h // 2, (h % 2) * 64:(h % 2) * 64 + 64, i * 512:(i + 1) * 512], in_=ybt[yi][1:65, :]),
                reads=[b_ybt[yi]], writes=[DB("ybT")[(h * 8 + i) % 32]], dma=True)

        gen = mlstm_gen()
        fox_load(0)
        emit_S(0)
        emit_S(1)
        for idx, (h, i, j) in enumerate(seq):
            if i == 0 and j == 0 and h + 1 < 8:
                fox_load(h + 1)
            if idx + 2 < len(seq):
                emit_S(idx + 2)
            emit_rest(idx)
            if idx % 6 == 5:
                next(gen, None)
        for _ in gen:
            pass
        P.flush()


def merge_pass(P, nc, C, I, SC, DB, h1, h1_b, h2, h2_b):
    with contextlib.ExitStack() as st:
        sb = lambda name, shape, dt: st.enter_context(nc.sbuf_tensor("mg" + name, shape, dt))
        ps = lambda name, shape, dt: st.enter_context(nc.psum_tensor("mg" + name, shape, dt))
        Wa = sb("Wa", [128, 4, D], BF16)
        Wb = sb("Wb", [128, 4, D], BF16)
        Wo = sb("Wo", [128, 8, D], BF16)
        b_W = P.buf("mgW")
        P.op("pool", lambda e: e.dma_start(out=Wa[:], in_=I["w_branch_a"].rearrange("(k p) d -> p k d", p=128)),
             writes=[b_W], dma=True)
        P.op("pool", lambda e: e.dma_start(out=Wb[:], in_=I["w_branch_b"].rearrange("(k p) d -> p k d", p=128)),
             writes=[b_W], dma=True)
        P.op("pool", lambda e: e.dma_start(out=Wo[:], in_=I["w_out"].rearrange("(k p) d -> p k d", p=128)),
             writes=[b_W], dma=True)
        gpost = sb("gpost", [128, D], F32)
        P.op("sp", lambda e: e.dma_start(out=gpost[:], in_=I["mix_post_g"].partition_broadcast(128)),
             writes=[b_W], dma=True)
        yaT = [sb("yaT%d" % i, [128, 4, 128], BF16) for i in range(2)]
        ybT = [sb("ybT%d" % i, [128, 4, 128], BF16) for i in range(2)]
        gab = [sb("gab%d" % i, [128, 2, D], BF16) for i in range(2)]
        hin = [sb("hin%d" % i, [128, D], F32) for i in range(2)]
        b_in = P.bufs_n("mgin", 2)
        b_hin = P.bufs_n("mghin", 2)
        t1 = [sb("t1%d" % i, [128, D], F32) for i in range(2)]
        t2 = [sb("t2%d" % i, [128, D], F32) for i in range(2)]
        mb = [sb("mb%d" % i, [128, D], BF16) for i in range(2)]
        mT = [sb("mT%d" % i, [128, 8, 128], BF16) for i in range(2)]
        junk = sb("junk", [128, D], BF16)
        hout = [sb("hout%d" % i, [128, D], F32) for i in range(2)]
        stat = sb("stat", [128, 6], F32)
        b_t1, b_t2, b_mb, b_mT = [P.bufs_n(n, 2) for n in ("t1", "t2", "mb", "mT")]
        b_junk = P.buf("mgjunk")
        b_hout = P.bufs_n("hout", 2)
        b_stat = P.bufs_n("mgstat", 2)
        pA = ps("pA", [128, D], F32)
        pB = ps("pB", [128, D], F32)
        pO = ps("pO", [128, D], F32)
        pt = ps("pt", [128, 8, 128], BF16)
        b_pA, b_pB, b_pO, b_pt = [P.buf(n) for n in ("pA", "pB", "mgpO", "mgpt")]
        h1v = h1.rearrange("(n p) d -> n p d", p=128)
        h2v = h2.rearrange("(n p) d -> n p d", p=128)

        def s1(n):
            ib = n % 2
            rows = slice(n * 128, (n + 1) * 128)
            cols = rows
            P.op("sp", lambda e: e.dma_start(out=yaT[ib][:], in_=SC["yaT"][:, :, cols].rearrange("k p t -> p k t")),
                 reads=[DB("yaT")[n]], writes=[b_in[ib]], dma=True)
            P.op("sp", lambda e: e.dma_start(out=ybT[ib][:], in_=SC["ybT"][:, :, cols].rearrange("k p t -> p k t")),
                 reads=DB("ybT"), writes=[b_in[ib]], dma=True)
            P.op("sp", lambda e: e.dma_start(out=gab[ib][:, 0, :], in_=SC["ga"][rows]),
                 reads=[DB("gab")[n]], writes=[b_in[ib]], dma=True)
            P.op("sp", lambda e: e.dma_start(out=gab[ib][:, 1, :], in_=SC["gb"][rows]),
                 reads=[DB("gab")[n]], writes=[b_in[ib]], dma=True)
            P.op("sp", lambda e: e.dma_start(out=hin[ib][:], in_=h1v[n]),
                 reads=[h1_b[n]], writes=[b_hin[ib]], dma=True)
            for hf in range(2):
                hs = slice(hf * 512, (hf + 1) * 512)
                for k in range(4):
                    P.op("pe", lambda e, k=k, hs=hs: e.matmul(pA[:, hs], lhsT=yaT[ib][:, k, :], rhs=Wa[:, k, hs],
                                                              start=(k == 0), stop=(k == 3)),
                         reads=[b_in[ib], b_W], writes=[b_pA])
            for hf in range(2):
                hs = slice(hf * 512, (hf + 1) * 512)
                for k in range(4):
                    P.op("pe", lambda e, k=k, hs=hs: e.matmul(pB[:, hs], lhsT=ybT[ib][:, k, :], rhs=Wb[:, k, hs],
                                                              start=(k == 0), stop=(k == 3)),
                         reads=[b_in[ib], b_W], writes=[b_pB])
            P.op("dve", lambda e: e.tensor_tensor(out=t1[ib][:], in0=pA[:], in1=gab[ib][:, 0, :], op=ALU.mult),
                 reads=[b_pA, b_in[ib]], writes=[b_t1[ib]])
            P.op("dve", lambda e: e.tensor_tensor(out=t2[ib][:], in0=pB[:], in1=gab[ib][:, 1, :], op=ALU.mult),
                 reads=[b_pB, b_in[ib]], writes=[b_t2[ib]])
            P.op("pool", lambda e: e.tensor_tensor(out=mb[ib][:], in0=t1[ib][:], in1=t2[ib][:], op=ALU.add),
                 reads=[b_t1[ib], b_t2[ib]], writes=[b_mb[ib]])

        def s2(n):
            ib = n % 2
            for k in range(8):
                P.op("pe", lambda e, k=k: e.transpose(out=pt[:, k, :], in_=mb[ib][:, k * 128:(k + 1) * 128],
                                                      identity=C.ident[:]),
                     reads=[b_mb[ib], C.b_const], writes=[b_pt])
            P.op("act", lambda e: e.activation(out=mT[ib][:], in_=pt[:], func=AF.Copy), reads=[b_pt], writes=[b_mT[ib]])
            for hf in range(2):
                hs = slice(hf * 512, (hf + 1) * 512)
                for k in range(8):
                    P.op("pe", lambda e, k=k, hs=hs: e.matmul(pO[:, hs], lhsT=mT[ib][:, k, :], rhs=Wo[:, k, hs],
                                                              start=(k == 0), stop=(k == 7)),
                         reads=[b_mT[ib], b_W], writes=[b_pO])
            si = n % 2
            ss, var, rstd = (stat[:, 3 * si + j:3 * si + j + 1] for j in range(3))
            rms_stats(P, C, pO[:], b_pO, junk[:], b_junk, ss, var, rstd, b_stat[si])
            P.op("dve", lambda e: e.scalar_tensor_tensor(
                out=hout[ib][:], in0=pO[:], scalar=rstd, in1=gpost[:], op0=ALU.mult, op1=ALU.mult),
                reads=[b_pO, b_stat[si], b_W], writes=[b_hout[ib]])
            P.op("pool", lambda e: e.tensor_tensor(out=hout[ib][:], in0=hout[ib][:], in1=hin[ib][:], op=ALU.add),
                 reads=[b_hout[ib], b_hin[ib]], writes=[b_hout[ib]])
            P.op("pool", lambda e: e.dma_start(out=h2v[n], in_=hout[ib][:]),
                 reads=[b_hout[ib]], writes=[h2_b[n]], dma=True)

        s1(0)
        for n in range(32):
            if n + 1 < 32:
                s1(n + 1)
            s2(n)
        P.flush()


def ple_pass(P, nc, C, I, uT3, uT3_b, h3, h3_b, out):
    with contextlib.ExitStack() as st:
        sb = lambda name, shape, dt: st.enter_context(nc.sbuf_tensor("pl" + name, shape, dt))
        ps = lambda name, shape, dt: st.enter_context(nc.psum_tensor("pl" + name, shape, dt))
        Wg = sb("Wg", [128, 8, D], BF16)
        Wp = sb("Wp", [128, 2, D], BF16)
        b_W = P.buf("plW")
        P.op("pool", lambda e: e.dma_start(out=Wg[:], in_=I["ple_w_gate"].rearrange("(k p) d -> p k d", p=128)),
             writes=[b_W], dma=True)
        P.op("pool", lambda e: e.dma_start(out=Wp[:], in_=I["ple_w_proj"].rearrange("(k p) d -> p k d", p=128)),
             writes=[b_W], dma=True)
        gpost = sb("gpost", [128, D], F32)
        bg = sb("bg", [128, D], F32)
        P.op("sp", lambda e: e.dma_start(out=gpost[:], in_=I["ple_post_g"].partition_broadcast(128)),
             writes=[b_W], dma=True)
        P.op("sp", lambda e: e.dma_start(out=bg[:], in_=I["ple_b_gate"].partition_broadcast(128)),
             writes=[b_W], dma=True)
        uT = [sb("uT%d" % i, [128, 8, 128], BF16) for i in range(2)]
        pin = [sb("pin%d" % i, [128, 256], F32) for i in range(2)]
        hin = [sb("hin%d" % i, [128, D], F32) for i in range(2)]
        b_in = P.bufs_n("plin", 2)
        b_hin = P.bufs_n("plhin", 2)
        pb = [sb("pb%d" % i, [128, 256], BF16) for i in range(2)]
        pT = [sb("pT%d" % i, [128, 2, 128], BF16) for i in range(2)]
        gt = [sb("gt%d" % i, [128, D], F32) for i in range(2)]
        ge = [sb("ge%d" % i, [128, D], F32) for i in range(2)]
        junk = sb("junk", [128, D], BF16)
        hout = [sb("hout%d" % i, [128, D], F32) for i in range(2)]
        stat = sb("stat", [128, 6], F32)
        b_pb, b_pT, b_gt, b_ge = [P.bufs_n(n, 2) for n in ("pb", "pT", "gt", "ge")]
        b_junk = P.buf("pljunk")
        b_hout = P.bufs_n("plhout", 2)
        b_stat = P.bufs_n("plstat", 2)
        pG = [ps("pG%d" % i, [128, D], F32) for i in range(2)]
        pE = ps("pE", [128, D], F32)
        ptp = ps("ptp", [128, 2, 128], BF16)
        b_pG = P.bufs_n("pG", 2)
        b_pE, b_ptp = P.buf("pE"), P.buf("ptp")
        pv = I["p"].rearrange("(n p) d -> n p d", p=128)
        h3v = h3.rearrange("(n p) d -> n p d", p=128)
        ov = out.rearrange("(n p) d -> n p d", p=128)

        def s1(n):
            ib = n % 2
            cols = slice(n * 128, (n + 1) * 128)
            P.op("sp", lambda e: e.dma_start(out=uT[ib][:], in_=uT3[:, :, cols].rearrange("k p t -> p k t")),
                 reads=[uT3_b[n]], writes=[b_in[ib]], dma=True)
            P.op("sp", lambda e: e.dma_start(out=pin[ib][:], in_=pv[n]), writes=[b_in[ib]], dma=True)
            P.op("sp", lambda e: e.dma_start(out=hin[ib][:], in_=h3v[n]), reads=[h3_b[n]],
                 writes=[b_hin[ib]], dma=True)
            P.op("act", lambda e: e.activation(out=pb[ib][:], in_=pin[ib][:], func=AF.Copy), reads=[b_in[ib]],
                 writes=[b_pb[ib]])
            for k in range(2):
                P.op("pe", lambda e, k=k: e.transpose(out=ptp[:, k, :], in_=pb[ib][:, k * 128:(k + 1) * 128],
                                                      identity=C.ident[:]),
                     reads=[b_pb[ib], C.b_const], writes=[b_ptp])
            P.op("dve", lambda e: e.tensor_copy(out=pT[ib][:], in_=ptp[:]), reads=[b_ptp], writes=[b_pT[ib]])
            for hf in range(2):
                hs = slice(hf * 512, (hf + 1) * 512)
                for k in range(8):
                    P.op("pe", lambda e, k=k, hs=hs: e.matmul(pG[ib][:, hs], lhsT=uT[ib][:, k, :], rhs=Wg[:, k, hs],
                                                              start=(k == 0), stop=(k == 7)),
                         reads=[b_in[ib], b_W], writes=[b_pG[ib]])

        def s2(n):
            ib = n % 2
            for hf in range(2):
                hs = slice(hf * 512, (hf + 1) * 512)
                for k in range(2):
                    P.op("pe", lambda e, k=k, hs=hs: e.matmul(pE[:, hs], lhsT=pT[ib][:, k, :], rhs=Wp[:, k, hs],
                                                              start=(k == 0), stop=(k == 1)),
                         reads=[b_pT[ib], b_W], writes=[b_pE])
            P.op("dve", lambda e: e.tensor_tensor(out=gt[ib][:], in0=pG[ib][:], in1=bg[:], op=ALU.add),
                 reads=[b_pG[ib], b_W], writes=[b_gt[ib]])
            P.op("act", lambda e: e.activation(out=gt[ib][:], in_=gt[ib][:], func=AF.Sigmoid), reads=[b_gt[ib]],
                 writes=[b_gt[ib]])
            P.op("dve", lambda e: e.tensor_tensor(out=ge[ib][:], in0=gt[ib][:], in1=pE[:], op=ALU.mult),
                 reads=[b_gt[ib], b_pE], writes=[b_ge[ib]])
            si = n % 2
            ss, var, rstd = (stat[:, 3 * si + j:3 * si + j + 1] for j in range(3))
            rms_stats(P, C, ge[ib][:], b_ge[ib], junk[:], b_junk, ss, var, rstd, b_stat[si])
            P.op("dve", lambda e: e.scalar_tensor_tensor(
                out=hout[ib][:], in0=ge[ib][:], scalar=rstd, in1=gpost[:], op0=ALU.mult, op1=ALU.mult),
                reads=[b_ge[ib], b_stat[si], b_W], writes=[b_hout[ib]])
            P.op("pool", lambda e: e.tensor_tensor(out=hout[ib][:], in0=hout[ib][:], in1=hin[ib][:], op=ALU.add),
                 reads=[b_hout[ib], b_hin[ib]], writes=[b_hout[ib]])
            P.op("pool", lambda e: e.dma_start(out=ov[n], in_=hout[ib][:]), reads=[b_hout[ib]], dma=True)

        s1(0)
        for n in range(32):
            if n + 1 < 32:
                s1(n + 1)
            s2(n)
        P.flush()


def build_program(debug=False, stage=99, only=None):
    nc = bass.Bass("TRN2", target_bir_lowering=False)
    I = {}

    def din(name, shape):
        I[name] = nc.dram_tensor(name, shape, F32, kind="ExternalInput").ap()
        return I[name]

    din("x", [S, D])
    din("p", [S, 256])
    for nm in ("ffn1", "ffn2"):
        din(nm + "_pre_g", [1, D])
        din(nm + "_w_gate", [D, DFF])
        din(nm + "_w_up", [D, DFF])
        din(nm + "_w_down", [DFF, D])
        din(nm + "_post_g", [1, D])
    din("mix_pre_g", [1, D])
    din("w_in", [D, INW])
    din("conv_w", [4, 512])
    din("conv_b", [1, 512])
    din("mlstm_i_bias", [1, 4])
    din("mlstm_f_bias", [1, 4])
    din("mlstm_norm_g", [1, 512])
    din("fox_f_bias", [1, 8])
    din("branch_gate_bias", [1, 2048])
    din("w_branch_a", [512, D])
    din("w_branch_b", [512, D])
    din("w_out", [D, D])
    din("mix_post_g", [1, D])
    din("ple_pre_g", [1, D])
    din("ple_w_gate", [D, D])
    din("ple_b_gate", [1, D])
    din("ple_w_proj", [256, D])
    din("ple_post_g", [1, D])

    skind = "ExternalOutput" if debug else "Internal"

    def dscr(name, shape, dt):
        return nc.dram_tensor(name, shape, dt, kind=skind).ap()

    out = nc.dram_tensor("out", [S, D], F32, kind="ExternalOutput").ap()
    h1 = dscr("h1", [S, D], F32)
    uT1 = dscr("uT1", [8, 128, S], BF16)
    gpre = dscr("gpre", [72, S], F32)
    SC = {
        "mqkT": dscr("mqkT", [4, 128, S], BF16),
        "vaugM": dscr("vaugM", [S, 4, 129], BF16),
        "so": dscr("so", [S, 512], BF16),
        "qaug": dscr("qaug", [8, 70, S], BF16),
        "kaug": dscr("kaug", [8, 70, S], BF16),
        "vaugF": dscr("vaugF", [S, 8, 65], BF16),
        "ga": dscr("ga", [S, D], BF16),
        "gb": dscr("gb", [S, D], BF16),
        "yaT": dscr("yaT", [4, 128, S], BF16),
        "ybT": dscr("ybT", [4, 128, S], BF16),
        "recd": dscr("recd", [64, 512], F32),
    }
    h2 = dscr("h2", [S, D], F32)
    h3 = dscr("h3", [S, D], F32)
    uT3 = dscr("uT3", [8, 128, S], BF16)

    with contextlib.ExitStack() as st:
        P = Prog(nc, st)
        C = Ctx()
        C.db = {}

        def db(name):
            if name not in C.db:
                C.db[name] = P.bufs_n("D" + name, 32)
            return C.db[name]

        setup_consts(P, nc, st, C)
        C.wthr = st.enter_context(nc.sbuf_tensor("wthr", [128, 32, 8], F32))
        C.decbc = st.enter_context(nc.sbuf_tensor("decbc", [128, 4, 32], F32))
        C.b_wthr = P.buf("wthr")
        C.b_decbc = P.buf("decbc")
        def want(name, st_no):
            return (name in only) if only is not None else (stage >= st_no)

        if want("ffn1", 1):
            ffn_pass(P, nc, C, "f1", I["x"], I["ffn1_w_gate"], I["ffn1_w_up"], I["ffn1_w_down"],
                     I["ffn1_pre_g"], I["ffn1_post_g"], h1, I["mix_pre_g"], uT1,
                     db("x"), db("h1"), db("uT1"), gate_w=I["w_in"], gate_dst=gpre, gate_b=db("gpre"))
        if want("gp", 2):
            aug_b = P.buf("augrows")
            gp_stage(P, nc, C, I, gpre, db("gpre"), SC["qaug"], SC["kaug"], aug_b)
            db("aug").append(aug_b)
        if want("win", 2):
            win_pass(P, nc, C, I, uT1, db("uT1"), SC, db)
        if want("mix", 3):
            mix_pass(P, nc, C, I, SC, db)
        if want("merge", 4):
            merge_pass(P, nc, C, I, SC, db, h1, db("h1"), h2, db("h2"))
        if want("ffn2", 5):
            ffn_pass(P, nc, C, "f2", h2, I["ffn2_w_gate"], I["ffn2_w_up"], I["ffn2_w_down"],
                     I["ffn2_pre_g"], I["ffn2_post_g"], h3, I["ple_pre_g"], uT3,
                     db("h2"), db("h3"), db("uT3"))
        if want("ple", 6):
            ple_pass(P, nc, C, I, uT3, db("uT3"), h3, db("h3"), out)
        P.flush(final=True)
    return nc

IN_NAMES = ["x", "p", "ffn1_pre_g", "ffn1_w_gate", "ffn1_w_up", "ffn1_w_down", "ffn1_post_g",
            "mix_pre_g", "w_in", "conv_w", "conv_b", "mlstm_i_bias", "mlstm_f_bias", "mlstm_norm_g",
            "fox_f_bias", "branch_gate_bias", "w_branch_a", "w_branch_b", "w_out", "mix_post_g",
            "ffn2_pre_g", "ffn2_w_gate", "ffn2_w_up", "ffn2_w_down", "ffn2_post_g",
            "ple_pre_g", "ple_w_gate", "ple_b_gate", "ple_w_proj", "ple_post_g"]


def make_in_maps(inputs, cores):
    maps = []
    shared = {}
    for k in IN_NAMES:
        if k in ("x", "p"):
            continue
        shared[k] = np.ascontiguousarray(np.asarray(inputs[k])[0], dtype=np.float32)
    x = np.asarray(inputs["x"])
    p = np.asarray(inputs["p"])
    for b in cores:
        m = dict(shared)
        m["x"] = np.ascontiguousarray(x[b], dtype=np.float32)
        m["p"] = np.ascontiguousarray(p[0, b], dtype=np.float32)
        maps.append(m)
    return maps


def kernel(**inputs):
    nc = build_program()
    maps = make_in_maps(inputs, list(range(8)))
    res = run_bass_kernel_spmd(nc, maps, core_ids=list(range(8)))
    return np.stack([np.asarray(r["out"], dtype=np.float32) for r in res.results], axis=0)
```

```python
import contextlib
import numpy as np
import concourse.bass as bass
import concourse.mybir as mybir
from concourse.bass_utils import run_bass_kernel_spmd

F32 = mybir.dt.float32
BF16 = mybir.dt.bfloat16
AF = mybir.ActivationFunctionType
ALU = mybir.AluOpType
AX = mybir.AxisListType

S = 4096
D = 1024
DFF = 2816
NFC = DFF // 128
NT = 8
TT = 512
EPS = 1e-6
INW = 5136

ENGS = ("pe", "act", "dve", "pool", "sp")


class Buf:
    __slots__ = ("name", "w", "r")

    def __init__(self, name=""):
        self.name = name
        self.w = None
        self.r = []


class Op:
    __slots__ = ("eng", "fn", "deps", "inc", "cnt", "dma", "sem", "emitted")

    def __init__(self, eng, fn, dma=False):
        self.eng = eng
        self.fn = fn
        self.deps = []
        self.inc = False
        self.cnt = 0
        self.dma = dma
        self.sem = None
        self.emitted = False


class Prog:
    def __init__(self, nc, st, n_dma_sems=20):
        self.nc = nc
        self.pending = {e: [] for e in ENGS}
        self.bufs = []
        self.nd = n_dma_sems
        self.esem = {e: st.enter_context(nc.semaphore("s_" + e)) for e in ENGS}
        self.dsem = {}
        for e in ("sp", "pool"):
            for s in range(n_dma_sems):
                self.dsem[(e, s)] = st.enter_context(nc.semaphore("d_%s_%d" % (e, s)))
        self.ecnt = {e: 0 for e in ENGS}
        self.dcnt = {e: 0 for e in ENGS}
        self.waited = {e: {} for e in ENGS}
        self.n_ops = 0

    def buf(self, name=""):
        b = Buf(name)
        self.bufs.append(b)
        return b

    def bufs_n(self, name, n):
        return [self.buf("%s%d" % (name, i)) for i in range(n)]

    def op(self, eng, fn, reads=(), writes=(), dma=False):
        o = Op(eng, fn, dma)
        seen = set()
        cand = []
        for b in reads:
            if b.w is not None:
                cand.append(b.w)
        for b in writes:
            if b.w is not None:
                cand.append(b.w)
            cand.extend(b.r)
        for d in cand:
            if d is o or id(d) in seen:
                continue
            seen.add(id(d))
            if d.eng == "pe" and eng == "pe" and not d.dma and not dma:
                continue
            o.deps.append(d)
            if not d.emitted:
                d.inc = True
        for b in reads:
            b.r.append(o)
        for b in writes:
            b.w = o
            b.r = []
        self.pending[eng].append(o)
        self.n_ops += 1
        return o

    def flush(self, final=False):
        nc = self.nc
        for b in self.bufs:
            if b.w is not None and not b.w.emitted:
                b.w.inc = True
            for r in b.r:
                if not r.emitted:
                    r.inc = True
        for e in ENGS:
            for o in self.pending[e]:
                if o.dma:
                    k = self.dcnt[e]
                    o.sem = (e, k % self.nd)
                    o.cnt = 16 * (k // self.nd + 1)
                    self.dcnt[e] = k + 1
                elif o.inc:
                    self.ecnt[e] += 1
                    o.cnt = self.ecnt[e]
        pending = self.pending
        self.pending = {e: [] for e in ENGS}

        def run(ename, eng):
            waited = self.waited[ename]
            for o in pending[ename]:
                for d in o.deps:
                    key = d.sem if d.dma else d.eng
                    if waited.get(key, 0) >= d.cnt:
                        continue
                    assert d.cnt > 0, (d.eng, ename)
                    eng.wait_ge(self.dsem[key] if d.dma else self.esem[key], d.cnt)
                    waited[key] = d.cnt
                if o.dma:
                    if o.cnt > 16 and waited.get(o.sem, 0) < o.cnt - 16:
                        eng.wait_ge(self.dsem[o.sem], o.cnt - 16)
                        waited[o.sem] = o.cnt - 16
                    o.fn(eng).then_inc(self.dsem[o.sem], 16)
                else:
                    ins = o.fn(eng)
                    if o.inc:
                        ins.then_inc(self.esem[o.eng], 1)
                o.emitted = True
            if ename == "sp" and final:
                for q in ("sp", "pool"):
                    k = self.dcnt[q]
                    for sl in range(min(self.nd, k)):
                        last = 16 * ((k - 1 - sl) // self.nd + 1)
                        eng.wait_ge(self.dsem[(q, sl)], last)

        with nc.Block() as block:
            @block.tensor
            def _(eng):
                run("pe", eng)

            @block.scalar
            def _(eng):
                run("act", eng)

            @block.vector
            def _(eng):
                run("dve", eng)

            @block.gpsimd
            def _(eng):
                run("pool", eng)

            @block.sync
            def _(eng):
                run("sp", eng)


def bcast_last(ap2d, n):
    return ap2d.unsqueeze(2).to_broadcast([ap2d.shape[0], ap2d.shape[1], n])


class Ctx:
    pass


def load_w_kmajor(P, nc, dst, src2d, n_kc, ncols, bufs, col_chunk=1408):
    v = src2d.rearrange("(kc p) n -> kc p n", p=128)
    mdl = 4 * col_chunk
    for k in range(n_kc):
        P.op("pool", lambda e, k=k: e.dma_start(out=dst[:, k, :], in_=v[k], max_dma_last_dim=mdl),
             writes=[bufs[k]], dma=True)


def setup_consts(P, nc, st, C):
    sb = lambda name, shape, dt: st.enter_context(nc.sbuf_tensor(name, shape, dt))
    C.identf = sb("identf", [128, 128], F32)
    C.ident = sb("ident", [128, 128], BF16)
    C.neghalf = sb("neghalf", [128, 1], F32)
    C.b_const = P.buf("const")
    identf, ident = C.identf, C.ident
    P.op("pool", lambda e: e.memset(identf[:], 0.0), writes=[C.b_const])
    P.op("pool", lambda e: e.affine_select(out=identf[:], in_=identf[:], pattern=[[-1, 128]],
                                             compare_op=ALU.not_equal, fill=1.0, base=0,
                                             channel_multiplier=1),
         reads=[C.b_const], writes=[C.b_const])
    P.op("dve", lambda e: e.tensor_copy(out=ident[:], in_=identf[:]), reads=[C.b_const], writes=[C.b_const])
    P.op("pool", lambda e: e.memset(C.neghalf[:], -0.5), reads=[C.b_const], writes=[C.b_const])


def rms_stats(P, C, src_ap, src_buf, junk, b_junk, ss, var, rstd, b_stat, n_feat=D):
    P.op("act", lambda e: e.activation(out=junk, in_=src_ap, func=AF.Square, accum_out=ss),
         reads=[src_buf], writes=[b_junk, b_stat])
    P.op("dve", lambda e: e.tensor_scalar(out=var, in0=ss, scalar1=1.0 / n_feat, scalar2=EPS,
                                          op0=ALU.mult, op1=ALU.add),
         reads=[b_stat], writes=[b_stat])
    P.op("pool", lambda e: e.tensor_tensor(out=rstd, in0=var, in1=C.neghalf[:], op=ALU.pow),
         reads=[b_stat, C.b_const], writes=[b_stat])


def ffn_pass(P, nc, C, tag, src_h, w_gate, w_up, w_down, pre_g, post_g, dst_h, next_g, dst_uT,
             src_b, dst_b, uT_b, gate_w=None, gate_dst=None, gate_b=None):
    with contextlib.ExitStack() as st:
        sb = lambda name, shape, dt: st.enter_context(nc.sbuf_tensor(tag + name, shape, dt))
        ps = lambda name, shape, dt: st.enter_context(nc.psum_tensor(tag + name, shape, dt))
        Wg = sb("Wg", [128, 8, DFF], BF16)
        Wu = sb("Wu", [128, 8, DFF], BF16)
        Wd = sb("Wd", [128, NFC, D], BF16)
        b_Wg = P.bufs_n("Wg", 8)
        b_Wu = P.bufs_n("Wu", 8)
        b_Wd = P.bufs_n("Wd", 2)
        load_w_kmajor(P, nc, Wg, w_gate, 8, DFF, b_Wg)
        load_w_kmajor(P, nc, Wu, w_up, 8, DFF, b_Wu)
        wdv = w_down.rearrange("(fc p) d -> p fc d", p=128)
        for hh in range(2):
            P.op("pool", lambda e, hh=hh: e.dma_start(out=Wd[:, hh * 11:(hh + 1) * 11, :],
                                                       in_=wdv[:, hh * 11:(hh + 1) * 11, :]),
                 writes=[b_Wd[hh]], dma=True)
        gpre = sb("gpre", [128, 8], F32)
        gnext = sb("gnext", [128, 8], F32)
        gpost = sb("gpost", [128, D], F32)
        b_par = P.buf("par")
        P.op("sp", lambda e: e.dma_start(out=gpre[:], in_=pre_g.rearrange("o (k p) -> p (o k)", p=128),
                                         allow_slow_non_contiguous=True),
             writes=[b_par], dma=True)
        P.op("sp", lambda e: e.dma_start(out=gnext[:], in_=next_g.rearrange("o (k p) -> p (o k)", p=128),
                                         allow_slow_non_contiguous=True),
             writes=[b_par], dma=True)
        P.op("sp", lambda e: e.dma_start(out=gpost[:], in_=post_g.partition_broadcast(128)),
             writes=[b_par], dma=True)
        if gate_w is not None:
            Wgt = sb("Wgt", [128, 8, 72], BF16)
            b_Wgt = P.buf("Wgt")
            P.op("pool", lambda e: e.memset(Wgt[:], 0.0), writes=[b_Wgt])
            gv = gate_w.rearrange("(kc p) n -> p kc n", p=128)
            for (c0, n, d0) in ((1540, 4, 0), (1536, 4, 32), (3080, 8, 64)):
                P.op("pool", lambda e, c0=c0, n=n, d0=d0: e.dma_start(
                    out=Wgt[:, :, d0:d0 + n], in_=gv[:, :, c0:c0 + n]),
                    reads=[], writes=[b_Wgt], dma=True)
            gsb = [sb("gsb%d" % i, [72, 128], F32) for i in range(2)]
            b_gsb = P.bufs_n("gsb", 2)

        NXB = 3
        xb = [sb("xb%d" % i, [128, D], F32) for i in range(NXB)]
        b_xb = P.bufs_n("xb", NXB)
        ubf = [sb("ubf%d" % i, [128, D], BF16) for i in range(2)]
        b_ubf = P.bufs_n("ubf", 2)
        junk = sb("junk", [128, D], BF16)
        b_junk = P.buf("junk")
        uT = sb("uT", [128, 8, TT], BF16)
        b_uT = P.bufs_n("uT", 4)
        aT = sb("aT", [128, NFC, TT], BF16)
        b_aT = P.bufs_n("aT", NFC)
        sg = [sb("sg%d" % i, [128, TT], F32) for i in range(2)]
        b_sg = P.bufs_n("sg", 2)
        hst = [sb("hst%d" % i, [128, D], F32) for i in range(2)]
        b_hst = P.bufs_n("hst", 2)
        u2T = [sb("u2T%d" % i, [128, 8, 128], BF16) for i in range(2)]
        b_u2T = P.bufs_n("u2T", 2)
        NST = 6
        stat = sb("stat", [128, 3 * NST], F32)
        b_stat = P.bufs_n("stat", NST)

        pt = ps("pt", [128, 8, 128], BF16)
        b_pt = P.buf("pt")
        pg = [ps("pg%d" % i, [128, TT], F32) for i in range(2)]
        pu = [ps("pu%d" % i, [128, TT], F32) for i in range(2)]
        b_pg = P.bufs_n("pg", 2)
        b_pu = P.bufs_n("pu", 2)
        pys = [ps("py%d" % i, [128, 512], F32) for i in range(3)]
        b_pys = P.bufs_n("py", 3)
        if gate_w is not None:
            pgt = pg[0][0:72, 0:128]
            b_pgt = b_pg[0]

        src_v = src_h.rearrange("(n p) d -> n p d", p=128)
        dst_v = dst_h.rearrange("(n p) d -> n p d", p=128)
        cnt = {"x": 0, "u": 0, "st": 0, "h": 0, "u2": 0, "sg": 0, "gs": 0, "py": 0}

        def norm_T(h_ap, h_buf, gcol, out_ap, out_bufs):
            si = cnt["st"] % NST
            cnt["st"] += 1
            ss, var, rstd = (stat[:, 3 * si + j:3 * si + j + 1] for j in range(3))
            rms_stats(P, C, h_ap, h_buf, junk[:], b_junk, ss, var, rstd, b_stat[si])
            ui = cnt["u"] % 2
            cnt["u"] += 1
            u = ubf[ui]
            P.op("dve", lambda e: e.tensor_scalar(out=u[:], in0=h_ap, scalar1=rstd, scalar2=None, op0=ALU.mult),
                 reads=[h_buf, b_stat[si]], writes=[b_ubf[ui]])
            for k in range(8):
                P.op("pe", lambda e, k=k: e.transpose(out=pt[:, k, :], in_=u[:, k * 128:(k + 1) * 128],
                                                      identity=C.ident[:]),
                     reads=[b_ubf[ui], C.b_const], writes=[b_pt])
            P.op("dve", lambda e: e.tensor_tensor(out=out_ap, in0=pt[:], in1=bcast_last(gcol[:], 128), op=ALU.mult),
                 reads=[b_pt, b_par], writes=out_bufs)

        def pre(i):
            for s in range(4):
                n = i * 4 + s
                xi = cnt["x"] % NXB
                cnt["x"] += 1
                P.op("sp", lambda e, n=n, xi=xi: e.dma_start(out=xb[xi][:], in_=src_v[n]),
                     reads=[src_b[n]], writes=[b_xb[xi]], dma=True)
                norm_T(xb[xi][:], b_xb[xi], gpre, uT[:, :, s * 128:(s + 1) * 128], [b_uT[s]])

        def gateup(i):
            for f in range(NFC):
                j = f % 2
                for k in range(8):
                    P.op("pe", lambda e, k=k, f=f, j=j: e.matmul(
                        pg[j][:], lhsT=Wg[:, k, f * 128:(f + 1) * 128], rhs=uT[:, k, :],
                        start=(k == 0), stop=(k == 7)),
                        reads=[b_Wg[k]] + b_uT, writes=[b_pg[j]])
                for k in range(8):
                    P.op("pe", lambda e, k=k, f=f, j=j: e.matmul(
                        pu[j][:], lhsT=Wu[:, k, f * 128:(f + 1) * 128], rhs=uT[:, k, :],
                        start=(k == 0), stop=(k == 7)),
                        reads=[b_Wu[k]] + b_uT, writes=[b_pu[j]])
                si = cnt["sg"] % 2
                cnt["sg"] += 1
                P.op("act", lambda e, j=j, si=si: e.activation(out=sg[si][:], in_=pg[j][:], func=AF.Silu),
                     reads=[b_pg[j]], writes=[b_sg[si]])
                P.op("dve", lambda e, j=j, si=si, f=f: e.tensor_tensor(out=aT[:, f, :], in0=sg[si][:], in1=pu[j][:],
                                                                   op=ALU.mult),
                     reads=[b_sg[si], b_pu[j]], writes=[b_aT[f]])

        def down_post(i):
            for s in range(4):
                n = i * 4 + s
                pyh = []
                for hf in range(2):
                    pi = cnt["py"] % 3
                    cnt["py"] += 1
                    pyh.append((pys[pi], b_pys[pi]))
                    for f in range(NFC):
                        P.op("pe", lambda e, f=f, s=s, hf=hf, pi=pi: e.matmul(
                            pys[pi][:], lhsT=aT[:, f, s * 128:(s + 1) * 128],
                            rhs=Wd[:, f, hf * 512:(hf + 1) * 512], start=(f == 0), stop=(f == NFC - 1)),
                            reads=[b_aT[f], b_Wd[f // 11]], writes=[b_pys[pi]])
                xi = cnt["x"] % NXB
                cnt["x"] += 1
                P.op("sp", lambda e, n=n, xi=xi: e.dma_start(out=xb[xi][:], in_=src_v[n]),
                     reads=[src_b[n]], writes=[b_xb[xi]], dma=True)
                si = cnt["st"] % NST
                cnt["st"] += 1
                ss, var, rstd = (stat[:, 3 * si + j:3 * si + j + 1] for j in range(3))
                P.op("act", lambda e, ss=ss, t=pyh[0][0]: e.activation(out=junk[:, 0:512], in_=t[:], func=AF.Square,
                                                                      accum_out=ss),
                     reads=[pyh[0][1]], writes=[b_junk, b_stat[si]])
                P.op("act", lambda e, var=var, t=pyh[1][0]: e.activation(out=junk[:, 512:1024], in_=t[:], func=AF.Square,
                                                                        accum_out=var),
                     reads=[pyh[1][1]], writes=[b_junk, b_stat[si]])
                P.op("dve", lambda e, ss=ss, var=var: e.tensor_tensor(out=var, in0=ss, in1=var, op=ALU.add),
                     reads=[b_stat[si]], writes=[b_stat[si]])
                P.op("dve", lambda e, var=var: e.tensor_scalar(out=var, in0=var, scalar1=1.0 / D, scalar2=EPS,
                                                              op0=ALU.mult, op1=ALU.add),
                     reads=[b_stat[si]], writes=[b_stat[si]])
                P.op("pool", lambda e, var=var, rstd=rstd: e.tensor_tensor(out=rstd, in0=var, in1=C.neghalf[:], op=ALU.pow),
                     reads=[b_stat[si], C.b_const], writes=[b_stat[si]])
                hi = cnt["h"] % 2
                cnt["h"] += 1
                hb = hst[hi]
                for hf in range(2):
                    hs = slice(hf * 512, (hf + 1) * 512)
                    P.op("dve", lambda e, hb=hb, rstd=rstd, t=pyh[hf][0], hs=hs: e.scalar_tensor_tensor(
                        out=hb[:, hs], in0=t[:], scalar=rstd, in1=gpost[:, hs], op0=ALU.mult, op1=ALU.mult),
                        reads=[pyh[hf][1], b_stat[si], b_par], writes=[b_hst[hi]])
                P.op("dve", lambda e, hb=hb, xi=xi: e.scalar_tensor_tensor(
                    out=hb[:], in0=hb[:], scalar=0.5, in1=xb[xi][:], op0=ALU.mult, op1=ALU.add),
                    reads=[b_hst[hi], b_xb[xi]], writes=[b_hst[hi]])
                P.op("sp", lambda e, hb=hb, n=n: e.dma_start(out=dst_v[n], in_=hb[:]),
                     reads=[b_hst[hi]], writes=[dst_b[n]], dma=True)
                ui2 = cnt["u2"] % 2
                cnt["u2"] += 1
                norm_T(hb[:], b_hst[hi], gnext, u2T[ui2][:], [b_u2T[ui2]])
                P.op("sp", lambda e, ui2=ui2, n=n: e.dma_start(
                    out=dst_uT[:, :, n * 128:(n + 1) * 128].rearrange("k p t -> p k t"), in_=u2T[ui2][:]),
                    reads=[b_u2T[ui2]], writes=[uT_b[n]], dma=True)
                if gate_w is not None:
                    for k in range(8):
                        P.op("pe", lambda e, k=k, ui2=ui2: e.matmul(
                            pgt, lhsT=Wgt[:, k, :], rhs=u2T[ui2][:, k, :], start=(k == 0), stop=(k == 7)),
                            reads=[b_Wgt, b_u2T[ui2]], writes=[b_pgt])
                    gi = cnt["gs"] % 2
                    cnt["gs"] += 1
                    P.op("act", lambda e, gi=gi: e.activation(out=gsb[gi][:], in_=pgt, func=AF.Copy),
                         reads=[b_pgt], writes=[b_gsb[gi]])
                    P.op("sp", lambda e, gi=gi, n=n: e.dma_start(out=gate_dst[:, n * 128:(n + 1) * 128], in_=gsb[gi][:]),
                         reads=[b_gsb[gi]], writes=[gate_b[n]], dma=True)

        pre(0)
        for i in range(NT):
            gateup(i)
            if i + 1 < NT:
                pre(i + 1)
            down_post(i)
        P.flush()


def gp_stage(P, nc, C, I, gpre, gpre_b, qaug, kaug, aug_b):
    with contextlib.ExitStack() as st:
        sb = lambda name, shape, dt: st.enter_context(nc.sbuf_tensor("gp" + name, shape, dt))
        ps = lambda name, shape, dt: st.enter_context(nc.psum_tensor("gp" + name, shape, dt))
        T0 = sb("T0", [72, S], F32)
        T1 = sb("T1", [72, S], F32)
        T2 = sb("T2", [72, S], F32)
        T3 = sb("T3", [72, S], F32)
        QR = sb("QR", [72, 3, S], BF16)
        KR = sb("KR", [72, 3, S], BF16)
        ONE = sb("ONE", [72, S], BF16)
        bcol = sb("bcol", [72, 1], F32)
        negb = sb("negb", [72, 1], F32)
        bicol = sb("bicol", [72, 1], F32)
        onec = sb("onec", [72, 1], F32)
        cm = sb("cm", [72, 32], F32)
        mce = sb("mce", [72, 32], F32)
        mprev = sb("mprev", [72, 32], F32)
        dec = sb("dec", [72, 32], F32)
        esel = sb("esel", [72, 4, 128], F32)
        bT0, bT1, bT2, bT3, bQR, bKR, bONE, bsm = [P.buf(n) for n in
                                                   ("T0", "T1", "T2", "T3", "QR", "KR", "ONE", "gsm")]
        ptm = ps("ptm", [128, 32, 8], F32)
        pdc = ps("pdc", [128, 4, 32], F32)
        b_ptm, b_pdc = P.buf("ptm"), P.buf("pdc")

        P.op("sp", lambda e: e.dma_start(out=T0[:], in_=gpre), reads=gpre_b, writes=[bT0], dma=True)
        P.op("sp", lambda e: e.dma_start(out=T3[0:4, :], in_=gpre[32:36, :]), reads=gpre_b, writes=[bT3], dma=True)
        P.op("dve", lambda e: e.memset(bcol[:], 0.0), writes=[bsm])
        P.op("dve", lambda e: e.memset(bicol[:], 0.0), reads=[bsm], writes=[bsm])
        P.op("dve", lambda e: e.memset(onec[:], 1.0), reads=[bsm], writes=[bsm])
        P.op("pool", lambda e: e.memset(ONE[:], 1.0), writes=[bONE])
        P.op("sp", lambda e: e.dma_start(out=bcol[0:4, :], in_=I["mlstm_f_bias"].rearrange("o n -> n o"),
                                         allow_slow_non_contiguous=True), reads=[bsm], writes=[bsm], dma=True)
        P.op("sp", lambda e: e.dma_start(out=bcol[64:72, :], in_=I["fox_f_bias"].rearrange("o n -> n o"),
                                         allow_slow_non_contiguous=True), reads=[bsm], writes=[bsm], dma=True)
        P.op("sp", lambda e: e.dma_start(out=bicol[0:4, :], in_=I["mlstm_i_bias"].rearrange("o n -> n o"),
                                         allow_slow_non_contiguous=True), reads=[bsm], writes=[bsm], dma=True)
        P.op("dve", lambda e: e.tensor_scalar(out=negb[0:72, :], in0=bcol[0:72, :], scalar1=-1.0, scalar2=None,
                                              op0=ALU.mult), reads=[bsm], writes=[bsm])
        R = slice(0, 72)
        P.op("act", lambda e: e.activation(out=T1[R, :], in_=T0[R, :], func=AF.Exp, scale=-1.0, bias=negb[R, :]),
             reads=[bT0, bsm], writes=[bT1])
        P.op("act", lambda e: e.activation(out=T1[R, :], in_=T1[R, :], func=AF.Ln, scale=1.0, bias=onec[R, :]),
             reads=[bT1, bsm], writes=[bT1])
        P.op("dve", lambda e: e.tensor_tensor_scan(out=T2[R, :], data0=T1[R, :], data1=T1[R, :], initial=0.0,
                                                   op0=ALU.add, op1=ALU.max), reads=[bT1], writes=[bT2])
        M = slice(0, 4)
        P.op("dve", lambda e: e.scalar_tensor_tensor(out=T3[M, :], in0=T3[M, :], scalar=bicol[M, :], in1=T2[M, :],
                                                     op0=ALU.add, op1=ALU.add), reads=[bT3, bT2, bsm], writes=[bT3])
        P.op("dve", lambda e: e.tensor_reduce(out=cm[M, :], in_=T3[M, :].rearrange("p (c l) -> p c l", l=128),
                                              axis=AX.X, op=ALU.max), reads=[bT3], writes=[bsm])
        P.op("dve", lambda e: e.tensor_tensor_scan(out=mce[M, :], data0=cm[M, :], data1=cm[M, :], initial=0.0,
                                                   op0=ALU.max, op1=ALU.max), reads=[bsm], writes=[bsm])
        P.op("dve", lambda e: e.tensor_tensor(out=T3[M, :].rearrange("p (c l) -> p c l", l=128),
                                              in0=T3[M, :].rearrange("p (c l) -> p c l", l=128),
                                              in1=bcast_last(mce[M, :], 128), op=ALU.subtract),
             reads=[bT3, bsm], writes=[bT3])
        P.op("act", lambda e: e.activation(out=T3[M, :], in_=T3[M, :], func=AF.Exp), reads=[bT3], writes=[bT3])
        P.op("dve", lambda e: e.tensor_tensor(out=T1[M, :].rearrange("p (c l) -> p c l", l=128),
                                              in0=T2[M, :].rearrange("p (c l) -> p c l", l=128),
                                              in1=bcast_last(mce[M, :], 128), op=ALU.subtract),
             reads=[bT2, bsm, bT1], writes=[bT1])
        P.op("act", lambda e: e.activation(out=T1[M, :], in_=T1[M, :], func=AF.Exp, scale=2.0), reads=[bT1], writes=[bT1])
        P.op("dve", lambda e: e.memset(mprev[M, :], 0.0), reads=[bsm], writes=[bsm])
        P.op("dve", lambda e: e.tensor_copy(out=mprev[M, 1:32], in_=mce[M, 0:31]), reads=[bsm], writes=[bsm])
        P.op("dve", lambda e: e.tensor_tensor(out=dec[M, :], in0=mprev[M, :], in1=mce[M, :], op=ALU.subtract),
             reads=[bsm], writes=[bsm])
        P.op("act", lambda e: e.activation(out=dec[M, :], in_=dec[M, :], func=AF.Exp), reads=[bsm], writes=[bsm])
        for c in range(32):
            P.op("pe", lambda e, c=c: e.transpose(out=ptm[:, c, 0:4], in_=T3[M, c * 128:(c + 1) * 128],
                                                  identity=C.identf[M, 0:4]),
                 reads=[bT3, C.b_const], writes=[b_ptm])
            P.op("pe", lambda e, c=c: e.transpose(out=ptm[:, c, 4:8], in_=T1[M, c * 128:(c + 1) * 128],
                                                  identity=C.identf[M, 0:4]),
                 reads=[bT1, C.b_const], writes=[b_ptm])
        P.op("dve", lambda e: e.tensor_copy(out=C.wthr[:], in_=ptm[:]), reads=[b_ptm], writes=[C.b_wthr])
        for h in range(4):
            P.op("dve", lambda e, h=h: e.tensor_copy(out=esel[M, h, :],
                                                     in_=C.identf[M, h:h + 1].to_broadcast([4, 128])),
                 reads=[C.b_const, bsm], writes=[bsm])
        for h in range(4):
            P.op("pe", lambda e, h=h: e.matmul(pdc[:, h, :], lhsT=esel[M, h, :], rhs=dec[M, :], start=True, stop=True),
                 reads=[bsm], writes=[b_pdc])
        P.op("dve", lambda e: e.tensor_copy(out=C.decbc[:], in_=pdc[:]), reads=[b_pdc], writes=[C.b_decbc])
        Fx = slice(64, 72)
        Fd = slice(64, 72)
        P.op("dve", lambda e: e.tensor_scalar(out=T0[Fx, :], in0=T2[Fx, :], scalar1=-1.0, scalar2=None, op0=ALU.mult),
             reads=[bT2, bT0], writes=[bT0])
        for part in range(3):
            P.op("dve", lambda e, part=part: e.tensor_copy(out=QR[Fx, part, :], in_=T0[Fx, :]),
                 reads=[bT0], writes=[bQR])
            if part < 2:
                P.op("dve", lambda e, part=part: e.tensor_tensor(out=T0[Fx, :], in0=T0[Fx, :], in1=QR[Fx, part, :],
                                                                 op=ALU.subtract), reads=[bT0, bQR], writes=[bT0])
        P.op("pool", lambda e: e.tensor_scalar(out=KR[Fx, :, :], in0=QR[Fx, :, :], scalar1=-1.0, scalar2=None,
                                               op0=ALU.mult), reads=[bQR], writes=[bKR])
        P.op("sp", lambda e: e.dma_start(out=qaug[:, 64:67, :], in_=QR[Fd, :, :]), reads=[bQR], writes=[aug_b], dma=True)
        P.op("sp", lambda e: e.dma_start(out=kaug[:, 67:70, :], in_=KR[Fd, :, :]), reads=[bKR], writes=[aug_b], dma=True)
        for r in range(3):
            P.op("sp", lambda e, r=r: e.dma_start(out=qaug[:, 67 + r, :], in_=ONE[Fd, :]), reads=[bONE],
                 writes=[aug_b], dma=True)
            P.op("sp", lambda e, r=r: e.dma_start(out=kaug[:, 64 + r, :], in_=ONE[Fd, :]), reads=[bONE],
                 writes=[aug_b], dma=True)
        P.flush()


def win_pass(P, nc, C, I, uT1, uT_b, SC, DB):
    w_in = I["w_in"]
    with contextlib.ExitStack() as st:
        sb = lambda name, shape, dt: st.enter_context(nc.sbuf_tensor("wi" + name, shape, dt))
        ps = lambda name, shape, dt: st.enter_context(nc.psum_tensor("wi" + name, shape, dt))
        W = sb("W", [128, 8, INW], BF16)
        b_W = P.bufs_n("Win", 8)
        wv = w_in.rearrange("(kc p) n -> kc p n", p=128)
        for k in range(8):
            P.op("pool", lambda e, k=k: e.dma_start(out=W[:, k, :], in_=wv[k], max_dma_last_dim=4 * 1284),
                 writes=[b_W[k]], dma=True)
        cw = sb("cw", [128, 4, 4], F32)
        cb = sb("cb", [128, 4], F32)
        gbias = sb("gbias", [128, 2048], F32)
        b_par = P.buf("wipar")
        for tap in range(4):
            P.op("sp", lambda e, tap=tap: e.dma_start(
                out=cw[:, :, tap], in_=I["conv_w"][tap:tap + 1, :].rearrange("o (c p) -> p (o c)", p=128),
                allow_slow_non_contiguous=True), writes=[b_par], dma=True)
        P.op("sp", lambda e: e.dma_start(out=cb[:], in_=I["conv_b"].rearrange("o (c p) -> p (o c)", p=128),
                                         allow_slow_non_contiguous=True), writes=[b_par], dma=True)
        P.op("sp", lambda e: e.dma_start(out=gbias[:], in_=I["branch_gate_bias"].partition_broadcast(128)),
             writes=[b_par], dma=True)
        uT = [sb("uT%d" % i, [128, 8, TT], BF16) for i in range(2)]
        b_uT = P.bufs_n("wiuT", 2)
        zq = sb("zq", [128, 4, 3 + TT], F32)
        b_zq = P.bufs_n("zq", 4)
        acc = [sb("acc%d" % i, [128, TT], F32) for i in range(2)]
        b_acc = P.bufs_n("acc", 2)
        fo = [sb("fo%d" % i, [128, TT], BF16) for i in range(3)]
        b_fo = P.bufs_n("fo", 3)
        tv = [sb("tv%d" % i, [128, 4, 129], BF16) for i in range(2)]
        b_tv = P.bufs_n("tv", 2)
        tf = [sb("tf%d" % i, [128, 8, 65], BF16) for i in range(2)]
        b_tf = P.bufs_n("tf", 2)
        tg = [sb("tg%d" % i, [128, 512], F32) for i in range(2)]
        b_tg = P.bufs_n("tg", 2)
        to = [sb("to%d" % i, [128, 512], BF16) for i in range(3)]
        b_to = P.bufs_n("to", 3)
        pf = [ps("pf%d" % i, [128, TT], F32) for i in range(2)]
        b_pf = P.bufs_n("pf", 2)
        pk = [ps("pk%d" % i, [128, 512], F32) for i in range(2)]
        b_pk = P.bufs_n("pk", 2)
        cnt = {"pf": 0, "pk": 0, "acc": 0, "fo": 0, "tv": 0, "tf": 0, "tg": 0, "to": 0}

        def rot(key, n):
            v = cnt[key] % n
            cnt[key] += 1
            return v

        for ch in range(4):
            P.op("dve", lambda e, ch=ch: e.memset(zq[:, ch, 0:3], 0.0), writes=[b_zq[ch]])
        for i in range(NT):
            ub = i % 2
            tcols = slice(i * TT, (i + 1) * TT)
            if i == 0:
                P.op("sp", lambda e: e.dma_start(out=uT[0][:], in_=uT1[:, :, 0:TT].rearrange("k p t -> p k t")),
                     reads=uT_b[0:4], writes=[b_uT[0]], dma=True)
            if i + 1 < NT:
                ncols = slice((i + 1) * TT, (i + 2) * TT)
                P.op("sp", lambda e, ub=ub, ncols=ncols: e.dma_start(
                    out=uT[1 - ub][:], in_=uT1[:, :, ncols].rearrange("k p t -> p k t")),
                    reads=uT_b[4 * i + 4:4 * i + 8], writes=[b_uT[1 - ub]], dma=True)
            fm = [("mqk", ch, ch * 128) for ch in range(4)] + \
                 [("fq", ch, 1544 + ch * 128) for ch in range(4)] + \
                 [("fk", ch, 2056 + ch * 128) for ch in range(4)]
            for (kind, ch, c0) in fm:
                j = rot("pf", 2)
                for k in range(8):
                    P.op("pe", lambda e, k=k, c0=c0, j=j, ub=ub: e.matmul(
                        pf[j][:], lhsT=W[:, k, c0:c0 + 128], rhs=uT[ub][:, k, :], start=(k == 0), stop=(k == 7)),
                        reads=[b_W[k], b_uT[ub]], writes=[b_pf[j]])
                if kind == "mqk":
                    P.op("act", lambda e, ch=ch, j=j: e.activation(out=zq[:, ch, 3:3 + TT], in_=pf[j][:], func=AF.Copy),
                         reads=[b_pf[j]], writes=[b_zq[ch]])
                    a = rot("acc", 2)
                    P.op("dve", lambda e, ch=ch, a=a: e.tensor_scalar(
                        out=acc[a][:], in0=zq[:, ch, 0:TT], scalar1=cw[:, ch, 0:1], scalar2=cb[:, ch:ch + 1],
                        op0=ALU.mult, op1=ALU.add), reads=[b_zq[ch], b_par], writes=[b_acc[a]])
                    for tap in range(1, 4):
                        P.op("dve", lambda e, ch=ch, a=a, tap=tap: e.scalar_tensor_tensor(
                            out=acc[a][:], in0=zq[:, ch, tap:tap + TT], scalar=cw[:, ch, tap:tap + 1], in1=acc[a][:],
                            op0=ALU.mult, op1=ALU.add), reads=[b_zq[ch], b_par, b_acc[a]], writes=[b_acc[a]])
                    P.op("dve", lambda e, ch=ch: e.tensor_copy(out=zq[:, ch, 0:3], in_=zq[:, ch, TT:TT + 3]),
                         reads=[b_zq[ch]], writes=[b_zq[ch]])
                    o = rot("fo", 3)
                    P.op("act", lambda e, a=a, o=o: e.activation(out=fo[o][:], in_=acc[a][:], func=AF.Silu),
                         reads=[b_acc[a]], writes=[b_fo[o]])
                    P.op("sp", lambda e, o=o, ch=ch, tcols=tcols: e.dma_start(out=SC["mqkT"][ch, :, tcols], in_=fo[o][:]),
                         reads=[b_fo[o]], writes=[DB("mqkT")[i]], dma=True)
                else:
                    o = rot("fo", 3)
                    sc = 0.125 if kind == "fq" else 1.0
                    P.op("act", lambda e, o=o, j=j, sc=sc: e.activation(out=fo[o][:], in_=pf[j][:], func=AF.Copy, scale=sc),
                         reads=[b_pf[j]], writes=[b_fo[o]])
                    dst = SC["qaug"] if kind == "fq" else SC["kaug"]
                    for hh in range(2):
                        P.op("sp", lambda e, o=o, ch=ch, dst=dst, tcols=tcols, hh=hh: e.dma_start(
                            out=dst[2 * ch + hh, 0:64, tcols], in_=fo[o][hh * 64:(hh + 1) * 64, :]),
                            reads=[b_fo[o]], writes=[DB("aug")[i]], dma=True)
            for s in range(4):
                n = i * 4 + s
                rows = slice(n * 128, (n + 1) * 128)
                groups = [("mv", 512), ("mo", 1024), ("fv", 2568), ("ga", 3088), ("ga", 3600), ("gb", 4112), ("gb", 4624)]
                for gi, (kind, c0) in enumerate(groups):
                    j = rot("pk", 2)
                    for k in range(8):
                        P.op("pe", lambda e, k=k, c0=c0, j=j, ub=ub, s=s: e.matmul(
                            pk[j][:], lhsT=uT[ub][:, k, s * 128:(s + 1) * 128], rhs=W[:, k, c0:c0 + 512],
                            start=(k == 0), stop=(k == 7)),
                            reads=[b_W[k], b_uT[ub]], writes=[b_pk[j]])
                    if kind == "mv":
                        t = rot("tv", 2)
                        c = n
                        P.op("dve", lambda e, t=t, j=j, c=c: e.tensor_tensor(
                            out=tv[t][:, :, 0:128], in0=pk[j][:].rearrange("p (h d) -> p h d", h=4),
                            in1=bcast_last(C.wthr[:, c, 0:4], 128), op=ALU.mult),
                            reads=[b_pk[j], C.b_wthr], writes=[b_tv[t]])
                        P.op("dve", lambda e, t=t, c=c: e.tensor_copy(out=tv[t][:, :, 128:129],
                                                                     in_=C.wthr[:, c, 0:4].unsqueeze(2)),
                             reads=[C.b_wthr, b_tv[t]], writes=[b_tv[t]])
                        P.op("sp", lambda e, t=t, rows=rows: e.dma_start(out=SC["vaugM"][rows], in_=tv[t][:]),
                             reads=[b_tv[t]], writes=[DB("vaugM")[n]], dma=True)
                    elif kind == "fv":
                        t = rot("tf", 2)
                        P.op("act", lambda e, t=t, j=j: e.activation(
                            out=tf[t][:, :, 1:65], in_=pk[j][:].rearrange("p (h d) -> p h d", h=8), func=AF.Copy),
                            reads=[b_pk[j]], writes=[b_tf[t]])
                        P.op("dve", lambda e, t=t: e.memset(tf[t][:, :, 0:1], 1.0), reads=[b_tf[t]], writes=[b_tf[t]])
                        P.op("sp", lambda e, t=t, rows=rows: e.dma_start(out=SC["vaugF"][rows], in_=tf[t][:]),
                             reads=[b_tf[t]], writes=[DB("vaugF")[n]], dma=True)
                    elif kind == "mo":
                        o = rot("to", 3)
                        P.op("act", lambda e, o=o, j=j: e.activation(out=to[o][:], in_=pk[j][:], func=AF.Sigmoid),
                             reads=[b_pk[j]], writes=[b_to[o]])
                        P.op("sp", lambda e, o=o, rows=rows: e.dma_start(out=SC["so"][rows], in_=to[o][:]),
                             reads=[b_to[o]], writes=[DB("so")[n]], dma=True)
                    else:
                        g = rot("tg", 2)
                        boff = c0 - 3088
                        P.op("dve", lambda e, g=g, j=j, boff=boff: e.tensor_tensor(
                            out=tg[g][:], in0=pk[j][:], in1=gbias[:, boff:boff + 512], op=ALU.add),
                            reads=[b_pk[j], b_par], writes=[b_tg[g]])
                        o = rot("to", 3)
                        P.op("act", lambda e, o=o, g=g: e.activation(out=to[o][:], in_=tg[g][:], func=AF.Sigmoid),
                             reads=[b_tg[g]], writes=[b_to[o]])
                        dcol = boff % 1024
                        dst = SC["ga"] if kind == "ga" else SC["gb"]
                        P.op("sp", lambda e, o=o, rows=rows, dst=dst, dcol=dcol: e.dma_start(
                            out=dst[rows, dcol:dcol + 512], in_=to[o][:]),
                            reads=[b_to[o]], writes=[DB("gab")[n]], dma=True)
        P.flush()


def mix_pass(P, nc, C, I, SC, DB):
    with contextlib.ExitStack() as st:
        sb = lambda name, shape, dt: st.enter_context(nc.sbuf_tensor("mx" + name, shape, dt))
        ps = lambda name, shape, dt: st.enter_context(nc.psum_tensor("mx" + name, shape, dt))
        mask01 = sb("mask01", [128, 128], F32)
        trim = sb("trim", [128, 128], BF16)
        trimf = sb("trimf", [128, 128], F32)
        onesr = sb("onesr", [1, 65], F32)
        gln = sb("gln", [128, 512], F32)
        b_c = P.buf("mxconst")
        P.op("pool", lambda e: e.memset(mask01[:], 1.0), writes=[b_c])
        P.op("pool", lambda e: e.affine_select(out=mask01[:], in_=mask01[:], pattern=[[1, 128]], compare_op=ALU.is_ge,
                                                 fill=0.0, base=0, channel_multiplier=-1), reads=[b_c], writes=[b_c])
        P.op("pool", lambda e: e.memset(trimf[:], 0.0), reads=[b_c], writes=[b_c])
        P.op("pool", lambda e: e.affine_select(out=trimf[:], in_=trimf[:], pattern=[[1, 128]], compare_op=ALU.is_ge,
                                                 fill=-30000.0, base=0, channel_multiplier=-1), reads=[b_c], writes=[b_c])
        P.op("dve", lambda e: e.tensor_copy(out=trim[:], in_=trimf[:]), reads=[b_c], writes=[b_c])
        P.op("dve", lambda e: e.memset(onesr[:], 1.0), reads=[b_c], writes=[b_c])
        P.op("sp", lambda e: e.dma_start(out=gln[:], in_=I["mlstm_norm_g"].partition_broadcast(128)),
             reads=[b_c], writes=[b_c], dma=True)
        mqz = [sb("mqz%d" % i, [128, S], BF16) for i in range(4)]
        mk = [sb("mk%d" % i, [128, S], BF16) for i in range(2)]
        b_mqk = P.buf("mqk")
        for h in range(4):
            P.op("pool", lambda e, h=h: e.memset(mqz[h][:], 0.0), writes=[b_mqk])
        for h in range(4):
            R = slice((h % 2) * 64, (h % 2) * 64 + 64)
            P.op("sp", lambda e, h=h, R=R: e.dma_start(out=mqz[h][R, :], in_=SC["mqkT"][h // 2, R, :]), reads=DB("mqkT"),
                 writes=[b_mqk], dma=True)
        for hp in range(2):
            P.op("sp", lambda e, hp=hp: e.dma_start(out=mk[hp][:], in_=SC["mqkT"][2 + hp]), reads=DB("mqkT"),
                 writes=[b_mqk], dma=True)
        Cst = [sb("Cst%d" % i, [128, 129], F32) for i in range(2)]
        Cb = [sb("Cb%d" % i, [128, 129], BF16) for i in range(2)]
        b_Cst = P.bufs_n("Cst", 2)
        b_Cb = P.bufs_n("Cb", 2)
        va = [sb("va%d" % i, [128, 4, 129], BF16) for i in range(2)]
        b_va = P.bufs_n("va", 2)
        sgo = [sb("sgo%d" % i, [128, 512], BF16) for i in range(2)]
        b_sgo = P.bufs_n("sgo", 2)
        Sm = [sb("Sm%d" % i, [128, 2, 128], BF16) for i in range(2)]
        b_Sm = P.bufs_n("Sm", 2)
        ktm = [sb("ktm%d" % i, [128, 128], BF16) for i in range(2)]
        b_ktm = P.bufs_n("ktm", 2)
        bst = [sb("bst%d" % i, [128, 2, 6], F32) for i in range(2)]
        bag = [sb("bag%d" % i, [128, 2, 2], F32) for i in range(2)]
        sm = [sb("sm%d" % i, [128, 2, 4], F32) for i in range(2)]
        b_sm = P.bufs_n("msm", 2)
        sq = [sb("sq%d" % i, [128, 2, 1], F32) for i in range(2)]
        b_sq = P.bufs_n("msq", 2)
        hn = [sb("hn%d" % i, [128, 512], F32) for i in range(2)]
        b_hn = P.bufs_n("hn", 2)
        ya = [sb("ya%d" % i, [128, 512], BF16) for i in range(2)]
        b_ya = P.bufs_n("ya", 2)
        yaT = [sb("yaT%d" % i, [128, 4, 128], BF16) for i in range(2)]
        b_yaT = P.bufs_n("yaTs", 2)
        pSm = ps("pSm", [128, 2, 128], F32)
        pOm = ps("pOm", [128, 2, 129], F32)
        pU = ps("pU", [128, 2, 129], F32)
        pT5 = ps("pT5", [128, 5, 128], BF16)
        pkt = pT5[:, 4, :]
        pyT = pT5[:, 0:4, :]
        b_pSm, b_pOm, b_pU, b_pkt, b_pyT = [P.buf(n) for n in ("pSm", "pOm", "pU", "pkt", "pyT")]

        def mlstm_chunk(c):
            cols = slice(c * 128, (c + 1) * 128)
            rows = slice(c * 128, (c + 1) * 128)
            vi = c % 2
            P.op("sp", lambda e: e.dma_start(out=va[vi][:], in_=SC["vaugM"][rows]), reads=[DB("vaugM")[c]],
                 writes=[b_va[vi]], dma=True)
            P.op("sp", lambda e: e.dma_start(out=sgo[vi][:], in_=SC["so"][rows]), reads=[DB("so")[c]],
                 writes=[b_sgo[vi]], dma=True)
            hnb = hn[vi]
            for hp in range(2):
                si = hp
                for hh in range(2):
                    P.op("pe", lambda e, hp=hp, hh=hh: e.matmul(
                        pSm[:, hh, :], lhsT=mk[hp][:, cols], rhs=mqz[2 * hp + hh][:, cols], start=True, stop=True),
                        reads=[b_mqk], writes=[b_pSm])
                P.op("pe", lambda e, hp=hp: e.transpose(out=pkt, in_=mk[hp][:, cols], identity=C.ident[:]),
                     reads=[b_mqk, C.b_const], writes=[b_pkt])
                P.op("dve", lambda e, si=si: e.scalar_tensor_tensor(
                    out=Sm[si][:], in0=pSm[:], scalar=0.125,
                    in1=mask01[:].unsqueeze(1).to_broadcast([128, 2, 128]), op0=ALU.mult, op1=ALU.mult),
                    reads=[b_pSm, b_c], writes=[b_Sm[si]])
                P.op("act", lambda e, si=si: e.activation(out=ktm[si][:], in_=pkt, func=AF.Copy, scale=0.125),
                     reads=[b_pkt], writes=[b_ktm[si]])
                yield
                for hh in range(2):
                    h = 2 * hp + hh
                    P.op("pe", lambda e, si=si, hh=hh, h=h: e.matmul(
                        pOm[:, hh, :], lhsT=Sm[si][:, hh, :], rhs=va[vi][:, h, :], start=True, stop=(c == 0)),
                        reads=[b_Sm[si], b_va[vi]], writes=[b_pOm])
                    if c > 0:
                        P.op("pe", lambda e, hp=hp, hh=hh, h=h: e.matmul(
                            pOm[:, hh, :], lhsT=mqz[h][:, cols], rhs=Cb[hp][:, :], start=False, stop=True),
                            reads=[b_mqk, b_Cb[hp]], writes=[b_pOm])
                for hh in range(2):
                    h = 2 * hp + hh
                    P.op("pe", lambda e, si=si, hh=hh, h=h: e.matmul(
                        pU[:, hh, :], lhsT=ktm[si][:], rhs=va[vi][:, h, :], start=True, stop=True),
                        reads=[b_ktm[si], b_va[vi]], writes=[b_pU])
                for hh in range(2):
                    h = 2 * hp + hh
                    R = slice(hh * 64, (hh + 1) * 64)
                    if c == 0:
                        P.op("dve", lambda e, hp=hp, hh=hh, R=R: e.tensor_copy(out=Cst[hp][R, :], in_=pU[R, hh, :]),
                             reads=[b_pU], writes=[b_Cst[hp]])
                    else:
                        P.op("dve", lambda e, hp=hp, hh=hh, R=R, h=h: e.scalar_tensor_tensor(
                            out=Cst[hp][R, :], in0=Cst[hp][R, :], scalar=C.decbc[R, h, c:c + 1], in1=pU[R, hh, :],
                            op0=ALU.mult, op1=ALU.add), reads=[b_pU, b_Cst[hp], C.b_decbc], writes=[b_Cst[hp]])
                    if c < 31:
                        P.op("dve", lambda e, hp=hp, R=R, h=h: e.tensor_scalar(
                            out=Cb[hp][R, :], in0=Cst[hp][R, :], scalar1=C.decbc[R, h, c + 1:c + 2], scalar2=None,
                            op0=ALU.mult), reads=[b_Cst[hp], C.b_decbc], writes=[b_Cb[hp]])
                smp, bstp, bagp, bsm = sm[hp], bst[hp], bag[hp], b_sm[hp]
                for hh in range(2):
                    P.op("dve", lambda e, hh=hh, bstp=bstp: e.bn_stats(out=bstp[:, hh, :], in_=pOm[:, hh, 0:128]),
                         reads=[b_pOm], writes=[bsm])
                    P.op("dve", lambda e, hh=hh, bstp=bstp, bagp=bagp: e.bn_aggr(out=bagp[:, hh, :], in_=bstp[:, hh, :]),
                         reads=[bsm], writes=[bsm])
                sqp, bsq = sq[hp], b_sq[hp]
                P.op("act", lambda e, sqp=sqp: e.activation(out=sqp[:], in_=pOm[:, :, 128:129], func=AF.Square),
                     reads=[b_pOm], writes=[bsq])
                P.op("dve", lambda e, hp=hp, smp=smp, sqp=sqp: e.tensor_tensor(
                    out=smp[:, :, 0:1], in0=sqp[:], in1=C.wthr[:, c, 4 + 2 * hp:6 + 2 * hp].unsqueeze(2),
                    op=ALU.max), reads=[C.b_wthr, bsm, bsq], writes=[bsm])
                P.op("dve", lambda e, smp=smp, bagp=bagp: e.scalar_tensor_tensor(
                    out=smp[:, :, 1:2], in0=smp[:, :, 0:1], scalar=EPS, in1=bagp[:, :, 1:2], op0=ALU.mult, op1=ALU.add),
                    reads=[bsm], writes=[bsm])
                P.op("pool", lambda e, smp=smp: e.tensor_tensor(
                    out=smp[:, :, 2:3], in0=smp[:, :, 1:2],
                    in1=C.neghalf[:].unsqueeze(1).to_broadcast([128, 2, 1]), op=ALU.pow),
                    reads=[bsm, C.b_const], writes=[bsm])
                for hh in range(2):
                    h = 2 * hp + hh
                    P.op("dve", lambda e, hh=hh, h=h, smp=smp, bagp=bagp: e.tensor_scalar(
                        out=hnb[:, h * 128:(h + 1) * 128], in0=pOm[:, hh, 0:128], scalar1=bagp[:, hh, 0:1],
                        scalar2=smp[:, hh, 2:3], op0=ALU.subtract, op1=ALU.mult),
                        reads=[b_pOm, bsm], writes=[b_hn[vi]])
                yield
            yi = c % 2
            P.op("pool", lambda e: e.tensor_tensor(out=hnb[:], in0=hnb[:], in1=gln[:], op=ALU.mult),
                 reads=[b_hn[vi], b_c], writes=[b_hn[vi]])
            P.op("dve", lambda e: e.tensor_tensor(out=ya[yi][:], in0=hnb[:], in1=sgo[vi][:], op=ALU.mult),
                 reads=[b_hn[vi], b_sgo[vi]], writes=[b_ya[yi]])
            yield
            for k in range(4):
                P.op("pe", lambda e, k=k: e.transpose(out=pyT[:, k, :], in_=ya[yi][:, k * 128:(k + 1) * 128],
                                                      identity=C.ident[:]),
                     reads=[b_ya[yi], C.b_const], writes=[b_pyT])
            P.op("act", lambda e: e.activation(out=yaT[yi][:], in_=pyT, func=AF.Copy), reads=[b_pyT],
                 writes=[b_yaT[yi]])
            P.op("sp", lambda e: e.dma_start(out=SC["yaT"][:, :, cols].rearrange("k p t -> p k t"), in_=yaT[yi][:]),
                 reads=[b_yaT[yi]], writes=[DB("yaT")[c]], dma=True)
            yield

        def mlstm_gen():
            for c in range(32):
                yield from mlstm_chunk(c)

        VF = sb("VF", [128, 32, 8 * 65], BF16)
        b_VF = P.buf("VF")
        P.op("sp", lambda e: e.dma_start(out=VF[:], in_=SC["vaugF"].rearrange("(j p) h e -> p j (h e)", p=128)),
             reads=DB("vaugF"), writes=[b_VF], dma=True)
        QA = [sb("QA%d" % i, [70, S], BF16) for i in range(2)]
        KA = [sb("KA%d" % i, [70, S], BF16) for i in range(2)]
        b_QA = P.bufs_n("QA", 2)
        b_KA = P.bufs_n("KA", 2)
        PT = [sb("PT%d" % i, [128, 512], BF16) for i in range(3)]
        b_PT = P.bufs_n("PT", 3)
        rec = [sb("rec%d" % i, [1, 512], F32) for i in range(2)]
        b_rec = P.bufs_n("rec", 2)
        b_recd = P.bufs_n("recd", 2)
        osb = [sb("osb%d" % i, [65, 512], F32) for i in range(2)]
        b_osb = P.bufs_n("osb", 2)
        bcs = [sb("bcs%d" % i, [65, 512], F32) for i in range(2)]
        b_bcs = P.bufs_n("bcs", 2)
        ybt = [sb("ybt%d" % i, [65, 512], BF16) for i in range(2)]
        b_ybt = P.bufs_n("ybt", 2)
        pS = [ps("pS%d" % i, [128, 512], F32) for i in range(3)]
        b_pS = P.bufs_n("pS", 3)
        slot_of = {}
        pO = ps("pO", [128, 512], F32)
        b_pO = P.buf("pO")
        cnt = {"s": 0, "y": 0}

        def fox_load(h):
            hb = h % 2
            P.op("sp", lambda e: e.dma_start(out=QA[hb][:], in_=SC["qaug"][h]), reads=DB("aug"), writes=[b_QA[hb]], dma=True)
            P.op("sp", lambda e: e.dma_start(out=KA[hb][:], in_=SC["kaug"][h]), reads=DB("aug"), writes=[b_KA[hb]], dma=True)

        seq = [(h, i, j) for h in range(8) for i in range(8) for j in range(4 * i + 4)]

        def emit_S(idx):
            h, i, j = seq[idx]
            hb = h % 2
            sj = cnt["s"] % 3
            cnt["s"] += 1
            slot_of[idx] = sj
            jj = j - 4 * i
            kc = slice(j * 128, (j + 1) * 128)
            rd = [b_KA[hb], b_QA[hb]]
            if jj < 0:
                P.op("pe", lambda e: e.matmul(
                    pS[sj][:, 0:512], lhsT=KA[hb][:, kc], rhs=QA[hb][:, i * 512:(i + 1) * 512], start=True, stop=True),
                    reads=rd, writes=[b_pS[sj]])
            else:
                qs = jj * 128
                wq = 512 - qs
                q0 = i * 512 + qs
                P.op("pe", lambda e: e.matmul(pS[sj][:, 0:128], lhsT=C.ident[:], rhs=trim[:], start=True, stop=False),
                     reads=[C.b_const, b_c], writes=[b_pS[sj]])
                P.op("pe", lambda e: e.matmul(
                    pS[sj][:, 0:128], lhsT=KA[hb][:, kc], rhs=QA[hb][:, q0:q0 + 128], start=False, stop=True),
                    reads=rd, writes=[b_pS[sj]])
                if wq > 128:
                    P.op("pe", lambda e: e.matmul(
                        pS[sj][:, 128:wq], lhsT=KA[hb][:, kc], rhs=QA[hb][:, q0 + 128:q0 + wq], start=True, stop=True),
                        reads=rd, writes=[b_pS[sj]])

        def emit_rest(idx):
            h, i, j = seq[idx]
            sj = slot_of[idx]
            nkb = 4 * i + 4
            jj = j - 4 * i
            qs = max(jj, 0) * 128
            wq = 512 - qs
            tj = idx % 3
            P.op("act", lambda e: e.activation(out=PT[tj][:, 0:wq], in_=pS[sj][:, 0:wq], func=AF.Exp),
                 reads=[b_pS[sj]], writes=[b_PT[tj]])
            P.op("pe", lambda e: e.matmul(
                pO[0:65, qs:512], lhsT=VF[:, j, h * 65:(h + 1) * 65], rhs=PT[tj][:, 0:wq],
                start=(j == 0), stop=(j == nkb - 1)),
                reads=[b_VF, b_PT[tj]], writes=[b_pO])
            if j < nkb - 1:
                return
            yi = cnt["y"] % 2
            cnt["y"] += 1
            u = h * 8 + i
            P.op("act", lambda e: e.activation(out=osb[yi][:], in_=pO[0:65, :], func=AF.Copy), reads=[b_pO],
                 writes=[b_osb[yi]])
            P.op("dve", lambda e: e.reciprocal(out=rec[yi][0:1, :], in_=osb[yi][0:1, :]), reads=[b_osb[yi]],
                 writes=[b_rec[yi]])
            P.op("sp", lambda e: e.dma_start(out=SC["recd"][u:u + 1, :], in_=rec[yi][0:1, :]), reads=[b_rec[yi]],
                 writes=[b_recd[yi]], dma=True)
            P.op("sp", lambda e: e.dma_start(out=bcs[yi][:], in_=SC["recd"][u:u + 1, :].partition_broadcast(65)),
                 reads=[b_recd[yi]], writes=[b_bcs[yi]], dma=True)
            P.op("dve", lambda e: e.tensor_tensor(out=ybt[yi][:], in0=osb[yi][:], in1=bcs[yi][:], op=ALU.mult),
                 reads=[b_osb[yi], b_bcs[yi]], writes=[b_ybt[yi]])
            P.op("sp", lambda e: e.dma_start(
                out=SC["ybT"][h // 2, (h % 2) * 64:(h % 2) * 64 + 64, i * 512:(i + 1) * 512], in_=ybt[yi][1:65, :]),
                reads=[b_ybt[yi]], writes=[DB("ybT")[(h * 8 + i) % 32]], dma=True)

        gen = mlstm_gen()
        fox_load(0)
        emit_S(0)
        emit_S(1)
        for idx, (h, i, j) in enumerate(seq):
            if i == 0 and j == 0 and h + 1 < 8:
                fox_load(h + 1)
            if idx + 2 < len(seq):
                emit_S(idx + 2)
            emit_rest(idx)
            if idx % 6 == 5:
                next(gen, None)
        for _ in gen:
            pass
        P.flush()


def merge_pass(P, nc, C, I, SC, DB, h1, h1_b, h2, h2_b):
    with contextlib.ExitStack() as st:
        sb = lambda name, shape, dt: st.enter_context(nc.sbuf_tensor("mg" + name, shape, dt))
        ps = lambda name, shape, dt: st.enter_context(nc.psum_tensor("mg" + name, shape, dt))
        Wa = sb("Wa", [128, 4, D], BF16)
        Wb = sb("Wb", [128, 4, D], BF16)
        Wo = sb("Wo", [128, 8, D], BF16)
        b_W = P.buf("mgW")
        P.op("pool", lambda e: e.dma_start(out=Wa[:], in_=I["w_branch_a"].rearrange("(k p) d -> p k d", p=128)),
             writes=[b_W], dma=True)
        P.op("pool", lambda e: e.dma_start(out=Wb[:], in_=I["w_branch_b"].rearrange("(k p) d -> p k d", p=128)),
             writes=[b_W], dma=True)
        P.op("pool", lambda e: e.dma_start(out=Wo[:], in_=I["w_out"].rearrange("(k p) d -> p k d", p=128)),
             writes=[b_W], dma=True)
        gpost = sb("gpost", [128, D], F32)
        P.op("sp", lambda e: e.dma_start(out=gpost[:], in_=I["mix_post_g"].partition_broadcast(128)),
             writes=[b_W], dma=True)
        yaT = [sb("yaT%d" % i, [128, 4, 128], BF16) for i in range(2)]
        ybT = [sb("ybT%d" % i, [128, 4, 128], BF16) for i in range(2)]
        gab = [sb("gab%d" % i, [128, 2, D], BF16) for i in range(2)]
        hin = [sb("hin%d" % i, [128, D], F32) for i in range(2)]
        b_in = P.bufs_n("mgin", 2)
        b_hin = P.bufs_n("mghin", 2)
        t1 = [sb("t1%d" % i, [128, D], F32) for i in range(2)]
        t2 = [sb("t2%d" % i, [128, D], F32) for i in range(2)]
        mb = [sb("mb%d" % i, [128, D], BF16) for i in range(2)]
        mT = [sb("mT%d" % i, [128, 8, 128], BF16) for i in range(2)]
        junk = sb("junk", [128, D], BF16)
        hout = [sb("hout%d" % i, [128, D], F32) for i in range(2)]
        stat = sb("stat", [128, 6], F32)
        b_t1, b_t2, b_mb, b_mT = [P.bufs_n(n, 2) for n in ("t1", "t2", "mb", "mT")]
        b_junk = P.buf("mgjunk")
        b_hout = P.bufs_n("hout", 2)
        b_stat = P.bufs_n("mgstat", 2)
        pA = ps("pA", [128, D], F32)
        pB = ps("pB", [128, D], F32)
        pO = ps("pO", [128, D], F32)
        pt = ps("pt", [128, 8, 128], BF16)
        b_pA, b_pB, b_pO, b_pt = [P.buf(n) for n in ("pA", "pB", "mgpO", "mgpt")]
        h1v = h1.rearrange("(n p) d -> n p d", p=128)
        h2v = h2.rearrange("(n p) d -> n p d", p=128)

        def s1(n):
            ib = n % 2
            rows = slice(n * 128, (n + 1) * 128)
            cols = rows
            P.op("sp", lambda e: e.dma_start(out=yaT[ib][:], in_=SC["yaT"][:, :, cols].rearrange("k p t -> p k t")),
                 reads=[DB("yaT")[n]], writes=[b_in[ib]], dma=True)
            P.op("sp", lambda e: e.dma_start(out=ybT[ib][:], in_=SC["ybT"][:, :, cols].rearrange("k p t -> p k t")),
                 reads=DB("ybT"), writes=[b_in[ib]], dma=True)
            P.op("sp", lambda e: e.dma_start(out=gab[ib][:, 0, :], in_=SC["ga"][rows]),
                 reads=[DB("gab")[n]], writes=[b_in[ib]], dma=True)
            P.op("sp", lambda e: e.dma_start(out=gab[ib][:, 1, :], in_=SC["gb"][rows]),
                 reads=[DB("gab")[n]], writes=[b_in[ib]], dma=True)
            P.op("sp", lambda e: e.dma_start(out=hin[ib][:], in_=h1v[n]),
                 reads=[h1_b[n]], writes=[b_hin[ib]], dma=True)
            for hf in range(2):
                hs = slice(hf * 512, (hf + 1) * 512)
                for k in range(4):
                    P.op("pe", lambda e, k=k, hs=hs: e.matmul(pA[:, hs], lhsT=yaT[ib][:, k, :], rhs=Wa[:, k, hs],
                                                              start=(k == 0), stop=(k == 3)),
                         reads=[b_in[ib], b_W], writes=[b_pA])
            for hf in range(2):
                hs = slice(hf * 512, (hf + 1) * 512)
                for k in range(4):
                    P.op("pe", lambda e, k=k, hs=hs: e.matmul(pB[:, hs], lhsT=ybT[ib][:, k, :], rhs=Wb[:, k, hs],
                                                              start=(k == 0), stop=(k == 3)),
                         reads=[b_in[ib], b_W], writes=[b_pB])
            P.op("dve", lambda e: e.tensor_tensor(out=t1[ib][:], in0=pA[:], in1=gab[ib][:, 0, :], op=ALU.mult),
                 reads=[b_pA, b_in[ib]], writes=[b_t1[ib]])
            P.op("dve", lambda e: e.tensor_tensor(out=t2[ib][:], in0=pB[:], in1=gab[ib][:, 1, :], op=ALU.mult),
                 reads=[b_pB, b_in[ib]], writes=[b_t2[ib]])
            P.op("pool", lambda e: e.tensor_tensor(out=mb[ib][:], in0=t1[ib][:], in1=t2[ib][:], op=ALU.add),
                 reads=[b_t1[ib], b_t2[ib]], writes=[b_mb[ib]])

        def s2(n):
            ib = n % 2
            for k in range(8):
                P.op("pe", lambda e, k=k: e.transpose(out=pt[:, k, :], in_=mb[ib][:, k * 128:(k + 1) * 128],
                                                      identity=C.ident[:]),
                     reads=[b_mb[ib], C.b_const], writes=[b_pt])
            P.op("act", lambda e: e.activation(out=mT[ib][:], in_=pt[:], func=AF.Copy), reads=[b_pt], writes=[b_mT[ib]])
            for hf in range(2):
                hs = slice(hf * 512, (hf + 1) * 512)
                for k in range(8):
                    P.op("pe", lambda e, k=k, hs=hs: e.matmul(pO[:, hs], lhsT=mT[ib][:, k, :], rhs=Wo[:, k, hs],
                                                              start=(k == 0), stop=(k == 7)),
                         reads=[b_mT[ib], b_W], writes=[b_pO])
            si = n % 2
            ss, var, rstd = (stat[:, 3 * si + j:3 * si + j + 1] for j in range(3))
            rms_stats(P, C, pO[:], b_pO, junk[:], b_junk, ss, var, rstd, b_stat[si])
            P.op("dve", lambda e: e.scalar_tensor_tensor(
                out=hout[ib][:], in0=pO[:], scalar=rstd, in1=gpost[:], op0=ALU.mult, op1=ALU.mult),
                reads=[b_pO, b_stat[si], b_W], writes=[b_hout[ib]])
            P.op("pool", lambda e: e.tensor_tensor(out=hout[ib][:], in0=hout[ib][:], in1=hin[ib][:], op=ALU.add),
                 reads=[b_hout[ib], b_hin[ib]], writes=[b_hout[ib]])
            P.op("pool", lambda e: e.dma_start(out=h2v[n], in_=hout[ib][:]),
                 reads=[b_hout[ib]], writes=[h2_b[n]], dma=True)

        s1(0)
        for n in range(32):
            if n + 1 < 32:
                s1(n + 1)
            s2(n)
        P.flush()


def ple_pass(P, nc, C, I, uT3, uT3_b, h3, h3_b, out):
    with contextlib.ExitStack() as st:
        sb = lambda name, shape, dt: st.enter_context(nc.sbuf_tensor("pl" + name, shape, dt))
        ps = lambda name, shape, dt: st.enter_context(nc.psum_tensor("pl" + name, shape, dt))
        Wg = sb("Wg", [128, 8, D], BF16)
        Wp = sb("Wp", [128, 2, D], BF16)
        b_W = P.buf("plW")
        P.op("pool", lambda e: e.dma_start(out=Wg[:], in_=I["ple_w_gate"].rearrange("(k p) d -> p k d", p=128)),
             writes=[b_W], dma=True)
        P.op("pool", lambda e: e.dma_start(out=Wp[:], in_=I["ple_w_proj"].rearrange("(k p) d -> p k d", p=128)),
             writes=[b_W], dma=True)
        gpost = sb("gpost", [128, D], F32)
        bg = sb("bg", [128, D], F32)
        P.op("sp", lambda e: e.dma_start(out=gpost[:], in_=I["ple_post_g"].partition_broadcast(128)),
             writes=[b_W], dma=True)
        P.op("sp", lambda e: e.dma_start(out=bg[:], in_=I["ple_b_gate"].partition_broadcast(128)),
             writes=[b_W], dma=True)
        uT = [sb("uT%d" % i, [128, 8, 128], BF16) for i in range(2)]
        pin = [sb("pin%d" % i, [128, 256], F32) for i in range(2)]
        hin = [sb("hin%d" % i, [128, D], F32) for i in range(2)]
        b_in = P.bufs_n("plin", 2)
        b_hin = P.bufs_n("plhin", 2)
        pb = [sb("pb%d" % i, [128, 256], BF16) for i in range(2)]
        pT = [sb("pT%d" % i, [128, 2, 128], BF16) for i in range(2)]
        gt = [sb("gt%d" % i, [128, D], F32) for i in range(2)]
        ge = [sb("ge%d" % i, [128, D], F32) for i in range(2)]
        junk = sb("junk", [128, D], BF16)
        hout = [sb("hout%d" % i, [128, D], F32) for i in range(2)]
        stat = sb("stat", [128, 6], F32)
        b_pb, b_pT, b_gt, b_ge = [P.bufs_n(n, 2) for n in ("pb", "pT", "gt", "ge")]
        b_junk = P.buf("pljunk")
        b_hout = P.bufs_n("plhout", 2)
        b_stat = P.bufs_n("plstat", 2)
        pG = [ps("pG%d" % i, [128, D], F32) for i in range(2)]
        pE = ps("pE", [128, D], F32)
        ptp = ps("ptp", [128, 2, 128], BF16)
        b_pG = P.bufs_n("pG", 2)
        b_pE, b_ptp = P.buf("pE"), P.buf("ptp")
        pv = I["p"].rearrange("(n p) d -> n p d", p=128)
        h3v = h3.rearrange("(n p) d -> n p d", p=128)
        ov = out.rearrange("(n p) d -> n p d", p=128)

        def s1(n):
            ib = n % 2
            cols = slice(n * 128, (n + 1) * 128)
            P.op("sp", lambda e: e.dma_start(out=uT[ib][:], in_=uT3[:, :, cols].rearrange("k p t -> p k t")),
                 reads=[uT3_b[n]], writes=[b_in[ib]], dma=True)
            P.op("sp", lambda e: e.dma_start(out=pin[ib][:], in_=pv[n]), writes=[b_in[ib]], dma=True)
            P.op("sp", lambda e: e.dma_start(out=hin[ib][:], in_=h3v[n]), reads=[h3_b[n]],
                 writes=[b_hin[ib]], dma=True)
            P.op("act", lambda e: e.activation(out=pb[ib][:], in_=pin[ib][:], func=AF.Copy), reads=[b_in[ib]],
                 writes=[b_pb[ib]])
            for k in range(2):
                P.op("pe", lambda e, k=k: e.transpose(out=ptp[:, k, :], in_=pb[ib][:, k * 128:(k + 1) * 128],
                                                      identity=C.ident[:]),
                     reads=[b_pb[ib], C.b_const], writes=[b_ptp])
            P.op("dve", lambda e: e.tensor_copy(out=pT[ib][:], in_=ptp[:]), reads=[b_ptp], writes=[b_pT[ib]])
            for hf in range(2):
                hs = slice(hf * 512, (hf + 1) * 512)
                for k in range(8):
                    P.op("pe", lambda e, k=k, hs=hs: e.matmul(pG[ib][:, hs], lhsT=uT[ib][:, k, :], rhs=Wg[:, k, hs],
                                                              start=(k == 0), stop=(k == 7)),
                         reads=[b_in[ib], b_W], writes=[b_pG[ib]])

        def s2(n):
            ib = n % 2
            for hf in range(2):
                hs = slice(hf * 512, (hf + 1) * 512)
                for k in range(2):
                    P.op("pe", lambda e, k=k, hs=hs: e.matmul(pE[:, hs], lhsT=pT[ib][:, k, :], rhs=Wp[:, k, hs],
                                                              start=(k == 0), stop=(k == 1)),
                         reads=[b_pT[ib], b_W], writes=[b_pE])
            P.op("dve", lambda e: e.tensor_tensor(out=gt[ib][:], in0=pG[ib][:], in1=bg[:], op=ALU.add),
                 reads=[b_pG[ib], b_W], writes=[b_gt[ib]])
            P.op("act", lambda e: e.activation(out=gt[ib][:], in_=gt[ib][:], func=AF.Sigmoid), reads=[b_gt[ib]],
                 writes=[b_gt[ib]])
            P.op("dve", lambda e: e.tensor_tensor(out=ge[ib][:], in0=gt[ib][:], in1=pE[:], op=ALU.mult),
                 reads=[b_gt[ib], b_pE], writes=[b_ge[ib]])
            si = n % 2
            ss, var, rstd = (stat[:, 3 * si + j:3 * si + j + 1] for j in range(3))
            rms_stats(P, C, ge[ib][:], b_ge[ib], junk[:], b_junk, ss, var, rstd, b_stat[si])
            P.op("dve", lambda e: e.scalar_tensor_tensor(
                out=hout[ib][:], in0=ge[ib][:], scalar=rstd, in1=gpost[:], op0=ALU.mult, op1=ALU.mult),
                reads=[b_ge[ib], b_stat[si], b_W], writes=[b_hout[ib]])
            P.op("pool", lambda e: e.tensor_tensor(out=hout[ib][:], in0=hout[ib][:], in1=hin[ib][:], op=ALU.add),
                 reads=[b_hout[ib], b_hin[ib]], writes=[b_hout[ib]])
            P.op("pool", lambda e: e.dma_start(out=ov[n], in_=hout[ib][:]), reads=[b_hout[ib]], dma=True)

        s1(0)
        for n in range(32):
            if n + 1 < 32:
                s1(n + 1)
            s2(n)
        P.flush()


def build_program(debug=False, stage=99, only=None):
    nc = bass.Bass("TRN2", target_bir_lowering=False)
    I = {}

    def din(name, shape):
        I[name] = nc.dram_tensor(name, shape, F32, kind="ExternalInput").ap()
        return I[name]

    din("x", [S, D])
    din("p", [S, 256])
    for nm in ("ffn1", "ffn2"):
        din(nm + "_pre_g", [1, D])
        din(nm + "_w_gate", [D, DFF])
        din(nm + "_w_up", [D, DFF])
        din(nm + "_w_down", [DFF, D])
        din(nm + "_post_g", [1, D])
    din("mix_pre_g", [1, D])
    din("w_in", [D, INW])
    din("conv_w", [4, 512])
    din("conv_b", [1, 512])
    din("mlstm_i_bias", [1, 4])
    din("mlstm_f_bias", [1, 4])
    din("mlstm_norm_g", [1, 512])
    din("fox_f_bias", [1, 8])
    din("branch_gate_bias", [1, 2048])
    din("w_branch_a", [512, D])
    din("w_branch_b", [512, D])
    din("w_out", [D, D])
    din("mix_post_g", [1, D])
    din("ple_pre_g", [1, D])
    din("ple_w_gate", [D, D])
    din("ple_b_gate", [1, D])
    din("ple_w_proj", [256, D])
    din("ple_post_g", [1, D])

    skind = "ExternalOutput" if debug else "Internal"

    def dscr(name, shape, dt):
        return nc.dram_tensor(name, shape, dt, kind=skind).ap()

    out = nc.dram_tensor("out", [S, D], F32, kind="ExternalOutput").ap()
    h1 = dscr("h1", [S, D], F32)
    uT1 = dscr("uT1", [8, 128, S], BF16)
    gpre = dscr("gpre", [72, S], F32)
    SC = {
        "mqkT": dscr("mqkT", [4, 128, S], BF16),
        "vaugM": dscr("vaugM", [S, 4, 129], BF16),
        "so": dscr("so", [S, 512], BF16),
        "qaug": dscr("qaug", [8, 70, S], BF16),
        "kaug": dscr("kaug", [8, 70, S], BF16),
        "vaugF": dscr("vaugF", [S, 8, 65], BF16),
        "ga": dscr("ga", [S, D], BF16),
        "gb": dscr("gb", [S, D], BF16),
        "yaT": dscr("yaT", [4, 128, S], BF16),
        "ybT": dscr("ybT", [4, 128, S], BF16),
        "recd": dscr("recd", [64, 512], F32),
    }
    h2 = dscr("h2", [S, D], F32)
    h3 = dscr("h3", [S, D], F32)
    uT3 = dscr("uT3", [8, 128, S], BF16)

    with contextlib.ExitStack() as st:
        P = Prog(nc, st)
        C = Ctx()
        C.db = {}

        def db(name):
            if name not in C.db:
                C.db[name] = P.bufs_n("D" + name, 32)
            return C.db[name]

        setup_consts(P, nc, st, C)
        C.wthr = st.enter_context(nc.sbuf_tensor("wthr", [128, 32, 8], F32))
        C.decbc = st.enter_context(nc.sbuf_tensor("decbc", [128, 4, 32], F32))
        C.b_wthr = P.buf("wthr")
        C.b_decbc = P.buf("decbc")
        def want(name, st_no):
            return (name in only) if only is not None else (stage >= st_no)

        if want("ffn1", 1):
            ffn_pass(P, nc, C, "f1", I["x"], I["ffn1_w_gate"], I["ffn1_w_up"], I["ffn1_w_down"],
                     I["ffn1_pre_g"], I["ffn1_post_g"], h1, I["mix_pre_g"], uT1,
                     db("x"), db("h1"), db("uT1"), gate_w=I["w_in"], gate_dst=gpre, gate_b=db("gpre"))
        if want("gp", 2):
            aug_b = P.buf("augrows")
            gp_stage(P, nc, C, I, gpre, db("gpre"), SC["qaug"], SC["kaug"], aug_b)
            db("aug").append(aug_b)
        if want("win", 2):
            win_pass(P, nc, C, I, uT1, db("uT1"), SC, db)
        if want("mix", 3):
            mix_pass(P, nc, C, I, SC, db)
        if want("merge", 4):
            merge_pass(P, nc, C, I, SC, db, h1, db("h1"), h2, db("h2"))
        if want("ffn2", 5):
            ffn_pass(P, nc, C, "f2", h2, I["ffn2_w_gate"], I["ffn2_w_up"], I["ffn2_w_down"],
                     I["ffn2_pre_g"], I["ffn2_post_g"], h3, I["ple_pre_g"], uT3,
                     db("h2"), db("h3"), db("uT3"))
        if want("ple", 6):
            ple_pass(P, nc, C, I, uT3, db("uT3"), h3, db("h3"), out)
        P.flush(final=True)
    return nc

IN_NAMES = ["x", "p", "ffn1_pre_g", "ffn1_w_gate", "ffn1_w_up", "ffn1_w_down", "ffn1_post_g",
            "mix_pre_g", "w_in", "conv_w", "conv_b", "mlstm_i_bias", "mlstm_f_bias", "mlstm_norm_g",
            "fox_f_bias", "branch_gate_bias", "w_branch_a", "w_branch_b", "w_out", "mix_post_g",
            "ffn2_pre_g", "ffn2_w_gate", "ffn2_w_up", "ffn2_w_down", "ffn2_post_g",
            "ple_pre_g", "ple_w_gate", "ple_b_gate", "ple_w_proj", "ple_post_g"]


def make_in_maps(inputs, cores):
    maps = []
    shared = {}
    for k in IN_NAMES:
        if k in ("x", "p"):
            continue
        shared[k] = np.ascontiguousarray(np.asarray(inputs[k])[0], dtype=np.float32)
    x = np.asarray(inputs["x"])
    p = np.asarray(inputs["p"])
    for b in cores:
        m = dict(shared)
        m["x"] = np.ascontiguousarray(x[b], dtype=np.float32)
        m["p"] = np.ascontiguousarray(p[0, b], dtype=np.float32)
        maps.append(m)
    return maps


def kernel(**inputs):
    nc = build_program()
    maps = make_in_maps(inputs, list(range(8)))
    res = run_bass_kernel_spmd(nc, maps, core_ids=list(range(8)))
    return np.stack([np.asarray(r["out"], dtype=np.float32) for r in res.results], axis=0)
```

```python
import contextlib
import numpy as np
import concourse.bass as bass
import concourse.mybir as mybir
from concourse.bass_utils import run_bass_kernel_spmd

F32 = mybir.dt.float32
BF16 = mybir.dt.bfloat16
AF = mybir.ActivationFunctionType
ALU = mybir.AluOpType
AX = mybir.AxisListType

S = 4096
D = 1024
DFF = 2816
NFC = DFF // 128
NT = 8
TT = 512
EPS = 1e-6
INW = 5136

ENGS = ("pe", "act", "dve", "pool", "sp")


class Buf:
    __slots__ = ("name", "w", "r")

    def __init__(self, name=""):
        self.name = name
        self.w = None
        self.r = []


class Op:
    __slots__ = ("eng", "fn", "deps", "inc", "cnt", "dma", "sem", "emitted")

    def __init__(self, eng, fn, dma=False):
        self.eng = eng
        self.fn = fn
        self.deps = []
        self.inc = False
        self.cnt = 0
        self.dma = dma
        self.sem = None
        self.emitted = False


class Prog:
    def __init__(self, nc, st, n_dma_sems=20):
        self.nc = nc
        self.pending = {e: [] for e in ENGS}
        self.bufs = []
        self.nd = n_dma_sems
        self.esem = {e: st.enter_context(nc.semaphore("s_" + e)) for e in ENGS}
        self.dsem = {}
        for e in ("sp", "pool"):
            for s in range(n_dma_sems):
                self.dsem[(e, s)] = st.enter_context(nc.semaphore("d_%s_%d" % (e, s)))
        self.ecnt = {e: 0 for e in ENGS}
        self.dcnt = {e: 0 for e in ENGS}
        self.waited = {e: {} for e in ENGS}
        self.n_ops = 0

    def buf(self, name=""):
        b = Buf(name)
        self.bufs.append(b)
        return b

    def bufs_n(self, name, n):
        return [self.buf("%s%d" % (name, i)) for i in range(n)]

    def op(self, eng, fn, reads=(), writes=(), dma=False):
        o = Op(eng, fn, dma)
        seen = set()
        cand = []
        for b in reads:
            if b.w is not None:
                cand.append(b.w)
        for b in writes:
            if b.w is not None:
                cand.append(b.w)
            cand.extend(b.r)
        for d in cand:
            if d is o or id(d) in seen:
                continue
            seen.add(id(d))
            if d.eng == "pe" and eng == "pe" and not d.dma and not dma:
                continue
            o.deps.append(d)
            if not d.emitted:
                d.inc = True
        for b in reads:
            b.r.append(o)
        for b in writes:
            b.w = o
            b.r = []
        self.pending[eng].append(o)
        self.n_ops += 1
        return o

    def flush(self, final=False):
        nc = self.nc
        for b in self.bufs:
            if b.w is not None and not b.w.emitted:
                b.w.inc = True
            for r in b.r:
                if not r.emitted:
                    r.inc = True
        for e in ENGS:
            for o in self.pending[e]:
                if o.dma:
                    k = self.dcnt[e]
                    o.sem = (e, k % self.nd)
                    o.cnt = 16 * (k // self.nd + 1)
                    self.dcnt[e] = k + 1
                elif o.inc:
                    self.ecnt[e] += 1
                    o.cnt = self.ecnt[e]
        pending = self.pending
        self.pending = {e: [] for e in ENGS}

        def run(ename, eng):
            waited = self.waited[ename]
            for o in pending[ename]:
                for d in o.deps:
                    key = d.sem if d.dma else d.eng
                    if waited.get(key, 0) >= d.cnt:
                        continue
                    assert d.cnt > 0, (d.eng, ename)
                    eng.wait_ge(self.dsem[key] if d.dma else self.esem[key], d.cnt)
                    waited[key] = d.cnt
                if o.dma:
                    if o.cnt > 16 and waited.get(o.sem, 0) < o.cnt - 16:
                        eng.wait_ge(self.dsem[o.sem], o.cnt - 16)
                        waited[o.sem] = o.cnt - 16
                    o.fn(eng).then_inc(self.dsem[o.sem], 16)
                else:
                    ins = o.fn(eng)
                    if o.inc:
                        ins.then_inc(self.esem[o.eng], 1)
                o.emitted = True
            if ename == "sp" and final:
                for q in ("sp", "pool"):
                    k = self.dcnt[q]
                    for sl in range(min(self.nd, k)):
                        last = 16 * ((k - 1 - sl) // self.nd + 1)
                        eng.wait_ge(self.dsem[(q, sl)], last)

        with nc.Block() as block:
            @block.tensor
            def _(eng):
                run("pe", eng)

            @block.scalar
            def _(eng):
                run("act", eng)

            @block.vector
            def _(eng):
                run("dve", eng)

            @block.gpsimd
            def _(eng):
                run("pool", eng)

            @block.sync
            def _(eng):
                run("sp", eng)


def bcast_last(ap2d, n):
    return ap2d.unsqueeze(2).to_broadcast([ap2d.shape[0], ap2d.shape[1], n])


class Ctx:
    pass


def load_w_kmajor(P, nc, dst, src2d, n_kc, ncols, bufs, col_chunk=1408):
    v = src2d.rearrange("(kc p) n -> kc p n", p=128)
    mdl = 4 * col_chunk
    for k in range(n_kc):
        P.op("pool", lambda e, k=k: e.dma_start(out=dst[:, k, :], in_=v[k], max_dma_last_dim=mdl),
             writes=[bufs[k]], dma=True)


def setup_consts(P, nc, st, C):
    sb = lambda name, shape, dt: st.enter_context(nc.sbuf_tensor(name, shape, dt))
    C.identf = sb("identf", [128, 128], F32)
    C.ident = sb("ident", [128, 128], BF16)
    C.neghalf = sb("neghalf", [128, 1], F32)
    C.b_const = P.buf("const")
    identf, ident = C.identf, C.ident
    P.op("pool", lambda e: e.memset(identf[:], 0.0), writes=[C.b_const])
    P.op("pool", lambda e: e.affine_select(out=identf[:], in_=identf[:], pattern=[[-1, 128]],
                                             compare_op=ALU.not_equal, fill=1.0, base=0,
                                             channel_multiplier=1),
         reads=[C.b_const], writes=[C.b_const])
    P.op("dve", lambda e: e.tensor_copy(out=ident[:], in_=identf[:]), reads=[C.b_const], writes=[C.b_const])
    P.op("pool", lambda e: e.memset(C.neghalf[:], -0.5), reads=[C.b_const], writes=[C.b_const])


def rms_stats(P, C, src_ap, src_buf, junk, b_junk, ss, var, rstd, b_stat, n_feat=D):
    P.op("act", lambda e: e.activation(out=junk, in_=src_ap, func=AF.Square, accum_out=ss),
         reads=[src_buf], writes=[b_junk, b_stat])
    P.op("dve", lambda e: e.tensor_scalar(out=var, in0=ss, scalar1=1.0 / n_feat, scalar2=EPS,
                                          op0=ALU.mult, op1=ALU.add),
         reads=[b_stat], writes=[b_stat])
    P.op("pool", lambda e: e.tensor_tensor(out=rstd, in0=var, in1=C.neghalf[:], op=ALU.pow),
         reads=[b_stat, C.b_const], writes=[b_stat])


def ffn_pass(P, nc, C, tag, src_h, w_gate, w_up, w_down, pre_g, post_g, dst_h, next_g, dst_uT,
             src_b, dst_b, uT_b, gate_w=None, gate_dst=None, gate_b=None):
    with contextlib.ExitStack() as st:
        sb = lambda name, shape, dt: st.enter_context(nc.sbuf_tensor(tag + name, shape, dt))
        ps = lambda name, shape, dt: st.enter_context(nc.psum_tensor(tag + name, shape, dt))
        Wg = sb("Wg", [128, 8, DFF], BF16)
        Wu = sb("Wu", [128, 8, DFF], BF16)
        Wd = sb("Wd", [128, NFC, D], BF16)
        b_Wg = P.bufs_n("Wg", 8)
        b_Wu = P.bufs_n("Wu", 8)
        b_Wd = P.bufs_n("Wd", 2)
        load_w_kmajor(P, nc, Wg, w_gate, 8, DFF, b_Wg)
        load_w_kmajor(P, nc, Wu, w_up, 8, DFF, b_Wu)
        wdv = w_down.rearrange("(fc p) d -> p fc d", p=128)
        for hh in range(2):
            P.op("pool", lambda e, hh=hh: e.dma_start(out=Wd[:, hh * 11:(hh + 1) * 11, :],
                                                       in_=wdv[:, hh * 11:(hh + 1) * 11, :]),
                 writes=[b_Wd[hh]], dma=True)
        gpre = sb("gpre", [128, 8], F32)
        gnext = sb("gnext", [128, 8], F32)
        gpost = sb("gpost", [128, D], F32)
        b_par = P.buf("par")
        P.op("sp", lambda e: e.dma_start(out=gpre[:], in_=pre_g.rearrange("o (k p) -> p (o k)", p=128),
                                         allow_slow_non_contiguous=True),
             writes=[b_par], dma=True)
        P.op("sp", lambda e: e.dma_start(out=gnext[:], in_=next_g.rearrange("o (k p) -> p (o k)", p=128),
                                         allow_slow_non_contiguous=True),
             writes=[b_par], dma=True)
        P.op("sp", lambda e: e.dma_start(out=gpost[:], in_=post_g.partition_broadcast(128)),
             writes=[b_par], dma=True)
        if gate_w is not None:
            Wgt = sb("Wgt", [128, 8, 72], BF16)
            b_Wgt = P.buf("Wgt")
            P.op("pool", lambda e: e.memset(Wgt[:], 0.0), writes=[b_Wgt])
            gv = gate_w.rearrange("(kc p) n -> p kc n", p=128)
            for (c0, n, d0) in ((1540, 4, 0), (1536, 4, 32), (3080, 8, 64)):
                P.op("pool", lambda e, c0=c0, n=n, d0=d0: e.dma_start(
                    out=Wgt[:, :, d0:d0 + n], in_=gv[:, :, c0:c0 + n]),
                    reads=[], writes=[b_Wgt], dma=True)
            gsb = [sb("gsb%d" % i, [72, 128], F32) for i in range(2)]
            b_gsb = P.bufs_n("gsb", 2)

        NXB = 3
        xb = [sb("xb%d" % i, [128, D], F32) for i in range(NXB)]
        b_xb = P.bufs_n("xb", NXB)
        ubf = [sb("ubf%d" % i, [128, D], BF16) for i in range(2)]
        b_ubf = P.bufs_n("ubf", 2)
        junk = sb("junk", [128, D], BF16)
        b_junk = P.buf("junk")
        uT = sb("uT", [128, 8, TT], BF16)
        b_uT = P.bufs_n("uT", 4)
        aT = sb("aT", [128, NFC, TT], BF16)
        b_aT = P.bufs_n("aT", NFC)
        sg = [sb("sg%d" % i, [128, TT], F32) for i in range(2)]
        b_sg = P.bufs_n("sg", 2)
        hst = [sb("hst%d" % i, [128, D], F32) for i in range(2)]
        b_hst = P.bufs_n("hst", 2)
        u2T = [sb("u2T%d" % i, [128, 8, 128], BF16) for i in range(2)]
        b_u2T = P.bufs_n("u2T", 2)
        NST = 6
        stat = sb("stat", [128, 3 * NST], F32)
        b_stat = P.bufs_n("stat", NST)

        pt = ps("pt", [128, 8, 128], BF16)
        b_pt = P.buf("pt")
        pg = [ps("pg%d" % i, [128, TT], F32) for i in range(2)]
        pu = [ps("pu%d" % i, [128, TT], F32) for i in range(2)]
        b_pg = P.bufs_n("pg", 2)
        b_pu = P.bufs_n("pu", 2)
        pys = [ps("py%d" % i, [128, 512], F32) for i in range(3)]
        b_pys = P.bufs_n("py", 3)
        if gate_w is not None:
            pgt = pu[1][0:72, 0:128]
            b_pgt = b_pu[1]

        src_v = src_h.rearrange("(n p) d -> n p d", p=128)
        dst_v = dst_h.rearrange("(n p) d -> n p d", p=128)
        cnt = {"x": 0, "u": 0, "st": 0, "h": 0, "u2": 0, "sg": 0, "gs": 0, "py": 0}
        pend = []

        def norm_T(h_ap, h_buf, gcol, out_ap, out_bufs, defer=False):
            si = cnt["st"] % NST
            cnt["st"] += 1
            ss, var, rstd = (stat[:, 3 * si + j:3 * si + j + 1] for j in range(3))
            rms_stats(P, C, h_ap, h_buf, junk[:], b_junk, ss, var, rstd, b_stat[si])
            ui = cnt["u"] % 2
            cnt["u"] += 1
            u = ubf[ui]
            P.op("dve", lambda e: e.tensor_scalar(out=u[:], in0=h_ap, scalar1=rstd, scalar2=None, op0=ALU.mult),
                 reads=[h_buf, b_stat[si]], writes=[b_ubf[ui]])
            def pe_part():
                for k in range(8):
                    P.op("pe", lambda e, k=k: e.transpose(out=pt[:, k, :], in_=u[:, k * 128:(k + 1) * 128],
                                                          identity=C.ident[:]),
                         reads=[b_ubf[ui], C.b_const], writes=[b_pt])
                P.op("dve", lambda e: e.tensor_tensor(out=out_ap, in0=pt[:], in1=bcast_last(gcol[:], 128), op=ALU.mult),
                     reads=[b_pt, b_par], writes=out_bufs)
            if defer:
                return pe_part
            pe_part()

        def pre(i):
            for s in range(4):
                n = i * 4 + s
                xi = cnt["x"] % NXB
                cnt["x"] += 1
                P.op("sp", lambda e, n=n, xi=xi: e.dma_start(out=xb[xi][:], in_=src_v[n]),
                     reads=[src_b[n]], writes=[b_xb[xi]], dma=True)
                norm_T(xb[xi][:], b_xb[xi], gpre, uT[:, :, s * 128:(s + 1) * 128], [b_uT[s]])

        def gateup(i):
            for f in range(NFC):
                j = f % 2
                for k in range(8):
                    P.op("pe", lambda e, k=k, f=f, j=j: e.matmul(
                        pg[j][:], lhsT=Wg[:, k, f * 128:(f + 1) * 128], rhs=uT[:, k, :],
                        start=(k == 0), stop=(k == 7)),
                        reads=[b_Wg[k]] + b_uT, writes=[b_pg[j]])
                for k in range(8):
                    P.op("pe", lambda e, k=k, f=f, j=j: e.matmul(
                        pu[j][:], lhsT=Wu[:, k, f * 128:(f + 1) * 128], rhs=uT[:, k, :],
                        start=(k == 0), stop=(k == 7)),
                        reads=[b_Wu[k]] + b_uT, writes=[b_pu[j]])
                if f == 0:
                    while pend:
                        pend.pop(0)()
                si = cnt["sg"] % 2
                cnt["sg"] += 1
                P.op("act", lambda e, j=j, si=si: e.activation(out=sg[si][:], in_=pg[j][:], func=AF.Silu),
                     reads=[b_pg[j]], writes=[b_sg[si]])
                P.op("dve", lambda e, j=j, si=si, f=f: e.tensor_tensor(out=aT[:, f, :], in0=sg[si][:], in1=pu[j][:],
                                                                   op=ALU.mult),
                     reads=[b_sg[si], b_pu[j]], writes=[b_aT[f]])

        def down_post(i):
            for s in range(4):
                n = i * 4 + s
                pyh = []
                for hf in range(2):
                    pi = cnt["py"] % 3
                    cnt["py"] += 1
                    pyh.append((pys[pi], b_pys[pi]))
                    for f in range(NFC):
                        P.op("pe", lambda e, f=f, s=s, hf=hf, pi=pi: e.matmul(
                            pys[pi][:], lhsT=aT[:, f, s * 128:(s + 1) * 128],
                            rhs=Wd[:, f, hf * 512:(hf + 1) * 512], start=(f == 0), stop=(f == NFC - 1)),
                            reads=[b_aT[f], b_Wd[f // 11]], writes=[b_pys[pi]])
                while pend:
                    pend.pop(0)()
                xi = cnt["x"] % NXB
                cnt["x"] += 1
                P.op("sp", lambda e, n=n, xi=xi: e.dma_start(out=xb[xi][:], in_=src_v[n]),
                     reads=[src_b[n]], writes=[b_xb[xi]], dma=True)
                si = cnt["st"] % NST
                cnt["st"] += 1
                ss, var, rstd = (stat[:, 3 * si + j:3 * si + j + 1] for j in range(3))
                P.op("act", lambda e, ss=ss, t=pyh[0][0]: e.activation(out=junk[:, 0:512], in_=t[:], func=AF.Square,
                                                                      accum_out=ss),
                     reads=[pyh[0][1]], writes=[b_junk, b_stat[si]])
                P.op("act", lambda e, var=var, t=pyh[1][0]: e.activation(out=junk[:, 512:1024], in_=t[:], func=AF.Square,
                                                                        accum_out=var),
                     reads=[pyh[1][1]], writes=[b_junk, b_stat[si]])
                P.op("dve", lambda e, ss=ss, var=var: e.tensor_tensor(out=var, in0=ss, in1=var, op=ALU.add),
                     reads=[b_stat[si]], writes=[b_stat[si]])
                P.op("dve", lambda e, var=var: e.tensor_scalar(out=var, in0=var, scalar1=1.0 / D, scalar2=EPS,
                                                              op0=ALU.mult, op1=ALU.add),
                     reads=[b_stat[si]], writes=[b_stat[si]])
                P.op("pool", lambda e, var=var, rstd=rstd: e.tensor_tensor(out=rstd, in0=var, in1=C.neghalf[:], op=ALU.pow),
                     reads=[b_stat[si], C.b_const], writes=[b_stat[si]])
                hi = cnt["h"] % 2
                cnt["h"] += 1
                hb = hst[hi]
                for hf in range(2):
                    hs = slice(hf * 512, (hf + 1) * 512)
                    P.op("dve", lambda e, hb=hb, rstd=rstd, t=pyh[hf][0], hs=hs: e.scalar_tensor_tensor(
                        out=hb[:, hs], in0=t[:], scalar=rstd, in1=gpost[:, hs], op0=ALU.mult, op1=ALU.mult),
                        reads=[pyh[hf][1], b_stat[si], b_par], writes=[b_hst[hi]])
                P.op("dve", lambda e, hb=hb, xi=xi: e.scalar_tensor_tensor(
                    out=hb[:], in0=hb[:], scalar=0.5, in1=xb[xi][:], op0=ALU.mult, op1=ALU.add),
                    reads=[b_hst[hi], b_xb[xi]], writes=[b_hst[hi]])
                P.op("sp", lambda e, hb=hb, n=n: e.dma_start(out=dst_v[n], in_=hb[:]),
                     reads=[b_hst[hi]], writes=[dst_b[n]], dma=True)
                ui2 = cnt["u2"] % 2
                cnt["u2"] += 1
                pe_part = norm_T(hb[:], b_hst[hi], gnext, u2T[ui2][:], [b_u2T[ui2]], defer=True)

                def tail(pe_part=pe_part, ui2=ui2, n=n):
                    pe_part()
                    P.op("sp", lambda e: e.dma_start(
                        out=dst_uT[:, :, n * 128:(n + 1) * 128].rearrange("k p t -> p k t"), in_=u2T[ui2][:]),
                        reads=[b_u2T[ui2]], writes=[uT_b[n]], dma=True)
                    if gate_w is not None:
                        for k in range(8):
                            P.op("pe", lambda e, k=k: e.matmul(
                                pgt, lhsT=Wgt[:, k, :], rhs=u2T[ui2][:, k, :], start=(k == 0), stop=(k == 7)),
                                reads=[b_Wgt, b_u2T[ui2]], writes=[b_pgt])
                        gi = cnt["gs"] % 2
                        cnt["gs"] += 1
                        P.op("act", lambda e: e.activation(out=gsb[gi][:], in_=pgt, func=AF.Copy),
                             reads=[b_pgt], writes=[b_gsb[gi]])
                        P.op("sp", lambda e: e.dma_start(out=gate_dst[:, n * 128:(n + 1) * 128], in_=gsb[gi][:]),
                             reads=[b_gsb[gi]], writes=[gate_b[n]], dma=True)
                pend.append(tail)

        pre(0)
        for i in range(NT):
            gateup(i)
            if i + 1 < NT:
                pre(i + 1)
            down_post(i)
        while pend:
            pend.pop(0)()
        P.flush()


def gp_stage(P, nc, C, I, gpre, gpre_b, qaug, kaug, aug_b):
    with contextlib.ExitStack() as st:
        sb = lambda name, shape, dt: st.enter_context(nc.sbuf_tensor("gp" + name, shape, dt))
        ps = lambda name, shape, dt: st.enter_context(nc.psum_tensor("gp" + name, shape, dt))
        T0 = sb("T0", [72, S], F32)
        T1 = sb("T1", [72, S], F32)
        T2 = sb("T2", [72, S], F32)
        T3 = sb("T3", [72, S], F32)
        QR = sb("QR", [72, 3, S], BF16)
        KR = sb("KR", [72, 3, S], BF16)
        ONE = sb("ONE", [72, S], BF16)
        bcol = sb("bcol", [72, 1], F32)
        negb = sb("negb", [72, 1], F32)
        bicol = sb("bicol", [72, 1], F32)
        onec = sb("onec", [72, 1], F32)
        cm = sb("cm", [72, 32], F32)
        mce = sb("mce", [72, 32], F32)
        mprev = sb("mprev", [72, 32], F32)
        dec = sb("dec", [72, 32], F32)
        esel = sb("esel", [72, 4, 128], F32)
        bT0, bT1, bT2, bT3, bQR, bKR, bONE, bsm = [P.buf(n) for n in
                                                   ("T0", "T1", "T2", "T3", "QR", "KR", "ONE", "gsm")]
        ptm = ps("ptm", [128, 32, 8], F32)
        pdc = ps("pdc", [128, 4, 32], F32)
        b_ptm, b_pdc = P.buf("ptm"), P.buf("pdc")

        P.op("sp", lambda e: e.dma_start(out=T0[:], in_=gpre), reads=gpre_b, writes=[bT0], dma=True)
        P.op("sp", lambda e: e.dma_start(out=T3[0:4, :], in_=gpre[32:36, :]), reads=gpre_b, writes=[bT3], dma=True)
        P.op("dve", lambda e: e.memset(bcol[:], 0.0), writes=[bsm])
        P.op("dve", lambda e: e.memset(bicol[:], 0.0), reads=[bsm], writes=[bsm])
        P.op("dve", lambda e: e.memset(onec[:], 1.0), reads=[bsm], writes=[bsm])
        P.op("pool", lambda e: e.memset(ONE[:], 1.0), writes=[bONE])
        P.op("sp", lambda e: e.dma_start(out=bcol[0:4, :], in_=I["mlstm_f_bias"].rearrange("o n -> n o"),
                                         allow_slow_non_contiguous=True), reads=[bsm], writes=[bsm], dma=True)
        P.op("sp", lambda e: e.dma_start(out=bcol[64:72, :], in_=I["fox_f_bias"].rearrange("o n -> n o"),
                                         allow_slow_non_contiguous=True), reads=[bsm], writes=[bsm], dma=True)
        P.op("sp", lambda e: e.dma_start(out=bicol[0:4, :], in_=I["mlstm_i_bias"].rearrange("o n -> n o"),
                                         allow_slow_non_contiguous=True), reads=[bsm], writes=[bsm], dma=True)
        P.op("dve", lambda e: e.tensor_scalar(out=negb[0:72, :], in0=bcol[0:72, :], scalar1=-1.0, scalar2=None,
                                              op0=ALU.mult), reads=[bsm], writes=[bsm])
        R = slice(0, 72)
        P.op("act", lambda e: e.activation(out=T1[R, :], in_=T0[R, :], func=AF.Exp, scale=-1.0, bias=negb[R, :]),
             reads=[bT0, bsm], writes=[bT1])
        P.op("act", lambda e: e.activation(out=T1[R, :], in_=T1[R, :], func=AF.Ln, scale=1.0, bias=onec[R, :]),
             reads=[bT1, bsm], writes=[bT1])
        P.op("dve", lambda e: e.tensor_tensor_scan(out=T2[R, :], data0=T1[R, :], data1=T1[R, :], initial=0.0,
                                                   op0=ALU.add, op1=ALU.max), reads=[bT1], writes=[bT2])
        M = slice(0, 4)
        P.op("dve", lambda e: e.scalar_tensor_tensor(out=T3[M, :], in0=T3[M, :], scalar=bicol[M, :], in1=T2[M, :],
                                                     op0=ALU.add, op1=ALU.add), reads=[bT3, bT2, bsm], writes=[bT3])
        P.op("dve", lambda e: e.tensor_reduce(out=cm[M, :], in_=T3[M, :].rearrange("p (c l) -> p c l", l=128),
                                              axis=AX.X, op=ALU.max), reads=[bT3], writes=[bsm])
        P.op("dve", lambda e: e.tensor_tensor_scan(out=mce[M, :], data0=cm[M, :], data1=cm[M, :], initial=0.0,
                                                   op0=ALU.max, op1=ALU.max), reads=[bsm], writes=[bsm])
        P.op("dve", lambda e: e.tensor_tensor(out=T3[M, :].rearrange("p (c l) -> p c l", l=128),
                                              in0=T3[M, :].rearrange("p (c l) -> p c l", l=128),
                                              in1=bcast_last(mce[M, :], 128), op=ALU.subtract),
             reads=[bT3, bsm], writes=[bT3])
        P.op("act", lambda e: e.activation(out=T3[M, :], in_=T3[M, :], func=AF.Exp), reads=[bT3], writes=[bT3])
        P.op("dve", lambda e: e.tensor_tensor(out=T1[M, :].rearrange("p (c l) -> p c l", l=128),
                                              in0=T2[M, :].rearrange("p (c l) -> p c l", l=128),
                                              in1=bcast_last(mce[M, :], 128), op=ALU.subtract),
             reads=[bT2, bsm, bT1], writes=[bT1])
        P.op("act", lambda e: e.activation(out=T1[M, :], in_=T1[M, :], func=AF.Exp, scale=2.0), reads=[bT1], writes=[bT1])
        P.op("dve", lambda e: e.memset(mprev[M, :], 0.0), reads=[bsm], writes=[bsm])
        P.op("dve", lambda e: e.tensor_copy(out=mprev[M, 1:32], in_=mce[M, 0:31]), reads=[bsm], writes=[bsm])
        P.op("dve", lambda e: e.tensor_tensor(out=dec[M, :], in0=mprev[M, :], in1=mce[M, :], op=ALU.subtract),
             reads=[bsm], writes=[bsm])
        P.op("act", lambda e: e.activation(out=dec[M, :], in_=dec[M, :], func=AF.Exp), reads=[bsm], writes=[bsm])
        for c in range(32):
            P.op("pe", lambda e, c=c: e.transpose(out=ptm[:, c, 0:4], in_=T3[M, c * 128:(c + 1) * 128],
                                                  identity=C.identf[M, 0:4]),
                 reads=[bT3, C.b_const], writes=[b_ptm])
            P.op("pe", lambda e, c=c: e.transpose(out=ptm[:, c, 4:8], in_=T1[M, c * 128:(c + 1) * 128],
                                                  identity=C.identf[M, 0:4]),
                 reads=[bT1, C.b_const], writes=[b_ptm])
        P.op("dve", lambda e: e.tensor_copy(out=C.wthr[:], in_=ptm[:]), reads=[b_ptm], writes=[C.b_wthr])
        for h in range(4):
            P.op("dve", lambda e, h=h: e.tensor_copy(out=esel[M, h, :],
                                                     in_=C.identf[M, h:h + 1].to_broadcast([4, 128])),
                 reads=[C.b_const, bsm], writes=[bsm])
        for h in range(4):
            P.op("pe", lambda e, h=h: e.matmul(pdc[:, h, :], lhsT=esel[M, h, :], rhs=dec[M, :], start=True, stop=True),
                 reads=[bsm], writes=[b_pdc])
        P.op("dve", lambda e: e.tensor_copy(out=C.decbc[:], in_=pdc[:]), reads=[b_pdc], writes=[C.b_decbc])
        Fx = slice(64, 72)
        Fd = slice(64, 72)
        P.op("dve", lambda e: e.tensor_scalar(out=T0[Fx, :], in0=T2[Fx, :], scalar1=-1.0, scalar2=None, op0=ALU.mult),
             reads=[bT2, bT0], writes=[bT0])
        for part in range(3):
            P.op("dve", lambda e, part=part: e.tensor_copy(out=QR[Fx, part, :], in_=T0[Fx, :]),
                 reads=[bT0], writes=[bQR])
            if part < 2:
                P.op("dve", lambda e, part=part: e.tensor_tensor(out=T0[Fx, :], in0=T0[Fx, :], in1=QR[Fx, part, :],
                                                                 op=ALU.subtract), reads=[bT0, bQR], writes=[bT0])
        P.op("pool", lambda e: e.tensor_scalar(out=KR[Fx, :, :], in0=QR[Fx, :, :], scalar1=-1.0, scalar2=None,
                                               op0=ALU.mult), reads=[bQR], writes=[bKR])
        P.op("sp", lambda e: e.dma_start(out=qaug[:, 64:67, :], in_=QR[Fd, :, :]), reads=[bQR], writes=[aug_b], dma=True)
        P.op("sp", lambda e: e.dma_start(out=kaug[:, 67:70, :], in_=KR[Fd, :, :]), reads=[bKR], writes=[aug_b], dma=True)
        for r in range(3):
            P.op("sp", lambda e, r=r: e.dma_start(out=qaug[:, 67 + r, :], in_=ONE[Fd, :]), reads=[bONE],
                 writes=[aug_b], dma=True)
            P.op("sp", lambda e, r=r: e.dma_start(out=kaug[:, 64 + r, :], in_=ONE[Fd, :]), reads=[bONE],
                 writes=[aug_b], dma=True)
        P.flush()


def win_pass(P, nc, C, I, uT1, uT_b, SC, DB):
    w_in = I["w_in"]
    with contextlib.ExitStack() as st:
        sb = lambda name, shape, dt: st.enter_context(nc.sbuf_tensor("wi" + name, shape, dt))
        ps = lambda name, shape, dt: st.enter_context(nc.psum_tensor("wi" + name, shape, dt))
        W = sb("W", [128, 8, INW], BF16)
        b_W = P.bufs_n("Win", 8)
        wv = w_in.rearrange("(kc p) n -> kc p n", p=128)
        for k in range(8):
            P.op("pool", lambda e, k=k: e.dma_start(out=W[:, k, :], in_=wv[k], max_dma_last_dim=4 * 1284),
                 writes=[b_W[k]], dma=True)
        cw = sb("cw", [128, 4, 4], F32)
        cb = sb("cb", [128, 4], F32)
        gbias = sb("gbias", [128, 2048], F32)
        b_par = P.buf("wipar")
        for tap in range(4):
            P.op("sp", lambda e, tap=tap: e.dma_start(
                out=cw[:, :, tap], in_=I["conv_w"][tap:tap + 1, :].rearrange("o (c p) -> p (o c)", p=128),
                allow_slow_non_contiguous=True), writes=[b_par], dma=True)
        P.op("sp", lambda e: e.dma_start(out=cb[:], in_=I["conv_b"].rearrange("o (c p) -> p (o c)", p=128),
                                         allow_slow_non_contiguous=True), writes=[b_par], dma=True)
        P.op("sp", lambda e: e.dma_start(out=gbias[:], in_=I["branch_gate_bias"].partition_broadcast(128)),
             writes=[b_par], dma=True)
        uT = [sb("uT%d" % i, [128, 8, TT], BF16) for i in range(2)]
        b_uT = P.bufs_n("wiuT", 2)
        zq = sb("zq", [128, 4, 3 + TT], F32)
        b_zq = P.bufs_n("zq", 4)
        acc = [sb("acc%d" % i, [128, TT], F32) for i in range(2)]
        b_acc = P.bufs_n("acc", 2)
        fo = [sb("fo%d" % i, [128, TT], BF16) for i in range(3)]
        b_fo = P.bufs_n("fo", 3)
        tv = [sb("tv%d" % i, [128, 4, 129], BF16) for i in range(2)]
        b_tv = P.bufs_n("tv", 2)
        tf = [sb("tf%d" % i, [128, 8, 65], BF16) for i in range(2)]
        b_tf = P.bufs_n("tf", 2)
        tg = [sb("tg%d" % i, [128, 512], F32) for i in range(2)]
        b_tg = P.bufs_n("tg", 2)
        to = [sb("to%d" % i, [128, 512], BF16) for i in range(3)]
        b_to = P.bufs_n("to", 3)
        pf = [ps("pf%d" % i, [128, TT], F32) for i in range(2)]
        b_pf = P.bufs_n("pf", 2)
        pk = [ps("pk%d" % i, [128, 512], F32) for i in range(2)]
        b_pk = P.bufs_n("pk", 2)
        cnt = {"pf": 0, "pk": 0, "acc": 0, "fo": 0, "tv": 0, "tf": 0, "tg": 0, "to": 0}

        def rot(key, n):
            v = cnt[key] % n
            cnt[key] += 1
            return v

        for ch in range(4):
            P.op("dve", lambda e, ch=ch: e.memset(zq[:, ch, 0:3], 0.0), writes=[b_zq[ch]])
        for i in range(NT):
            ub = i % 2
            tcols = slice(i * TT, (i + 1) * TT)
            if i == 0:
                P.op("sp", lambda e: e.dma_start(out=uT[0][:], in_=uT1[:, :, 0:TT].rearrange("k p t -> p k t")),
                     reads=uT_b[0:4], writes=[b_uT[0]], dma=True)
            if i + 1 < NT:
                ncols = slice((i + 1) * TT, (i + 2) * TT)
                P.op("sp", lambda e, ub=ub, ncols=ncols: e.dma_start(
                    out=uT[1 - ub][:], in_=uT1[:, :, ncols].rearrange("k p t -> p k t")),
                    reads=uT_b[4 * i + 4:4 * i + 8], writes=[b_uT[1 - ub]], dma=True)
            fm = [("mqk", ch, ch * 128) for ch in range(4)] + \
                 [("fq", ch, 1544 + ch * 128) for ch in range(4)] + \
                 [("fk", ch, 2056 + ch * 128) for ch in range(4)]
            for (kind, ch, c0) in fm:
                j = rot("pf", 2)
                for k in range(8):
                    P.op("pe", lambda e, k=k, c0=c0, j=j, ub=ub: e.matmul(
                        pf[j][:], lhsT=W[:, k, c0:c0 + 128], rhs=uT[ub][:, k, :], start=(k == 0), stop=(k == 7)),
                        reads=[b_W[k], b_uT[ub]], writes=[b_pf[j]])
                if kind == "mqk":
                    P.op("act", lambda e, ch=ch, j=j: e.activation(out=zq[:, ch, 3:3 + TT], in_=pf[j][:], func=AF.Copy),
                         reads=[b_pf[j]], writes=[b_zq[ch]])
                    a = rot("acc", 2)
                    P.op("dve", lambda e, ch=ch, a=a: e.tensor_scalar(
                        out=acc[a][:], in0=zq[:, ch, 0:TT], scalar1=cw[:, ch, 0:1], scalar2=cb[:, ch:ch + 1],
                        op0=ALU.mult, op1=ALU.add), reads=[b_zq[ch], b_par], writes=[b_acc[a]])
                    for tap in range(1, 4):
                        P.op("dve", lambda e, ch=ch, a=a, tap=tap: e.scalar_tensor_tensor(
                            out=acc[a][:], in0=zq[:, ch, tap:tap + TT], scalar=cw[:, ch, tap:tap + 1], in1=acc[a][:],
                            op0=ALU.mult, op1=ALU.add), reads=[b_zq[ch], b_par, b_acc[a]], writes=[b_acc[a]])
                    P.op("dve", lambda e, ch=ch: e.tensor_copy(out=zq[:, ch, 0:3], in_=zq[:, ch, TT:TT + 3]),
                         reads=[b_zq[ch]], writes=[b_zq[ch]])
                    o = rot("fo", 3)
                    P.op("act", lambda e, a=a, o=o: e.activation(out=fo[o][:], in_=acc[a][:], func=AF.Silu),
                         reads=[b_acc[a]], writes=[b_fo[o]])
                    P.op("sp", lambda e, o=o, ch=ch, tcols=tcols: e.dma_start(out=SC["mqkT"][ch, :, tcols], in_=fo[o][:]),
                         reads=[b_fo[o]], writes=[DB("mqkT")[i]], dma=True)
                else:
                    o = rot("fo", 3)
                    sc = 0.125 if kind == "fq" else 1.0
                    P.op("act", lambda e, o=o, j=j, sc=sc: e.activation(out=fo[o][:], in_=pf[j][:], func=AF.Copy, scale=sc),
                         reads=[b_pf[j]], writes=[b_fo[o]])
                    dst = SC["qaug"] if kind == "fq" else SC["kaug"]
                    for hh in range(2):
                        P.op("sp", lambda e, o=o, ch=ch, dst=dst, tcols=tcols, hh=hh: e.dma_start(
                            out=dst[2 * ch + hh, 0:64, tcols], in_=fo[o][hh * 64:(hh + 1) * 64, :]),
                            reads=[b_fo[o]], writes=[DB("aug")[i]], dma=True)
            for s in range(4):
                n = i * 4 + s
                rows = slice(n * 128, (n + 1) * 128)
                groups = [("mv", 512), ("mo", 1024), ("fv", 2568), ("ga", 3088), ("ga", 3600), ("gb", 4112), ("gb", 4624)]
                for gi, (kind, c0) in enumerate(groups):
                    j = rot("pk", 2)
                    for k in range(8):
                        P.op("pe", lambda e, k=k, c0=c0, j=j, ub=ub, s=s: e.matmul(
                            pk[j][:], lhsT=uT[ub][:, k, s * 128:(s + 1) * 128], rhs=W[:, k, c0:c0 + 512],
                            start=(k == 0), stop=(k == 7)),
                            reads=[b_W[k], b_uT[ub]], writes=[b_pk[j]])
                    if kind == "mv":
                        t = rot("tv", 2)
                        c = n
                        P.op("dve", lambda e, t=t, j=j, c=c: e.tensor_tensor(
                            out=tv[t][:, :, 0:128], in0=pk[j][:].rearrange("p (h d) -> p h d", h=4),
                            in1=bcast_last(C.wthr[:, c, 0:4], 128), op=ALU.mult),
                            reads=[b_pk[j], C.b_wthr], writes=[b_tv[t]])
                        P.op("dve", lambda e, t=t, c=c: e.tensor_copy(out=tv[t][:, :, 128:129],
                                                                     in_=C.wthr[:, c, 0:4].unsqueeze(2)),
                             reads=[C.b_wthr, b_tv[t]], writes=[b_tv[t]])
                        P.op("sp", lambda e, t=t, rows=rows: e.dma_start(out=SC["vaugM"][rows], in_=tv[t][:]),
                             reads=[b_tv[t]], writes=[DB("vaugM")[n]], dma=True)
                    elif kind == "fv":
                        t = rot("tf", 2)
                        P.op("act", lambda e, t=t, j=j: e.activation(
                            out=tf[t][:, :, 1:65], in_=pk[j][:].rearrange("p (h d) -> p h d", h=8), func=AF.Copy),
                            reads=[b_pk[j]], writes=[b_tf[t]])
                        P.op("dve", lambda e, t=t: e.memset(tf[t][:, :, 0:1], 1.0), reads=[b_tf[t]], writes=[b_tf[t]])
                        P.op("sp", lambda e, t=t, rows=rows: e.dma_start(out=SC["vaugF"][rows], in_=tf[t][:]),
                             reads=[b_tf[t]], writes=[DB("vaugF")[n]], dma=True)
                    elif kind == "mo":
                        o = rot("to", 3)
                        P.op("act", lambda e, o=o, j=j: e.activation(out=to[o][:], in_=pk[j][:], func=AF.Sigmoid),
                             reads=[b_pk[j]], writes=[b_to[o]])
                        P.op("sp", lambda e, o=o, rows=rows: e.dma_start(out=SC["so"][rows], in_=to[o][:]),
                             reads=[b_to[o]], writes=[DB("so")[n]], dma=True)
                    else:
                        g = rot("tg", 2)
                        boff = c0 - 3088
                        P.op("dve", lambda e, g=g, j=j, boff=boff: e.tensor_tensor(
                            out=tg[g][:], in0=pk[j][:], in1=gbias[:, boff:boff + 512], op=ALU.add),
                            reads=[b_pk[j], b_par], writes=[b_tg[g]])
                        o = rot("to", 3)
                        P.op("act", lambda e, o=o, g=g: e.activation(out=to[o][:], in_=tg[g][:], func=AF.Sigmoid),
                             reads=[b_tg[g]], writes=[b_to[o]])
                        dcol = boff % 1024
                        dst = SC["ga"] if kind == "ga" else SC["gb"]
                        P.op("sp", lambda e, o=o, rows=rows, dst=dst, dcol=dcol: e.dma_start(
                            out=dst[rows, dcol:dcol + 512], in_=to[o][:]),
                            reads=[b_to[o]], writes=[DB("gab")[n]], dma=True)
        P.flush()


def mix_pass(P, nc, C, I, SC, DB):
    with contextlib.ExitStack() as st:
        sb = lambda name, shape, dt: st.enter_context(nc.sbuf_tensor("mx" + name, shape, dt))
        ps = lambda name, shape, dt: st.enter_context(nc.psum_tensor("mx" + name, shape, dt))
        mask01 = sb("mask01", [128, 128], F32)
        trim = sb("trim", [128, 128], BF16)
        trimf = sb("trimf", [128, 128], F32)
        onesr = sb("onesr", [1, 65], F32)
        gln = sb("gln", [128, 512], F32)
        b_c = P.buf("mxconst")
        P.op("pool", lambda e: e.memset(mask01[:], 1.0), writes=[b_c])
        P.op("pool", lambda e: e.affine_select(out=mask01[:], in_=mask01[:], pattern=[[1, 128]], compare_op=ALU.is_ge,
                                                 fill=0.0, base=0, channel_multiplier=-1), reads=[b_c], writes=[b_c])
        P.op("pool", lambda e: e.memset(trimf[:], 0.0), reads=[b_c], writes=[b_c])
        P.op("pool", lambda e: e.affine_select(out=trimf[:], in_=trimf[:], pattern=[[1, 128]], compare_op=ALU.is_ge,
                                                 fill=-30000.0, base=0, channel_multiplier=-1), reads=[b_c], writes=[b_c])
        P.op("dve", lambda e: e.tensor_copy(out=trim[:], in_=trimf[:]), reads=[b_c], writes=[b_c])
        P.op("dve", lambda e: e.memset(onesr[:], 1.0), reads=[b_c], writes=[b_c])
        P.op("sp", lambda e: e.dma_start(out=gln[:], in_=I["mlstm_norm_g"].partition_broadcast(128)),
             reads=[b_c], writes=[b_c], dma=True)
        mqz = [sb("mqz%d" % i, [128, S], BF16) for i in range(4)]
        mk = [sb("mk%d" % i, [128, S], BF16) for i in range(2)]
        b_mqk = P.buf("mqk")
        for h in range(4):
            P.op("pool", lambda e, h=h: e.memset(mqz[h][:], 0.0), writes=[b_mqk])
        for h in range(4):
            R = slice((h % 2) * 64, (h % 2) * 64 + 64)
            P.op("sp", lambda e, h=h, R=R: e.dma_start(out=mqz[h][R, :], in_=SC["mqkT"][h // 2, R, :]), reads=DB("mqkT"),
                 writes=[b_mqk], dma=True)
        for hp in range(2):
            P.op("sp", lambda e, hp=hp: e.dma_start(out=mk[hp][:], in_=SC["mqkT"][2 + hp]), reads=DB("mqkT"),
                 writes=[b_mqk], dma=True)
        Cst = [sb("Cst%d" % i, [128, 129], F32) for i in range(2)]
        Cb = [sb("Cb%d" % i, [128, 129], BF16) for i in range(2)]
        b_Cst = P.bufs_n("Cst", 2)
        b_Cb = P.bufs_n("Cb", 2)
        va = [sb("va%d" % i, [128, 4, 129], BF16) for i in range(2)]
        b_va = P.bufs_n("va", 2)
        sgo = [sb("sgo%d" % i, [128, 512], BF16) for i in range(2)]
        b_sgo = P.bufs_n("sgo", 2)
        Sm = [sb("Sm%d" % i, [128, 2, 128], BF16) for i in range(2)]
        b_Sm = P.bufs_n("Sm", 2)
        ktm = [sb("ktm%d" % i, [128, 128], BF16) for i in range(2)]
        b_ktm = P.bufs_n("ktm", 2)
        bst = [sb("bst%d" % i, [128, 2, 6], F32) for i in range(2)]
        bag = [sb("bag%d" % i, [128, 2, 2], F32) for i in range(2)]
        sm = [sb("sm%d" % i, [128, 2, 4], F32) for i in range(2)]
        b_sm = P.bufs_n("msm", 2)
        sq = [sb("sq%d" % i, [128, 2, 1], F32) for i in range(2)]
        b_sq = P.bufs_n("msq", 2)
        hn = [sb("hn%d" % i, [128, 512], F32) for i in range(2)]
        b_hn = P.bufs_n("hn", 2)
        ya = [sb("ya%d" % i, [128, 512], BF16) for i in range(2)]
        b_ya = P.bufs_n("ya", 2)
        yaT = [sb("yaT%d" % i, [128, 4, 128], BF16) for i in range(2)]
        b_yaT = P.bufs_n("yaTs", 2)
        pSm = ps("pSm", [128, 2, 128], F32)
        pOm = ps("pOm", [128, 2, 129], F32)
        pU = ps("pU", [128, 2, 129], F32)
        pT5 = ps("pT5", [128, 5, 128], BF16)
        pkt = pT5[:, 4, :]
        pyT = pT5[:, 0:4, :]
        b_pSm, b_pOm, b_pU, b_pkt, b_pyT = [P.buf(n) for n in ("pSm", "pOm", "pU", "pkt", "pyT")]

        def mlstm_chunk(c):
            cols = slice(c * 128, (c + 1) * 128)
            rows = slice(c * 128, (c + 1) * 128)
            vi = c % 2
            P.op("sp", lambda e: e.dma_start(out=va[vi][:], in_=SC["vaugM"][rows]), reads=[DB("vaugM")[c]],
                 writes=[b_va[vi]], dma=True)
            P.op("sp", lambda e: e.dma_start(out=sgo[vi][:], in_=SC["so"][rows]), reads=[DB("so")[c]],
                 writes=[b_sgo[vi]], dma=True)
            hnb = hn[vi]
            for hp in range(2):
                si = hp
                for hh in range(2):
                    P.op("pe", lambda e, hp=hp, hh=hh: e.matmul(
                        pSm[:, hh, :], lhsT=mk[hp][:, cols], rhs=mqz[2 * hp + hh][:, cols], start=True, stop=True),
                        reads=[b_mqk], writes=[b_pSm])
                P.op("pe", lambda e, hp=hp: e.transpose(out=pkt, in_=mk[hp][:, cols], identity=C.ident[:]),
                     reads=[b_mqk, C.b_const], writes=[b_pkt])
                P.op("dve", lambda e, si=si: e.scalar_tensor_tensor(
                    out=Sm[si][:], in0=pSm[:], scalar=0.125,
                    in1=mask01[:].unsqueeze(1).to_broadcast([128, 2, 128]), op0=ALU.mult, op1=ALU.mult),
                    reads=[b_pSm, b_c], writes=[b_Sm[si]])
                P.op("act", lambda e, si=si: e.activation(out=ktm[si][:], in_=pkt, func=AF.Copy, scale=0.125),
                     reads=[b_pkt], writes=[b_ktm[si]])
                yield
                for hh in range(2):
                    h = 2 * hp + hh
                    P.op("pe", lambda e, si=si, hh=hh, h=h: e.matmul(
                        pOm[:, hh, :], lhsT=Sm[si][:, hh, :], rhs=va[vi][:, h, :], start=True, stop=(c == 0)),
                        reads=[b_Sm[si], b_va[vi]], writes=[b_pOm])
                    if c > 0:
                        P.op("pe", lambda e, hp=hp, hh=hh, h=h: e.matmul(
                            pOm[:, hh, :], lhsT=mqz[h][:, cols], rhs=Cb[hp][:, :], start=False, stop=True),
                            reads=[b_mqk, b_Cb[hp]], writes=[b_pOm])
                for hh in range(2):
                    h = 2 * hp + hh
                    P.op("pe", lambda e, si=si, hh=hh, h=h: e.matmul(
                        pU[:, hh, :], lhsT=ktm[si][:], rhs=va[vi][:, h, :], start=True, stop=True),
                        reads=[b_ktm[si], b_va[vi]], writes=[b_pU])
                for hh in range(2):
                    h = 2 * hp + hh
                    R = slice(hh * 64, (hh + 1) * 64)
                    if c == 0:
                        P.op("dve", lambda e, hp=hp, hh=hh, R=R: e.tensor_copy(out=Cst[hp][R, :], in_=pU[R, hh, :]),
                             reads=[b_pU], writes=[b_Cst[hp]])
                    else:
                        P.op("dve", lambda e, hp=hp, hh=hh, R=R, h=h: e.scalar_tensor_tensor(
                            out=Cst[hp][R, :], in0=Cst[hp][R, :], scalar=C.decbc[R, h, c:c + 1], in1=pU[R, hh, :],
                            op0=ALU.mult, op1=ALU.add), reads=[b_pU, b_Cst[hp], C.b_decbc], writes=[b_Cst[hp]])
                    if c < 31:
                        P.op("dve", lambda e, hp=hp, R=R, h=h: e.tensor_scalar(
                            out=Cb[hp][R, :], in0=Cst[hp][R, :], scalar1=C.decbc[R, h, c + 1:c + 2], scalar2=None,
                            op0=ALU.mult), reads=[b_Cst[hp], C.b_decbc], writes=[b_Cb[hp]])
                smp, bstp, bagp, bsm = sm[hp], bst[hp], bag[hp], b_sm[hp]
                for hh in range(2):
                    P.op("dve", lambda e, hh=hh, bstp=bstp: e.bn_stats(out=bstp[:, hh, :], in_=pOm[:, hh, 0:128]),
                         reads=[b_pOm], writes=[bsm])
                    P.op("dve", lambda e, hh=hh, bstp=bstp, bagp=bagp: e.bn_aggr(out=bagp[:, hh, :], in_=bstp[:, hh, :]),
                         reads=[bsm], writes=[bsm])
                sqp, bsq = sq[hp], b_sq[hp]
                P.op("act", lambda e, sqp=sqp: e.activation(out=sqp[:], in_=pOm[:, :, 128:129], func=AF.Square),
                     reads=[b_pOm], writes=[bsq])
                P.op("dve", lambda e, hp=hp, smp=smp, sqp=sqp: e.tensor_tensor(
                    out=smp[:, :, 0:1], in0=sqp[:], in1=C.wthr[:, c, 4 + 2 * hp:6 + 2 * hp].unsqueeze(2),
                    op=ALU.max), reads=[C.b_wthr, bsm, bsq], writes=[bsm])
                P.op("dve", lambda e, smp=smp, bagp=bagp: e.scalar_tensor_tensor(
                    out=smp[:, :, 1:2], in0=smp[:, :, 0:1], scalar=EPS, in1=bagp[:, :, 1:2], op0=ALU.mult, op1=ALU.add),
                    reads=[bsm], writes=[bsm])
                P.op("pool", lambda e, smp=smp: e.tensor_tensor(
                    out=smp[:, :, 2:3], in0=smp[:, :, 1:2],
                    in1=C.neghalf[:].unsqueeze(1).to_broadcast([128, 2, 1]), op=ALU.pow),
                    reads=[bsm, C.b_const], writes=[bsm])
                for hh in range(2):
                    h = 2 * hp + hh
                    P.op("dve", lambda e, hh=hh, h=h, smp=smp, bagp=bagp: e.tensor_scalar(
                        out=hnb[:, h * 128:(h + 1) * 128], in0=pOm[:, hh, 0:128], scalar1=bagp[:, hh, 0:1],
                        scalar2=smp[:, hh, 2:3], op0=ALU.subtract, op1=ALU.mult),
                        reads=[b_pOm, bsm], writes=[b_hn[vi]])
                yield
            yi = c % 2
            P.op("pool", lambda e: e.tensor_tensor(out=hnb[:], in0=hnb[:], in1=gln[:], op=ALU.mult),
                 reads=[b_hn[vi], b_c], writes=[b_hn[vi]])
            P.op("dve", lambda e: e.tensor_tensor(out=ya[yi][:], in0=hnb[:], in1=sgo[vi][:], op=ALU.mult),
                 reads=[b_hn[vi], b_sgo[vi]], writes=[b_ya[yi]])
            yield
            for k in range(4):
                P.op("pe", lambda e, k=k: e.transpose(out=pyT[:, k, :], in_=ya[yi][:, k * 128:(k + 1) * 128],
                                                      identity=C.ident[:]),
                     reads=[b_ya[yi], C.b_const], writes=[b_pyT])
            P.op("act", lambda e: e.activation(out=yaT[yi][:], in_=pyT, func=AF.Copy), reads=[b_pyT],
                 writes=[b_yaT[yi]])
            P.op("sp", lambda e: e.dma_start(out=SC["yaT"][:, :, cols].rearrange("k p t -> p k t"), in_=yaT[yi][:]),
                 reads=[b_yaT[yi]], writes=[DB("yaT")[c]], dma=True)
            yield

        def mlstm_gen():
            for c in range(32):
                yield from mlstm_chunk(c)

        VF = sb("VF", [128, 32, 8 * 65], BF16)
        b_VF = P.buf("VF")
        P.op("sp", lambda e: e.dma_start(out=VF[:], in_=SC["vaugF"].rearrange("(j p) h e -> p j (h e)", p=128)),
             reads=DB("vaugF"), writes=[b_VF], dma=True)
        QA = [sb("QA%d" % i, [70, S], BF16) for i in range(2)]
        KA = [sb("KA%d" % i, [70, S], BF16) for i in range(2)]
        b_QA = P.bufs_n("QA", 2)
        b_KA = P.bufs_n("KA", 2)
        PT = [sb("PT%d" % i, [128, 512], BF16) for i in range(3)]
        b_PT = P.bufs_n("PT", 3)
        rec = [sb("rec%d" % i, [1, 512], F32) for i in range(2)]
        b_rec = P.bufs_n("rec", 2)
        b_recd = P.bufs_n("recd", 2)
        osb = [sb("osb%d" % i, [65, 512], F32) for i in range(2)]
        b_osb = P.bufs_n("osb", 2)
        bcs = [sb("bcs%d" % i, [65, 512], F32) for i in range(2)]
        b_bcs = P.bufs_n("bcs", 2)
        ybt = [sb("ybt%d" % i, [65, 512], BF16) for i in range(2)]
        b_ybt = P.bufs_n("ybt", 2)
        pS = [ps("pS%d" % i, [128, 512], F32) for i in range(3)]
        b_pS = P.bufs_n("pS", 3)
        slot_of = {}
        pO = ps("pO", [128, 512], F32)
        b_pO = P.buf("pO")
        cnt = {"s": 0, "y": 0}

        def fox_load(h):
            hb = h % 2
            P.op("sp", lambda e: e.dma_start(out=QA[hb][:], in_=SC["qaug"][h]), reads=DB("aug"), writes=[b_QA[hb]], dma=True)
            P.op("sp", lambda e: e.dma_start(out=KA[hb][:], in_=SC["kaug"][h]), reads=DB("aug"), writes=[b_KA[hb]], dma=True)

        seq = [(h, i, j) for h in range(8) for i in range(8) for j in range(4 * i + 4)]

        def emit_S(idx):
            h, i, j = seq[idx]
            hb = h % 2
            sj = cnt["s"] % 3
            cnt["s"] += 1
            slot_of[idx] = sj
            jj = j - 4 * i
            kc = slice(j * 128, (j + 1) * 128)
            rd = [b_KA[hb], b_QA[hb]]
            if jj < 0:
                P.op("pe", lambda e: e.matmul(
                    pS[sj][:, 0:512], lhsT=KA[hb][:, kc], rhs=QA[hb][:, i * 512:(i + 1) * 512], start=True, stop=True),
                    reads=rd, writes=[b_pS[sj]])
            else:
                qs = jj * 128
                wq = 512 - qs
                q0 = i * 512 + qs
                P.op("pe", lambda e: e.matmul(pS[sj][:, 0:128], lhsT=C.ident[:], rhs=trim[:], start=True, stop=False),
                     reads=[C.b_const, b_c], writes=[b_pS[sj]])
                P.op("pe", lambda e: e.matmul(
                    pS[sj][:, 0:128], lhsT=KA[hb][:, kc], rhs=QA[hb][:, q0:q0 + 128], start=False, stop=True),
                    reads=rd, writes=[b_pS[sj]])
                if wq > 128:
                    P.op("pe", lambda e: e.matmul(
                        pS[sj][:, 128:wq], lhsT=KA[hb][:, kc], rhs=QA[hb][:, q0 + 128:q0 + wq], start=True, stop=True),
                        reads=rd, writes=[b_pS[sj]])

        def emit_rest(idx):
            h, i, j = seq[idx]
            sj = slot_of[idx]
            nkb = 4 * i + 4
            jj = j - 4 * i
            qs = max(jj, 0) * 128
            wq = 512 - qs
            tj = idx % 3
            P.op("act", lambda e: e.activation(out=PT[tj][:, 0:wq], in_=pS[sj][:, 0:wq], func=AF.Exp),
                 reads=[b_pS[sj]], writes=[b_PT[tj]])
            P.op("pe", lambda e: e.matmul(
                pO[0:65, qs:512], lhsT=VF[:, j, h * 65:(h + 1) * 65], rhs=PT[tj][:, 0:wq],
                start=(j == 0), stop=(j == nkb - 1)),
                reads=[b_VF, b_PT[tj]], writes=[b_pO])
            if j < nkb - 1:
                return
            yi = cnt["y"] % 2
            cnt["y"] += 1
            u = h * 8 + i
            P.op("act", lambda e: e.activation(out=osb[yi][:], in_=pO[0:65, :], func=AF.Copy), reads=[b_pO],
                 writes=[b_osb[yi]])
            P.op("dve", lambda e: e.reciprocal(out=rec[yi][0:1, :], in_=osb[yi][0:1, :]), reads=[b_osb[yi]],
                 writes=[b_rec[yi]])
            P.op("sp", lambda e: e.dma_start(out=SC["recd"][u:u + 1, :], in_=rec[yi][0:1, :]), reads=[b_rec[yi]],
                 writes=[b_recd[yi]], dma=True)
            P.op("sp", lambda e: e.dma_start(out=bcs[yi][:], in_=SC["recd"][u:u + 1, :].partition_broadcast(65)),
                 reads=[b_recd[yi]], writes=[b_bcs[yi]], dma=True)
            P.op("dve", lambda e: e.tensor_tensor(out=ybt[yi][:], in0=osb[yi][:], in1=bcs[yi][:], op=ALU.mult),
                 reads=[b_osb[yi], b_bcs[yi]], writes=[b_ybt[yi]])
            P.op("sp", lambda e: e.dma_start(
                out=SC["ybT"][h // 2, (h % 2) * 64:(h % 2) * 64 + 64, i * 512:(i + 1) * 512], in_=ybt[yi][1:65, :]),
                reads=[b_ybt[yi]], writes=[DB("ybT")[(h * 8 + i) % 32]], dma=True)

        gen = mlstm_gen()
        fox_load(0)
        emit_S(0)
        emit_S(1)
        for idx, (h, i, j) in enumerate(seq):
            if i == 0 and j == 0 and h + 1 < 8:
                fox_load(h + 1)
            if idx + 2 < len(seq):
                emit_S(idx + 2)
            emit_rest(idx)
            if idx % 6 == 5:
                next(gen, None)
        for _ in gen:
            pass
        P.flush()


def merge_pass(P, nc, C, I, SC, DB, h1, h1_b, h2, h2_b):
    with contextlib.ExitStack() as st:
        sb = lambda name, shape, dt: st.enter_context(nc.sbuf_tensor("mg" + name, shape, dt))
        ps = lambda name, shape, dt: st.enter_context(nc.psum_tensor("mg" + name, shape, dt))
        Wa = sb("Wa", [128, 4, D], BF16)
        Wb = sb("Wb", [128, 4, D], BF16)
        Wo = sb("Wo", [128, 8, D], BF16)
        b_W = P.buf("mgW")
        P.op("pool", lambda e: e.dma_start(out=Wa[:], in_=I["w_branch_a"].rearrange("(k p) d -> p k d", p=128)),
             writes=[b_W], dma=True)
        P.op("pool", lambda e: e.dma_start(out=Wb[:], in_=I["w_branch_b"].rearrange("(k p) d -> p k d", p=128)),
             writes=[b_W], dma=True)
        P.op("pool", lambda e: e.dma_start(out=Wo[:], in_=I["w_out"].rearrange("(k p) d -> p k d", p=128)),
             writes=[b_W], dma=True)
        gpost = sb("gpost", [128, D], F32)
        P.op("sp", lambda e: e.dma_start(out=gpost[:], in_=I["mix_post_g"].partition_broadcast(128)),
             writes=[b_W], dma=True)
        yaT = [sb("yaT%d" % i, [128, 4, 128], BF16) for i in range(2)]
        ybT = [sb("ybT%d" % i, [128, 4, 128], BF16) for i in range(2)]
        gab = [sb("gab%d" % i, [128, 2, D], BF16) for i in range(2)]
        hin = [sb("hin%d" % i, [128, D], F32) for i in range(2)]
        b_in = P.bufs_n("mgin", 2)
        b_hin = P.bufs_n("mghin", 2)
        t1 = [sb("t1%d" % i, [128, D], F32) for i in range(2)]
        t2 = [sb("t2%d" % i, [128, D], F32) for i in range(2)]
        mb = [sb("mb%d" % i, [128, D], BF16) for i in range(2)]
        mT = [sb("mT%d" % i, [128, 8, 128], BF16) for i in range(2)]
        junk = sb("junk", [128, D], BF16)
        hout = [sb("hout%d" % i, [128, D], F32) for i in range(2)]
        stat = sb("stat", [128, 6], F32)
        b_t1, b_t2, b_mb, b_mT = [P.bufs_n(n, 2) for n in ("t1", "t2", "mb", "mT")]
        b_junk = P.buf("mgjunk")
        b_hout = P.bufs_n("hout", 2)
        b_stat = P.bufs_n("mgstat", 2)
        pA = ps("pA", [128, D], F32)
        pB = ps("pB", [128, D], F32)
        pO = ps("pO", [128, D], F32)
        pt = ps("pt", [128, 8, 128], BF16)
        b_pA, b_pB, b_pO, b_pt = [P.buf(n) for n in ("pA", "pB", "mgpO", "mgpt")]
        h1v = h1.rearrange("(n p) d -> n p d", p=128)
        h2v = h2.rearrange("(n p) d -> n p d", p=128)

        def s1(n):
            ib = n % 2
            rows = slice(n * 128, (n + 1) * 128)
            cols = rows
            P.op("sp", lambda e: e.dma_start(out=yaT[ib][:], in_=SC["yaT"][:, :, cols].rearrange("k p t -> p k t")),
                 reads=[DB("yaT")[n]], writes=[b_in[ib]], dma=True)
            P.op("sp", lambda e: e.dma_start(out=ybT[ib][:], in_=SC["ybT"][:, :, cols].rearrange("k p t -> p k t")),
                 reads=DB("ybT"), writes=[b_in[ib]], dma=True)
            P.op("sp", lambda e: e.dma_start(out=gab[ib][:, 0, :], in_=SC["ga"][rows]),
                 reads=[DB("gab")[n]], writes=[b_in[ib]], dma=True)
            P.op("sp", lambda e: e.dma_start(out=gab[ib][:, 1, :], in_=SC["gb"][rows]),
                 reads=[DB("gab")[n]], writes=[b_in[ib]], dma=True)
            P.op("sp", lambda e: e.dma_start(out=hin[ib][:], in_=h1v[n]),
                 reads=[h1_b[n]], writes=[b_hin[ib]], dma=True)
            for hf in range(2):
                hs = slice(hf * 512, (hf + 1) * 512)
                for k in range(4):
                    P.op("pe", lambda e, k=k, hs=hs: e.matmul(pA[:, hs], lhsT=yaT[ib][:, k, :], rhs=Wa[:, k, hs],
                                                              start=(k == 0), stop=(k == 3)),
                         reads=[b_in[ib], b_W], writes=[b_pA])
            for hf in range(2):
                hs = slice(hf * 512, (hf + 1) * 512)
                for k in range(4):
                    P.op("pe", lambda e, k=k, hs=hs: e.matmul(pB[:, hs], lhsT=ybT[ib][:, k, :], rhs=Wb[:, k, hs],
                                                              start=(k == 0), stop=(k == 3)),
                         reads=[b_in[ib], b_W], writes=[b_pB])
            P.op("dve", lambda e: e.tensor_tensor(out=t1[ib][:], in0=pA[:], in1=gab[ib][:, 0, :], op=ALU.mult),
                 reads=[b_pA, b_in[ib]], writes=[b_t1[ib]])
            P.op("dve", lambda e: e.tensor_tensor(out=t2[ib][:], in0=pB[:], in1=gab[ib][:, 1, :], op=ALU.mult),
                 reads=[b_pB, b_in[ib]], writes=[b_t2[ib]])
            P.op("pool", lambda e: e.tensor_tensor(out=mb[ib][:], in0=t1[ib][:], in1=t2[ib][:], op=ALU.add),
                 reads=[b_t1[ib], b_t2[ib]], writes=[b_mb[ib]])

        def s2(n):
            ib = n % 2
            for k in range(8):
                P.op("pe", lambda e, k=k: e.transpose(out=pt[:, k, :], in_=mb[ib][:, k * 128:(k + 1) * 128],
                                                      identity=C.ident[:]),
                     reads=[b_mb[ib], C.b_const], writes=[b_pt])
            P.op("act", lambda e: e.activation(out=mT[ib][:], in_=pt[:], func=AF.Copy), reads=[b_pt], writes=[b_mT[ib]])
            for hf in range(2):
                hs = slice(hf * 512, (hf + 1) * 512)
                for k in range(8):
                    P.op("pe", lambda e, k=k, hs=hs: e.matmul(pO[:, hs], lhsT=mT[ib][:, k, :], rhs=Wo[:, k, hs],
                                                              start=(k == 0), stop=(k == 7)),
                         reads=[b_mT[ib], b_W], writes=[b_pO])
            si = n % 2
            ss, var, rstd = (stat[:, 3 * si + j:3 * si + j + 1] for j in range(3))
            rms_stats(P, C, pO[:], b_pO, junk[:], b_junk, ss, var, rstd, b_stat[si])
            P.op("dve", lambda e: e.scalar_tensor_tensor(
                out=hout[ib][:], in0=pO[:], scalar=rstd, in1=gpost[:], op0=ALU.mult, op1=ALU.mult),
                reads=[b_pO, b_stat[si], b_W], writes=[b_hout[ib]])
            P.op("pool", lambda e: e.tensor_tensor(out=hout[ib][:], in0=hout[ib][:], in1=hin[ib][:], op=ALU.add),
                 reads=[b_hout[ib], b_hin[ib]], writes=[b_hout[ib]])
            P.op("pool", lambda e: e.dma_start(out=h2v[n], in_=hout[ib][:]),
                 reads=[b_hout[ib]], writes=[h2_b[n]], dma=True)

        s1(0)
        for n in range(32):
            if n + 1 < 32:
                s1(n + 1)
            s2(n)
        P.flush()


def ple_pass(P, nc, C, I, uT3, uT3_b, h3, h3_b, out):
    with contextlib.ExitStack() as st:
        sb = lambda name, shape, dt: st.enter_context(nc.sbuf_tensor("pl" + name, shape, dt))
        ps = lambda name, shape, dt: st.enter_context(nc.psum_tensor("pl" + name, shape, dt))
        Wg = sb("Wg", [128, 8, D], BF16)
        Wp = sb("Wp", [128, 2, D], BF16)
        b_W = P.buf("plW")
        P.op("pool", lambda e: e.dma_start(out=Wg[:], in_=I["ple_w_gate"].rearrange("(k p) d -> p k d", p=128)),
             writes=[b_W], dma=True)
        P.op("pool", lambda e: e.dma_start(out=Wp[:], in_=I["ple_w_proj"].rearrange("(k p) d -> p k d", p=128)),
             writes=[b_W], dma=True)
        gpost = sb("gpost", [128, D], F32)
        bg = sb("bg", [128, D], F32)
        P.op("sp", lambda e: e.dma_start(out=gpost[:], in_=I["ple_post_g"].partition_broadcast(128)),
             writes=[b_W], dma=True)
        P.op("sp", lambda e: e.dma_start(out=bg[:], in_=I["ple_b_gate"].partition_broadcast(128)),
             writes=[b_W], dma=True)
        uT = [sb("uT%d" % i, [128, 8, 128], BF16) for i in range(2)]
        pin = [sb("pin%d" % i, [128, 256], F32) for i in range(2)]
        hin = [sb("hin%d" % i, [128, D], F32) for i in range(2)]
        b_in = P.bufs_n("plin", 2)
        b_hin = P.bufs_n("plhin", 2)
        pb = [sb("pb%d" % i, [128, 256], BF16) for i in range(2)]
        pT = [sb("pT%d" % i, [128, 2, 128], BF16) for i in range(2)]
        gt = [sb("gt%d" % i, [128, D], F32) for i in range(2)]
        ge = [sb("ge%d" % i, [128, D], F32) for i in range(2)]
        junk = sb("junk", [128, D], BF16)
        hout = [sb("hout%d" % i, [128, D], F32) for i in range(2)]
        stat = sb("stat", [128, 6], F32)
        b_pb, b_pT, b_gt, b_ge = [P.bufs_n(n, 2) for n in ("pb", "pT", "gt", "ge")]
        b_junk = P.buf("pljunk")
        b_hout = P.bufs_n("plhout", 2)
        b_stat = P.bufs_n("plstat", 2)
        pG = [ps("pG%d" % i, [128, D], F32) for i in range(2)]
        pE = ps("pE", [128, D], F32)
        ptp = ps("ptp", [128, 2, 128], BF16)
        b_pG = P.bufs_n("pG", 2)
        b_pE, b_ptp = P.buf("pE"), P.buf("ptp")
        pv = I["p"].rearrange("(n p) d -> n p d", p=128)
        h3v = h3.rearrange("(n p) d -> n p d", p=128)
        ov = out.rearrange("(n p) d -> n p d", p=128)

        def s1(n):
            ib = n % 2
            cols = slice(n * 128, (n + 1) * 128)
            P.op("sp", lambda e: e.dma_start(out=uT[ib][:], in_=uT3[:, :, cols].rearrange("k p t -> p k t")),
                 reads=[uT3_b[n]], writes=[b_in[ib]], dma=True)
            P.op("sp", lambda e: e.dma_start(out=pin[ib][:], in_=pv[n]), writes=[b_in[ib]], dma=True)
            P.op("sp", lambda e: e.dma_start(out=hin[ib][:], in_=h3v[n]), reads=[h3_b[n]],
                 writes=[b_hin[ib]], dma=True)
            P.op("act", lambda e: e.activation(out=pb[ib][:], in_=pin[ib][:], func=AF.Copy), reads=[b_in[ib]],
                 writes=[b_pb[ib]])
            for k in range(2):
                P.op("pe", lambda e, k=k: e.transpose(out=ptp[:, k, :], in_=pb[ib][:, k * 128:(k + 1) * 128],
                                                      identity=C.ident[:]),
                     reads=[b_pb[ib], C.b_const], writes=[b_ptp])
            P.op("dve", lambda e: e.tensor_copy(out=pT[ib][:], in_=ptp[:]), reads=[b_ptp], writes=[b_pT[ib]])
            for hf in range(2):
                hs = slice(hf * 512, (hf + 1) * 512)
                for k in range(8):
                    P.op("pe", lambda e, k=k, hs=hs: e.matmul(pG[ib][:, hs], lhsT=uT[ib][:, k, :], rhs=Wg[:, k, hs],
                                                              start=(k == 0), stop=(k == 7)),
                         reads=[b_in[ib], b_W], writes=[b_pG[ib]])

        def s2(n):
            ib = n % 2
            for hf in range(2):
                hs = slice(hf * 512, (hf + 1) * 512)
                for k in range(2):
                    P.op("pe", lambda e, k=k, hs=hs: e.matmul(pE[:, hs], lhsT=pT[ib][:, k, :], rhs=Wp[:, k, hs],
                                                              start=(k == 0), stop=(k == 1)),
                         reads=[b_pT[ib], b_W], writes=[b_pE])
            P.op("dve", lambda e: e.tensor_tensor(out=gt[ib][:], in0=pG[ib][:], in1=bg[:], op=ALU.add),
                 reads=[b_pG[ib], b_W], writes=[b_gt[ib]])
            P.op("act", lambda e: e.activation(out=gt[ib][:], in_=gt[ib][:], func=AF.Sigmoid), reads=[b_gt[ib]],
                 writes=[b_gt[ib]])
            P.op("dve", lambda e: e.tensor_tensor(out=ge[ib][:], in0=gt[ib][:], in1=pE[:], op=ALU.mult),
                 reads=[b_gt[ib], b_pE], writes=[b_ge[ib]])
            si = n % 2
            ss, var, rstd = (stat[:, 3 * si + j:3 * si + j + 1] for j in range(3))
            rms_stats(P, C, ge[ib][:], b_ge[ib], junk[:], b_junk, ss, var, rstd, b_stat[si])
            P.op("dve", lambda e: e.scalar_tensor_tensor(
                out=hout[ib][:], in0=ge[ib][:], scalar=rstd, in1=gpost[:], op0=ALU.mult, op1=ALU.mult),
                reads=[b_ge[ib], b_stat[si], b_W], writes=[b_hout[ib]])
            P.op("pool", lambda e: e.tensor_tensor(out=hout[ib][:], in0=hout[ib][:], in1=hin[ib][:], op=ALU.add),
                 reads=[b_hout[ib], b_hin[ib]], writes=[b_hout[ib]])
            P.op("pool", lambda e: e.dma_start(out=ov[n], in_=hout[ib][:]), reads=[b_hout[ib]], dma=True)

        s1(0)
        for n in range(32):
            if n + 1 < 32:
                s1(n + 1)
            s2(n)
        P.flush()


def build_program(debug=False, stage=99, only=None):
    nc = bass.Bass("TRN2", target_bir_lowering=False)
    I = {}

    def din(name, shape):
        I[name] = nc.dram_tensor(name, shape, F32, kind="ExternalInput").ap()
        return I[name]

    din("x", [S, D])
    din("p", [S, 256])
    for nm in ("ffn1", "ffn2"):
        din(nm + "_pre_g", [1, D])
        din(nm + "_w_gate", [D, DFF])
        din(nm + "_w_up", [D, DFF])
        din(nm + "_w_down", [DFF, D])
        din(nm + "_post_g", [1, D])
    din("mix_pre_g", [1, D])
    din("w_in", [D, INW])
    din("conv_w", [4, 512])
    din("conv_b", [1, 512])
    din("mlstm_i_bias", [1, 4])
    din("mlstm_f_bias", [1, 4])
    din("mlstm_norm_g", [1, 512])
    din("fox_f_bias", [1, 8])
    din("branch_gate_bias", [1, 2048])
    din("w_branch_a", [512, D])
    din("w_branch_b", [512, D])
    din("w_out", [D, D])
    din("mix_post_g", [1, D])
    din("ple_pre_g", [1, D])
    din("ple_w_gate", [D, D])
    din("ple_b_gate", [1, D])
    din("ple_w_proj", [256, D])
    din("ple_post_g", [1, D])

    skind = "ExternalOutput" if debug else "Internal"

    def dscr(name, shape, dt):
        return nc.dram_tensor(name, shape, dt, kind=skind).ap()

    out = nc.dram_tensor("out", [S, D], F32, kind="ExternalOutput").ap()
    h1 = dscr("h1", [S, D], F32)
    uT1 = dscr("uT1", [8, 128, S], BF16)
    gpre = dscr("gpre", [72, S], F32)
    SC = {
        "mqkT": dscr("mqkT", [4, 128, S], BF16),
        "vaugM": dscr("vaugM", [S, 4, 129], BF16),
        "so": dscr("so", [S, 512], BF16),
        "qaug": dscr("qaug", [8, 70, S], BF16),
        "kaug": dscr("kaug", [8, 70, S], BF16),
        "vaugF": dscr("vaugF", [S, 8, 65], BF16),
        "ga": dscr("ga", [S, D], BF16),
        "gb": dscr("gb", [S, D], BF16),
        "yaT": dscr("yaT", [4, 128, S], BF16),
        "ybT": dscr("ybT", [4, 128, S], BF16),
        "recd": dscr("recd", [64, 512], F32),
    }
    h2 = dscr("h2", [S, D], F32)
    h3 = dscr("h3", [S, D], F32)
    uT3 = dscr("uT3", [8, 128, S], BF16)

    with contextlib.ExitStack() as st:
        P = Prog(nc, st)
        C = Ctx()
        C.db = {}

        def db(name):
            if name not in C.db:
                C.db[name] = P.bufs_n("D" + name, 32)
            return C.db[name]

        setup_consts(P, nc, st, C)
        C.wthr = st.enter_context(nc.sbuf_tensor("wthr", [128, 32, 8], F32))
        C.decbc = st.enter_context(nc.sbuf_tensor("decbc", [128, 4, 32], F32))
        C.b_wthr = P.buf("wthr")
        C.b_decbc = P.buf("decbc")
        def want(name, st_no):
            return (name in only) if only is not None else (stage >= st_no)

        if want("ffn1", 1):
            ffn_pass(P, nc, C, "f1", I["x"], I["ffn1_w_gate"], I["ffn1_w_up"], I["ffn1_w_down"],
                     I["ffn1_pre_g"], I["ffn1_post_g"], h1, I["mix_pre_g"], uT1,
                     db("x"), db("h1"), db("uT1"), gate_w=I["w_in"], gate_dst=gpre, gate_b=db("gpre"))
        if want("gp", 2):
            aug_b = P.buf("augrows")
            gp_stage(P, nc, C, I, gpre, db("gpre"), SC["qaug"], SC["kaug"], aug_b)
            db("aug").append(aug_b)
        if want("win", 2):
            win_pass(P, nc, C, I, uT1, db("uT1"), SC, db)
        if want("mix", 3):
            mix_pass(P, nc, C, I, SC, db)
        if want("merge", 4):
            merge_pass(P, nc, C, I, SC, db, h1, db("h1"), h2, db("h2"))
        if want("ffn2", 5):
            ffn_pass(P, nc, C, "f2", h2, I["ffn2_w_gate"], I["ffn2_w_up"], I["ffn2_w_down"],
                     I["ffn2_pre_g"], I["ffn2_post_g"], h3, I["ple_pre_g"], uT3,
                     db("h2"), db("h3"), db("uT3"))
        if want("ple", 6):
            ple_pass(P, nc, C, I, uT3, db("uT3"), h3, db("h3"), out)
        P.flush(final=True)
    return nc

IN_NAMES = ["x", "p", "ffn1_pre_g", "ffn1_w_gate", "ffn1_w_up", "ffn1_w_down", "ffn1_post_g",
            "mix_pre_g", "w_in", "conv_w", "conv_b", "mlstm_i_bias", "mlstm_f_bias", "mlstm_norm_g",
            "fox_f_bias", "branch_gate_bias", "w_branch_a", "w_branch_b", "w_out", "mix_post_g",
            "ffn2_pre_g", "ffn2_w_gate", "ffn2_w_up", "ffn2_w_down", "ffn2_post_g",
            "ple_pre_g", "ple_w_gate", "ple_b_gate", "ple_w_proj", "ple_post_g"]


def make_in_maps(inputs, cores):
    maps = []
    shared = {}
    for k in IN_NAMES:
        if k in ("x", "p"):
            continue
        shared[k] = np.ascontiguousarray(np.asarray(inputs[k])[0], dtype=np.float32)
    x = np.asarray(inputs["x"])
    p = np.asarray(inputs["p"])
    for b in cores:
        m = dict(shared)
        m["x"] = np.ascontiguousarray(x[b], dtype=np.float32)
        m["p"] = np.ascontiguousarray(p[0, b], dtype=np.float32)
        maps.append(m)
    return maps


def kernel(**inputs):
    nc = build_program()
    maps = make_in_maps(inputs, list(range(8)))
    res = run_bass_kernel_spmd(nc, maps, core_ids=list(range(8)))
    return np.stack([np.asarray(r["out"], dtype=np.float32) for r in res.results], axis=0)
```

```python
import contextlib
import numpy as np
import concourse.bass as bass
import concourse.mybir as mybir
from concourse.bass_utils import run_bass_kernel_spmd

F32 = mybir.dt.float32
BF16 = mybir.dt.bfloat16
AF = mybir.ActivationFunctionType
ALU = mybir.AluOpType
AX = mybir.AxisListType

S = 4096
D = 1024
DFF = 2816
NFC = DFF // 128
NT = 8
TT = 512
EPS = 1e-6
INW = 5136

ENGS = ("pe", "act", "dve", "pool", "sp")


class Buf:
    __slots__ = ("name", "w", "r")

    def __init__(self, name=""):
        self.name = name
        self.w = None
        self.r = []


class Op:
    __slots__ = ("eng", "fn", "deps", "inc", "cnt", "dma", "sem", "emitted")

    def __init__(self, eng, fn, dma=False):
        self.eng = eng
        self.fn = fn
        self.deps = []
        self.inc = False
        self.cnt = 0
        self.dma = dma
        self.sem = None
        self.emitted = False


class Prog:
    def __init__(self, nc, st, n_dma_sems=20):
        self.nc = nc
        self.pending = {e: [] for e in ENGS}
        self.bufs = []
        self.nd = n_dma_sems
        self.esem = {e: st.enter_context(nc.semaphore("s_" + e)) for e in ENGS}
        self.dsem = {}
        for e in ("sp", "pool"):
            for s in range(n_dma_sems):
                self.dsem[(e, s)] = st.enter_context(nc.semaphore("d_%s_%d" % (e, s)))
        self.ecnt = {e: 0 for e in ENGS}
        self.dcnt = {e: 0 for e in ENGS}
        self.waited = {e: {} for e in ENGS}
        self.n_ops = 0

    def buf(self, name=""):
        b = Buf(name)
        self.bufs.append(b)
        return b

    def bufs_n(self, name, n):
        return [self.buf("%s%d" % (name, i)) for i in range(n)]

    def op(self, eng, fn, reads=(), writes=(), dma=False):
        o = Op(eng, fn, dma)
        seen = set()
        cand = []
        for b in reads:
            if b.w is not None:
                cand.append(b.w)
        for b in writes:
            if b.w is not None:
                cand.append(b.w)
            cand.extend(b.r)
        for d in cand:
            if d is o or id(d) in seen:
                continue
            seen.add(id(d))
            if d.eng == "pe" and eng == "pe" and not d.dma and not dma:
                continue
            o.deps.append(d)
            if not d.emitted:
                d.inc = True
        for b in reads:
            b.r.append(o)
        for b in writes:
            b.w = o
            b.r = []
        self.pending[eng].append(o)
        self.n_ops += 1
        return o

    def flush(self, final=False):
        nc = self.nc
        for b in self.bufs:
            if b.w is not None and not b.w.emitted:
                b.w.inc = True
            for r in b.r:
                if not r.emitted:
                    r.inc = True
        for e in ENGS:
            for o in self.pending[e]:
                if o.dma:
                    k = self.dcnt[e]
                    o.sem = (e, k % self.nd)
                    o.cnt = 16 * (k // self.nd + 1)
                    self.dcnt[e] = k + 1
                elif o.inc:
                    self.ecnt[e] += 1
                    o.cnt = self.ecnt[e]
        pending = self.pending
        self.pending = {e: [] for e in ENGS}

        def run(ename, eng):
            waited = self.waited[ename]
            for o in pending[ename]:
                for d in o.deps:
                    key = d.sem if d.dma else d.eng
                    if waited.get(key, 0) >= d.cnt:
                        continue
                    assert d.cnt > 0, (d.eng, ename)
                    eng.wait_ge(self.dsem[key] if d.dma else self.esem[key], d.cnt)
                    waited[key] = d.cnt
                if o.dma:
                    if o.cnt > 16 and waited.get(o.sem, 0) < o.cnt - 16:
                        eng.wait_ge(self.dsem[o.sem], o.cnt - 16)
                        waited[o.sem] = o.cnt - 16
                    o.fn(eng).then_inc(self.dsem[o.sem], 16)
                else:
                    ins = o.fn(eng)
                    if o.inc:
                        ins.then_inc(self.esem[o.eng], 1)
                o.emitted = True
            if ename == "sp" and final:
                for q in ("sp", "pool"):
                    k = self.dcnt[q]
                    for sl in range(min(self.nd, k)):
                        last = 16 * ((k - 1 - sl) // self.nd + 1)
                        eng.wait_ge(self.dsem[(q, sl)], last)

        with nc.Block() as block:
            @block.tensor
            def _(eng):
                run("pe", eng)

            @block.scalar
            def _(eng):
                run("act", eng)

            @block.vector
            def _(eng):
                run("dve", eng)

            @block.gpsimd
            def _(eng):
                run("pool", eng)

            @block.sync
            def _(eng):
                run("sp", eng)


def bcast_last(ap2d, n):
    return ap2d.unsqueeze(2).to_broadcast([ap2d.shape[0], ap2d.shape[1], n])


class Ctx:
    pass


def load_w_kmajor(P, nc, dst, src2d, n_kc, ncols, bufs, col_chunk=1408):
    v = src2d.rearrange("(kc p) n -> kc p n", p=128)
    mdl = 4 * col_chunk
    for k in range(n_kc):
        P.op("pool", lambda e, k=k: e.dma_start(out=dst[:, k, :], in_=v[k], max_dma_last_dim=mdl),
             writes=[bufs[k]], dma=True)


def setup_consts(P, nc, st, C):
    sb = lambda name, shape, dt: st.enter_context(nc.sbuf_tensor(name, shape, dt))
    C.identf = sb("identf", [128, 128], F32)
    C.ident = sb("ident", [128, 128], BF16)
    C.neghalf = sb("neghalf", [128, 1], F32)
    C.b_const = P.buf("const")
    identf, ident = C.identf, C.ident
    P.op("pool", lambda e: e.memset(identf[:], 0.0), writes=[C.b_const])
    P.op("pool", lambda e: e.affine_select(out=identf[:], in_=identf[:], pattern=[[-1, 128]],
                                             compare_op=ALU.not_equal, fill=1.0, base=0,
                                             channel_multiplier=1),
         reads=[C.b_const], writes=[C.b_const])
    P.op("dve", lambda e: e.tensor_copy(out=ident[:], in_=identf[:]), reads=[C.b_const], writes=[C.b_const])
    P.op("pool", lambda e: e.memset(C.neghalf[:], -0.5), reads=[C.b_const], writes=[C.b_const])


def rms_stats(P, C, src_ap, src_buf, junk, b_junk, ss, var, rstd, b_stat, n_feat=D):
    P.op("act", lambda e: e.activation(out=junk, in_=src_ap, func=AF.Square, accum_out=ss),
         reads=[src_buf], writes=[b_junk, b_stat])
    P.op("dve", lambda e: e.tensor_scalar(out=var, in0=ss, scalar1=1.0 / n_feat, scalar2=EPS,
                                          op0=ALU.mult, op1=ALU.add),
         reads=[b_stat], writes=[b_stat])
    P.op("pool", lambda e: e.tensor_tensor(out=rstd, in0=var, in1=C.neghalf[:], op=ALU.pow),
         reads=[b_stat, C.b_const], writes=[b_stat])


def ffn_pass(P, nc, C, tag, src_h, w_gate, w_up, w_down, pre_g, post_g, dst_h, next_g, dst_uT,
             src_b, dst_b, uT_b, gate_w=None, gate_dst=None, gate_b=None):
    with contextlib.ExitStack() as st:
        sb = lambda name, shape, dt: st.enter_context(nc.sbuf_tensor(tag + name, shape, dt))
        ps = lambda name, shape, dt: st.enter_context(nc.psum_tensor(tag + name, shape, dt))
        Wg = sb("Wg", [128, 8, DFF], BF16)
        Wu = sb("Wu", [128, 8, DFF], BF16)
        Wd = sb("Wd", [128, NFC, D], BF16)
        b_Wg = P.bufs_n("Wg", 8)
        b_Wu = P.bufs_n("Wu", 8)
        b_Wd = P.bufs_n("Wd", 2)
        load_w_kmajor(P, nc, Wg, w_gate, 8, DFF, b_Wg)
        load_w_kmajor(P, nc, Wu, w_up, 8, DFF, b_Wu)
        wdv = w_down.rearrange("(fc p) d -> p fc d", p=128)
        for hh in range(2):
            P.op("pool", lambda e, hh=hh: e.dma_start(out=Wd[:, hh * 11:(hh + 1) * 11, :],
                                                       in_=wdv[:, hh * 11:(hh + 1) * 11, :]),
                 writes=[b_Wd[hh]], dma=True)
        gpre = sb("gpre", [128, 8], F32)
        gnext = sb("gnext", [128, 8], F32)
        gpost = sb("gpost", [128, D], F32)
        b_par = P.buf("par")
        P.op("sp", lambda e: e.dma_start(out=gpre[:], in_=pre_g.rearrange("o (k p) -> p (o k)", p=128),
                                         allow_slow_non_contiguous=True),
             writes=[b_par], dma=True)
        P.op("sp", lambda e: e.dma_start(out=gnext[:], in_=next_g.rearrange("o (k p) -> p (o k)", p=128),
                                         allow_slow_non_contiguous=True),
             writes=[b_par], dma=True)
        P.op("sp", lambda e: e.dma_start(out=gpost[:], in_=post_g.partition_broadcast(128)),
             writes=[b_par], dma=True)
        if gate_w is not None:
            Wgt = sb("Wgt", [128, 8, 72], BF16)
            b_Wgt = P.buf("Wgt")
            P.op("pool", lambda e: e.memset(Wgt[:], 0.0), writes=[b_Wgt])
            gv = gate_w.rearrange("(kc p) n -> p kc n", p=128)
            for (c0, n, d0) in ((1540, 4, 0), (1536, 4, 32), (3080, 8, 64)):
                P.op("pool", lambda e, c0=c0, n=n, d0=d0: e.dma_start(
                    out=Wgt[:, :, d0:d0 + n], in_=gv[:, :, c0:c0 + n]),
                    reads=[], writes=[b_Wgt], dma=True)
            gsb = [sb("gsb%d" % i, [72, 128], F32) for i in range(2)]
            b_gsb = P.bufs_n("gsb", 2)

        NXB = 3
        xb = [sb("xb%d" % i, [128, D], F32) for i in range(NXB)]
        b_xb = P.bufs_n("xb", NXB)
        ubf = [sb("ubf%d" % i, [128, D], BF16) for i in range(2)]
        b_ubf = P.bufs_n("ubf", 2)
        junk = sb("junk", [128, D], BF16)
        b_junk = P.buf("junk")
        uT = sb("uT", [128, 8, TT], BF16)
        b_uT = P.bufs_n("uT", 4)
        aT = sb("aT", [128, NFC, TT], BF16)
        b_aT = P.bufs_n("aT", NFC)
        sg = [sb("sg%d" % i, [128, TT], F32) for i in range(2)]
        b_sg = P.bufs_n("sg", 2)
        hst = [sb("hst%d" % i, [128, D], F32) for i in range(2)]
        b_hst = P.bufs_n("hst", 2)
        u2T = [sb("u2T%d" % i, [128, 8, 128], BF16) for i in range(2)]
        b_u2T = P.bufs_n("u2T", 2)
        NST = 6
        stat = sb("stat", [128, 3 * NST], F32)
        b_stat = P.bufs_n("stat", NST)

        pt = ps("pt", [128, 8, 128], BF16)
        b_pt = P.buf("pt")
        pg = [ps("pg%d" % i, [128, TT], F32) for i in range(2)]
        pu = [ps("pu%d" % i, [128, TT], F32) for i in range(2)]
        b_pg = P.bufs_n("pg", 2)
        b_pu = P.bufs_n("pu", 2)
        pys = [ps("py%d" % i, [128, 512], F32) for i in range(3)]
        b_pys = P.bufs_n("py", 3)
        if gate_w is not None:
            pgt = pu[1][0:72, 0:128]
            b_pgt = b_pu[1]

        src_v = src_h.rearrange("(n p) d -> n p d", p=128)
        dst_v = dst_h.rearrange("(n p) d -> n p d", p=128)
        cnt = {"x": 0, "u": 0, "st": 0, "h": 0, "u2": 0, "sg": 0, "gs": 0, "py": 0}
        pend = []

        def norm_T(h_ap, h_buf, gcol, out_ap, out_bufs, defer=False):
            si = cnt["st"] % NST
            cnt["st"] += 1
            ss, var, rstd = (stat[:, 3 * si + j:3 * si + j + 1] for j in range(3))
            rms_stats(P, C, h_ap, h_buf, junk[:], b_junk, ss, var, rstd, b_stat[si])
            ui = cnt["u"] % 2
            cnt["u"] += 1
            u = ubf[ui]
            P.op("dve", lambda e: e.tensor_scalar(out=u[:], in0=h_ap, scalar1=rstd, scalar2=None, op0=ALU.mult),
                 reads=[h_buf, b_stat[si]], writes=[b_ubf[ui]])
            def pe_part():
                for k in range(8):
                    P.op("pe", lambda e, k=k: e.transpose(out=pt[:, k, :], in_=u[:, k * 128:(k + 1) * 128],
                                                          identity=C.ident[:]),
                         reads=[b_ubf[ui], C.b_const], writes=[b_pt])
                P.op("dve", lambda e: e.tensor_tensor(out=out_ap, in0=pt[:], in1=bcast_last(gcol[:], 128), op=ALU.mult),
                     reads=[b_pt, b_par], writes=out_bufs)
            if defer:
                return pe_part
            pe_part()

        def pre(i):
            for s in range(4):
                n = i * 4 + s
                xi = cnt["x"] % NXB
                cnt["x"] += 1
                P.op("sp", lambda e, n=n, xi=xi: e.dma_start(out=xb[xi][:], in_=src_v[n]),
                     reads=[src_b[n]], writes=[b_xb[xi]], dma=True)
                norm_T(xb[xi][:], b_xb[xi], gpre, uT[:, :, s * 128:(s + 1) * 128], [b_uT[s]])

        def gateup(i):
            for f in range(NFC):
                j = f % 2
                for k in range(8):
                    P.op("pe", lambda e, k=k, f=f, j=j: e.matmul(
                        pg[j][:], lhsT=Wg[:, k, f * 128:(f + 1) * 128], rhs=uT[:, k, :],
                        start=(k == 0), stop=(k == 7)),
                        reads=[b_Wg[k]] + b_uT, writes=[b_pg[j]])
                for k in range(8):
                    P.op("pe", lambda e, k=k, f=f, j=j: e.matmul(
                        pu[j][:], lhsT=Wu[:, k, f * 128:(f + 1) * 128], rhs=uT[:, k, :],
                        start=(k == 0), stop=(k == 7)),
                        reads=[b_Wu[k]] + b_uT, writes=[b_pu[j]])
                if f == 0:
                    while pend:
                        pend.pop(0)()
                si = cnt["sg"] % 2
                cnt["sg"] += 1
                P.op("act", lambda e, j=j, si=si: e.activation(out=sg[si][:], in_=pg[j][:], func=AF.Silu),
                     reads=[b_pg[j]], writes=[b_sg[si]])
                P.op("dve", lambda e, j=j, si=si, f=f: e.tensor_tensor(out=aT[:, f, :], in0=sg[si][:], in1=pu[j][:],
                                                                   op=ALU.mult),
                     reads=[b_sg[si], b_pu[j]], writes=[b_aT[f]])

        def down_post(i):
            for s in range(4):
                n = i * 4 + s
                pyh = []
                for hf in range(2):
                    pi = cnt["py"] % 3
                    cnt["py"] += 1
                    pyh.append((pys[pi], b_pys[pi]))
                    for f in range(NFC):
                        P.op("pe", lambda e, f=f, s=s, hf=hf, pi=pi: e.matmul(
                            pys[pi][:], lhsT=aT[:, f, s * 128:(s + 1) * 128],
                            rhs=Wd[:, f, hf * 512:(hf + 1) * 512], start=(f == 0), stop=(f == NFC - 1)),
                            reads=[b_aT[f], b_Wd[f // 11]], writes=[b_pys[pi]])
                while pend:
                    pend.pop(0)()
                xi = cnt["x"] % NXB
                cnt["x"] += 1
                P.op("sp", lambda e, n=n, xi=xi: e.dma_start(out=xb[xi][:], in_=src_v[n]),
                     reads=[src_b[n]], writes=[b_xb[xi]], dma=True)
                si = cnt["st"] % NST
                cnt["st"] += 1
                ss, var, rstd = (stat[:, 3 * si + j:3 * si + j + 1] for j in range(3))
                P.op("act", lambda e, ss=ss, t=pyh[0][0]: e.activation(out=junk[:, 0:512], in_=t[:], func=AF.Square,
                                                                      accum_out=ss),
                     reads=[pyh[0][1]], writes=[b_junk, b_stat[si]])
                P.op("act", lambda e, var=var, t=pyh[1][0]: e.activation(out=junk[:, 512:1024], in_=t[:], func=AF.Square,
                                                                        accum_out=var),
                     reads=[pyh[1][1]], writes=[b_junk, b_stat[si]])
                P.op("dve", lambda e, ss=ss, var=var: e.tensor_tensor(out=var, in0=ss, in1=var, op=ALU.add),
                     reads=[b_stat[si]], writes=[b_stat[si]])
                P.op("dve", lambda e, var=var: e.tensor_scalar(out=var, in0=var, scalar1=1.0 / D, scalar2=EPS,
                                                              op0=ALU.mult, op1=ALU.add),
                     reads=[b_stat[si]], writes=[b_stat[si]])
                P.op("pool", lambda e, var=var, rstd=rstd: e.tensor_tensor(out=rstd, in0=var, in1=C.neghalf[:], op=ALU.pow),
                     reads=[b_stat[si], C.b_const], writes=[b_stat[si]])
                hi = cnt["h"] % 2
                cnt["h"] += 1
                hb = hst[hi]
                for hf in range(2):
                    hs = slice(hf * 512, (hf + 1) * 512)
                    P.op("dve", lambda e, hb=hb, rstd=rstd, t=pyh[hf][0], hs=hs: e.scalar_tensor_tensor(
                        out=hb[:, hs], in0=t[:], scalar=rstd, in1=gpost[:, hs], op0=ALU.mult, op1=ALU.mult),
                        reads=[pyh[hf][1], b_stat[si], b_par], writes=[b_hst[hi]])
                P.op("dve", lambda e, hb=hb, xi=xi: e.scalar_tensor_tensor(
                    out=hb[:], in0=hb[:], scalar=0.5, in1=xb[xi][:], op0=ALU.mult, op1=ALU.add),
                    reads=[b_hst[hi], b_xb[xi]], writes=[b_hst[hi]])
                P.op("sp", lambda e, hb=hb, n=n: e.dma_start(out=dst_v[n], in_=hb[:]),
                     reads=[b_hst[hi]], writes=[dst_b[n]], dma=True)
                ui2 = cnt["u2"] % 2
                cnt["u2"] += 1
                pe_part = norm_T(hb[:], b_hst[hi], gnext, u2T[ui2][:], [b_u2T[ui2]], defer=True)

                def tail(pe_part=pe_part, ui2=ui2, n=n):
                    pe_part()
                    P.op("sp", lambda e: e.dma_start(
                        out=dst_uT[:, :, n * 128:(n + 1) * 128].rearrange("k p t -> p k t"), in_=u2T[ui2][:]),
                        reads=[b_u2T[ui2]], writes=[uT_b[n]], dma=True)
                    if gate_w is not None:
                        for k in range(8):
                            P.op("pe", lambda e, k=k: e.matmul(
                                pgt, lhsT=Wgt[:, k, :], rhs=u2T[ui2][:, k, :], start=(k == 0), stop=(k == 7)),
                                reads=[b_Wgt, b_u2T[ui2]], writes=[b_pgt])
                        gi = cnt["gs"] % 2
                        cnt["gs"] += 1
                        P.op("act", lambda e: e.activation(out=gsb[gi][:], in_=pgt, func=AF.Copy),
                             reads=[b_pgt], writes=[b_gsb[gi]])
                        P.op("sp", lambda e: e.dma_start(out=gate_dst[:, n * 128:(n + 1) * 128], in_=gsb[gi][:]),
                             reads=[b_gsb[gi]], writes=[gate_b[n]], dma=True)
                pend.append(tail)

        pre(0)
        for i in range(NT):
            gateup(i)
            if i + 1 < NT:
                pre(i + 1)
            down_post(i)
        while pend:
            pend.pop(0)()
        P.flush()


def gp_stage(P, nc, C, I, gpre, gpre_b, qaug, kaug, aug_b):
    with contextlib.ExitStack() as st:
        sb = lambda name, shape, dt: st.enter_context(nc.sbuf_tensor("gp" + name, shape, dt))
        ps = lambda name, shape, dt: st.enter_context(nc.psum_tensor("gp" + name, shape, dt))
        T0 = sb("T0", [72, S], F32)
        T1 = sb("T1", [72, S], F32)
        T2 = sb("T2", [72, S], F32)
        T3 = sb("T3", [72, S], F32)
        QR = sb("QR", [72, 3, S], BF16)
        KR = sb("KR", [72, 3, S], BF16)
        ONE = sb("ONE", [72, S], BF16)
        bcol = sb("bcol", [72, 1], F32)
        negb = sb("negb", [72, 1], F32)
        bicol = sb("bicol", [72, 1], F32)
        onec = sb("onec", [72, 1], F32)
        cm = sb("cm", [72, 32], F32)
        mce = sb("mce", [72, 32], F32)
        mprev = sb("mprev", [72, 32], F32)
        dec = sb("dec", [72, 32], F32)
        esel = sb("esel", [72, 4, 128], F32)
        bT0, bT1, bT2, bT3, bQR, bKR, bONE, bsm = [P.buf(n) for n in
                                                   ("T0", "T1", "T2", "T3", "QR", "KR", "ONE", "gsm")]
        ptm = ps("ptm", [128, 32, 8], F32)
        pdc = ps("pdc", [128, 4, 32], F32)
        b_ptm, b_pdc = P.buf("ptm"), P.buf("pdc")

        P.op("sp", lambda e: e.dma_start(out=T0[:], in_=gpre), reads=gpre_b, writes=[bT0], dma=True)
        P.op("sp", lambda e: e.dma_start(out=T3[0:4, :], in_=gpre[32:36, :]), reads=gpre_b, writes=[bT3], dma=True)
        P.op("dve", lambda e: e.memset(bcol[:], 0.0), writes=[bsm])
        P.op("dve", lambda e: e.memset(bicol[:], 0.0), reads=[bsm], writes=[bsm])
        P.op("dve", lambda e: e.memset(onec[:], 1.0), reads=[bsm], writes=[bsm])
        P.op("pool", lambda e: e.memset(ONE[:], 1.0), writes=[bONE])
        P.op("sp", lambda e: e.dma_start(out=bcol[0:4, :], in_=I["mlstm_f_bias"].rearrange("o n -> n o"),
                                         allow_slow_non_contiguous=True), reads=[bsm], writes=[bsm], dma=True)
        P.op("sp", lambda e: e.dma_start(out=bcol[64:72, :], in_=I["fox_f_bias"].rearrange("o n -> n o"),
                                         allow_slow_non_contiguous=True), reads=[bsm], writes=[bsm], dma=True)
        P.op("sp", lambda e: e.dma_start(out=bicol[0:4, :], in_=I["mlstm_i_bias"].rearrange("o n -> n o"),
                                         allow_slow_non_contiguous=True), reads=[bsm], writes=[bsm], dma=True)
        P.op("dve", lambda e: e.tensor_scalar(out=negb[0:72, :], in0=bcol[0:72, :], scalar1=-1.0, scalar2=None,
                                              op0=ALU.mult), reads=[bsm], writes=[bsm])
        R = slice(0, 72)
        P.op("act", lambda e: e.activation(out=T1[R, :], in_=T0[R, :], func=AF.Exp, scale=-1.0, bias=negb[R, :]),
             reads=[bT0, bsm], writes=[bT1])
        P.op("act", lambda e: e.activation(out=T1[R, :], in_=T1[R, :], func=AF.Ln, scale=1.0, bias=onec[R, :]),
             reads=[bT1, bsm], writes=[bT1])
        P.op("dve", lambda e: e.tensor_tensor_scan(out=T2[R, :], data0=T1[R, :], data1=T1[R, :], initial=0.0,
                                                   op0=ALU.add, op1=ALU.max), reads=[bT1], writes=[bT2])
        M = slice(0, 4)
        P.op("dve", lambda e: e.scalar_tensor_tensor(out=T3[M, :], in0=T3[M, :], scalar=bicol[M, :], in1=T2[M, :],
                                                     op0=ALU.add, op1=ALU.add), reads=[bT3, bT2, bsm], writes=[bT3])
        P.op("dve", lambda e: e.tensor_reduce(out=cm[M, :], in_=T3[M, :].rearrange("p (c l) -> p c l", l=128),
                                              axis=AX.X, op=ALU.max), reads=[bT3], writes=[bsm])
        P.op("dve", lambda e: e.tensor_tensor_scan(out=mce[M, :], data0=cm[M, :], data1=cm[M, :], initial=0.0,
                                                   op0=ALU.max, op1=ALU.max), reads=[bsm], writes=[bsm])
        P.op("dve", lambda e: e.tensor_tensor(out=T3[M, :].rearrange("p (c l) -> p c l", l=128),
                                              in0=T3[M, :].rearrange("p (c l) -> p c l", l=128),
                                              in1=bcast_last(mce[M, :], 128), op=ALU.subtract),
             reads=[bT3, bsm], writes=[bT3])
        P.op("act", lambda e: e.activation(out=T3[M, :], in_=T3[M, :], func=AF.Exp), reads=[bT3], writes=[bT3])
        P.op("dve", lambda e: e.tensor_tensor(out=T1[M, :].rearrange("p (c l) -> p c l", l=128),
                                              in0=T2[M, :].rearrange("p (c l) -> p c l", l=128),
                                              in1=bcast_last(mce[M, :], 128), op=ALU.subtract),
             reads=[bT2, bsm, bT1], writes=[bT1])
        P.op("act", lambda e: e.activation(out=T1[M, :], in_=T1[M, :], func=AF.Exp, scale=2.0), reads=[bT1], writes=[bT1])
        P.op("dve", lambda e: e.memset(mprev[M, :], 0.0), reads=[bsm], writes=[bsm])
        P.op("dve", lambda e: e.tensor_copy(out=mprev[M, 1:32], in_=mce[M, 0:31]), reads=[bsm], writes=[bsm])
        P.op("dve", lambda e: e.tensor_tensor(out=dec[M, :], in0=mprev[M, :], in1=mce[M, :], op=ALU.subtract),
             reads=[bsm], writes=[bsm])
        P.op("act", lambda e: e.activation(out=dec[M, :], in_=dec[M, :], func=AF.Exp), reads=[bsm], writes=[bsm])
        for c in range(32):
            P.op("pe", lambda e, c=c: e.transpose(out=ptm[:, c, 0:4], in_=T3[M, c * 128:(c + 1) * 128],
                                                  identity=C.identf[M, 0:4]),
                 reads=[bT3, C.b_const], writes=[b_ptm])
            P.op("pe", lambda e, c=c: e.transpose(out=ptm[:, c, 4:8], in_=T1[M, c * 128:(c + 1) * 128],
                                                  identity=C.identf[M, 0:4]),
                 reads=[bT1, C.b_const], writes=[b_ptm])
        P.op("dve", lambda e: e.tensor_copy(out=C.wthr[:], in_=ptm[:]), reads=[b_ptm], writes=[C.b_wthr])
        for h in range(4):
            P.op("dve", lambda e, h=h: e.tensor_copy(out=esel[M, h, :],
                                                     in_=C.identf[M, h:h + 1].to_broadcast([4, 128])),
                 reads=[C.b_const, bsm], writes=[bsm])
        for h in range(4):
            P.op("pe", lambda e, h=h: e.matmul(pdc[:, h, :], lhsT=esel[M, h, :], rhs=dec[M, :], start=True, stop=True),
                 reads=[bsm], writes=[b_pdc])
        P.op("dve", lambda e: e.tensor_copy(out=C.decbc[:], in_=pdc[:]), reads=[b_pdc], writes=[C.b_decbc])
        Fx = slice(64, 72)
        Fd = slice(64, 72)
        P.op("dve", lambda e: e.tensor_scalar(out=T0[Fx, :], in0=T2[Fx, :], scalar1=-1.0, scalar2=None, op0=ALU.mult),
             reads=[bT2, bT0], writes=[bT0])
        for part in range(3):
            P.op("dve", lambda e, part=part: e.tensor_copy(out=QR[Fx, part, :], in_=T0[Fx, :]),
                 reads=[bT0], writes=[bQR])
            if part < 2:
                P.op("dve", lambda e, part=part: e.tensor_tensor(out=T0[Fx, :], in0=T0[Fx, :], in1=QR[Fx, part, :],
                                                                 op=ALU.subtract), reads=[bT0, bQR], writes=[bT0])
        P.op("pool", lambda e: e.tensor_scalar(out=KR[Fx, :, :], in0=QR[Fx, :, :], scalar1=-1.0, scalar2=None,
                                               op0=ALU.mult), reads=[bQR], writes=[bKR])
        P.op("sp", lambda e: e.dma_start(out=qaug[:, 64:67, :], in_=QR[Fd, :, :]), reads=[bQR], writes=[aug_b], dma=True)
        P.op("sp", lambda e: e.dma_start(out=kaug[:, 67:70, :], in_=KR[Fd, :, :]), reads=[bKR], writes=[aug_b], dma=True)
        for r in range(3):
            P.op("sp", lambda e, r=r: e.dma_start(out=qaug[:, 67 + r, :], in_=ONE[Fd, :]), reads=[bONE],
                 writes=[aug_b], dma=True)
            P.op("sp", lambda e, r=r: e.dma_start(out=kaug[:, 64 + r, :], in_=ONE[Fd, :]), reads=[bONE],
                 writes=[aug_b], dma=True)
        P.flush()


def win_pass(P, nc, C, I, uT1, uT_b, SC, DB):
    w_in = I["w_in"]
    with contextlib.ExitStack() as st:
        sb = lambda name, shape, dt: st.enter_context(nc.sbuf_tensor("wi" + name, shape, dt))
        ps = lambda name, shape, dt: st.enter_context(nc.psum_tensor("wi" + name, shape, dt))
        W = sb("W", [128, 8, INW], BF16)
        b_W = P.bufs_n("Win", 8)
        wv = w_in.rearrange("(kc p) n -> kc p n", p=128)
        for k in range(8):
            P.op("pool", lambda e, k=k: e.dma_start(out=W[:, k, :], in_=wv[k], max_dma_last_dim=4 * 1284),
                 writes=[b_W[k]], dma=True)
        cw = sb("cw", [128, 4, 4], F32)
        cb = sb("cb", [128, 4], F32)
        gbias = sb("gbias", [128, 2048], F32)
        b_par = P.buf("wipar")
        for tap in range(4):
            P.op("sp", lambda e, tap=tap: e.dma_start(
                out=cw[:, :, tap], in_=I["conv_w"][tap:tap + 1, :].rearrange("o (c p) -> p (o c)", p=128),
                allow_slow_non_contiguous=True), writes=[b_par], dma=True)
        P.op("sp", lambda e: e.dma_start(out=cb[:], in_=I["conv_b"].rearrange("o (c p) -> p (o c)", p=128),
                                         allow_slow_non_contiguous=True), writes=[b_par], dma=True)
        P.op("sp", lambda e: e.dma_start(out=gbias[:], in_=I["branch_gate_bias"].partition_broadcast(128)),
             writes=[b_par], dma=True)
        uT = [sb("uT%d" % i, [128, 8, TT], BF16) for i in range(2)]
        b_uT = P.bufs_n("wiuT", 2)
        zq = sb("zq", [128, 4, 3 + TT], F32)
        b_zq = P.bufs_n("zq", 4)
        acc = [sb("acc%d" % i, [128, TT], F32) for i in range(2)]
        b_acc = P.bufs_n("acc", 2)
        fo = [sb("fo%d" % i, [128, TT], BF16) for i in range(3)]
        b_fo = P.bufs_n("fo", 3)
        tv = [sb("tv%d" % i, [128, 4, 129], BF16) for i in range(2)]
        b_tv = P.bufs_n("tv", 2)
        tf = [sb("tf%d" % i, [128, 8, 65], BF16) for i in range(2)]
        b_tf = P.bufs_n("tf", 2)
        tg = [sb("tg%d" % i, [128, 512], F32) for i in range(2)]
        b_tg = P.bufs_n("tg", 2)
        to = [sb("to%d" % i, [128, 512], BF16) for i in range(3)]
        b_to = P.bufs_n("to", 3)
        pf = [ps("pf%d" % i, [128, TT], F32) for i in range(2)]
        b_pf = P.bufs_n("pf", 2)
        pk = [ps("pk%d" % i, [128, 512], F32) for i in range(2)]
        b_pk = P.bufs_n("pk", 2)
        cnt = {"pf": 0, "pk": 0, "acc": 0, "fo": 0, "tv": 0, "tf": 0, "tg": 0, "to": 0}

        def rot(key, n):
            v = cnt[key] % n
            cnt[key] += 1
            return v

        for ch in range(4):
            P.op("dve", lambda e, ch=ch: e.memset(zq[:, ch, 0:3], 0.0), writes=[b_zq[ch]])
        for i in range(NT):
            ub = i % 2
            tcols = slice(i * TT, (i + 1) * TT)
            if i == 0:
                P.op("sp", lambda e: e.dma_start(out=uT[0][:], in_=uT1[:, :, 0:TT].rearrange("k p t -> p k t")),
                     reads=uT_b[0:4], writes=[b_uT[0]], dma=True)
            if i + 1 < NT:
                ncols = slice((i + 1) * TT, (i + 2) * TT)
                P.op("sp", lambda e, ub=ub, ncols=ncols: e.dma_start(
                    out=uT[1 - ub][:], in_=uT1[:, :, ncols].rearrange("k p t -> p k t")),
                    reads=uT_b[4 * i + 4:4 * i + 8], writes=[b_uT[1 - ub]], dma=True)
            fm = [("mqk", ch, ch * 128) for ch in range(4)] + \
                 [("fq", ch, 1544 + ch * 128) for ch in range(4)] + \
                 [("fk", ch, 2056 + ch * 128) for ch in range(4)]
            for (kind, ch, c0) in fm:
                j = rot("pf", 2)
                for k in range(8):
                    P.op("pe", lambda e, k=k, c0=c0, j=j, ub=ub: e.matmul(
                        pf[j][:], lhsT=W[:, k, c0:c0 + 128], rhs=uT[ub][:, k, :], start=(k == 0), stop=(k == 7)),
                        reads=[b_W[k], b_uT[ub]], writes=[b_pf[j]])
                if kind == "mqk":
                    P.op("act", lambda e, ch=ch, j=j: e.activation(out=zq[:, ch, 3:3 + TT], in_=pf[j][:], func=AF.Copy),
                         reads=[b_pf[j]], writes=[b_zq[ch]])
                    a = rot("acc", 2)
                    P.op("dve", lambda e, ch=ch, a=a: e.tensor_scalar(
                        out=acc[a][:], in0=zq[:, ch, 0:TT], scalar1=cw[:, ch, 0:1], scalar2=cb[:, ch:ch + 1],
                        op0=ALU.mult, op1=ALU.add), reads=[b_zq[ch], b_par], writes=[b_acc[a]])
                    for tap in range(1, 4):
                        P.op("dve", lambda e, ch=ch, a=a, tap=tap: e.scalar_tensor_tensor(
                            out=acc[a][:], in0=zq[:, ch, tap:tap + TT], scalar=cw[:, ch, tap:tap + 1], in1=acc[a][:],
                            op0=ALU.mult, op1=ALU.add), reads=[b_zq[ch], b_par, b_acc[a]], writes=[b_acc[a]])
                    P.op("dve", lambda e, ch=ch: e.tensor_copy(out=zq[:, ch, 0:3], in_=zq[:, ch, TT:TT + 3]),
                         reads=[b_zq[ch]], writes=[b_zq[ch]])
                    o = rot("fo", 3)
                    P.op("act", lambda e, a=a, o=o: e.activation(out=fo[o][:], in_=acc[a][:], func=AF.Silu),
                         reads=[b_acc[a]], writes=[b_fo[o]])
                    P.op("sp", lambda e, o=o, ch=ch, tcols=tcols: e.dma_start(out=SC["mqkT"][ch, :, tcols], in_=fo[o][:]),
                         reads=[b_fo[o]], writes=[DB("mqkT")[i]], dma=True)
                else:
                    o = rot("fo", 3)
                    sc = 0.125 if kind == "fq" else 1.0
                    P.op("act", lambda e, o=o, j=j, sc=sc: e.activation(out=fo[o][:], in_=pf[j][:], func=AF.Copy, scale=sc),
                         reads=[b_pf[j]], writes=[b_fo[o]])
                    dst = SC["qaug"] if kind == "fq" else SC["kaug"]
                    for hh in range(2):
                        P.op("sp", lambda e, o=o, ch=ch, dst=dst, tcols=tcols, hh=hh: e.dma_start(
                            out=dst[2 * ch + hh, 0:64, tcols], in_=fo[o][hh * 64:(hh + 1) * 64, :]),
                            reads=[b_fo[o]], writes=[DB("aug")[i]], dma=True)
            for s in range(4):
                n = i * 4 + s
                rows = slice(n * 128, (n + 1) * 128)
                groups = [("mv", 512), ("mo", 1024), ("fv", 2568), ("ga", 3088), ("ga", 3600), ("gb", 4112), ("gb", 4624)]
                for gi, (kind, c0) in enumerate(groups):
                    j = rot("pk", 2)
                    for k in range(8):
                        P.op("pe", lambda e, k=k, c0=c0, j=j, ub=ub, s=s: e.matmul(
                            pk[j][:], lhsT=uT[ub][:, k, s * 128:(s + 1) * 128], rhs=W[:, k, c0:c0 + 512],
                            start=(k == 0), stop=(k == 7)),
                            reads=[b_W[k], b_uT[ub]], writes=[b_pk[j]])
                    if kind == "mv":
                        t = rot("tv", 2)
                        c = n
                        P.op("dve", lambda e, t=t, j=j, c=c: e.tensor_tensor(
                            out=tv[t][:, :, 0:128], in0=pk[j][:].rearrange("p (h d) -> p h d", h=4),
                            in1=bcast_last(C.wthr[:, c, 0:4], 128), op=ALU.mult),
                            reads=[b_pk[j], C.b_wthr], writes=[b_tv[t]])
                        P.op("dve", lambda e, t=t, c=c: e.tensor_copy(out=tv[t][:, :, 128:129],
                                                                     in_=C.wthr[:, c, 0:4].unsqueeze(2)),
                             reads=[C.b_wthr, b_tv[t]], writes=[b_tv[t]])
                        P.op("sp", lambda e, t=t, rows=rows: e.dma_start(out=SC["vaugM"][rows], in_=tv[t][:]),
                             reads=[b_tv[t]], writes=[DB("vaugM")[n]], dma=True)
                    elif kind == "fv":
                        t = rot("tf", 2)
                        P.op("act", lambda e, t=t, j=j: e.activation(
                            out=tf[t][:, :, 1:65], in_=pk[j][:].rearrange("p (h d) -> p h d", h=8), func=AF.Copy),
                            reads=[b_pk[j]], writes=[b_tf[t]])
                        P.op("dve", lambda e, t=t: e.memset(tf[t][:, :, 0:1], 1.0), reads=[b_tf[t]], writes=[b_tf[t]])
                        P.op("sp", lambda e, t=t, rows=rows: e.dma_start(out=SC["vaugF"][rows], in_=tf[t][:]),
                             reads=[b_tf[t]], writes=[DB("vaugF")[n]], dma=True)
                    elif kind == "mo":
                        o = rot("to", 3)
                        P.op("act", lambda e, o=o, j=j: e.activation(out=to[o][:], in_=pk[j][:], func=AF.Sigmoid),
                             reads=[b_pk[j]], writes=[b_to[o]])
                        P.op("sp", lambda e, o=o, rows=rows: e.dma_start(out=SC["so"][rows], in_=to[o][:]),
                             reads=[b_to[o]], writes=[DB("so")[n]], dma=True)
                    else:
                        g = rot("tg", 2)
                        boff = c0 - 3088
                        P.op("dve", lambda e, g=g, j=j, boff=boff: e.tensor_tensor(
                            out=tg[g][:], in0=pk[j][:], in1=gbias[:, boff:boff + 512], op=ALU.add),
                            reads=[b_pk[j], b_par], writes=[b_tg[g]])
                        o = rot("to", 3)
                        P.op("act", lambda e, o=o, g=g: e.activation(out=to[o][:], in_=tg[g][:], func=AF.Sigmoid),
                             reads=[b_tg[g]], writes=[b_to[o]])
                        dcol = boff % 1024
                        dst = SC["ga"] if kind == "ga" else SC["gb"]
                        P.op("sp", lambda e, o=o, rows=rows, dst=dst, dcol=dcol: e.dma_start(
                            out=dst[rows, dcol:dcol + 512], in_=to[o][:]),
                            reads=[b_to[o]], writes=[DB("gab")[n]], dma=True)
        P.flush()


def mix_pass(P, nc, C, I, SC, DB):
    with contextlib.ExitStack() as st:
        sb = lambda name, shape, dt: st.enter_context(nc.sbuf_tensor("mx" + name, shape, dt))
        ps = lambda name, shape, dt: st.enter_context(nc.psum_tensor("mx" + name, shape, dt))
        mask01 = sb("mask01", [128, 128], F32)
        trim = sb("trim", [128, 128], BF16)
        trimf = sb("trimf", [128, 128], F32)
        onesr = sb("onesr", [1, 65], F32)
        gln = sb("gln", [128, 512], F32)
        b_c = P.buf("mxconst")
        P.op("pool", lambda e: e.memset(mask01[:], 1.0), writes=[b_c])
        P.op("pool", lambda e: e.affine_select(out=mask01[:], in_=mask01[:], pattern=[[1, 128]], compare_op=ALU.is_ge,
                                                 fill=0.0, base=0, channel_multiplier=-1), reads=[b_c], writes=[b_c])
        P.op("pool", lambda e: e.memset(trimf[:], 0.0), reads=[b_c], writes=[b_c])
        P.op("pool", lambda e: e.affine_select(out=trimf[:], in_=trimf[:], pattern=[[1, 128]], compare_op=ALU.is_ge,
                                                 fill=-30000.0, base=0, channel_multiplier=-1), reads=[b_c], writes=[b_c])
        P.op("dve", lambda e: e.tensor_copy(out=trim[:], in_=trimf[:]), reads=[b_c], writes=[b_c])
        P.op("dve", lambda e: e.memset(onesr[:], 1.0), reads=[b_c], writes=[b_c])
        P.op("sp", lambda e: e.dma_start(out=gln[:], in_=I["mlstm_norm_g"].partition_broadcast(128)),
             reads=[b_c], writes=[b_c], dma=True)
        mqz = [sb("mqz%d" % i, [128, S], BF16) for i in range(4)]
        mk = [sb("mk%d" % i, [128, S], BF16) for i in range(2)]
        b_mqk = P.buf("mqk")
        for h in range(4):
            P.op("pool", lambda e, h=h: e.memset(mqz[h][:], 0.0), writes=[b_mqk])
        for h in range(4):
            R = slice((h % 2) * 64, (h % 2) * 64 + 64)
            P.op("sp", lambda e, h=h, R=R: e.dma_start(out=mqz[h][R, :], in_=SC["mqkT"][h // 2, R, :]), reads=DB("mqkT"),
                 writes=[b_mqk], dma=True)
        for hp in range(2):
            P.op("sp", lambda e, hp=hp: e.dma_start(out=mk[hp][:], in_=SC["mqkT"][2 + hp]), reads=DB("mqkT"),
                 writes=[b_mqk], dma=True)
        Cst = [sb("Cst%d" % i, [128, 129], F32) for i in range(2)]
        Cb = [sb("Cb%d" % i, [128, 129], BF16) for i in range(2)]
        b_Cst = P.bufs_n("Cst", 2)
        b_Cb = P.bufs_n("Cb", 2)
        va = [sb("va%d" % i, [128, 4, 129], BF16) for i in range(2)]
        b_va = P.bufs_n("va", 2)
        sgo = [sb("sgo%d" % i, [128, 512], BF16) for i in range(2)]
        b_sgo = P.bufs_n("sgo", 2)
        Sm = [sb("Sm%d" % i, [128, 2, 128], BF16) for i in range(2)]
        b_Sm = P.bufs_n("Sm", 2)
        ktm = [sb("ktm%d" % i, [128, 128], BF16) for i in range(2)]
        b_ktm = P.bufs_n("ktm", 2)
        bst = [sb("bst%d" % i, [128, 2, 6], F32) for i in range(2)]
        bag = [sb("bag%d" % i, [128, 2, 2], F32) for i in range(2)]
        sm = [sb("sm%d" % i, [128, 2, 4], F32) for i in range(2)]
        b_sm = P.bufs_n("msm", 2)
        sq = [sb("sq%d" % i, [128, 2, 1], F32) for i in range(2)]
        b_sq = P.bufs_n("msq", 2)
        hn = [sb("hn%d" % i, [128, 512], F32) for i in range(2)]
        b_hn = P.bufs_n("hn", 2)
        ya = [sb("ya%d" % i, [128, 512], BF16) for i in range(2)]
        b_ya = P.bufs_n("ya", 2)
        yaT = [sb("yaT%d" % i, [128, 4, 128], BF16) for i in range(2)]
        b_yaT = P.bufs_n("yaTs", 2)
        pSm = ps("pSm", [128, 2, 128], F32)
        pOm = ps("pOm", [128, 2, 129], F32)
        pU = ps("pU", [128, 2, 129], F32)
        pT5 = ps("pT5", [128, 5, 128], BF16)
        pkt = pT5[:, 4, :]
        pyT = pT5[:, 0:4, :]
        b_pSm, b_pOm, b_pU, b_pkt, b_pyT = [P.buf(n) for n in ("pSm", "pOm", "pU", "pkt", "pyT")]

        def mlstm_chunk(c):
            cols = slice(c * 128, (c + 1) * 128)
            rows = slice(c * 128, (c + 1) * 128)
            vi = c % 2
            P.op("sp", lambda e: e.dma_start(out=va[vi][:], in_=SC["vaugM"][rows]), reads=[DB("vaugM")[c]],
                 writes=[b_va[vi]], dma=True)
            P.op("sp", lambda e: e.dma_start(out=sgo[vi][:], in_=SC["so"][rows]), reads=[DB("so")[c]],
                 writes=[b_sgo[vi]], dma=True)
            hnb = hn[vi]
            for hp in range(2):
                si = hp
                for hh in range(2):
                    P.op("pe", lambda e, hp=hp, hh=hh: e.matmul(
                        pSm[:, hh, :], lhsT=mk[hp][:, cols], rhs=mqz[2 * hp + hh][:, cols], start=True, stop=True),
                        reads=[b_mqk], writes=[b_pSm])
                P.op("pe", lambda e, hp=hp: e.transpose(out=pkt, in_=mk[hp][:, cols], identity=C.ident[:]),
                     reads=[b_mqk, C.b_const], writes=[b_pkt])
                P.op("dve", lambda e, si=si: e.scalar_tensor_tensor(
                    out=Sm[si][:], in0=pSm[:], scalar=0.125,
                    in1=mask01[:].unsqueeze(1).to_broadcast([128, 2, 128]), op0=ALU.mult, op1=ALU.mult),
                    reads=[b_pSm, b_c], writes=[b_Sm[si]])
                P.op("act", lambda e, si=si: e.activation(out=ktm[si][:], in_=pkt, func=AF.Copy, scale=0.125),
                     reads=[b_pkt], writes=[b_ktm[si]])
                yield
                for hh in range(2):
                    h = 2 * hp + hh
                    P.op("pe", lambda e, si=si, hh=hh, h=h: e.matmul(
                        pOm[:, hh, :], lhsT=Sm[si][:, hh, :], rhs=va[vi][:, h, :], start=True, stop=(c == 0)),
                        reads=[b_Sm[si], b_va[vi]], writes=[b_pOm])
                    if c > 0:
                        P.op("pe", lambda e, hp=hp, hh=hh, h=h: e.matmul(
                            pOm[:, hh, :], lhsT=mqz[h][:, cols], rhs=Cb[hp][:, :], start=False, stop=True),
                            reads=[b_mqk, b_Cb[hp]], writes=[b_pOm])
                for hh in range(2):
                    h = 2 * hp + hh
                    P.op("pe", lambda e, si=si, hh=hh, h=h: e.matmul(
                        pU[:, hh, :], lhsT=ktm[si][:], rhs=va[vi][:, h, :], start=True, stop=True),
                        reads=[b_ktm[si], b_va[vi]], writes=[b_pU])
                for hh in range(2):
                    h = 2 * hp + hh
                    R = slice(hh * 64, (hh + 1) * 64)
                    if c == 0:
                        P.op("dve", lambda e, hp=hp, hh=hh, R=R: e.tensor_copy(out=Cst[hp][R, :], in_=pU[R, hh, :]),
                             reads=[b_pU], writes=[b_Cst[hp]])
                    else:
                        P.op("dve", lambda e, hp=hp, hh=hh, R=R, h=h: e.scalar_tensor_tensor(
                            out=Cst[hp][R, :], in0=Cst[hp][R, :], scalar=C.decbc[R, h, c:c + 1], in1=pU[R, hh, :],
                            op0=ALU.mult, op1=ALU.add), reads=[b_pU, b_Cst[hp], C.b_decbc], writes=[b_Cst[hp]])
                    if c < 31:
                        P.op("dve", lambda e, hp=hp, R=R, h=h: e.tensor_scalar(
                            out=Cb[hp][R, :], in0=Cst[hp][R, :], scalar1=C.decbc[R, h, c + 1:c + 2], scalar2=None,
                            op0=ALU.mult), reads=[b_Cst[hp], C.b_decbc], writes=[b_Cb[hp]])
                smp, bstp, bagp, bsm = sm[hp], bst[hp], bag[hp], b_sm[hp]
                for hh in range(2):
                    P.op("dve", lambda e, hh=hh, bstp=bstp: e.bn_stats(out=bstp[:, hh, :], in_=pOm[:, hh, 0:128]),
                         reads=[b_pOm], writes=[bsm])
                    P.op("dve", lambda e, hh=hh, bstp=bstp, bagp=bagp: e.bn_aggr(out=bagp[:, hh, :], in_=bstp[:, hh, :]),
                         reads=[bsm], writes=[bsm])
                sqp, bsq = sq[hp], b_sq[hp]
                P.op("act", lambda e, sqp=sqp: e.activation(out=sqp[:], in_=pOm[:, :, 128:129], func=AF.Square),
                     reads=[b_pOm], writes=[bsq])
                P.op("dve", lambda e, hp=hp, smp=smp, sqp=sqp: e.tensor_tensor(
                    out=smp[:, :, 0:1], in0=sqp[:], in1=C.wthr[:, c, 4 + 2 * hp:6 + 2 * hp].unsqueeze(2),
                    op=ALU.max), reads=[C.b_wthr, bsm, bsq], writes=[bsm])
                P.op("dve", lambda e, smp=smp, bagp=bagp: e.scalar_tensor_tensor(
                    out=smp[:, :, 1:2], in0=smp[:, :, 0:1], scalar=EPS, in1=bagp[:, :, 1:2], op0=ALU.mult, op1=ALU.add),
                    reads=[bsm], writes=[bsm])
                P.op("pool", lambda e, smp=smp: e.tensor_tensor(
                    out=smp[:, :, 2:3], in0=smp[:, :, 1:2],
                    in1=C.neghalf[:].unsqueeze(1).to_broadcast([128, 2, 1]), op=ALU.pow),
                    reads=[bsm, C.b_const], writes=[bsm])
                for hh in range(2):
                    h = 2 * hp + hh
                    P.op("dve", lambda e, hh=hh, h=h, smp=smp, bagp=bagp: e.tensor_scalar(
                        out=hnb[:, h * 128:(h + 1) * 128], in0=pOm[:, hh, 0:128], scalar1=bagp[:, hh, 0:1],
                        scalar2=smp[:, hh, 2:3], op0=ALU.subtract, op1=ALU.mult),
                        reads=[b_pOm, bsm], writes=[b_hn[vi]])
                yield
            yi = c % 2
            P.op("pool", lambda e: e.tensor_tensor(out=hnb[:], in0=hnb[:], in1=gln[:], op=ALU.mult),
                 reads=[b_hn[vi], b_c], writes=[b_hn[vi]])
            P.op("dve", lambda e: e.tensor_tensor(out=ya[yi][:], in0=hnb[:], in1=sgo[vi][:], op=ALU.mult),
                 reads=[b_hn[vi], b_sgo[vi]], writes=[b_ya[yi]])
            yield
            for k in range(4):
                P.op("pe", lambda e, k=k: e.transpose(out=pyT[:, k, :], in_=ya[yi][:, k * 128:(k + 1) * 128],
                                                      identity=C.ident[:]),
                     reads=[b_ya[yi], C.b_const], writes=[b_pyT])
            P.op("act", lambda e: e.activation(out=yaT[yi][:], in_=pyT, func=AF.Copy), reads=[b_pyT],
                 writes=[b_yaT[yi]])
            P.op("sp", lambda e: e.dma_start(out=SC["yaT"][:, :, cols].rearrange("k p t -> p k t"), in_=yaT[yi][:]),
                 reads=[b_yaT[yi]], writes=[DB("yaT")[c]], dma=True)
            yield

        def mlstm_gen():
            for c in range(32):
                yield from mlstm_chunk(c)

        VF = sb("VF", [128, 32, 8 * 65], BF16)
        b_VF = P.buf("VF")
        P.op("sp", lambda e: e.dma_start(out=VF[:], in_=SC["vaugF"].rearrange("(j p) h e -> p j (h e)", p=128)),
             reads=DB("vaugF"), writes=[b_VF], dma=True)
        QA = [sb("QA%d" % i, [70, S], BF16) for i in range(2)]
        KA = [sb("KA%d" % i, [70, S], BF16) for i in range(2)]
        b_QA = P.bufs_n("QA", 2)
        b_KA = P.bufs_n("KA", 2)
        PT = [sb("PT%d" % i, [128, 512], BF16) for i in range(3)]
        b_PT = P.bufs_n("PT", 3)
        rec = [sb("rec%d" % i, [1, 512], F32) for i in range(2)]
        b_rec = P.bufs_n("rec", 2)
        b_recd = P.bufs_n("recd", 2)
        osb = [sb("osb%d" % i, [65, 512], F32) for i in range(2)]
        b_osb = P.bufs_n("osb", 2)
        bcs = [sb("bcs%d" % i, [65, 512], F32) for i in range(2)]
        b_bcs = P.bufs_n("bcs", 2)
        ybt = [sb("ybt%d" % i, [65, 512], BF16) for i in range(2)]
        b_ybt = P.bufs_n("ybt", 2)
        pS = [ps("pS%d" % i, [128, 512], F32) for i in range(3)]
        b_pS = P.bufs_n("pS", 3)
        slot_of = {}
        pO = ps("pO", [128, 512], F32)
        b_pO = P.buf("pO")
        cnt = {"s": 0, "y": 0}

        def fox_load(h):
            hb = h % 2
            P.op("sp", lambda e: e.dma_start(out=QA[hb][:], in_=SC["qaug"][h]), reads=DB("aug"), writes=[b_QA[hb]], dma=True)
            P.op("sp", lambda e: e.dma_start(out=KA[hb][:], in_=SC["kaug"][h]), reads=DB("aug"), writes=[b_KA[hb]], dma=True)

        seq = [(h, i, j) for h in range(8) for i in range(8) for j in range(4 * i + 4)]

        def emit_S(idx):
            h, i, j = seq[idx]
            hb = h % 2
            sj = cnt["s"] % 3
            cnt["s"] += 1
            slot_of[idx] = sj
            jj = j - 4 * i
            kc = slice(j * 128, (j + 1) * 128)
            rd = [b_KA[hb], b_QA[hb]]
            if jj < 0:
                P.op("pe", lambda e: e.matmul(
                    pS[sj][:, 0:512], lhsT=KA[hb][:, kc], rhs=QA[hb][:, i * 512:(i + 1) * 512], start=True, stop=True),
                    reads=rd, writes=[b_pS[sj]])
            else:
                qs = jj * 128
                wq = 512 - qs
                q0 = i * 512 + qs
                P.op("pe", lambda e: e.matmul(pS[sj][:, 0:128], lhsT=C.ident[:], rhs=trim[:], start=True, stop=False),
                     reads=[C.b_const, b_c], writes=[b_pS[sj]])
                P.op("pe", lambda e: e.matmul(
                    pS[sj][:, 0:128], lhsT=KA[hb][:, kc], rhs=QA[hb][:, q0:q0 + 128], start=False, stop=True),
                    reads=rd, writes=[b_pS[sj]])
                if wq > 128:
                    P.op("pe", lambda e: e.matmul(
                        pS[sj][:, 128:wq], lhsT=KA[hb][:, kc], rhs=QA[hb][:, q0 + 128:q0 + wq], start=True, stop=True),
                        reads=rd, writes=[b_pS[sj]])

        def emit_rest(idx):
            h, i, j = seq[idx]
            sj = slot_of[idx]
            nkb = 4 * i + 4
            jj = j - 4 * i
            qs = max(jj, 0) * 128
            wq = 512 - qs
            tj = idx % 3
            P.op("act", lambda e: e.activation(out=PT[tj][:, 0:wq], in_=pS[sj][:, 0:wq], func=AF.Exp),
                 reads=[b_pS[sj]], writes=[b_PT[tj]])
            P.op("pe", lambda e: e.matmul(
                pO[0:65, qs:512], lhsT=VF[:, j, h * 65:(h + 1) * 65], rhs=PT[tj][:, 0:wq],
                start=(j == 0), stop=(j == nkb - 1)),
                reads=[b_VF, b_PT[tj]], writes=[b_pO])
            if j < nkb - 1:
                return
            yi = cnt["y"] % 2
            cnt["y"] += 1
            u = h * 8 + i
            P.op("act", lambda e: e.activation(out=osb[yi][:], in_=pO[0:65, :], func=AF.Copy), reads=[b_pO],
                 writes=[b_osb[yi]])
            P.op("dve", lambda e: e.reciprocal(out=rec[yi][0:1, :], in_=osb[yi][0:1, :]), reads=[b_osb[yi]],
                 writes=[b_rec[yi]])
            P.op("sp", lambda e: e.dma_start(out=SC["recd"][u:u + 1, :], in_=rec[yi][0:1, :]), reads=[b_rec[yi]],
                 writes=[b_recd[yi]], dma=True)
            P.op("sp", lambda e: e.dma_start(out=bcs[yi][:], in_=SC["recd"][u:u + 1, :].partition_broadcast(65)),
                 reads=[b_recd[yi]], writes=[b_bcs[yi]], dma=True)
            P.op("dve", lambda e: e.tensor_tensor(out=ybt[yi][:], in0=osb[yi][:], in1=bcs[yi][:], op=ALU.mult),
                 reads=[b_osb[yi], b_bcs[yi]], writes=[b_ybt[yi]])
            P.op("sp", lambda e: e.dma_start(
                out=SC["ybT"][h // 2, (h % 2) * 64:(h % 2) * 64 + 64, i * 512:(i + 1) * 512], in_=ybt[yi][1:65, :]),
                reads=[b_ybt[yi]], writes=[DB("ybT")[(h * 8 + i) % 32]], dma=True)

        gen = mlstm_gen()
        fox_load(0)
        emit_S(0)
        emit_S(1)
        for idx, (h, i, j) in enumerate(seq):
            if i == 0 and j == 0 and h + 1 < 8:
                fox_load(h + 1)
            if idx + 2 < len(seq):
                emit_S(idx + 2)
            emit_rest(idx)
            if idx % 6 == 5:
                next(gen, None)
        for _ in gen:
            pass
        P.flush()


def merge_pass(P, nc, C, I, SC, DB, h1, h1_b, h2, h2_b):
    with contextlib.ExitStack() as st:
        sb = lambda name, shape, dt: st.enter_context(nc.sbuf_tensor("mg" + name, shape, dt))
        ps = lambda name, shape, dt: st.enter_context(nc.psum_tensor("mg" + name, shape, dt))
        Wa = sb("Wa", [128, 4, D], BF16)
        Wb = sb("Wb", [128, 4, D], BF16)
        Wo = sb("Wo", [128, 8, D], BF16)
        b_W = P.buf("mgW")
        P.op("pool", lambda e: e.dma_start(out=Wa[:], in_=I["w_branch_a"].rearrange("(k p) d -> p k d", p=128)),
             writes=[b_W], dma=True)
        P.op("pool", lambda e: e.dma_start(out=Wb[:], in_=I["w_branch_b"].rearrange("(k p) d -> p k d", p=128)),
             writes=[b_W], dma=True)
        P.op("pool", lambda e: e.dma_start(out=Wo[:], in_=I["w_out"].rearrange("(k p) d -> p k d", p=128)),
             writes=[b_W], dma=True)
        gpost = sb("gpost", [128, D], F32)
        P.op("sp", lambda e: e.dma_start(out=gpost[:], in_=I["mix_post_g"].partition_broadcast(128)),
             writes=[b_W], dma=True)
        yaT = [sb("yaT%d" % i, [128, 4, 128], BF16) for i in range(2)]
        ybT = [sb("ybT%d" % i, [128, 4, 128], BF16) for i in range(2)]
        gab = [sb("gab%d" % i, [128, 2, D], BF16) for i in range(2)]
        hin = [sb("hin%d" % i, [128, D], F32) for i in range(2)]
        b_in = P.bufs_n("mgin", 2)
        b_inb = P.bufs_n("mginb", 2)
        b_ga = P.bufs_n("mgga", 2)
        b_gb = P.bufs_n("mggb", 2)
        b_hin = P.bufs_n("mghin", 2)
        t1 = [sb("t1%d" % i, [128, D], F32) for i in range(2)]
        t2 = [sb("t2%d" % i, [128, D], F32) for i in range(2)]
        mb = [sb("mb%d" % i, [128, D], BF16) for i in range(2)]
        mT = [sb("mT%d" % i, [128, 8, 128], BF16) for i in range(2)]
        junk = sb("junk", [128, D], BF16)
        hout = [sb("hout%d" % i, [128, D], F32) for i in range(2)]
        stat = sb("stat", [128, 6], F32)
        b_t1, b_t2, b_mb, b_mT = [P.bufs_n(n, 2) for n in ("t1", "t2", "mb", "mT")]
        b_junk = P.buf("mgjunk")
        b_hout = P.bufs_n("hout", 2)
        b_stat = P.bufs_n("mgstat", 2)
        pA = ps("pA", [128, D], F32)
        pB = ps("pB", [128, D], F32)
        pO = ps("pO", [128, D], F32)
        pt = ps("pt", [128, 8, 128], BF16)
        b_pA, b_pB, b_pO, b_pt = [P.buf(n) for n in ("pA", "pB", "mgpO", "mgpt")]
        h1v = h1.rearrange("(n p) d -> n p d", p=128)
        h2v = h2.rearrange("(n p) d -> n p d", p=128)

        def s1(n):
            ib = n % 2
            rows = slice(n * 128, (n + 1) * 128)
            cols = rows
            P.op("sp", lambda e: e.dma_start(out=yaT[ib][:], in_=SC["yaT"][:, :, cols].rearrange("k p t -> p k t")),
                 reads=[DB("yaT")[n]], writes=[b_in[ib]], dma=True)
            P.op("sp", lambda e: e.dma_start(out=ybT[ib][:], in_=SC["ybT"][:, :, cols].rearrange("k p t -> p k t")),
                 reads=DB("ybT"), writes=[b_inb[ib]], dma=True)
            P.op("sp", lambda e: e.dma_start(out=gab[ib][:, 0, :], in_=SC["ga"][rows]),
                 reads=[DB("gab")[n]], writes=[b_ga[ib]], dma=True)
            P.op("sp", lambda e: e.dma_start(out=gab[ib][:, 1, :], in_=SC["gb"][rows]),
                 reads=[DB("gab")[n]], writes=[b_gb[ib]], dma=True)
            P.op("sp", lambda e: e.dma_start(out=hin[ib][:], in_=h1v[n]),
                 reads=[h1_b[n]], writes=[b_hin[ib]], dma=True)
            for hf in range(2):
                hs = slice(hf * 512, (hf + 1) * 512)
                for k in range(4):
                    P.op("pe", lambda e, k=k, hs=hs: e.matmul(pA[:, hs], lhsT=yaT[ib][:, k, :], rhs=Wa[:, k, hs],
                                                              start=(k == 0), stop=(k == 3)),
                         reads=[b_in[ib], b_W], writes=[b_pA])
            for hf in range(2):
                hs = slice(hf * 512, (hf + 1) * 512)
                for k in range(4):
                    P.op("pe", lambda e, k=k, hs=hs: e.matmul(pB[:, hs], lhsT=ybT[ib][:, k, :], rhs=Wb[:, k, hs],
                                                              start=(k == 0), stop=(k == 3)),
                         reads=[b_inb[ib], b_W], writes=[b_pB])
            P.op("dve", lambda e: e.tensor_tensor(out=t1[ib][:], in0=pA[:], in1=gab[ib][:, 0, :], op=ALU.mult),
                 reads=[b_pA, b_ga[ib]], writes=[b_t1[ib]])
            P.op("dve", lambda e: e.tensor_tensor(out=t2[ib][:], in0=pB[:], in1=gab[ib][:, 1, :], op=ALU.mult),
                 reads=[b_pB, b_gb[ib]], writes=[b_t2[ib]])
            P.op("pool", lambda e: e.tensor_tensor(out=mb[ib][:], in0=t1[ib][:], in1=t2[ib][:], op=ALU.add),
                 reads=[b_t1[ib], b_t2[ib]], writes=[b_mb[ib]])

        def s2(n):
            ib = n % 2
            for k in range(8):
                P.op("pe", lambda e, k=k: e.transpose(out=pt[:, k, :], in_=mb[ib][:, k * 128:(k + 1) * 128],
                                                      identity=C.ident[:]),
                     reads=[b_mb[ib], C.b_const], writes=[b_pt])
            P.op("act", lambda e: e.activation(out=mT[ib][:], in_=pt[:], func=AF.Copy), reads=[b_pt], writes=[b_mT[ib]])
            for hf in range(2):
                hs = slice(hf * 512, (hf + 1) * 512)
                for k in range(8):
                    P.op("pe", lambda e, k=k, hs=hs: e.matmul(pO[:, hs], lhsT=mT[ib][:, k, :], rhs=Wo[:, k, hs],
                                                              start=(k == 0), stop=(k == 7)),
                         reads=[b_mT[ib], b_W], writes=[b_pO])
            si = n % 2
            ss, var, rstd = (stat[:, 3 * si + j:3 * si + j + 1] for j in range(3))
            rms_stats(P, C, pO[:], b_pO, junk[:], b_junk, ss, var, rstd, b_stat[si])
            P.op("dve", lambda e: e.scalar_tensor_tensor(
                out=hout[ib][:], in0=pO[:], scalar=rstd, in1=gpost[:], op0=ALU.mult, op1=ALU.mult),
                reads=[b_pO, b_stat[si], b_W], writes=[b_hout[ib]])
            P.op("pool", lambda e: e.tensor_tensor(out=hout[ib][:], in0=hout[ib][:], in1=hin[ib][:], op=ALU.add),
                 reads=[b_hout[ib], b_hin[ib]], writes=[b_hout[ib]])
            P.op("pool", lambda e: e.dma_start(out=h2v[n], in_=hout[ib][:]),
                 reads=[b_hout[ib]], writes=[h2_b[n]], dma=True)

        s1(0)
        for n in range(32):
            if n + 1 < 32:
                s1(n + 1)
            s2(n)
        P.flush()


def ple_pass(P, nc, C, I, uT3, uT3_b, h3, h3_b, out):
    with contextlib.ExitStack() as st:
        sb = lambda name, shape, dt: st.enter_context(nc.sbuf_tensor("pl" + name, shape, dt))
        ps = lambda name, shape, dt: st.enter_context(nc.psum_tensor("pl" + name, shape, dt))
        Wg = sb("Wg", [128, 8, D], BF16)
        Wp = sb("Wp", [128, 2, D], BF16)
        b_W = P.buf("plW")
        P.op("pool", lambda e: e.dma_start(out=Wg[:], in_=I["ple_w_gate"].rearrange("(k p) d -> p k d", p=128)),
             writes=[b_W], dma=True)
        P.op("pool", lambda e: e.dma_start(out=Wp[:], in_=I["ple_w_proj"].rearrange("(k p) d -> p k d", p=128)),
             writes=[b_W], dma=True)
        gpost = sb("gpost", [128, D], F32)
        bg = sb("bg", [128, D], F32)
        P.op("sp", lambda e: e.dma_start(out=gpost[:], in_=I["ple_post_g"].partition_broadcast(128)),
             writes=[b_W], dma=True)
        P.op("sp", lambda e: e.dma_start(out=bg[:], in_=I["ple_b_gate"].partition_broadcast(128)),
             writes=[b_W], dma=True)
        uT = [sb("uT%d" % i, [128, 8, 128], BF16) for i in range(2)]
        pin = [sb("pin%d" % i, [128, 256], F32) for i in range(2)]
        hin = [sb("hin%d" % i, [128, D], F32) for i in range(2)]
        b_in = P.bufs_n("plin", 2)
        b_pin = P.bufs_n("plpin", 2)
        b_hin = P.bufs_n("plhin", 2)
        pb = [sb("pb%d" % i, [128, 256], BF16) for i in range(2)]
        pT = [sb("pT%d" % i, [128, 2, 128], BF16) for i in range(2)]
        gt = [sb("gt%d" % i, [128, D], F32) for i in range(2)]
        ge = [sb("ge%d" % i, [128, D], F32) for i in range(2)]
        junk = sb("junk", [128, D], BF16)
        hout = [sb("hout%d" % i, [128, D], F32) for i in range(2)]
        stat = sb("stat", [128, 6], F32)
        b_pb, b_pT, b_gt, b_ge = [P.bufs_n(n, 2) for n in ("pb", "pT", "gt", "ge")]
        b_junk = P.buf("pljunk")
        b_hout = P.bufs_n("plhout", 2)
        b_stat = P.bufs_n("plstat", 2)
        pG = [ps("pG%d" % i, [128, D], F32) for i in range(2)]
        pE = ps("pE", [128, D], F32)
        ptp = ps("ptp", [128, 2, 128], BF16)
        b_pG = P.bufs_n("pG", 2)
        b_pE, b_ptp = P.buf("pE"), P.buf("ptp")
        pv = I["p"].rearrange("(n p) d -> n p d", p=128)
        h3v = h3.rearrange("(n p) d -> n p d", p=128)
        ov = out.rearrange("(n p) d -> n p d", p=128)

        def s1(n):
            ib = n % 2
            cols = slice(n * 128, (n + 1) * 128)
            P.op("sp", lambda e: e.dma_start(out=uT[ib][:], in_=uT3[:, :, cols].rearrange("k p t -> p k t")),
                 reads=[uT3_b[n]], writes=[b_in[ib]], dma=True)
            P.op("sp", lambda e: e.dma_start(out=pin[ib][:], in_=pv[n]), writes=[b_pin[ib]], dma=True)
            P.op("sp", lambda e: e.dma_start(out=hin[ib][:], in_=h3v[n]), reads=[h3_b[n]],
                 writes=[b_hin[ib]], dma=True)
            P.op("act", lambda e: e.activation(out=pb[ib][:], in_=pin[ib][:], func=AF.Copy), reads=[b_pin[ib]],
                 writes=[b_pb[ib]])
            for k in range(2):
                P.op("pe", lambda e, k=k: e.transpose(out=ptp[:, k, :], in_=pb[ib][:, k * 128:(k + 1) * 128],
                                                      identity=C.ident[:]),
                     reads=[b_pb[ib], C.b_const], writes=[b_ptp])
            P.op("dve", lambda e: e.tensor_copy(out=pT[ib][:], in_=ptp[:]), reads=[b_ptp], writes=[b_pT[ib]])
            for hf in range(2):
                hs = slice(hf * 512, (hf + 1) * 512)
                for k in range(8):
                    P.op("pe", lambda e, k=k, hs=hs: e.matmul(pG[ib][:, hs], lhsT=uT[ib][:, k, :], rhs=Wg[:, k, hs],
                                                              start=(k == 0), stop=(k == 7)),
                         reads=[b_in[ib], b_W], writes=[b_pG[ib]])

        def s2(n):
            ib = n % 2
            for hf in range(2):
                hs = slice(hf * 512, (hf + 1) * 512)
                for k in range(2):
                    P.op("pe", lambda e, k=k, hs=hs: e.matmul(pE[:, hs], lhsT=pT[ib][:, k, :], rhs=Wp[:, k, hs],
                                                              start=(k == 0), stop=(k == 1)),
                         reads=[b_pT[ib], b_W], writes=[b_pE])
            P.op("dve", lambda e: e.tensor_tensor(out=gt[ib][:], in0=pG[ib][:], in1=bg[:], op=ALU.add),
                 reads=[b_pG[ib], b_W], writes=[b_gt[ib]])
            P.op("act", lambda e: e.activation(out=gt[ib][:], in_=gt[ib][:], func=AF.Sigmoid), reads=[b_gt[ib]],
                 writes=[b_gt[ib]])
            P.op("dve", lambda e: e.tensor_tensor(out=ge[ib][:], in0=gt[ib][:], in1=pE[:], op=ALU.mult),
                 reads=[b_gt[ib], b_pE], writes=[b_ge[ib]])
            si = n % 2
            ss, var, rstd = (stat[:, 3 * si + j:3 * si + j + 1] for j in range(3))
            rms_stats(P, C, ge[ib][:], b_ge[ib], junk[:], b_junk, ss, var, rstd, b_stat[si])
            P.op("dve", lambda e: e.scalar_tensor_tensor(
                out=hout[ib][:], in0=ge[ib][:], scalar=rstd, in1=gpost[:], op0=ALU.mult, op1=ALU.mult),
                reads=[b_ge[ib], b_stat[si], b_W], writes=[b_hout[ib]])
            P.op("pool", lambda e: e.tensor_tensor(out=hout[ib][:], in0=hout[ib][:], in1=hin[ib][:], op=ALU.add),
                 reads=[b_hout[ib], b_hin[ib]], writes=[b_hout[ib]])
            P.op("pool", lambda e: e.dma_start(out=ov[n], in_=hout[ib][:]), reads=[b_hout[ib]], dma=True)

        s1(0)
        for n in range(32):
            if n + 1 < 32:
                s1(n + 1)
            s2(n)
        P.flush()


def build_program(debug=False, stage=99, only=None):
    nc = bass.Bass("TRN2", target_bir_lowering=False)
    I = {}

    def din(name, shape):
        I[name] = nc.dram_tensor(name, shape, F32, kind="ExternalInput").ap()
        return I[name]

    din("x", [S, D])
    din("p", [S, 256])
    for nm in ("ffn1", "ffn2"):
        din(nm + "_pre_g", [1, D])
        din(nm + "_w_gate", [D, DFF])
        din(nm + "_w_up", [D, DFF])
        din(nm + "_w_down", [DFF, D])
        din(nm + "_post_g", [1, D])
    din("mix_pre_g", [1, D])
    din("w_in", [D, INW])
    din("conv_w", [4, 512])
    din("conv_b", [1, 512])
    din("mlstm_i_bias", [1, 4])
    din("mlstm_f_bias", [1, 4])
    din("mlstm_norm_g", [1, 512])
    din("fox_f_bias", [1, 8])
    din("branch_gate_bias", [1, 2048])
    din("w_branch_a", [512, D])
    din("w_branch_b", [512, D])
    din("w_out", [D, D])
    din("mix_post_g", [1, D])
    din("ple_pre_g", [1, D])
    din("ple_w_gate", [D, D])
    din("ple_b_gate", [1, D])
    din("ple_w_proj", [256, D])
    din("ple_post_g", [1, D])

    skind = "ExternalOutput" if debug else "Internal"

    def dscr(name, shape, dt):
        return nc.dram_tensor(name, shape, dt, kind=skind).ap()

    out = nc.dram_tensor("out", [S, D], F32, kind="ExternalOutput").ap()
    h1 = dscr("h1", [S, D], F32)
    uT1 = dscr("uT1", [8, 128, S], BF16)
    gpre = dscr("gpre", [72, S], F32)
    SC = {
        "mqkT": dscr("mqkT", [4, 128, S], BF16),
        "vaugM": dscr("vaugM", [S, 4, 129], BF16),
        "so": dscr("so", [S, 512], BF16),
        "qaug": dscr("qaug", [8, 70, S], BF16),
        "kaug": dscr("kaug", [8, 70, S], BF16),
        "vaugF": dscr("vaugF", [S, 8, 65], BF16),
        "ga": dscr("ga", [S, D], BF16),
        "gb": dscr("gb", [S, D], BF16),
        "yaT": dscr("yaT", [4, 128, S], BF16),
        "ybT": dscr("ybT", [4, 128, S], BF16),
        "recd": dscr("recd", [64, 512], F32),
    }
    h2 = dscr("h2", [S, D], F32)
    h3 = dscr("h3", [S, D], F32)
    uT3 = dscr("uT3", [8, 128, S], BF16)

    with contextlib.ExitStack() as st:
        P = Prog(nc, st)
        C = Ctx()
        C.db = {}

        def db(name):
            if name not in C.db:
                C.db[name] = P.bufs_n("D" + name, 32)
            return C.db[name]

        setup_consts(P, nc, st, C)
        C.wthr = st.enter_context(nc.sbuf_tensor("wthr", [128, 32, 8], F32))
        C.decbc = st.enter_context(nc.sbuf_tensor("decbc", [128, 4, 32], F32))
        C.b_wthr = P.buf("wthr")
        C.b_decbc = P.buf("decbc")
        def want(name, st_no):
            return (name in only) if only is not None else (stage >= st_no)

        if want("ffn1", 1):
            ffn_pass(P, nc, C, "f1", I["x"], I["ffn1_w_gate"], I["ffn1_w_up"], I["ffn1_w_down"],
                     I["ffn1_pre_g"], I["ffn1_post_g"], h1, I["mix_pre_g"], uT1,
                     db("x"), db("h1"), db("uT1"), gate_w=I["w_in"], gate_dst=gpre, gate_b=db("gpre"))
        if want("gp", 2):
            aug_b = P.buf("augrows")
            gp_stage(P, nc, C, I, gpre, db("gpre"), SC["qaug"], SC["kaug"], aug_b)
            db("aug").append(aug_b)
        if want("win", 2):
            win_pass(P, nc, C, I, uT1, db("uT1"), SC, db)
        if want("mix", 3):
            mix_pass(P, nc, C, I, SC, db)
        if want("merge", 4):
            merge_pass(P, nc, C, I, SC, db, h1, db("h1"), h2, db("h2"))
        if want("ffn2", 5):
            ffn_pass(P, nc, C, "f2", h2, I["ffn2_w_gate"], I["ffn2_w_up"], I["ffn2_w_down"],
                     I["ffn2_pre_g"], I["ffn2_post_g"], h3, I["ple_pre_g"], uT3,
                     db("h2"), db("h3"), db("uT3"))
        if want("ple", 6):
            ple_pass(P, nc, C, I, uT3, db("uT3"), h3, db("h3"), out)
        P.flush(final=True)
    return nc

IN_NAMES = ["x", "p", "ffn1_pre_g", "ffn1_w_gate", "ffn1_w_up", "ffn1_w_down", "ffn1_post_g",
            "mix_pre_g", "w_in", "conv_w", "conv_b", "mlstm_i_bias", "mlstm_f_bias", "mlstm_norm_g",
            "fox_f_bias", "branch_gate_bias", "w_branch_a", "w_branch_b", "w_out", "mix_post_g",
            "ffn2_pre_g", "ffn2_w_gate", "ffn2_w_up", "ffn2_w_down", "ffn2_post_g",
            "ple_pre_g", "ple_w_gate", "ple_b_gate", "ple_w_proj", "ple_post_g"]


def make_in_maps(inputs, cores):
    maps = []
    shared = {}
    for k in IN_NAMES:
        if k in ("x", "p"):
            continue
        shared[k] = np.ascontiguousarray(np.asarray(inputs[k])[0], dtype=np.float32)
    x = np.asarray(inputs["x"])
    p = np.asarray(inputs["p"])
    for b in cores:
        m = dict(shared)
        m["x"] = np.ascontiguousarray(x[b], dtype=np.float32)
        m["p"] = np.ascontiguousarray(p[0, b], dtype=np.float32)
        maps.append(m)
    return maps


def kernel(**inputs):
    nc = build_program()
    maps = make_in_maps(inputs, list(range(8)))
    res = run_bass_kernel_spmd(nc, maps, core_ids=list(range(8)))
    return np.stack([np.asarray(r["out"], dtype=np.float32) for r in res.results], axis=0)
```

```python
import contextlib
import numpy as np
import concourse.bass as bass
import concourse.mybir as mybir
from concourse.bass_utils import run_bass_kernel_spmd

F32 = mybir.dt.float32
BF16 = mybir.dt.bfloat16
AF = mybir.ActivationFunctionType
ALU = mybir.AluOpType
AX = mybir.AxisListType

S = 4096
D = 1024
DFF = 2816
NFC = DFF // 128
NT = 8
TT = 512
EPS = 1e-6
INW = 5136

ENGS = ("pe", "act", "dve", "pool", "sp")


class Buf:
    __slots__ = ("name", "w", "r")

    def __init__(self, name=""):
        self.name = name
        self.w = None
        self.r = []


class Op:
    __slots__ = ("eng", "fn", "deps", "inc", "cnt", "dma", "sem", "emitted")

    def __init__(self, eng, fn, dma=False):
        self.eng = eng
        self.fn = fn
        self.deps = []
        self.inc = False
        self.cnt = 0
        self.dma = dma
        self.sem = None
        self.emitted = False


class Prog:
    def __init__(self, nc, st, n_dma_sems=20):
        self.nc = nc
        self.pending = {e: [] for e in ENGS}
        self.bufs = []
        self.nd = n_dma_sems
        self.esem = {e: st.enter_context(nc.semaphore("s_" + e)) for e in ENGS}
        self.dsem = {}
        for e in ("sp", "pool"):
            for s in range(n_dma_sems):
                self.dsem[(e, s)] = st.enter_context(nc.semaphore("d_%s_%d" % (e, s)))
        self.ecnt = {e: 0 for e in ENGS}
        self.dcnt = {e: 0 for e in ENGS}
        self.waited = {e: {} for e in ENGS}
        self.n_ops = 0

    def buf(self, name=""):
        b = Buf(name)
        self.bufs.append(b)
        return b

    def bufs_n(self, name, n):
        return [self.buf("%s%d" % (name, i)) for i in range(n)]

    def op(self, eng, fn, reads=(), writes=(), dma=False):
        o = Op(eng, fn, dma)
        seen = set()
        cand = []
        for b in reads:
            if b.w is not None:
                cand.append(b.w)
        for b in writes:
            if b.w is not None:
                cand.append(b.w)
            cand.extend(b.r)
        for d in cand:
            if d is o or id(d) in seen:
                continue
            seen.add(id(d))
            if d.eng == "pe" and eng == "pe" and not d.dma and not dma:
                continue
            o.deps.append(d)
            if not d.emitted:
                d.inc = True
        for b in reads:
            b.r.append(o)
        for b in writes:
            b.w = o
            b.r = []
        self.pending[eng].append(o)
        self.n_ops += 1
        return o

    def flush(self, final=False):
        nc = self.nc
        for b in self.bufs:
            if b.w is not None and not b.w.emitted:
                b.w.inc = True
            for r in b.r:
                if not r.emitted:
                    r.inc = True
        for e in ENGS:
            for o in self.pending[e]:
                if o.dma:
                    k = self.dcnt[e]
                    o.sem = (e, k % self.nd)
                    o.cnt = 16 * (k // self.nd + 1)
                    self.dcnt[e] = k + 1
                elif o.inc:
                    self.ecnt[e] += 1
                    o.cnt = self.ecnt[e]
        pending = self.pending
        self.pending = {e: [] for e in ENGS}

        def run(ename, eng):
            waited = self.waited[ename]
            for o in pending[ename]:
                for d in o.deps:
                    key = d.sem if d.dma else d.eng
                    if waited.get(key, 0) >= d.cnt:
                        continue
                    assert d.cnt > 0, (d.eng, ename)
                    eng.wait_ge(self.dsem[key] if d.dma else self.esem[key], d.cnt)
                    waited[key] = d.cnt
                if o.dma:
                    if o.cnt > 16 and waited.get(o.sem, 0) < o.cnt - 16:
                        eng.wait_ge(self.dsem[o.sem], o.cnt - 16)
                        waited[o.sem] = o.cnt - 16
                    o.fn(eng).then_inc(self.dsem[o.sem], 16)
                else:
                    ins = o.fn(eng)
                    if o.inc:
                        ins.then_inc(self.esem[o.eng], 1)
                o.emitted = True
            if ename == "sp" and final:
                for q in ("sp", "pool"):
                    k = self.dcnt[q]
                    for sl in range(min(self.nd, k)):
                        last = 16 * ((k - 1 - sl) // self.nd + 1)
                        eng.wait_ge(self.dsem[(q, sl)], last)

        with nc.Block() as block:
            @block.tensor
            def _(eng):
                run("pe", eng)

            @block.scalar
            def _(eng):
                run("act", eng)

            @block.vector
            def _(eng):
                run("dve", eng)

            @block.gpsimd
            def _(eng):
                run("pool", eng)

            @block.sync
            def _(eng):
                run("sp", eng)


def bcast_last(ap2d, n):
    return ap2d.unsqueeze(2).to_broadcast([ap2d.shape[0], ap2d.shape[1], n])


class Ctx:
    pass


def load_w_kmajor(P, nc, dst, src2d, n_kc, ncols, bufs, col_chunk=1408):
    v = src2d.rearrange("(kc p) n -> kc p n", p=128)
    mdl = 4 * col_chunk
    for k in range(n_kc):
        P.op("pool", lambda e, k=k: e.dma_start(out=dst[:, k, :], in_=v[k], max_dma_last_dim=mdl),
             writes=[bufs[k]], dma=True)


def setup_consts(P, nc, st, C):
    sb = lambda name, shape, dt: st.enter_context(nc.sbuf_tensor(name, shape, dt))
    C.identf = sb("identf", [128, 128], F32)
    C.ident = sb("ident", [128, 128], BF16)
    C.neghalf = sb("neghalf", [128, 1], F32)
    C.b_const = P.buf("const")
    identf, ident = C.identf, C.ident
    P.op("pool", lambda e: e.memset(identf[:], 0.0), writes=[C.b_const])
    P.op("pool", lambda e: e.affine_select(out=identf[:], in_=identf[:], pattern=[[-1, 128]],
                                             compare_op=ALU.not_equal, fill=1.0, base=0,
                                             channel_multiplier=1),
         reads=[C.b_const], writes=[C.b_const])
    P.op("dve", lambda e: e.tensor_copy(out=ident[:], in_=identf[:]), reads=[C.b_const], writes=[C.b_const])
    P.op("pool", lambda e: e.memset(C.neghalf[:], -0.5), reads=[C.b_const], writes=[C.b_const])


def rms_stats(P, C, src_ap, src_buf, junk, b_junk, ss, var, rstd, b_stat, n_feat=D):
    P.op("act", lambda e: e.activation(out=junk, in_=src_ap, func=AF.Square, accum_out=ss),
         reads=[src_buf], writes=[b_junk, b_stat])
    P.op("dve", lambda e: e.tensor_scalar(out=var, in0=ss, scalar1=1.0 / n_feat, scalar2=EPS,
                                          op0=ALU.mult, op1=ALU.add),
         reads=[b_stat], writes=[b_stat])
    P.op("pool", lambda e: e.tensor_tensor(out=rstd, in0=var, in1=C.neghalf[:], op=ALU.pow),
         reads=[b_stat, C.b_const], writes=[b_stat])


def ffn_pass(P, nc, C, tag, src_h, w_gate, w_up, w_down, pre_g, post_g, dst_h, next_g, dst_uT,
             src_b, dst_b, uT_b, gate_w=None, gate_dst=None, gate_b=None):
    with contextlib.ExitStack() as st:
        sb = lambda name, shape, dt: st.enter_context(nc.sbuf_tensor(tag + name, shape, dt))
        ps = lambda name, shape, dt: st.enter_context(nc.psum_tensor(tag + name, shape, dt))
        Wg = sb("Wg", [128, 8, DFF], BF16)
        Wu = sb("Wu", [128, 8, DFF], BF16)
        Wd = sb("Wd", [128, NFC, D], BF16)
        b_Wg = P.bufs_n("Wg", 8)
        b_Wu = P.bufs_n("Wu", 8)
        b_Wd = P.bufs_n("Wd", 2)
        load_w_kmajor(P, nc, Wg, w_gate, 8, DFF, b_Wg)
        load_w_kmajor(P, nc, Wu, w_up, 8, DFF, b_Wu)
        wdv = w_down.rearrange("(fc p) d -> p fc d", p=128)
        for hh in range(2):
            P.op("pool", lambda e, hh=hh: e.dma_start(out=Wd[:, hh * 11:(hh + 1) * 11, :],
                                                       in_=wdv[:, hh * 11:(hh + 1) * 11, :]),
                 writes=[b_Wd[hh]], dma=True)
        gpre = sb("gpre", [128, 8], F32)
        gnext = sb("gnext", [128, 8], F32)
        gpost = sb("gpost", [128, D], F32)
        b_par = P.buf("par")
        P.op("sp", lambda e: e.dma_start(out=gpre[:], in_=pre_g.rearrange("o (k p) -> p (o k)", p=128),
                                         allow_slow_non_contiguous=True),
             writes=[b_par], dma=True)
        P.op("sp", lambda e: e.dma_start(out=gnext[:], in_=next_g.rearrange("o (k p) -> p (o k)", p=128),
                                         allow_slow_non_contiguous=True),
             writes=[b_par], dma=True)
        P.op("sp", lambda e: e.dma_start(out=gpost[:], in_=post_g.partition_broadcast(128)),
             writes=[b_par], dma=True)
        if gate_w is not None:
            Wgt = sb("Wgt", [128, 8, 72], BF16)
            b_Wgt = P.buf("Wgt")
            P.op("pool", lambda e: e.memset(Wgt[:], 0.0), writes=[b_Wgt])
            gv = gate_w.rearrange("(kc p) n -> p kc n", p=128)
            for (c0, n, d0) in ((1540, 4, 0), (1536, 4, 32), (3080, 8, 64)):
                P.op("pool", lambda e, c0=c0, n=n, d0=d0: e.dma_start(
                    out=Wgt[:, :, d0:d0 + n], in_=gv[:, :, c0:c0 + n]),
                    reads=[], writes=[b_Wgt], dma=True)
            gsb = [sb("gsb%d" % i, [72, 128], F32) for i in range(2)]
            b_gsb = P.bufs_n("gsb", 2)

        NXB = 3
        xb = [sb("xb%d" % i, [128, D], F32) for i in range(NXB)]
        b_xb = P.bufs_n("xb", NXB)
        ubf = [sb("ubf%d" % i, [128, D], BF16) for i in range(2)]
        b_ubf = P.bufs_n("ubf", 2)
        junk = sb("junk", [128, D], BF16)
        b_junk = P.buf("junk")
        uT = sb("uT", [128, 8, TT], BF16)
        b_uT = P.bufs_n("uT", 4)
        aT = sb("aT", [128, NFC, TT], BF16)
        b_aT = P.bufs_n("aT", NFC)
        sg = [sb("sg%d" % i, [128, TT], F32) for i in range(2)]
        b_sg = P.bufs_n("sg", 2)
        hst = [sb("hst%d" % i, [128, D], F32) for i in range(2)]
        b_hst = P.bufs_n("hst", 2)
        u2T = [sb("u2T%d" % i, [128, 8, 128], BF16) for i in range(2)]
        b_u2T = P.bufs_n("u2T", 2)
        NST = 6
        stat = sb("stat", [128, 3 * NST], F32)
        b_stat = P.bufs_n("stat", NST)

        pt = ps("pt", [128, 8, 128], BF16)
        b_pt = P.buf("pt")
        pg = [ps("pg%d" % i, [128, TT], F32) for i in range(2)]
        pu = [ps("pu%d" % i, [128, TT], F32) for i in range(2)]
        b_pg = P.bufs_n("pg", 2)
        b_pu = P.bufs_n("pu", 2)
        pys = [ps("py%d" % i, [128, 512], F32) for i in range(3)]
        b_pys = P.bufs_n("py", 3)
        if gate_w is not None:
            pgt = pu[1][0:72, 0:128]
            b_pgt = b_pu[1]

        src_v = src_h.rearrange("(n p) d -> n p d", p=128)
        dst_v = dst_h.rearrange("(n p) d -> n p d", p=128)
        cnt = {"x": 0, "u": 0, "st": 0, "h": 0, "u2": 0, "sg": 0, "gs": 0, "py": 0}
        pend = []

        def norm_T(h_ap, h_buf, gcol, out_ap, out_bufs, defer=False):
            si = cnt["st"] % NST
            cnt["st"] += 1
            ss, var, rstd = (stat[:, 3 * si + j:3 * si + j + 1] for j in range(3))
            rms_stats(P, C, h_ap, h_buf, junk[:], b_junk, ss, var, rstd, b_stat[si])
            ui = cnt["u"] % 2
            cnt["u"] += 1
            u = ubf[ui]
            P.op("dve", lambda e: e.tensor_scalar(out=u[:], in0=h_ap, scalar1=rstd, scalar2=None, op0=ALU.mult),
                 reads=[h_buf, b_stat[si]], writes=[b_ubf[ui]])
            def pe_part():
                for k in range(8):
                    P.op("pe", lambda e, k=k: e.transpose(out=pt[:, k, :], in_=u[:, k * 128:(k + 1) * 128],
                                                          identity=C.ident[:]),
                         reads=[b_ubf[ui], C.b_const], writes=[b_pt])
                P.op("dve", lambda e: e.tensor_tensor(out=out_ap, in0=pt[:], in1=bcast_last(gcol[:], 128), op=ALU.mult),
                     reads=[b_pt, b_par], writes=out_bufs)
            if defer:
                return pe_part
            pe_part()

        def pre(i):
            for s in range(4):
                n = i * 4 + s
                xi = cnt["x"] % NXB
                cnt["x"] += 1
                P.op("sp", lambda e, n=n, xi=xi: e.dma_start(out=xb[xi][:], in_=src_v[n]),
                     reads=[src_b[n]], writes=[b_xb[xi]], dma=True)
                norm_T(xb[xi][:], b_xb[xi], gpre, uT[:, :, s * 128:(s + 1) * 128], [b_uT[s]])

        def gateup(i):
            for f in range(NFC):
                j = f % 2
                for k in range(8):
                    P.op("pe", lambda e, k=k, f=f, j=j: e.matmul(
                        pg[j][:], lhsT=Wg[:, k, f * 128:(f + 1) * 128], rhs=uT[:, k, :],
                        start=(k == 0), stop=(k == 7)),
                        reads=[b_Wg[k]] + b_uT, writes=[b_pg[j]])
                for k in range(8):
                    P.op("pe", lambda e, k=k, f=f, j=j: e.matmul(
                        pu[j][:], lhsT=Wu[:, k, f * 128:(f + 1) * 128], rhs=uT[:, k, :],
                        start=(k == 0), stop=(k == 7)),
                        reads=[b_Wu[k]] + b_uT, writes=[b_pu[j]])
                if f == 0:
                    while pend:
                        pend.pop(0)()
                si = cnt["sg"] % 2
                cnt["sg"] += 1
                P.op("act", lambda e, j=j, si=si: e.activation(out=sg[si][:], in_=pg[j][:], func=AF.Silu),
                     reads=[b_pg[j]], writes=[b_sg[si]])
                P.op("dve", lambda e, j=j, si=si, f=f: e.tensor_tensor(out=aT[:, f, :], in0=sg[si][:], in1=pu[j][:],
                                                                   op=ALU.mult),
                     reads=[b_sg[si], b_pu[j]], writes=[b_aT[f]])

        def down_post(i):
            for s in range(4):
                n = i * 4 + s
                pyh = []
                for hf in range(2):
                    pi = cnt["py"] % 3
                    cnt["py"] += 1
                    pyh.append((pys[pi], b_pys[pi]))
                    for f in range(NFC):
                        P.op("pe", lambda e, f=f, s=s, hf=hf, pi=pi: e.matmul(
                            pys[pi][:], lhsT=aT[:, f, s * 128:(s + 1) * 128],
                            rhs=Wd[:, f, hf * 512:(hf + 1) * 512], start=(f == 0), stop=(f == NFC - 1)),
                            reads=[b_aT[f], b_Wd[f // 11]], writes=[b_pys[pi]])
                while pend:
                    pend.pop(0)()
                xi = cnt["x"] % NXB
                cnt["x"] += 1
                P.op("sp", lambda e, n=n, xi=xi: e.dma_start(out=xb[xi][:], in_=src_v[n]),
                     reads=[src_b[n]], writes=[b_xb[xi]], dma=True)
                si = cnt["st"] % NST
                cnt["st"] += 1
                ss, var, rstd = (stat[:, 3 * si + j:3 * si + j + 1] for j in range(3))
                P.op("act", lambda e, ss=ss, t=pyh[0][0]: e.activation(out=junk[:, 0:512], in_=t[:], func=AF.Square,
                                                                      accum_out=ss),
                     reads=[pyh[0][1]], writes=[b_junk, b_stat[si]])
                P.op("act", lambda e, var=var, t=pyh[1][0]: e.activation(out=junk[:, 512:1024], in_=t[:], func=AF.Square,
                                                                        accum_out=var),
                     reads=[pyh[1][1]], writes=[b_junk, b_stat[si]])
                P.op("dve", lambda e, ss=ss, var=var: e.tensor_tensor(out=var, in0=ss, in1=var, op=ALU.add),
                     reads=[b_stat[si]], writes=[b_stat[si]])
                P.op("dve", lambda e, var=var: e.tensor_scalar(out=var, in0=var, scalar1=1.0 / D, scalar2=EPS,
                                                              op0=ALU.mult, op1=ALU.add),
                     reads=[b_stat[si]], writes=[b_stat[si]])
                P.op("pool", lambda e, var=var, rstd=rstd: e.tensor_tensor(out=rstd, in0=var, in1=C.neghalf[:], op=ALU.pow),
                     reads=[b_stat[si], C.b_const], writes=[b_stat[si]])
                hi = cnt["h"] % 2
                cnt["h"] += 1
                hb = hst[hi]
                for hf in range(2):
                    hs = slice(hf * 512, (hf + 1) * 512)
                    P.op("dve", lambda e, hb=hb, rstd=rstd, t=pyh[hf][0], hs=hs: e.scalar_tensor_tensor(
                        out=hb[:, hs], in0=t[:], scalar=rstd, in1=gpost[:, hs], op0=ALU.mult, op1=ALU.mult),
                        reads=[pyh[hf][1], b_stat[si], b_par], writes=[b_hst[hi]])
                P.op("dve", lambda e, hb=hb, xi=xi: e.scalar_tensor_tensor(
                    out=hb[:], in0=hb[:], scalar=0.5, in1=xb[xi][:], op0=ALU.mult, op1=ALU.add),
                    reads=[b_hst[hi], b_xb[xi]], writes=[b_hst[hi]])
                P.op("sp", lambda e, hb=hb, n=n: e.dma_start(out=dst_v[n], in_=hb[:]),
                     reads=[b_hst[hi]], writes=[dst_b[n]], dma=True)
                ui2 = cnt["u2"] % 2
                cnt["u2"] += 1
                pe_part = norm_T(hb[:], b_hst[hi], gnext, u2T[ui2][:], [b_u2T[ui2]], defer=True)

                def tail(pe_part=pe_part, ui2=ui2, n=n):
                    pe_part()
                    P.op("sp", lambda e: e.dma_start(
                        out=dst_uT[:, :, n * 128:(n + 1) * 128].rearrange("k p t -> p k t"), in_=u2T[ui2][:]),
                        reads=[b_u2T[ui2]], writes=[uT_b[n]], dma=True)
                    if gate_w is not None:
                        for k in range(8):
                            P.op("pe", lambda e, k=k: e.matmul(
                                pgt, lhsT=Wgt[:, k, :], rhs=u2T[ui2][:, k, :], start=(k == 0), stop=(k == 7)),
                                reads=[b_Wgt, b_u2T[ui2]], writes=[b_pgt])
                        gi = cnt["gs"] % 2
                        cnt["gs"] += 1
                        P.op("act", lambda e: e.activation(out=gsb[gi][:], in_=pgt, func=AF.Copy),
                             reads=[b_pgt], writes=[b_gsb[gi]])
                        P.op("sp", lambda e: e.dma_start(out=gate_dst[:, n * 128:(n + 1) * 128], in_=gsb[gi][:]),
                             reads=[b_gsb[gi]], writes=[gate_b[n]], dma=True)
                pend.append(tail)

        pre(0)
        for i in range(NT):
            gateup(i)
            if i + 1 < NT:
                pre(i + 1)
            down_post(i)
        while pend:
            pend.pop(0)()
        P.flush()


def gp_stage(P, nc, C, I, gpre, gpre_b, qaug, kaug, aug_b):
    with contextlib.ExitStack() as st:
        sb = lambda name, shape, dt: st.enter_context(nc.sbuf_tensor("gp" + name, shape, dt))
        ps = lambda name, shape, dt: st.enter_context(nc.psum_tensor("gp" + name, shape, dt))
        T0 = sb("T0", [72, S], F32)
        T1 = sb("T1", [72, S], F32)
        T2 = sb("T2", [72, S], F32)
        T3 = sb("T3", [72, S], F32)
        QR = sb("QR", [72, 3, S], BF16)
        KR = sb("KR", [72, 3, S], BF16)
        ONE = sb("ONE", [72, S], BF16)
        bcol = sb("bcol", [72, 1], F32)
        negb = sb("negb", [72, 1], F32)
        bicol = sb("bicol", [72, 1], F32)
        onec = sb("onec", [72, 1], F32)
        cm = sb("cm", [72, 32], F32)
        mce = sb("mce", [72, 32], F32)
        mprev = sb("mprev", [72, 32], F32)
        dec = sb("dec", [72, 32], F32)
        esel = sb("esel", [72, 4, 128], F32)
        bT0, bT1, bT2, bT3, bQR, bKR, bONE, bsm = [P.buf(n) for n in
                                                   ("T0", "T1", "T2", "T3", "QR", "KR", "ONE", "gsm")]
        ptm = ps("ptm", [128, 32, 8], F32)
        pdc = ps("pdc", [128, 4, 32], F32)
        b_ptm, b_pdc = P.buf("ptm"), P.buf("pdc")

        P.op("sp", lambda e: e.dma_start(out=T0[:], in_=gpre), reads=gpre_b, writes=[bT0], dma=True)
        P.op("sp", lambda e: e.dma_start(out=T3[0:4, :], in_=gpre[32:36, :]), reads=gpre_b, writes=[bT3], dma=True)
        P.op("dve", lambda e: e.memset(bcol[:], 0.0), writes=[bsm])
        P.op("dve", lambda e: e.memset(bicol[:], 0.0), reads=[bsm], writes=[bsm])
        P.op("dve", lambda e: e.memset(onec[:], 1.0), reads=[bsm], writes=[bsm])
        P.op("pool", lambda e: e.memset(ONE[:], 1.0), writes=[bONE])
        P.op("sp", lambda e: e.dma_start(out=bcol[0:4, :], in_=I["mlstm_f_bias"].rearrange("o n -> n o"),
                                         allow_slow_non_contiguous=True), reads=[bsm], writes=[bsm], dma=True)
        P.op("sp", lambda e: e.dma_start(out=bcol[64:72, :], in_=I["fox_f_bias"].rearrange("o n -> n o"),
                                         allow_slow_non_contiguous=True), reads=[bsm], writes=[bsm], dma=True)
        P.op("sp", lambda e: e.dma_start(out=bicol[0:4, :], in_=I["mlstm_i_bias"].rearrange("o n -> n o"),
                                         allow_slow_non_contiguous=True), reads=[bsm], writes=[bsm], dma=True)
        P.op("dve", lambda e: e.tensor_scalar(out=negb[0:72, :], in0=bcol[0:72, :], scalar1=-1.0, scalar2=None,
                                              op0=ALU.mult), reads=[bsm], writes=[bsm])
        R = slice(0, 72)
        P.op("act", lambda e: e.activation(out=T1[R, :], in_=T0[R, :], func=AF.Exp, scale=-1.0, bias=negb[R, :]),
             reads=[bT0, bsm], writes=[bT1])
        P.op("act", lambda e: e.activation(out=T1[R, :], in_=T1[R, :], func=AF.Ln, scale=1.0, bias=onec[R, :]),
             reads=[bT1, bsm], writes=[bT1])
        P.op("dve", lambda e: e.tensor_tensor_scan(out=T2[R, :], data0=T1[R, :], data1=T1[R, :], initial=0.0,
                                                   op0=ALU.add, op1=ALU.max), reads=[bT1], writes=[bT2])
        M = slice(0, 4)
        P.op("dve", lambda e: e.scalar_tensor_tensor(out=T3[M, :], in0=T3[M, :], scalar=bicol[M, :], in1=T2[M, :],
                                                     op0=ALU.add, op1=ALU.add), reads=[bT3, bT2, bsm], writes=[bT3])
        P.op("dve", lambda e: e.tensor_reduce(out=cm[M, :], in_=T3[M, :].rearrange("p (c l) -> p c l", l=128),
                                              axis=AX.X, op=ALU.max), reads=[bT3], writes=[bsm])
        P.op("dve", lambda e: e.tensor_tensor_scan(out=mce[M, :], data0=cm[M, :], data1=cm[M, :], initial=0.0,
                                                   op0=ALU.max, op1=ALU.max), reads=[bsm], writes=[bsm])
        P.op("dve", lambda e: e.tensor_tensor(out=T3[M, :].rearrange("p (c l) -> p c l", l=128),
                                              in0=T3[M, :].rearrange("p (c l) -> p c l", l=128),
                                              in1=bcast_last(mce[M, :], 128), op=ALU.subtract),
             reads=[bT3, bsm], writes=[bT3])
        P.op("act", lambda e: e.activation(out=T3[M, :], in_=T3[M, :], func=AF.Exp), reads=[bT3], writes=[bT3])
        P.op("dve", lambda e: e.tensor_tensor(out=T1[M, :].rearrange("p (c l) -> p c l", l=128),
                                              in0=T2[M, :].rearrange("p (c l) -> p c l", l=128),
                                              in1=bcast_last(mce[M, :], 128), op=ALU.subtract),
             reads=[bT2, bsm, bT1], writes=[bT1])
        P.op("act", lambda e: e.activation(out=T1[M, :], in_=T1[M, :], func=AF.Exp, scale=2.0), reads=[bT1], writes=[bT1])
        P.op("dve", lambda e: e.memset(mprev[M, :], 0.0), reads=[bsm], writes=[bsm])
        P.op("dve", lambda e: e.tensor_copy(out=mprev[M, 1:32], in_=mce[M, 0:31]), reads=[bsm], writes=[bsm])
        P.op("dve", lambda e: e.tensor_tensor(out=dec[M, :], in0=mprev[M, :], in1=mce[M, :], op=ALU.subtract),
             reads=[bsm], writes=[bsm])
        P.op("act", lambda e: e.activation(out=dec[M, :], in_=dec[M, :], func=AF.Exp), reads=[bsm], writes=[bsm])
        for c in range(32):
            P.op("pe", lambda e, c=c: e.transpose(out=ptm[:, c, 0:4], in_=T3[M, c * 128:(c + 1) * 128],
                                                  identity=C.identf[M, 0:4]),
                 reads=[bT3, C.b_const], writes=[b_ptm])
            P.op("pe", lambda e, c=c: e.transpose(out=ptm[:, c, 4:8], in_=T1[M, c * 128:(c + 1) * 128],
                                                  identity=C.identf[M, 0:4]),
                 reads=[bT1, C.b_const], writes=[b_ptm])
        P.op("dve", lambda e: e.tensor_copy(out=C.wthr[:], in_=ptm[:]), reads=[b_ptm], writes=[C.b_wthr])
        for h in range(4):
            P.op("dve", lambda e, h=h: e.tensor_copy(out=esel[M, h, :],
                                                     in_=C.identf[M, h:h + 1].to_broadcast([4, 128])),
                 reads=[C.b_const, bsm], writes=[bsm])
        for h in range(4):
            P.op("pe", lambda e, h=h: e.matmul(pdc[:, h, :], lhsT=esel[M, h, :], rhs=dec[M, :], start=True, stop=True),
                 reads=[bsm], writes=[b_pdc])
        P.op("dve", lambda e: e.tensor_copy(out=C.decbc[:], in_=pdc[:]), reads=[b_pdc], writes=[C.b_decbc])
        Fx = slice(64, 72)
        Fd = slice(64, 72)
        P.op("dve", lambda e: e.tensor_scalar(out=T0[Fx, :], in0=T2[Fx, :], scalar1=-1.0, scalar2=None, op0=ALU.mult),
             reads=[bT2, bT0], writes=[bT0])
        for part in range(3):
            P.op("dve", lambda e, part=part: e.tensor_copy(out=QR[Fx, part, :], in_=T0[Fx, :]),
                 reads=[bT0], writes=[bQR])
            if part < 2:
                P.op("dve", lambda e, part=part: e.tensor_tensor(out=T0[Fx, :], in0=T0[Fx, :], in1=QR[Fx, part, :],
                                                                 op=ALU.subtract), reads=[bT0, bQR], writes=[bT0])
        P.op("pool", lambda e: e.tensor_scalar(out=KR[Fx, :, :], in0=QR[Fx, :, :], scalar1=-1.0, scalar2=None,
                                               op0=ALU.mult), reads=[bQR], writes=[bKR])
        P.op("sp", lambda e: e.dma_start(out=qaug[:, 64:67, :], in_=QR[Fd, :, :]), reads=[bQR], writes=[aug_b], dma=True)
        P.op("sp", lambda e: e.dma_start(out=kaug[:, 67:70, :], in_=KR[Fd, :, :]), reads=[bKR], writes=[aug_b], dma=True)
        for r in range(3):
            P.op("sp", lambda e, r=r: e.dma_start(out=qaug[:, 67 + r, :], in_=ONE[Fd, :]), reads=[bONE],
                 writes=[aug_b], dma=True)
            P.op("sp", lambda e, r=r: e.dma_start(out=kaug[:, 64 + r, :], in_=ONE[Fd, :]), reads=[bONE],
                 writes=[aug_b], dma=True)
        P.flush()


def win_pass(P, nc, C, I, uT1, uT_b, SC, DB):
    w_in = I["w_in"]
    with contextlib.ExitStack() as st:
        sb = lambda name, shape, dt: st.enter_context(nc.sbuf_tensor("wi" + name, shape, dt))
        ps = lambda name, shape, dt: st.enter_context(nc.psum_tensor("wi" + name, shape, dt))
        W = sb("W", [128, 8, INW], BF16)
        b_W = P.bufs_n("Win", 8)
        wv = w_in.rearrange("(kc p) n -> kc p n", p=128)
        for k in range(8):
            P.op("pool", lambda e, k=k: e.dma_start(out=W[:, k, :], in_=wv[k], max_dma_last_dim=4 * 1284),
                 writes=[b_W[k]], dma=True)
        cw = sb("cw", [128, 4, 4], F32)
        cb = sb("cb", [128, 4], F32)
        gbias = sb("gbias", [128, 2048], F32)
        b_par = P.buf("wipar")
        for tap in range(4):
            P.op("sp", lambda e, tap=tap: e.dma_start(
                out=cw[:, :, tap], in_=I["conv_w"][tap:tap + 1, :].rearrange("o (c p) -> p (o c)", p=128),
                allow_slow_non_contiguous=True), writes=[b_par], dma=True)
        P.op("sp", lambda e: e.dma_start(out=cb[:], in_=I["conv_b"].rearrange("o (c p) -> p (o c)", p=128),
                                         allow_slow_non_contiguous=True), writes=[b_par], dma=True)
        P.op("sp", lambda e: e.dma_start(out=gbias[:], in_=I["branch_gate_bias"].partition_broadcast(128)),
             writes=[b_par], dma=True)
        uT = [sb("uT%d" % i, [128, 8, TT], BF16) for i in range(2)]
        b_uT = P.bufs_n("wiuT", 2)
        zq = sb("zq", [128, 4, 3 + TT], F32)
        b_zq = P.bufs_n("zq", 4)
        acc = [sb("acc%d" % i, [128, TT], F32) for i in range(2)]
        b_acc = P.bufs_n("acc", 2)
        fo = [sb("fo%d" % i, [128, TT], BF16) for i in range(3)]
        b_fo = P.bufs_n("fo", 3)
        tv = [sb("tv%d" % i, [128, 4, 129], BF16) for i in range(2)]
        b_tv = P.bufs_n("tv", 2)
        tf = [sb("tf%d" % i, [128, 8, 65], BF16) for i in range(2)]
        b_tf = P.bufs_n("tf", 2)
        tg = [sb("tg%d" % i, [128, 512], F32) for i in range(2)]
        b_tg = P.bufs_n("tg", 2)
        to = [sb("to%d" % i, [128, 512], BF16) for i in range(3)]
        b_to = P.bufs_n("to", 3)
        pf = [ps("pf%d" % i, [128, TT], F32) for i in range(2)]
        b_pf = P.bufs_n("pf", 2)
        pk = [ps("pk%d" % i, [128, 512], F32) for i in range(2)]
        b_pk = P.bufs_n("pk", 2)
        cnt = {"pf": 0, "pk": 0, "acc": 0, "fo": 0, "tv": 0, "tf": 0, "tg": 0, "to": 0}

        def rot(key, n):
            v = cnt[key] % n
            cnt[key] += 1
            return v

        for ch in range(4):
            P.op("dve", lambda e, ch=ch: e.memset(zq[:, ch, 0:3], 0.0), writes=[b_zq[ch]])
        for i in range(NT):
            ub = i % 2
            tcols = slice(i * TT, (i + 1) * TT)
            if i == 0:
                P.op("sp", lambda e: e.dma_start(out=uT[0][:], in_=uT1[:, :, 0:TT].rearrange("k p t -> p k t")),
                     reads=uT_b[0:4], writes=[b_uT[0]], dma=True)
            if i + 1 < NT:
                ncols = slice((i + 1) * TT, (i + 2) * TT)
                P.op("sp", lambda e, ub=ub, ncols=ncols: e.dma_start(
                    out=uT[1 - ub][:], in_=uT1[:, :, ncols].rearrange("k p t -> p k t")),
                    reads=uT_b[4 * i + 4:4 * i + 8], writes=[b_uT[1 - ub]], dma=True)
            fm = [("mqk", ch, ch * 128) for ch in range(4)] + \
                 [("fq", ch, 1544 + ch * 128) for ch in range(4)] + \
                 [("fk", ch, 2056 + ch * 128) for ch in range(4)]
            for (kind, ch, c0) in fm:
                j = rot("pf", 2)
                for k in range(8):
                    P.op("pe", lambda e, k=k, c0=c0, j=j, ub=ub: e.matmul(
                        pf[j][:], lhsT=W[:, k, c0:c0 + 128], rhs=uT[ub][:, k, :], start=(k == 0), stop=(k == 7)),
                        reads=[b_W[k], b_uT[ub]], writes=[b_pf[j]])
                if kind == "mqk":
                    P.op("act", lambda e, ch=ch, j=j: e.activation(out=zq[:, ch, 3:3 + TT], in_=pf[j][:], func=AF.Copy),
                         reads=[b_pf[j]], writes=[b_zq[ch]])
                    a = rot("acc", 2)
                    P.op("dve", lambda e, ch=ch, a=a: e.tensor_scalar(
                        out=acc[a][:], in0=zq[:, ch, 0:TT], scalar1=cw[:, ch, 0:1], scalar2=cb[:, ch:ch + 1],
                        op0=ALU.mult, op1=ALU.add), reads=[b_zq[ch], b_par], writes=[b_acc[a]])
                    for tap in range(1, 4):
                        P.op("dve", lambda e, ch=ch, a=a, tap=tap: e.scalar_tensor_tensor(
                            out=acc[a][:], in0=zq[:, ch, tap:tap + TT], scalar=cw[:, ch, tap:tap + 1], in1=acc[a][:],
                            op0=ALU.mult, op1=ALU.add), reads=[b_zq[ch], b_par, b_acc[a]], writes=[b_acc[a]])
                    P.op("dve", lambda e, ch=ch: e.tensor_copy(out=zq[:, ch, 0:3], in_=zq[:, ch, TT:TT + 3]),
                         reads=[b_zq[ch]], writes=[b_zq[ch]])
                    o = rot("fo", 3)
                    P.op("act", lambda e, a=a, o=o: e.activation(out=fo[o][:], in_=acc[a][:], func=AF.Silu),
                         reads=[b_acc[a]], writes=[b_fo[o]])
                    P.op("sp", lambda e, o=o, ch=ch, tcols=tcols: e.dma_start(out=SC["mqkT"][ch, :, tcols], in_=fo[o][:]),
                         reads=[b_fo[o]], writes=[DB("mqkT")[i]], dma=True)
                else:
                    o = rot("fo", 3)
                    sc = 0.125 if kind == "fq" else 1.0
                    P.op("act", lambda e, o=o, j=j, sc=sc: e.activation(out=fo[o][:], in_=pf[j][:], func=AF.Copy, scale=sc),
                         reads=[b_pf[j]], writes=[b_fo[o]])
                    dst = SC["qaug"] if kind == "fq" else SC["kaug"]
                    for hh in range(2):
                        P.op("sp", lambda e, o=o, ch=ch, dst=dst, tcols=tcols, hh=hh: e.dma_start(
                            out=dst[2 * ch + hh, 0:64, tcols], in_=fo[o][hh * 64:(hh + 1) * 64, :]),
                            reads=[b_fo[o]], writes=[DB("aug")[i]], dma=True)
            for s in range(4):
                n = i * 4 + s
                rows = slice(n * 128, (n + 1) * 128)
                groups = [("mv", 512), ("mo", 1024), ("fv", 2568), ("ga", 3088), ("ga", 3600), ("gb", 4112), ("gb", 4624)]
                for gi, (kind, c0) in enumerate(groups):
                    j = rot("pk", 2)
                    for k in range(8):
                        P.op("pe", lambda e, k=k, c0=c0, j=j, ub=ub, s=s: e.matmul(
                            pk[j][:], lhsT=uT[ub][:, k, s * 128:(s + 1) * 128], rhs=W[:, k, c0:c0 + 512],
                            start=(k == 0), stop=(k == 7)),
                            reads=[b_W[k], b_uT[ub]], writes=[b_pk[j]])
                    if kind == "mv":
                        t = rot("tv", 2)
                        c = n
                        P.op("dve", lambda e, t=t, j=j, c=c: e.tensor_tensor(
                            out=tv[t][:, :, 0:128], in0=pk[j][:].rearrange("p (h d) -> p h d", h=4),
                            in1=bcast_last(C.wthr[:, c, 0:4], 128), op=ALU.mult),
                            reads=[b_pk[j], C.b_wthr], writes=[b_tv[t]])
                        P.op("dve", lambda e, t=t, c=c: e.tensor_copy(out=tv[t][:, :, 128:129],
                                                                     in_=C.wthr[:, c, 0:4].unsqueeze(2)),
                             reads=[C.b_wthr, b_tv[t]], writes=[b_tv[t]])
                        P.op("sp", lambda e, t=t, rows=rows: e.dma_start(out=SC["vaugM"][rows], in_=tv[t][:]),
                             reads=[b_tv[t]], writes=[DB("vaugM")[n]], dma=True)
                    elif kind == "fv":
                        t = rot("tf", 2)
                        P.op("act", lambda e, t=t, j=j: e.activation(
                            out=tf[t][:, :, 1:65], in_=pk[j][:].rearrange("p (h d) -> p h d", h=8), func=AF.Copy),
                            reads=[b_pk[j]], writes=[b_tf[t]])
                        P.op("dve", lambda e, t=t: e.memset(tf[t][:, :, 0:1], 1.0), reads=[b_tf[t]], writes=[b_tf[t]])
                        P.op("sp", lambda e, t=t, rows=rows: e.dma_start(out=SC["vaugF"][rows], in_=tf[t][:]),
                             reads=[b_tf[t]], writes=[DB("vaugF")[n]], dma=True)
                    elif kind == "mo":
                        o = rot("to", 3)
                        P.op("act", lambda e, o=o, j=j: e.activation(out=to[o][:], in_=pk[j][:], func=AF.Sigmoid),
                             reads=[b_pk[j]], writes=[b_to[o]])
                        P.op("sp", lambda e, o=o, rows=rows: e.dma_start(out=SC["so"][rows], in_=to[o][:]),
                             reads=[b_to[o]], writes=[DB("so")[n]], dma=True)
                    else:
                        g = rot("tg", 2)
                        boff = c0 - 3088
                        P.op("dve", lambda e, g=g, j=j, boff=boff: e.tensor_tensor(
                            out=tg[g][:], in0=pk[j][:], in1=gbias[:, boff:boff + 512], op=ALU.add),
                            reads=[b_pk[j], b_par], writes=[b_tg[g]])
                        o = rot("to", 3)
                        P.op("act", lambda e, o=o, g=g: e.activation(out=to[o][:], in_=tg[g][:], func=AF.Sigmoid),
                             reads=[b_tg[g]], writes=[b_to[o]])
                        dcol = boff % 1024
                        dst = SC["ga"] if kind == "ga" else SC["gb"]
                        P.op("sp", lambda e, o=o, rows=rows, dst=dst, dcol=dcol: e.dma_start(
                            out=dst[rows, dcol:dcol + 512], in_=to[o][:]),
                            reads=[b_to[o]], writes=[DB("gab")[n]], dma=True)
        P.flush()


def mix_pass(P, nc, C, I, SC, DB):
    with contextlib.ExitStack() as st:
        sb = lambda name, shape, dt: st.enter_context(nc.sbuf_tensor("mx" + name, shape, dt))
        ps = lambda name, shape, dt: st.enter_context(nc.psum_tensor("mx" + name, shape, dt))
        mask01 = sb("mask01", [128, 128], F32)
        trim = sb("trim", [128, 128], BF16)
        trimf = sb("trimf", [128, 128], F32)
        onesr = sb("onesr", [1, 65], F32)
        gln = sb("gln", [128, 512], F32)
        b_c = P.buf("mxconst")
        P.op("pool", lambda e: e.memset(mask01[:], 1.0), writes=[b_c])
        P.op("pool", lambda e: e.affine_select(out=mask01[:], in_=mask01[:], pattern=[[1, 128]], compare_op=ALU.is_ge,
                                                 fill=0.0, base=0, channel_multiplier=-1), reads=[b_c], writes=[b_c])
        P.op("pool", lambda e: e.memset(trimf[:], 0.0), reads=[b_c], writes=[b_c])
        P.op("pool", lambda e: e.affine_select(out=trimf[:], in_=trimf[:], pattern=[[1, 128]], compare_op=ALU.is_ge,
                                                 fill=-30000.0, base=0, channel_multiplier=-1), reads=[b_c], writes=[b_c])
        P.op("dve", lambda e: e.tensor_copy(out=trim[:], in_=trimf[:]), reads=[b_c], writes=[b_c])
        P.op("dve", lambda e: e.memset(onesr[:], 1.0), reads=[b_c], writes=[b_c])
        P.op("sp", lambda e: e.dma_start(out=gln[:], in_=I["mlstm_norm_g"].partition_broadcast(128)),
             reads=[b_c], writes=[b_c], dma=True)
        mqz = [sb("mqz%d" % i, [128, S], BF16) for i in range(4)]
        mk = [sb("mk%d" % i, [128, S], BF16) for i in range(2)]
        b_mqk = P.buf("mqk")
        for h in range(4):
            P.op("pool", lambda e, h=h: e.memset(mqz[h][:], 0.0), writes=[b_mqk])
        for h in range(4):
            R = slice((h % 2) * 64, (h % 2) * 64 + 64)
            P.op("sp", lambda e, h=h, R=R: e.dma_start(out=mqz[h][R, :], in_=SC["mqkT"][h // 2, R, :]), reads=DB("mqkT"),
                 writes=[b_mqk], dma=True)
        for hp in range(2):
            P.op("sp", lambda e, hp=hp: e.dma_start(out=mk[hp][:], in_=SC["mqkT"][2 + hp]), reads=DB("mqkT"),
                 writes=[b_mqk], dma=True)
        Cst = [sb("Cst%d" % i, [128, 129], F32) for i in range(2)]
        Cb = [sb("Cb%d" % i, [128, 129], BF16) for i in range(2)]
        b_Cst = P.bufs_n("Cst", 2)
        b_Cb = P.bufs_n("Cb", 2)
        va = [sb("va%d" % i, [128, 4, 129], BF16) for i in range(2)]
        b_va = P.bufs_n("va", 2)
        sgo = [sb("sgo%d" % i, [128, 512], BF16) for i in range(2)]
        b_sgo = P.bufs_n("sgo", 2)
        Sm = [sb("Sm%d" % i, [128, 2, 128], BF16) for i in range(2)]
        b_Sm = P.bufs_n("Sm", 2)
        ktm = [sb("ktm%d" % i, [128, 128], BF16) for i in range(2)]
        b_ktm = P.bufs_n("ktm", 2)
        bst = [sb("bst%d" % i, [128, 2, 6], F32) for i in range(2)]
        bag = [sb("bag%d" % i, [128, 2, 2], F32) for i in range(2)]
        sm = [sb("sm%d" % i, [128, 2, 4], F32) for i in range(2)]
        b_sm = P.bufs_n("msm", 2)
        Osb = [sb("Osb%d" % i, [128, 2, 129], F32) for i in range(2)]
        b_Osb = P.bufs_n("Osb", 2)
        sq = [sb("sq%d" % i, [128, 2, 1], F32) for i in range(2)]
        b_sq = P.bufs_n("msq", 2)
        hn = [sb("hn%d" % i, [128, 512], F32) for i in range(2)]
        b_hn = P.bufs_n("hn", 2)
        ya = [sb("ya%d" % i, [128, 512], BF16) for i in range(2)]
        b_ya = P.bufs_n("ya", 2)
        yaT = [sb("yaT%d" % i, [128, 4, 128], BF16) for i in range(2)]
        b_yaT = P.bufs_n("yaTs", 2)
        pSm = ps("pSm", [128, 2, 128], F32)
        pOm = ps("pOm", [128, 2, 129], F32)
        pU = ps("pU", [128, 2, 129], F32)
        pT5 = ps("pT5", [128, 5, 128], BF16)
        pkt = pT5[:, 4, :]
        pyT = pT5[:, 0:4, :]
        b_pSm, b_pOm, b_pU, b_pkt, b_pyT = [P.buf(n) for n in ("pSm", "pOm", "pU", "pkt", "pyT")]

        def mlstm_chunk(c):
            cols = slice(c * 128, (c + 1) * 128)
            rows = slice(c * 128, (c + 1) * 128)
            vi = c % 2
            P.op("sp", lambda e: e.dma_start(out=va[vi][:], in_=SC["vaugM"][rows]), reads=[DB("vaugM")[c]],
                 writes=[b_va[vi]], dma=True)
            P.op("sp", lambda e: e.dma_start(out=sgo[vi][:], in_=SC["so"][rows]), reads=[DB("so")[c]],
                 writes=[b_sgo[vi]], dma=True)
            hnb = hn[vi]
            for hp in range(2):
                si = hp
                for hh in range(2):
                    P.op("pe", lambda e, hp=hp, hh=hh: e.matmul(
                        pSm[:, hh, :], lhsT=mk[hp][:, cols], rhs=mqz[2 * hp + hh][:, cols], start=True, stop=True),
                        reads=[b_mqk], writes=[b_pSm])
                P.op("pe", lambda e, hp=hp: e.transpose(out=pkt, in_=mk[hp][:, cols], identity=C.ident[:]),
                     reads=[b_mqk, C.b_const], writes=[b_pkt])
                P.op("dve", lambda e, si=si: e.scalar_tensor_tensor(
                    out=Sm[si][:], in0=pSm[:], scalar=0.125,
                    in1=mask01[:].unsqueeze(1).to_broadcast([128, 2, 128]), op0=ALU.mult, op1=ALU.mult),
                    reads=[b_pSm, b_c], writes=[b_Sm[si]])
                P.op("act", lambda e, si=si: e.activation(out=ktm[si][:], in_=pkt, func=AF.Copy, scale=0.125),
                     reads=[b_pkt], writes=[b_ktm[si]])
                yield
            for hp in range(2):
                si = hp
                for hh in range(2):
                    h = 2 * hp + hh
                    P.op("pe", lambda e, si=si, hh=hh, h=h: e.matmul(
                        pOm[:, hh, :], lhsT=Sm[si][:, hh, :], rhs=va[vi][:, h, :], start=True, stop=(c == 0)),
                        reads=[b_Sm[si], b_va[vi]], writes=[b_pOm])
                    if c > 0:
                        P.op("pe", lambda e, hp=hp, hh=hh, h=h: e.matmul(
                            pOm[:, hh, :], lhsT=mqz[h][:, cols], rhs=Cb[hp][:, :], start=False, stop=True),
                            reads=[b_mqk, b_Cb[hp]], writes=[b_pOm])
                osp, bos = Osb[hp], b_Osb[hp]
                P.op("act", lambda e, osp=osp: e.activation(out=osp[:], in_=pOm[:], func=AF.Copy),
                     reads=[b_pOm], writes=[bos])
                for hh in range(2):
                    h = 2 * hp + hh
                    P.op("pe", lambda e, si=si, hh=hh, h=h: e.matmul(
                        pU[:, hh, :], lhsT=ktm[si][:], rhs=va[vi][:, h, :], start=True, stop=True),
                        reads=[b_ktm[si], b_va[vi]], writes=[b_pU])
                for hh in range(2):
                    h = 2 * hp + hh
                    R = slice(hh * 64, (hh + 1) * 64)
                    if c == 0:
                        P.op("dve", lambda e, hp=hp, hh=hh, R=R: e.tensor_copy(out=Cst[hp][R, :], in_=pU[R, hh, :]),
                             reads=[b_pU], writes=[b_Cst[hp]])
                    else:
                        P.op("dve", lambda e, hp=hp, hh=hh, R=R, h=h: e.scalar_tensor_tensor(
                            out=Cst[hp][R, :], in0=Cst[hp][R, :], scalar=C.decbc[R, h, c:c + 1], in1=pU[R, hh, :],
                            op0=ALU.mult, op1=ALU.add), reads=[b_pU, b_Cst[hp], C.b_decbc], writes=[b_Cst[hp]])
                    if c < 31:
                        P.op("dve", lambda e, hp=hp, R=R, h=h: e.tensor_scalar(
                            out=Cb[hp][R, :], in0=Cst[hp][R, :], scalar1=C.decbc[R, h, c + 1:c + 2], scalar2=None,
                            op0=ALU.mult), reads=[b_Cst[hp], C.b_decbc], writes=[b_Cb[hp]])
                smp, bstp, bagp, bsm = sm[hp], bst[hp], bag[hp], b_sm[hp]
                for hh in range(2):
                    P.op("dve", lambda e, hh=hh, bstp=bstp, hp=hp: e.bn_stats(out=bstp[:, hh, :], in_=Osb[hp][:, hh, 0:128]),
                         reads=[bos], writes=[bsm])
                    P.op("dve", lambda e, hh=hh, bstp=bstp, bagp=bagp: e.bn_aggr(out=bagp[:, hh, :], in_=bstp[:, hh, :]),
                         reads=[bsm], writes=[bsm])
                sqp, bsq = sq[hp], b_sq[hp]
                P.op("act", lambda e, sqp=sqp, osp=osp: e.activation(out=sqp[:], in_=osp[:, :, 128:129], func=AF.Square),
                     reads=[bos], writes=[bsq])
                P.op("dve", lambda e, hp=hp, smp=smp, sqp=sqp: e.tensor_tensor(
                    out=smp[:, :, 0:1], in0=sqp[:], in1=C.wthr[:, c, 4 + 2 * hp:6 + 2 * hp].unsqueeze(2),
                    op=ALU.max), reads=[C.b_wthr, bsm, bsq], writes=[bsm])
                P.op("dve", lambda e, smp=smp, bagp=bagp: e.scalar_tensor_tensor(
                    out=smp[:, :, 1:2], in0=smp[:, :, 0:1], scalar=EPS, in1=bagp[:, :, 1:2], op0=ALU.mult, op1=ALU.add),
                    reads=[bsm], writes=[bsm])
                P.op("pool", lambda e, smp=smp: e.tensor_tensor(
                    out=smp[:, :, 2:3], in0=smp[:, :, 1:2],
                    in1=C.neghalf[:].unsqueeze(1).to_broadcast([128, 2, 1]), op=ALU.pow),
                    reads=[bsm, C.b_const], writes=[bsm])
                for hh in range(2):
                    h = 2 * hp + hh
                    P.op("dve", lambda e, hh=hh, h=h, smp=smp, bagp=bagp, osp=osp: e.tensor_scalar(
                        out=hnb[:, h * 128:(h + 1) * 128], in0=osp[:, hh, 0:128], scalar1=bagp[:, hh, 0:1],
                        scalar2=smp[:, hh, 2:3], op0=ALU.subtract, op1=ALU.mult),
                        reads=[bos, bsm], writes=[b_hn[vi]])
                yield
            yi = c % 2
            P.op("pool", lambda e: e.tensor_tensor(out=hnb[:], in0=hnb[:], in1=gln[:], op=ALU.mult),
                 reads=[b_hn[vi], b_c], writes=[b_hn[vi]])
            P.op("dve", lambda e: e.tensor_tensor(out=ya[yi][:], in0=hnb[:], in1=sgo[vi][:], op=ALU.mult),
                 reads=[b_hn[vi], b_sgo[vi]], writes=[b_ya[yi]])
            yield
            for k in range(4):
                P.op("pe", lambda e, k=k: e.transpose(out=pyT[:, k, :], in_=ya[yi][:, k * 128:(k + 1) * 128],
                                                      identity=C.ident[:]),
                     reads=[b_ya[yi], C.b_const], writes=[b_pyT])
            P.op("act", lambda e: e.activation(out=yaT[yi][:], in_=pyT, func=AF.Copy), reads=[b_pyT],
                 writes=[b_yaT[yi]])
            P.op("sp", lambda e: e.dma_start(out=SC["yaT"][:, :, cols].rearrange("k p t -> p k t"), in_=yaT[yi][:]),
                 reads=[b_yaT[yi]], writes=[DB("yaT")[c]], dma=True)
            yield

        def mlstm_gen():
            for c in range(32):
                yield from mlstm_chunk(c)

        VF = sb("VF", [128, 32, 8 * 65], BF16)
        b_VF = P.buf("VF")
        P.op("sp", lambda e: e.dma_start(out=VF[:], in_=SC["vaugF"].rearrange("(j p) h e -> p j (h e)", p=128)),
             reads=DB("vaugF"), writes=[b_VF], dma=True)
        QA = [sb("QA%d" % i, [70, S], BF16) for i in range(2)]
        KA = [sb("KA%d" % i, [70, S], BF16) for i in range(2)]
        b_QA = P.bufs_n("QA", 2)
        b_KA = P.bufs_n("KA", 2)
        PT = [sb("PT%d" % i, [128, 512], BF16) for i in range(3)]
        b_PT = P.bufs_n("PT", 3)
        rec = [sb("rec%d" % i, [1, 512], F32) for i in range(2)]
        b_rec = P.bufs_n("rec", 2)
        b_recd = P.bufs_n("recd", 2)
        osb = [sb("osb%d" % i, [65, 512], F32) for i in range(2)]
        b_osb = P.bufs_n("osb", 2)
        bcs = [sb("bcs%d" % i, [65, 512], F32) for i in range(2)]
        b_bcs = P.bufs_n("bcs", 2)
        ybt = [sb("ybt%d" % i, [65, 512], BF16) for i in range(2)]
        b_ybt = P.bufs_n("ybt", 2)
        pS = [ps("pS%d" % i, [128, 512], F32) for i in range(3)]
        b_pS = P.bufs_n("pS", 3)
        slot_of = {}
        pO = ps("pO", [128, 512], F32)
        b_pO = P.buf("pO")
        cnt = {"s": 0, "y": 0}

        def fox_load(h):
            hb = h % 2
            P.op("sp", lambda e: e.dma_start(out=QA[hb][:], in_=SC["qaug"][h]), reads=DB("aug"), writes=[b_QA[hb]], dma=True)
            P.op("sp", lambda e: e.dma_start(out=KA[hb][:], in_=SC["kaug"][h]), reads=DB("aug"), writes=[b_KA[hb]], dma=True)

        seq = [(h, i, j) for h in range(8) for i in range(8) for j in range(4 * i + 4)]

        def emit_S(idx):
            h, i, j = seq[idx]
            hb = h % 2
            sj = cnt["s"] % 3
            cnt["s"] += 1
            slot_of[idx] = sj
            jj = j - 4 * i
            kc = slice(j * 128, (j + 1) * 128)
            rd = [b_KA[hb], b_QA[hb]]
            if jj < 0:
                P.op("pe", lambda e: e.matmul(
                    pS[sj][:, 0:512], lhsT=KA[hb][:, kc], rhs=QA[hb][:, i * 512:(i + 1) * 512], start=True, stop=True),
                    reads=rd, writes=[b_pS[sj]])
            else:
                qs = jj * 128
                wq = 512 - qs
                q0 = i * 512 + qs
                P.op("pe", lambda e: e.matmul(pS[sj][:, 0:128], lhsT=C.ident[:], rhs=trim[:], start=True, stop=False),
                     reads=[C.b_const, b_c], writes=[b_pS[sj]])
                P.op("pe", lambda e: e.matmul(
                    pS[sj][:, 0:128], lhsT=KA[hb][:, kc], rhs=QA[hb][:, q0:q0 + 128], start=False, stop=True),
                    reads=rd, writes=[b_pS[sj]])
                if wq > 128:
                    P.op("pe", lambda e: e.matmul(
                        pS[sj][:, 128:wq], lhsT=KA[hb][:, kc], rhs=QA[hb][:, q0 + 128:q0 + wq], start=True, stop=True),
                        reads=rd, writes=[b_pS[sj]])

        def emit_rest(idx):
            h, i, j = seq[idx]
            sj = slot_of[idx]
            nkb = 4 * i + 4
            jj = j - 4 * i
            qs = max(jj, 0) * 128
            wq = 512 - qs
            tj = idx % 3
            P.op("act", lambda e: e.activation(out=PT[tj][:, 0:wq], in_=pS[sj][:, 0:wq], func=AF.Exp),
                 reads=[b_pS[sj]], writes=[b_PT[tj]])
            P.op("pe", lambda e: e.matmul(
                pO[0:65, qs:512], lhsT=VF[:, j, h * 65:(h + 1) * 65], rhs=PT[tj][:, 0:wq],
                start=(j == 0), stop=(j == nkb - 1)),
                reads=[b_VF, b_PT[tj]], writes=[b_pO])
            if j < nkb - 1:
                return
            yi = cnt["y"] % 2
            cnt["y"] += 1
            u = h * 8 + i
            P.op("act", lambda e: e.activation(out=osb[yi][:], in_=pO[0:65, :], func=AF.Copy), reads=[b_pO],
                 writes=[b_osb[yi]])
            P.op("dve", lambda e: e.reciprocal(out=rec[yi][0:1, :], in_=osb[yi][0:1, :]), reads=[b_osb[yi]],
                 writes=[b_rec[yi]])
            P.op("sp", lambda e: e.dma_start(out=SC["recd"][u:u + 1, :], in_=rec[yi][0:1, :]), reads=[b_rec[yi]],
                 writes=[b_recd[yi]], dma=True)
            P.op("sp", lambda e: e.dma_start(out=bcs[yi][:], in_=SC["recd"][u:u + 1, :].partition_broadcast(65)),
                 reads=[b_recd[yi]], writes=[b_bcs[yi]], dma=True)
            P.op("dve", lambda e: e.tensor_tensor(out=ybt[yi][:], in0=osb[yi][:], in1=bcs[yi][:], op=ALU.mult),
                 reads=[b_osb[yi], b_bcs[yi]], writes=[b_ybt[yi]])
            P.op("sp", lambda e: e.dma_start(
                out=SC["ybT"][h // 2, (h % 2) * 64:(h % 2) * 64 + 64, i * 512:(i + 1) * 512], in_=ybt[yi][1:65, :]),
                reads=[b_ybt[yi]], writes=[DB("ybT")[(h * 8 + i) % 32]], dma=True)

        gen = mlstm_gen()
        fox_load(0)
        emit_S(0)
        emit_S(1)
        for idx, (h, i, j) in enumerate(seq):
            if i == 0 and j == 0 and h + 1 < 8:
                fox_load(h + 1)
            if idx + 2 < len(seq):
                emit_S(idx + 2)
            emit_rest(idx)
            if idx % 6 == 5:
                next(gen, None)
        for _ in gen:
            pass
        P.flush()


def merge_pass(P, nc, C, I, SC, DB, h1, h1_b, h2, h2_b):
    with contextlib.ExitStack() as st:
        sb = lambda name, shape, dt: st.enter_context(nc.sbuf_tensor("mg" + name, shape, dt))
        ps = lambda name, shape, dt: st.enter_context(nc.psum_tensor("mg" + name, shape, dt))
        Wa = sb("Wa", [128, 4, D], BF16)
        Wb = sb("Wb", [128, 4, D], BF16)
        Wo = sb("Wo", [128, 8, D], BF16)
        b_W = P.buf("mgW")
        P.op("pool", lambda e: e.dma_start(out=Wa[:], in_=I["w_branch_a"].rearrange("(k p) d -> p k d", p=128)),
             writes=[b_W], dma=True)
        P.op("pool", lambda e: e.dma_start(out=Wb[:], in_=I["w_branch_b"].rearrange("(k p) d -> p k d", p=128)),
             writes=[b_W], dma=True)
        P.op("pool", lambda e: e.dma_start(out=Wo[:], in_=I["w_out"].rearrange("(k p) d -> p k d", p=128)),
             writes=[b_W], dma=True)
        gpost = sb("gpost", [128, D], F32)
        P.op("sp", lambda e: e.dma_start(out=gpost[:], in_=I["mix_post_g"].partition_broadcast(128)),
             writes=[b_W], dma=True)
        yaT = [sb("yaT%d" % i, [128, 4, 128], BF16) for i in range(2)]
        ybT = [sb("ybT%d" % i, [128, 4, 128], BF16) for i in range(2)]
        gab = [sb("gab%d" % i, [128, 2, D], BF16) for i in range(2)]
        hin = [sb("hin%d" % i, [128, D], F32) for i in range(2)]
        b_in = P.bufs_n("mgin", 2)
        b_inb = P.bufs_n("mginb", 2)
        b_ga = P.bufs_n("mgga", 2)
        b_gb = P.bufs_n("mggb", 2)
        b_hin = P.bufs_n("mghin", 2)
        t1 = [sb("t1%d" % i, [128, D], F32) for i in range(2)]
        t2 = [sb("t2%d" % i, [128, D], F32) for i in range(2)]
        mb = [sb("mb%d" % i, [128, D], BF16) for i in range(2)]
        mT = [sb("mT%d" % i, [128, 8, 128], BF16) for i in range(2)]
        junk = sb("junk", [128, D], BF16)
        hout = [sb("hout%d" % i, [128, D], F32) for i in range(2)]
        stat = sb("stat", [128, 6], F32)
        b_t1, b_t2, b_mb, b_mT = [P.bufs_n(n, 2) for n in ("t1", "t2", "mb", "mT")]
        b_junk = P.buf("mgjunk")
        b_hout = P.bufs_n("hout", 2)
        b_stat = P.bufs_n("mgstat", 2)
        pA = ps("pA", [128, D], F32)
        pB = ps("pB", [128, D], F32)
        pO = ps("pO", [128, D], F32)
        pt = ps("pt", [128, 8, 128], BF16)
        b_pA, b_pB, b_pO, b_pt = [P.buf(n) for n in ("pA", "pB", "mgpO", "mgpt")]
        h1v = h1.rearrange("(n p) d -> n p d", p=128)
        h2v = h2.rearrange("(n p) d -> n p d", p=128)

        def s1(n):
            ib = n % 2
            rows = slice(n * 128, (n + 1) * 128)
            cols = rows
            P.op("sp", lambda e: e.dma_start(out=yaT[ib][:], in_=SC["yaT"][:, :, cols].rearrange("k p t -> p k t")),
                 reads=[DB("yaT")[n]], writes=[b_in[ib]], dma=True)
            P.op("sp", lambda e: e.dma_start(out=ybT[ib][:], in_=SC["ybT"][:, :, cols].rearrange("k p t -> p k t")),
                 reads=DB("ybT"), writes=[b_inb[ib]], dma=True)
            P.op("sp", lambda e: e.dma_start(out=gab[ib][:, 0, :], in_=SC["ga"][rows]),
                 reads=[DB("gab")[n]], writes=[b_ga[ib]], dma=True)
            P.op("sp", lambda e: e.dma_start(out=gab[ib][:, 1, :], in_=SC["gb"][rows]),
                 reads=[DB("gab")[n]], writes=[b_gb[ib]], dma=True)
            P.op("sp", lambda e: e.dma_start(out=hin[ib][:], in_=h1v[n]),
                 reads=[h1_b[n]], writes=[b_hin[ib]], dma=True)
            for hf in range(2):
                hs = slice(hf * 512, (hf + 1) * 512)
                for k in range(4):
                    P.op("pe", lambda e, k=k, hs=hs: e.matmul(pA[:, hs], lhsT=yaT[ib][:, k, :], rhs=Wa[:, k, hs],
                                                              start=(k == 0), stop=(k == 3)),
                         reads=[b_in[ib], b_W], writes=[b_pA])
            for hf in range(2):
                hs = slice(hf * 512, (hf + 1) * 512)
                for k in range(4):
                    P.op("pe", lambda e, k=k, hs=hs: e.matmul(pB[:, hs], lhsT=ybT[ib][:, k, :], rhs=Wb[:, k, hs],
                                                              start=(k == 0), stop=(k == 3)),
                         reads=[b_inb[ib], b_W], writes=[b_pB])
            P.op("dve", lambda e: e.tensor_tensor(out=t1[ib][:], in0=pA[:], in1=gab[ib][:, 0, :], op=ALU.mult),
                 reads=[b_pA, b_ga[ib]], writes=[b_t1[ib]])
            P.op("dve", lambda e: e.tensor_tensor(out=t2[ib][:], in0=pB[:], in1=gab[ib][:, 1, :], op=ALU.mult),
                 reads=[b_pB, b_gb[ib]], writes=[b_t2[ib]])
            P.op("pool", lambda e: e.tensor_tensor(out=mb[ib][:], in0=t1[ib][:], in1=t2[ib][:], op=ALU.add),
                 reads=[b_t1[ib], b_t2[ib]], writes=[b_mb[ib]])

        def s2(n):
            ib = n % 2
            for k in range(8):
                P.op("pe", lambda e, k=k: e.transpose(out=pt[:, k, :], in_=mb[ib][:, k * 128:(k + 1) * 128],
                                                      identity=C.ident[:]),
                     reads=[b_mb[ib], C.b_const], writes=[b_pt])
            P.op("act", lambda e: e.activation(out=mT[ib][:], in_=pt[:], func=AF.Copy), reads=[b_pt], writes=[b_mT[ib]])
            for hf in range(2):
                hs = slice(hf * 512, (hf + 1) * 512)
                for k in range(8):
                    P.op("pe", lambda e, k=k, hs=hs: e.matmul(pO[:, hs], lhsT=mT[ib][:, k, :], rhs=Wo[:, k, hs],
                                                              start=(k == 0), stop=(k == 7)),
                         reads=[b_mT[ib], b_W], writes=[b_pO])
            si = n % 2
            ss, var, rstd = (stat[:, 3 * si + j:3 * si + j + 1] for j in range(3))
            rms_stats(P, C, pO[:], b_pO, junk[:], b_junk, ss, var, rstd, b_stat[si])
            P.op("dve", lambda e: e.scalar_tensor_tensor(
                out=hout[ib][:], in0=pO[:], scalar=rstd, in1=gpost[:], op0=ALU.mult, op1=ALU.mult),
                reads=[b_pO, b_stat[si], b_W], writes=[b_hout[ib]])
            P.op("pool", lambda e: e.tensor_tensor(out=hout[ib][:], in0=hout[ib][:], in1=hin[ib][:], op=ALU.add),
                 reads=[b_hout[ib], b_hin[ib]], writes=[b_hout[ib]])
            P.op("pool", lambda e: e.dma_start(out=h2v[n], in_=hout[ib][:]),
                 reads=[b_hout[ib]], writes=[h2_b[n]], dma=True)

        s1(0)
        for n in range(32):
            if n + 1 < 32:
                s1(n + 1)
            s2(n)
        P.flush()


def ple_pass(P, nc, C, I, uT3, uT3_b, h3, h3_b, out):
    with contextlib.ExitStack() as st:
        sb = lambda name, shape, dt: st.enter_context(nc.sbuf_tensor("pl" + name, shape, dt))
        ps = lambda name, shape, dt: st.enter_context(nc.psum_tensor("pl" + name, shape, dt))
        Wg = sb("Wg", [128, 8, D], BF16)
        Wp = sb("Wp", [128, 2, D], BF16)
        b_W = P.buf("plW")
        P.op("pool", lambda e: e.dma_start(out=Wg[:], in_=I["ple_w_gate"].rearrange("(k p) d -> p k d", p=128)),
             writes=[b_W], dma=True)
        P.op("pool", lambda e: e.dma_start(out=Wp[:], in_=I["ple_w_proj"].rearrange("(k p) d -> p k d", p=128)),
             writes=[b_W], dma=True)
        gpost = sb("gpost", [128, D], F32)
        bg = sb("bg", [128, D], F32)
        P.op("sp", lambda e: e.dma_start(out=gpost[:], in_=I["ple_post_g"].partition_broadcast(128)),
             writes=[b_W], dma=True)
        P.op("sp", lambda e: e.dma_start(out=bg[:], in_=I["ple_b_gate"].partition_broadcast(128)),
             writes=[b_W], dma=True)
        uT = [sb("uT%d" % i, [128, 8, 128], BF16) for i in range(2)]
        pin = [sb("pin%d" % i, [128, 256], F32) for i in range(2)]
        hin = [sb("hin%d" % i, [128, D], F32) for i in range(2)]
        b_in = P.bufs_n("plin", 2)
        b_pin = P.bufs_n("plpin", 2)
        b_hin = P.bufs_n("plhin", 2)
        pb = [sb("pb%d" % i, [128, 256], BF16) for i in range(2)]
        pT = [sb("pT%d" % i, [128, 2, 128], BF16) for i in range(2)]
        gt = [sb("gt%d" % i, [128, D], F32) for i in range(2)]
        ge = [sb("ge%d" % i, [128, D], F32) for i in range(2)]
        junk = sb("junk", [128, D], BF16)
        hout = [sb("hout%d" % i, [128, D], F32) for i in range(2)]
        stat = sb("stat", [128, 6], F32)
        b_pb, b_pT, b_gt, b_ge = [P.bufs_n(n, 2) for n in ("pb", "pT", "gt", "ge")]
        b_junk = P.buf("pljunk")
        b_hout = P.bufs_n("plhout", 2)
        b_stat = P.bufs_n("plstat", 2)
        pG = [ps("pG%d" % i, [128, D], F32) for i in range(2)]
        pE = ps("pE", [128, D], F32)
        ptp = ps("ptp", [128, 2, 128], BF16)
        b_pG = P.bufs_n("pG", 2)
        b_pE, b_ptp = P.buf("pE"), P.buf("ptp")
        pv = I["p"].rearrange("(n p) d -> n p d", p=128)
        h3v = h3.rearrange("(n p) d -> n p d", p=128)
        ov = out.rearrange("(n p) d -> n p d", p=128)

        def s1(n):
            ib = n % 2
            cols = slice(n * 128, (n + 1) * 128)
            P.op("sp", lambda e: e.dma_start(out=uT[ib][:], in_=uT3[:, :, cols].rearrange("k p t -> p k t")),
                 reads=[uT3_b[n]], writes=[b_in[ib]], dma=True)
            P.op("sp", lambda e: e.dma_start(out=pin[ib][:], in_=pv[n]), writes=[b_pin[ib]], dma=True)
            P.op("sp", lambda e: e.dma_start(out=hin[ib][:], in_=h3v[n]), reads=[h3_b[n]],
                 writes=[b_hin[ib]], dma=True)
            P.op("act", lambda e: e.activation(out=pb[ib][:], in_=pin[ib][:], func=AF.Copy), reads=[b_pin[ib]],
                 writes=[b_pb[ib]])
            for k in range(2):
                P.op("pe", lambda e, k=k: e.transpose(out=ptp[:, k, :], in_=pb[ib][:, k * 128:(k + 1) * 128],
                                                      identity=C.ident[:]),
                     reads=[b_pb[ib], C.b_const], writes=[b_ptp])
            P.op("dve", lambda e: e.tensor_copy(out=pT[ib][:], in_=ptp[:]), reads=[b_ptp], writes=[b_pT[ib]])
            for hf in range(2):
                hs = slice(hf * 512, (hf + 1) * 512)
                for k in range(8):
                    P.op("pe", lambda e, k=k, hs=hs: e.matmul(pG[ib][:, hs], lhsT=uT[ib][:, k, :], rhs=Wg[:, k, hs],
                                                              start=(k == 0), stop=(k == 7)),
                         reads=[b_in[ib], b_W], writes=[b_pG[ib]])

        def s2(n):
            ib = n % 2
            for hf in range(2):
                hs = slice(hf * 512, (hf + 1) * 512)
                for k in range(2):
                    P.op("pe", lambda e, k=k, hs=hs: e.matmul(pE[:, hs], lhsT=pT[ib][:, k, :], rhs=Wp[:, k, hs],
                                                              start=(k == 0), stop=(k == 1)),
                         reads=[b_pT[ib], b_W], writes=[b_pE])
            P.op("dve", lambda e: e.tensor_tensor(out=gt[ib][:], in0=pG[ib][:], in1=bg[:], op=ALU.add),
                 reads=[b_pG[ib], b_W], writes=[b_gt[ib]])
            P.op("act", lambda e: e.activation(out=gt[ib][:], in_=gt[ib][:], func=AF.Sigmoid), reads=[b_gt[ib]],
                 writes=[b_gt[ib]])
            P.op("dve", lambda e: e.tensor_tensor(out=ge[ib][:], in0=gt[ib][:], in1=pE[:], op=ALU.mult),
                 reads=[b_gt[ib], b_pE], writes=[b_ge[ib]])
            si = n % 2
            ss, var, rstd = (stat[:, 3 * si + j:3 * si + j + 1] for j in range(3))
            rms_stats(P, C, ge[ib][:], b_ge[ib], junk[:], b_junk, ss, var, rstd, b_stat[si])
            P.op("dve", lambda e: e.scalar_tensor_tensor(
                out=hout[ib][:], in0=ge[ib][:], scalar=rstd, in1=gpost[:], op0=ALU.mult, op1=ALU.mult),
                reads=[b_ge[ib], b_stat[si], b_W], writes=[b_hout[ib]])
            P.op("pool", lambda e: e.tensor_tensor(out=hout[ib][:], in0=hout[ib][:], in1=hin[ib][:], op=ALU.add),
                 reads=[b_hout[ib], b_hin[ib]], writes=[b_hout[ib]])
            P.op("pool", lambda e: e.dma_start(out=ov[n], in_=hout[ib][:]), reads=[b_hout[ib]], dma=True)

        s1(0)
        for n in range(32):
            if n + 1 < 32:
                s1(n + 1)
            s2(n)
        P.flush()


def build_program(debug=False, stage=99, only=None):
    nc = bass.Bass("TRN2", target_bir_lowering=False)
    I = {}

    def din(name, shape):
        I[name] = nc.dram_tensor(name, shape, F32, kind="ExternalInput").ap()
        return I[name]

    din("x", [S, D])
    din("p", [S, 256])
    for nm in ("ffn1", "ffn2"):
        din(nm + "_pre_g", [1, D])
        din(nm + "_w_gate", [D, DFF])
        din(nm + "_w_up", [D, DFF])
        din(nm + "_w_down", [DFF, D])
        din(nm + "_post_g", [1, D])
    din("mix_pre_g", [1, D])
    din("w_in", [D, INW])
    din("conv_w", [4, 512])
    din("conv_b", [1, 512])
    din("mlstm_i_bias", [1, 4])
    din("mlstm_f_bias", [1, 4])
    din("mlstm_norm_g", [1, 512])
    din("fox_f_bias", [1, 8])
    din("branch_gate_bias", [1, 2048])
    din("w_branch_a", [512, D])
    din("w_branch_b", [512, D])
    din("w_out", [D, D])
    din("mix_post_g", [1, D])
    din("ple_pre_g", [1, D])
    din("ple_w_gate", [D, D])
    din("ple_b_gate", [1, D])
    din("ple_w_proj", [256, D])
    din("ple_post_g", [1, D])

    skind = "ExternalOutput" if debug else "Internal"

    def dscr(name, shape, dt):
        return nc.dram_tensor(name, shape, dt, kind=skind).ap()

    out = nc.dram_tensor("out", [S, D], F32, kind="ExternalOutput").ap()
    h1 = dscr("h1", [S, D], F32)
    uT1 = dscr("uT1", [8, 128, S], BF16)
    gpre = dscr("gpre", [72, S], F32)
    SC = {
        "mqkT": dscr("mqkT", [4, 128, S], BF16),
        "vaugM": dscr("vaugM", [S, 4, 129], BF16),
        "so": dscr("so", [S, 512], BF16),
        "qaug": dscr("qaug", [8, 70, S], BF16),
        "kaug": dscr("kaug", [8, 70, S], BF16),
        "vaugF": dscr("vaugF", [S, 8, 65], BF16),
        "ga": dscr("ga", [S, D], BF16),
        "gb": dscr("gb", [S, D], BF16),
        "yaT": dscr("yaT", [4, 128, S], BF16),
        "ybT": dscr("ybT", [4, 128, S], BF16),
        "recd": dscr("recd", [64, 512], F32),
    }
    h2 = dscr("h2", [S, D], F32)
    h3 = dscr("h3", [S, D], F32)
    uT3 = dscr("uT3", [8, 128, S], BF16)

    with contextlib.ExitStack() as st:
        P = Prog(nc, st)
        C = Ctx()
        C.db = {}

        def db(name):
            if name not in C.db:
                C.db[name] = P.bufs_n("D" + name, 32)
            return C.db[name]

        setup_consts(P, nc, st, C)
        C.wthr = st.enter_context(nc.sbuf_tensor("wthr", [128, 32, 8], F32))
        C.decbc = st.enter_context(nc.sbuf_tensor("decbc", [128, 4, 32], F32))
        C.b_wthr = P.buf("wthr")
        C.b_decbc = P.buf("decbc")
        def want(name, st_no):
            return (name in only) if only is not None else (stage >= st_no)

        if want("ffn1", 1):
            ffn_pass(P, nc, C, "f1", I["x"], I["ffn1_w_gate"], I["ffn1_w_up"], I["ffn1_w_down"],
                     I["ffn1_pre_g"], I["ffn1_post_g"], h1, I["mix_pre_g"], uT1,
                     db("x"), db("h1"), db("uT1"), gate_w=I["w_in"], gate_dst=gpre, gate_b=db("gpre"))
        if want("gp", 2):
            aug_b = P.buf("augrows")
            gp_stage(P, nc, C, I, gpre, db("gpre"), SC["qaug"], SC["kaug"], aug_b)
            db("aug").append(aug_b)
        if want("win", 2):
            win_pass(P, nc, C, I, uT1, db("uT1"), SC, db)
        if want("mix", 3):
            mix_pass(P, nc, C, I, SC, db)
        if want("merge", 4):
            merge_pass(P, nc, C, I, SC, db, h1, db("h1"), h2, db("h2"))
        if want("ffn2", 5):
            ffn_pass(P, nc, C, "f2", h2, I["ffn2_w_gate"], I["ffn2_w_up"], I["ffn2_w_down"],
                     I["ffn2_pre_g"], I["ffn2_post_g"], h3, I["ple_pre_g"], uT3,
                     db("h2"), db("h3"), db("uT3"))
        if want("ple", 6):
            ple_pass(P, nc, C, I, uT3, db("uT3"), h3, db("h3"), out)
        P.flush(final=True)
    return nc

IN_NAMES = ["x", "p", "ffn1_pre_g", "ffn1_w_gate", "ffn1_w_up", "ffn1_w_down", "ffn1_post_g",
            "mix_pre_g", "w_in", "conv_w", "conv_b", "mlstm_i_bias", "mlstm_f_bias", "mlstm_norm_g",
            "fox_f_bias", "branch_gate_bias", "w_branch_a", "w_branch_b", "w_out", "mix_post_g",
            "ffn2_pre_g", "ffn2_w_gate", "ffn2_w_up", "ffn2_w_down", "ffn2_post_g",
            "ple_pre_g", "ple_w_gate", "ple_b_gate", "ple_w_proj", "ple_post_g"]


def make_in_maps(inputs, cores):
    maps = []
    shared = {}
    for k in IN_NAMES:
        if k in ("x", "p"):
            continue
        shared[k] = np.ascontiguousarray(np.asarray(inputs[k])[0], dtype=np.float32)
    x = np.asarray(inputs["x"])
    p = np.asarray(inputs["p"])
    for b in cores:
        m = dict(shared)
        m["x"] = np.ascontiguousarray(x[b], dtype=np.float32)
        m["p"] = np.ascontiguousarray(p[0, b], dtype=np.float32)
        maps.append(m)
    return maps


def kernel(**inputs):
    nc = build_program()
    maps = make_in_maps(inputs, list(range(8)))
    res = run_bass_kernel_spmd(nc, maps, core_ids=list(range(8)))
    return np.stack([np.asarray(r["out"], dtype=np.float32) for r in res.results], axis=0)
```

```python
import contextlib
import numpy as np
import concourse.bass as bass
import concourse.mybir as mybir
from concourse.bass_utils import run_bass_kernel_spmd

F32 = mybir.dt.float32
BF16 = mybir.dt.bfloat16
AF = mybir.ActivationFunctionType
ALU = mybir.AluOpType
AX = mybir.AxisListType

S = 4096
D = 1024
DFF = 2816
NFC = DFF // 128
NT = 8
TT = 512
EPS = 1e-6
INW = 5136

ENGS = ("pe", "act", "dve", "pool", "sp")


class Buf:
    __slots__ = ("name", "w", "r")

    def __init__(self, name=""):
        self.name = name
        self.w = None
        self.r = []


class Op:
    __slots__ = ("eng", "fn", "deps", "inc", "cnt", "dma", "sem", "emitted")

    def __init__(self, eng, fn, dma=False):
        self.eng = eng
        self.fn = fn
        self.deps = []
        self.inc = False
        self.cnt = 0
        self.dma = dma
        self.sem = None
        self.emitted = False


class Prog:
    def __init__(self, nc, st, n_dma_sems=20):
        self.nc = nc
        self.pending = {e: [] for e in ENGS}
        self.bufs = []
        self.nd = n_dma_sems
        self.esem = {e: st.enter_context(nc.semaphore("s_" + e)) for e in ENGS}
        self.dsem = {}
        for e in ("sp", "pool"):
            for s in range(n_dma_sems):
                self.dsem[(e, s)] = st.enter_context(nc.semaphore("d_%s_%d" % (e, s)))
        self.ecnt = {e: 0 for e in ENGS}
        self.dcnt = {e: 0 for e in ENGS}
        self.waited = {e: {} for e in ENGS}
        self.n_ops = 0

    def buf(self, name=""):
        b = Buf(name)
        self.bufs.append(b)
        return b

    def bufs_n(self, name, n):
        return [self.buf("%s%d" % (name, i)) for i in range(n)]

    def op(self, eng, fn, reads=(), writes=(), dma=False):
        o = Op(eng, fn, dma)
        seen = set()
        cand = []
        for b in reads:
            if b.w is not None:
                cand.append(b.w)
        for b in writes:
            if b.w is not None:
                cand.append(b.w)
            cand.extend(b.r)
        for d in cand:
            if d is o or id(d) in seen:
                continue
            seen.add(id(d))
            if d.eng == "pe" and eng == "pe" and not d.dma and not dma:
                continue
            o.deps.append(d)
            if not d.emitted:
                d.inc = True
        for b in reads:
            b.r.append(o)
        for b in writes:
            b.w = o
            b.r = []
        self.pending[eng].append(o)
        self.n_ops += 1
        return o

    def flush(self, final=False):
        nc = self.nc
        for b in self.bufs:
            if b.w is not None and not b.w.emitted:
                b.w.inc = True
            for r in b.r:
                if not r.emitted:
                    r.inc = True
        for e in ENGS:
            for o in self.pending[e]:
                if o.dma:
                    k = self.dcnt[e]
                    o.sem = (e, k % self.nd)
                    o.cnt = 16 * (k // self.nd + 1)
                    self.dcnt[e] = k + 1
                elif o.inc:
                    self.ecnt[e] += 1
                    o.cnt = self.ecnt[e]
        pending = self.pending
        self.pending = {e: [] for e in ENGS}

        def run(ename, eng):
            waited = self.waited[ename]
            for o in pending[ename]:
                for d in o.deps:
                    key = d.sem if d.dma else d.eng
                    if waited.get(key, 0) >= d.cnt:
                        continue
                    assert d.cnt > 0, (d.eng, ename)
                    eng.wait_ge(self.dsem[key] if d.dma else self.esem[key], d.cnt)
                    waited[key] = d.cnt
                if o.dma:
                    if o.cnt > 16 and waited.get(o.sem, 0) < o.cnt - 16:
                        eng.wait_ge(self.dsem[o.sem], o.cnt - 16)
                        waited[o.sem] = o.cnt - 16
                    o.fn(eng).then_inc(self.dsem[o.sem], 16)
                else:
                    ins = o.fn(eng)
                    if o.inc:
                        ins.then_inc(self.esem[o.eng], 1)
                o.emitted = True
            if ename == "sp":
                for q in ("sp", "pool"):
                    k = self.dcnt[q]
                    for sl in range(min(self.nd, k)):
                        last = 16 * ((k - 1 - sl) // self.nd + 1)
                        eng.wait_ge(self.dsem[(q, sl)], last)

        with nc.Block() as block:
            @block.tensor
            def _(eng):
                run("pe", eng)

            @block.scalar
            def _(eng):
                run("act", eng)

            @block.vector
            def _(eng):
                run("dve", eng)

            @block.gpsimd
            def _(eng):
                run("pool", eng)

            @block.sync
            def _(eng):
                run("sp", eng)


def bcast_last(ap2d, n):
    return ap2d.unsqueeze(2).to_broadcast([ap2d.shape[0], ap2d.shape[1], n])


class Ctx:
    pass


def load_w_kmajor(P, nc, dst, src2d, n_kc, ncols, bufs, col_chunk=1408):
    v = src2d.rearrange("(kc p) n -> kc p n", p=128)
    mdl = 4 * col_chunk
    for k in range(n_kc):
        P.op("pool", lambda e, k=k: e.dma_start(out=dst[:, k, :], in_=v[k], max_dma_last_dim=mdl),
             writes=[bufs[k]], dma=True)


def setup_consts(P, nc, st, C):
    sb = lambda name, shape, dt: st.enter_context(nc.sbuf_tensor(name, shape, dt))
    C.identf = sb("identf", [128, 128], F32)
    C.ident = sb("ident", [128, 128], BF16)
    C.neghalf = sb("neghalf", [128, 1], F32)
    C.b_const = P.buf("const")
    identf, ident = C.identf, C.ident
    P.op("pool", lambda e: e.memset(identf[:], 0.0), writes=[C.b_const])
    P.op("pool", lambda e: e.affine_select(out=identf[:], in_=identf[:], pattern=[[-1, 128]],
                                             compare_op=ALU.not_equal, fill=1.0, base=0,
                                             channel_multiplier=1),
         reads=[C.b_const], writes=[C.b_const])
    P.op("dve", lambda e: e.tensor_copy(out=ident[:], in_=identf[:]), reads=[C.b_const], writes=[C.b_const])
    P.op("pool", lambda e: e.memset(C.neghalf[:], -0.5), reads=[C.b_const], writes=[C.b_const])


def rms_stats(P, C, src_ap, src_buf, junk, b_junk, ss, var, rstd, b_stat, n_feat=D):
    P.op("act", lambda e: e.activation(out=junk, in_=src_ap, func=AF.Square, accum_out=ss),
         reads=[src_buf], writes=[b_junk, b_stat])
    P.op("dve", lambda e: e.tensor_scalar(out=var, in0=ss, scalar1=1.0 / n_feat, scalar2=EPS,
                                          op0=ALU.mult, op1=ALU.add),
         reads=[b_stat], writes=[b_stat])
    P.op("pool", lambda e: e.tensor_tensor(out=rstd, in0=var, in1=C.neghalf[:], op=ALU.pow),
         reads=[b_stat, C.b_const], writes=[b_stat])


def ffn_pass(P, nc, C, tag, src_h, w_gate, w_up, w_down, pre_g, post_g, dst_h, next_g, dst_uT,
             src_b, dst_b, uT_b, gate_w=None, gate_dst=None, gate_b=None):
    with contextlib.ExitStack() as st:
        sb = lambda name, shape, dt: st.enter_context(nc.sbuf_tensor(tag + name, shape, dt))
        ps = lambda name, shape, dt: st.enter_context(nc.psum_tensor(tag + name, shape, dt))
        Wg = sb("Wg", [128, 8, DFF], BF16)
        Wu = sb("Wu", [128, 8, DFF], BF16)
        Wd = sb("Wd", [128, NFC, D], BF16)
        b_Wg = P.bufs_n("Wg", 8)
        b_Wu = P.bufs_n("Wu", 8)
        b_Wd = P.bufs_n("Wd", 2)
        load_w_kmajor(P, nc, Wg, w_gate, 8, DFF, b_Wg)
        load_w_kmajor(P, nc, Wu, w_up, 8, DFF, b_Wu)
        wdv = w_down.rearrange("(fc p) d -> p fc d", p=128)
        for hh in range(2):
            P.op("pool", lambda e, hh=hh: e.dma_start(out=Wd[:, hh * 11:(hh + 1) * 11, :],
                                                       in_=wdv[:, hh * 11:(hh + 1) * 11, :]),
                 writes=[b_Wd[hh]], dma=True)
        gpre = sb("gpre", [128, 8], F32)
        gnext = sb("gnext", [128, 8], F32)
        gpost = sb("gpost", [128, D], F32)
        b_par = P.buf("par")
        P.op("sp", lambda e: e.dma_start(out=gpre[:], in_=pre_g.rearrange("o (k p) -> p (o k)", p=128),
                                         allow_slow_non_contiguous=True),
             writes=[b_par], dma=True)
        P.op("sp", lambda e: e.dma_start(out=gnext[:], in_=next_g.rearrange("o (k p) -> p (o k)", p=128),
                                         allow_slow_non_contiguous=True),
             writes=[b_par], dma=True)
        P.op("sp", lambda e: e.dma_start(out=gpost[:], in_=post_g.partition_broadcast(128)),
             writes=[b_par], dma=True)
        if gate_w is not None:
            Wgt = sb("Wgt", [128, 8, 72], BF16)
            b_Wgt = P.buf("Wgt")
            P.op("pool", lambda e: e.memset(Wgt[:], 0.0), writes=[b_Wgt])
            gv = gate_w.rearrange("(kc p) n -> p kc n", p=128)
            for (c0, n, d0) in ((1540, 4, 0), (1536, 4, 32), (3080, 8, 64)):
                P.op("pool", lambda e, c0=c0, n=n, d0=d0: e.dma_start(
                    out=Wgt[:, :, d0:d0 + n], in_=gv[:, :, c0:c0 + n]),
                    reads=[], writes=[b_Wgt], dma=True)
            gsb = [sb("gsb%d" % i, [72, 128], F32) for i in range(2)]
            b_gsb = P.bufs_n("gsb", 2)

        NXB = 3
        xb = [sb("xb%d" % i, [128, D], F32) for i in range(NXB)]
        b_xb = P.bufs_n("xb", NXB)
        ubf = [sb("ubf%d" % i, [128, D], BF16) for i in range(2)]
        b_ubf = P.bufs_n("ubf", 2)
        junk = sb("junk", [128, D], BF16)
        b_junk = P.buf("junk")
        uT = sb("uT", [128, 8, TT], BF16)
        b_uT = P.bufs_n("uT", 4)
        aT = sb("aT", [128, NFC, TT], BF16)
        b_aT = P.bufs_n("aT", NFC)
        sg = [sb("sg%d" % i, [128, TT], F32) for i in range(2)]
        b_sg = P.bufs_n("sg", 2)
        hst = [sb("hst%d" % i, [128, D], F32) for i in range(2)]
        b_hst = P.bufs_n("hst", 2)
        u2T = [sb("u2T%d" % i, [128, 8, 128], BF16) for i in range(2)]
        b_u2T = P.bufs_n("u2T", 2)
        NST = 6
        stat = sb("stat", [128, 3 * NST], F32)
        b_stat = P.bufs_n("stat", NST)

        pt = ps("pt", [128, 8, 128], BF16)
        b_pt = P.buf("pt")
        pg = [ps("pg%d" % i, [128, TT], F32) for i in range(2)]
        pu = [ps("pu%d" % i, [128, TT], F32) for i in range(2)]
        b_pg = P.bufs_n("pg", 2)
        b_pu = P.bufs_n("pu", 2)
        pys = [ps("py%d" % i, [128, 512], F32) for i in range(3)]
        b_pys = P.bufs_n("py", 3)
        if gate_w is not None:
            pgt = pu[1][0:72, 0:128]
            b_pgt = b_pu[1]

        src_v = src_h.rearrange("(n p) d -> n p d", p=128)
        dst_v = dst_h.rearrange("(n p) d -> n p d", p=128)
        cnt = {"x": 0, "u": 0, "st": 0, "h": 0, "u2": 0, "sg": 0, "gs": 0, "py": 0}
        pend = []

        def norm_T(h_ap, h_buf, gcol, out_ap, out_bufs, defer=False):
            si = cnt["st"] % NST
            cnt["st"] += 1
            ss, var, rstd = (stat[:, 3 * si + j:3 * si + j + 1] for j in range(3))
            rms_stats(P, C, h_ap, h_buf, junk[:], b_junk, ss, var, rstd, b_stat[si])
            ui = cnt["u"] % 2
            cnt["u"] += 1
            u = ubf[ui]
            P.op("dve", lambda e: e.tensor_scalar(out=u[:], in0=h_ap, scalar1=rstd, scalar2=None, op0=ALU.mult),
                 reads=[h_buf, b_stat[si]], writes=[b_ubf[ui]])
            def pe_part():
                for k in range(8):
                    P.op("pe", lambda e, k=k: e.transpose(out=pt[:, k, :], in_=u[:, k * 128:(k + 1) * 128],
                                                          identity=C.ident[:]),
                         reads=[b_ubf[ui], C.b_const], writes=[b_pt])
                P.op("dve", lambda e: e.tensor_tensor(out=out_ap, in0=pt[:], in1=bcast_last(gcol[:], 128), op=ALU.mult),
                     reads=[b_pt, b_par], writes=out_bufs)
            if defer:
                return pe_part
            pe_part()

        def pre(i):
            for s in range(4):
                n = i * 4 + s
                xi = cnt["x"] % NXB
                cnt["x"] += 1
                P.op("sp", lambda e, n=n, xi=xi: e.dma_start(out=xb[xi][:], in_=src_v[n]),
                     reads=[src_b[n]], writes=[b_xb[xi]], dma=True)
                norm_T(xb[xi][:], b_xb[xi], gpre, uT[:, :, s * 128:(s + 1) * 128], [b_uT[s]])

        def gateup(i):
            for f in range(NFC):
                j = f % 2
                for k in range(8):
                    P.op("pe", lambda e, k=k, f=f, j=j: e.matmul(
                        pg[j][:], lhsT=Wg[:, k, f * 128:(f + 1) * 128], rhs=uT[:, k, :],
                        start=(k == 0), stop=(k == 7)),
                        reads=[b_Wg[k]] + b_uT, writes=[b_pg[j]])
                for k in range(8):
                    P.op("pe", lambda e, k=k, f=f, j=j: e.matmul(
                        pu[j][:], lhsT=Wu[:, k, f * 128:(f + 1) * 128], rhs=uT[:, k, :],
                        start=(k == 0), stop=(k == 7)),
                        reads=[b_Wu[k]] + b_uT, writes=[b_pu[j]])
                if f == 0:
                    while pend:
                        pend.pop(0)()
                si = cnt["sg"] % 2
                cnt["sg"] += 1
                P.op("act", lambda e, j=j, si=si: e.activation(out=sg[si][:], in_=pg[j][:], func=AF.Silu),
                     reads=[b_pg[j]], writes=[b_sg[si]])
                P.op("dve", lambda e, j=j, si=si, f=f: e.tensor_tensor(out=aT[:, f, :], in0=sg[si][:], in1=pu[j][:],
                                                                   op=ALU.mult),
                     reads=[b_sg[si], b_pu[j]], writes=[b_aT[f]])

        def down_post(i):
            for s in range(4):
                n = i * 4 + s
                pyh = []
                for hf in range(2):
                    pi = cnt["py"] % 3
                    cnt["py"] += 1
                    pyh.append((pys[pi], b_pys[pi]))
                    for f in range(NFC):
                        P.op("pe", lambda e, f=f, s=s, hf=hf, pi=pi: e.matmul(
                            pys[pi][:], lhsT=aT[:, f, s * 128:(s + 1) * 128],
                            rhs=Wd[:, f, hf * 512:(hf + 1) * 512], start=(f == 0), stop=(f == NFC - 1)),
                            reads=[b_aT[f], b_Wd[f // 11]], writes=[b_pys[pi]])
                while pend:
                    pend.pop(0)()
                xi = cnt["x"] % NXB
                cnt["x"] += 1
                P.op("sp", lambda e, n=n, xi=xi: e.dma_start(out=xb[xi][:], in_=src_v[n]),
                     reads=[src_b[n]], writes=[b_xb[xi]], dma=True)
                si = cnt["st"] % NST
                cnt["st"] += 1
                ss, var, rstd = (stat[:, 3 * si + j:3 * si + j + 1] for j in range(3))
                P.op("act", lambda e, ss=ss, t=pyh[0][0]: e.activation(out=junk[:, 0:512], in_=t[:], func=AF.Square,
                                                                      accum_out=ss),
                     reads=[pyh[0][1]], writes=[b_junk, b_stat[si]])
                P.op("act", lambda e, var=var, t=pyh[1][0]: e.activation(out=junk[:, 512:1024], in_=t[:], func=AF.Square,
                                                                        accum_out=var),
                     reads=[pyh[1][1]], writes=[b_junk, b_stat[si]])
                P.op("dve", lambda e, ss=ss, var=var: e.tensor_tensor(out=var, in0=ss, in1=var, op=ALU.add),
                     reads=[b_stat[si]], writes=[b_stat[si]])
                P.op("dve", lambda e, var=var: e.tensor_scalar(out=var, in0=var, scalar1=1.0 / D, scalar2=EPS,
                                                              op0=ALU.mult, op1=ALU.add),
                     reads=[b_stat[si]], writes=[b_stat[si]])
                P.op("pool", lambda e, var=var, rstd=rstd: e.tensor_tensor(out=rstd, in0=var, in1=C.neghalf[:], op=ALU.pow),
                     reads=[b_stat[si], C.b_const], writes=[b_stat[si]])
                hi = cnt["h"] % 2
                cnt["h"] += 1
                hb = hst[hi]
                for hf in range(2):
                    hs = slice(hf * 512, (hf + 1) * 512)
                    P.op("dve", lambda e, hb=hb, rstd=rstd, t=pyh[hf][0], hs=hs: e.scalar_tensor_tensor(
                        out=hb[:, hs], in0=t[:], scalar=rstd, in1=gpost[:, hs], op0=ALU.mult, op1=ALU.mult),
                        reads=[pyh[hf][1], b_stat[si], b_par], writes=[b_hst[hi]])
                P.op("dve", lambda e, hb=hb, xi=xi: e.scalar_tensor_tensor(
                    out=hb[:], in0=hb[:], scalar=0.5, in1=xb[xi][:], op0=ALU.mult, op1=ALU.add),
                    reads=[b_hst[hi], b_xb[xi]], writes=[b_hst[hi]])
                P.op("sp", lambda e, hb=hb, n=n: e.dma_start(out=dst_v[n], in_=hb[:]),
                     reads=[b_hst[hi]], writes=[dst_b[n]], dma=True)
                ui2 = cnt["u2"] % 2
                cnt["u2"] += 1
                pe_part = norm_T(hb[:], b_hst[hi], gnext, u2T[ui2][:], [b_u2T[ui2]], defer=True)

                def tail(pe_part=pe_part, ui2=ui2, n=n):
                    pe_part()
                    P.op("sp", lambda e: e.dma_start(
                        out=dst_uT[:, :, n * 128:(n + 1) * 128].rearrange("k p t -> p k t"), in_=u2T[ui2][:]),
                        reads=[b_u2T[ui2]], writes=[uT_b[n]], dma=True)
                    if gate_w is not None:
                        for k in range(8):
                            P.op("pe", lambda e, k=k: e.matmul(
                                pgt, lhsT=Wgt[:, k, :], rhs=u2T[ui2][:, k, :], start=(k == 0), stop=(k == 7)),
                                reads=[b_Wgt, b_u2T[ui2]], writes=[b_pgt])
                        gi = cnt["gs"] % 2
                        cnt["gs"] += 1
                        P.op("act", lambda e: e.activation(out=gsb[gi][:], in_=pgt, func=AF.Copy),
                             reads=[b_pgt], writes=[b_gsb[gi]])
                        P.op("sp", lambda e: e.dma_start(out=gate_dst[:, n * 128:(n + 1) * 128], in_=gsb[gi][:]),
                             reads=[b_gsb[gi]], writes=[gate_b[n]], dma=True)
                pend.append(tail)

        pre(0)
        for i in range(NT):
            gateup(i)
            if i + 1 < NT:
                pre(i + 1)
            down_post(i)
        while pend:
            pend.pop(0)()
        P.flush()


def gp_stage(P, nc, C, I, gpre, gpre_b, qaug, kaug, aug_b):
    with contextlib.ExitStack() as st:
        sb = lambda name, shape, dt: st.enter_context(nc.sbuf_tensor("gp" + name, shape, dt))
        ps = lambda name, shape, dt: st.enter_context(nc.psum_tensor("gp" + name, shape, dt))
        T0 = sb("T0", [72, S], F32)
        T1 = sb("T1", [72, S], F32)
        T2 = sb("T2", [72, S], F32)
        T3 = sb("T3", [72, S], F32)
        QR = sb("QR", [72, 3, S], BF16)
        KR = sb("KR", [72, 3, S], BF16)
        ONE = sb("ONE", [72, S], BF16)
        bcol = sb("bcol", [72, 1], F32)
        negb = sb("negb", [72, 1], F32)
        bicol = sb("bicol", [72, 1], F32)
        onec = sb("onec", [72, 1], F32)
        cm = sb("cm", [72, 32], F32)
        mce = sb("mce", [72, 32], F32)
        mprev = sb("mprev", [72, 32], F32)
        dec = sb("dec", [72, 32], F32)
        esel = sb("esel", [72, 4, 128], F32)
        bT0, bT1, bT2, bT3, bQR, bKR, bONE, bsm = [P.buf(n) for n in
                                                   ("T0", "T1", "T2", "T3", "QR", "KR", "ONE", "gsm")]
        ptm = ps("ptm", [128, 32, 8], F32)
        pdc = ps("pdc", [128, 4, 32], F32)
        b_ptm, b_pdc = P.buf("ptm"), P.buf("pdc")

        P.op("sp", lambda e: e.dma_start(out=T0[:], in_=gpre), reads=gpre_b, writes=[bT0], dma=True)
        P.op("sp", lambda e: e.dma_start(out=T3[0:4, :], in_=gpre[32:36, :]), reads=gpre_b, writes=[bT3], dma=True)
        P.op("dve", lambda e: e.memset(bcol[:], 0.0), writes=[bsm])
        P.op("dve", lambda e: e.memset(bicol[:], 0.0), reads=[bsm], writes=[bsm])
        P.op("dve", lambda e: e.memset(onec[:], 1.0), reads=[bsm], writes=[bsm])
        P.op("pool", lambda e: e.memset(ONE[:], 1.0), writes=[bONE])
        P.op("sp", lambda e: e.dma_start(out=bcol[0:4, :], in_=I["mlstm_f_bias"].rearrange("o n -> n o"),
                                         allow_slow_non_contiguous=True), reads=[bsm], writes=[bsm], dma=True)
        P.op("sp", lambda e: e.dma_start(out=bcol[64:72, :], in_=I["fox_f_bias"].rearrange("o n -> n o"),
                                         allow_slow_non_contiguous=True), reads=[bsm], writes=[bsm], dma=True)
        P.op("sp", lambda e: e.dma_start(out=bicol[0:4, :], in_=I["mlstm_i_bias"].rearrange("o n -> n o"),
                                         allow_slow_non_contiguous=True), reads=[bsm], writes=[bsm], dma=True)
        P.op("dve", lambda e: e.tensor_scalar(out=negb[0:72, :], in0=bcol[0:72, :], scalar1=-1.0, scalar2=None,
                                              op0=ALU.mult), reads=[bsm], writes=[bsm])
        R = slice(0, 72)
        P.op("act", lambda e: e.activation(out=T1[R, :], in_=T0[R, :], func=AF.Exp, scale=-1.0, bias=negb[R, :]),
             reads=[bT0, bsm], writes=[bT1])
        P.op("act", lambda e: e.activation(out=T1[R, :], in_=T1[R, :], func=AF.Ln, scale=1.0, bias=onec[R, :]),
             reads=[bT1, bsm], writes=[bT1])
        P.op("dve", lambda e: e.tensor_tensor_scan(out=T2[R, :], data0=T1[R, :], data1=T1[R, :], initial=0.0,
                                                   op0=ALU.add, op1=ALU.max), reads=[bT1], writes=[bT2])
        M = slice(0, 4)
        P.op("dve", lambda e: e.scalar_tensor_tensor(out=T3[M, :], in0=T3[M, :], scalar=bicol[M, :], in1=T2[M, :],
                                                     op0=ALU.add, op1=ALU.add), reads=[bT3, bT2, bsm], writes=[bT3])
        P.op("dve", lambda e: e.tensor_reduce(out=cm[M, :], in_=T3[M, :].rearrange("p (c l) -> p c l", l=128),
                                              axis=AX.X, op=ALU.max), reads=[bT3], writes=[bsm])
        P.op("dve", lambda e: e.tensor_tensor_scan(out=mce[M, :], data0=cm[M, :], data1=cm[M, :], initial=0.0,
                                                   op0=ALU.max, op1=ALU.max), reads=[bsm], writes=[bsm])
        P.op("dve", lambda e: e.tensor_tensor(out=T3[M, :].rearrange("p (c l) -> p c l", l=128),
                                              in0=T3[M, :].rearrange("p (c l) -> p c l", l=128),
                                              in1=bcast_last(mce[M, :], 128), op=ALU.subtract),
             reads=[bT3, bsm], writes=[bT3])
        P.op("act", lambda e: e.activation(out=T3[M, :], in_=T3[M, :], func=AF.Exp), reads=[bT3], writes=[bT3])
        P.op("dve", lambda e: e.tensor_tensor(out=T1[M, :].rearrange("p (c l) -> p c l", l=128),
                                              in0=T2[M, :].rearrange("p (c l) -> p c l", l=128),
                                              in1=bcast_last(mce[M, :], 128), op=ALU.subtract),
             reads=[bT2, bsm, bT1], writes=[bT1])
        P.op("act", lambda e: e.activation(out=T1[M, :], in_=T1[M, :], func=AF.Exp, scale=2.0), reads=[bT1], writes=[bT1])
        P.op("dve", lambda e: e.memset(mprev[M, :], 0.0), reads=[bsm], writes=[bsm])
        P.op("dve", lambda e: e.tensor_copy(out=mprev[M, 1:32], in_=mce[M, 0:31]), reads=[bsm], writes=[bsm])
        P.op("dve", lambda e: e.tensor_tensor(out=dec[M, :], in0=mprev[M, :], in1=mce[M, :], op=ALU.subtract),
             reads=[bsm], writes=[bsm])
        P.op("act", lambda e: e.activation(out=dec[M, :], in_=dec[M, :], func=AF.Exp), reads=[bsm], writes=[bsm])
        for c in range(32):
            P.op("pe", lambda e, c=c: e.transpose(out=ptm[:, c, 0:4], in_=T3[M, c * 128:(c + 1) * 128],
                                                  identity=C.identf[M, 0:4]),
                 reads=[bT3, C.b_const], writes=[b_ptm])
            P.op("pe", lambda e, c=c: e.transpose(out=ptm[:, c, 4:8], in_=T1[M, c * 128:(c + 1) * 128],
                                                  identity=C.identf[M, 0:4]),
                 reads=[bT1, C.b_const], writes=[b_ptm])
        P.op("dve", lambda e: e.tensor_copy(out=C.wthr[:], in_=ptm[:]), reads=[b_ptm], writes=[C.b_wthr])
        for h in range(4):
            P.op("dve", lambda e, h=h: e.tensor_copy(out=esel[M, h, :],
                                                     in_=C.identf[M, h:h + 1].to_broadcast([4, 128])),
                 reads=[C.b_const, bsm], writes=[bsm])
        for h in range(4):
            P.op("pe", lambda e, h=h: e.matmul(pdc[:, h, :], lhsT=esel[M, h, :], rhs=dec[M, :], start=True, stop=True),
                 reads=[bsm], writes=[b_pdc])
        P.op("dve", lambda e: e.tensor_copy(out=C.decbc[:], in_=pdc[:]), reads=[b_pdc], writes=[C.b_decbc])
        Fx = slice(64, 72)
        Fd = slice(64, 72)
        P.op("dve", lambda e: e.tensor_scalar(out=T0[Fx, :], in0=T2[Fx, :], scalar1=-1.0, scalar2=None, op0=ALU.mult),
             reads=[bT2, bT0], writes=[bT0])
        for part in range(3):
            P.op("dve", lambda e, part=part: e.tensor_copy(out=QR[Fx, part, :], in_=T0[Fx, :]),
                 reads=[bT0], writes=[bQR])
            if part < 2:
                P.op("dve", lambda e, part=part: e.tensor_tensor(out=T0[Fx, :], in0=T0[Fx, :], in1=QR[Fx, part, :],
                                                                 op=ALU.subtract), reads=[bT0, bQR], writes=[bT0])
        P.op("pool", lambda e: e.tensor_scalar(out=KR[Fx, :, :], in0=QR[Fx, :, :], scalar1=-1.0, scalar2=None,
                                               op0=ALU.mult), reads=[bQR], writes=[bKR])
        P.op("sp", lambda e: e.dma_start(out=qaug[:, 64:67, :], in_=QR[Fd, :, :]), reads=[bQR], writes=[aug_b], dma=True)
        P.op("sp", lambda e: e.dma_start(out=kaug[:, 67:70, :], in_=KR[Fd, :, :]), reads=[bKR], writes=[aug_b], dma=True)
        for r in range(3):
            P.op("sp", lambda e, r=r: e.dma_start(out=qaug[:, 67 + r, :], in_=ONE[Fd, :]), reads=[bONE],
                 writes=[aug_b], dma=True)
            P.op("sp", lambda e, r=r: e.dma_start(out=kaug[:, 64 + r, :], in_=ONE[Fd, :]), reads=[bONE],
                 writes=[aug_b], dma=True)
        P.flush()


def win_pass(P, nc, C, I, uT1, uT_b, SC, DB):
    w_in = I["w_in"]
    with contextlib.ExitStack() as st:
        sb = lambda name, shape, dt: st.enter_context(nc.sbuf_tensor("wi" + name, shape, dt))
        ps = lambda name, shape, dt: st.enter_context(nc.psum_tensor("wi" + name, shape, dt))
        W = sb("W", [128, 8, INW], BF16)
        b_W = P.bufs_n("Win", 8)
        wv = w_in.rearrange("(kc p) n -> kc p n", p=128)
        for k in range(8):
            P.op("pool", lambda e, k=k: e.dma_start(out=W[:, k, :], in_=wv[k], max_dma_last_dim=4 * 1284),
                 writes=[b_W[k]], dma=True)
        cw = sb("cw", [128, 4, 4], F32)
        cb = sb("cb", [128, 4], F32)
        gbias = sb("gbias", [128, 2048], F32)
        b_par = P.buf("wipar")
        for tap in range(4):
            P.op("sp", lambda e, tap=tap: e.dma_start(
                out=cw[:, :, tap], in_=I["conv_w"][tap:tap + 1, :].rearrange("o (c p) -> p (o c)", p=128),
                allow_slow_non_contiguous=True), writes=[b_par], dma=True)
        P.op("sp", lambda e: e.dma_start(out=cb[:], in_=I["conv_b"].rearrange("o (c p) -> p (o c)", p=128),
                                         allow_slow_non_contiguous=True), writes=[b_par], dma=True)
        P.op("sp", lambda e: e.dma_start(out=gbias[:], in_=I["branch_gate_bias"].partition_broadcast(128)),
             writes=[b_par], dma=True)
        uT = [sb("uT%d" % i, [128, 8, TT], BF16) for i in range(2)]
        b_uT = P.bufs_n("wiuT", 2)
        zq = sb("zq", [128, 4, 3 + TT], F32)
        b_zq = P.bufs_n("zq", 4)
        acc = [sb("acc%d" % i, [128, TT], F32) for i in range(2)]
        b_acc = P.bufs_n("acc", 2)
        fo = [sb("fo%d" % i, [128, TT], BF16) for i in range(3)]
        b_fo = P.bufs_n("fo", 3)
        tv = [sb("tv%d" % i, [128, 4, 129], BF16) for i in range(2)]
        b_tv = P.bufs_n("tv", 2)
        tf = [sb("tf%d" % i, [128, 8, 65], BF16) for i in range(2)]
        b_tf = P.bufs_n("tf", 2)
        tg = [sb("tg%d" % i, [128, 512], F32) for i in range(2)]
        b_tg = P.bufs_n("tg", 2)
        to = [sb("to%d" % i, [128, 512], BF16) for i in range(3)]
        b_to = P.bufs_n("to", 3)
        pf = [ps("pf%d" % i, [128, TT], F32) for i in range(2)]
        b_pf = P.bufs_n("pf", 2)
        pk = [ps("pk%d" % i, [128, 512], F32) for i in range(2)]
        b_pk = P.bufs_n("pk", 2)
        cnt = {"pf": 0, "pk": 0, "acc": 0, "fo": 0, "tv": 0, "tf": 0, "tg": 0, "to": 0}

        def rot(key, n):
            v = cnt[key] % n
            cnt[key] += 1
            return v

        for ch in range(4):
            P.op("dve", lambda e, ch=ch: e.memset(zq[:, ch, 0:3], 0.0), writes=[b_zq[ch]])
        for i in range(NT):
            ub = i % 2
            tcols = slice(i * TT, (i + 1) * TT)
            if i == 0:
                P.op("sp", lambda e: e.dma_start(out=uT[0][:], in_=uT1[:, :, 0:TT].rearrange("k p t -> p k t")),
                     reads=uT_b[0:4], writes=[b_uT[0]], dma=True)
            if i + 1 < NT:
                ncols = slice((i + 1) * TT, (i + 2) * TT)
                P.op("sp", lambda e, ub=ub, ncols=ncols: e.dma_start(
                    out=uT[1 - ub][:], in_=uT1[:, :, ncols].rearrange("k p t -> p k t")),
                    reads=uT_b[4 * i + 4:4 * i + 8], writes=[b_uT[1 - ub]], dma=True)
            fm = [("mqk", ch, ch * 128) for ch in range(4)] + \
                 [("fq", ch, 1544 + ch * 128) for ch in range(4)] + \
                 [("fk", ch, 2056 + ch * 128) for ch in range(4)]
            for (kind, ch, c0) in fm:
                j = rot("pf", 2)
                for k in range(8):
                    P.op("pe", lambda e, k=k, c0=c0, j=j, ub=ub: e.matmul(
                        pf[j][:], lhsT=W[:, k, c0:c0 + 128], rhs=uT[ub][:, k, :], start=(k == 0), stop=(k == 7)),
                        reads=[b_W[k], b_uT[ub]], writes=[b_pf[j]])
                if kind == "mqk":
                    P.op("act", lambda e, ch=ch, j=j: e.activation(out=zq[:, ch, 3:3 + TT], in_=pf[j][:], func=AF.Copy),
                         reads=[b_pf[j]], writes=[b_zq[ch]])
                    a = rot("acc", 2)
                    P.op("dve", lambda e, ch=ch, a=a: e.tensor_scalar(
                        out=acc[a][:], in0=zq[:, ch, 0:TT], scalar1=cw[:, ch, 0:1], scalar2=cb[:, ch:ch + 1],
                        op0=ALU.mult, op1=ALU.add), reads=[b_zq[ch], b_par], writes=[b_acc[a]])
                    for tap in range(1, 4):
                        P.op("dve", lambda e, ch=ch, a=a, tap=tap: e.scalar_tensor_tensor(
                            out=acc[a][:], in0=zq[:, ch, tap:tap + TT], scalar=cw[:, ch, tap:tap + 1], in1=acc[a][:],
                            op0=ALU.mult, op1=ALU.add), reads=[b_zq[ch], b_par, b_acc[a]], writes=[b_acc[a]])
                    P.op("dve", lambda e, ch=ch: e.tensor_copy(out=zq[:, ch, 0:3], in_=zq[:, ch, TT:TT + 3]),
                         reads=[b_zq[ch]], writes=[b_zq[ch]])
                    o = rot("fo", 3)
                    P.op("act", lambda e, a=a, o=o: e.activation(out=fo[o][:], in_=acc[a][:], func=AF.Silu),
                         reads=[b_acc[a]], writes=[b_fo[o]])
                    P.op("sp", lambda e, o=o, ch=ch, tcols=tcols: e.dma_start(out=SC["mqkT"][ch, :, tcols], in_=fo[o][:]),
                         reads=[b_fo[o]], writes=[DB("mqkT")[i]], dma=True)
                else:
                    o = rot("fo", 3)
                    sc = 0.125 if kind == "fq" else 1.0
                    P.op("act", lambda e, o=o, j=j, sc=sc: e.activation(out=fo[o][:], in_=pf[j][:], func=AF.Copy, scale=sc),
                         reads=[b_pf[j]], writes=[b_fo[o]])
                    dst = SC["qaug"] if kind == "fq" else SC["kaug"]
                    for hh in range(2):
                        P.op("sp", lambda e, o=o, ch=ch, dst=dst, tcols=tcols, hh=hh: e.dma_start(
                            out=dst[2 * ch + hh, 0:64, tcols], in_=fo[o][hh * 64:(hh + 1) * 64, :]),
                            reads=[b_fo[o]], writes=[DB("aug")[i]], dma=True)
            for s in range(4):
                n = i * 4 + s
                rows = slice(n * 128, (n + 1) * 128)
                groups = [("mv", 512), ("mo", 1024), ("fv", 2568), ("ga", 3088), ("ga", 3600), ("gb", 4112), ("gb", 4624)]
                for gi, (kind, c0) in enumerate(groups):
                    j = rot("pk", 2)
                    for k in range(8):
                        P.op("pe", lambda e, k=k, c0=c0, j=j, ub=ub, s=s: e.matmul(
                            pk[j][:], lhsT=uT[ub][:, k, s * 128:(s + 1) * 128], rhs=W[:, k, c0:c0 + 512],
                            start=(k == 0), stop=(k == 7)),
                            reads=[b_W[k], b_uT[ub]], writes=[b_pk[j]])
                    if kind == "mv":
                        t = rot("tv", 2)
                        c = n
                        P.op("dve", lambda e, t=t, j=j, c=c: e.tensor_tensor(
                            out=tv[t][:, :, 0:128], in0=pk[j][:].rearrange("p (h d) -> p h d", h=4),
                            in1=bcast_last(C.wthr[:, c, 0:4], 128), op=ALU.mult),
                            reads=[b_pk[j], C.b_wthr], writes=[b_tv[t]])
                        P.op("dve", lambda e, t=t, c=c: e.tensor_copy(out=tv[t][:, :, 128:129],
                                                                     in_=C.wthr[:, c, 0:4].unsqueeze(2)),
                             reads=[C.b_wthr, b_tv[t]], writes=[b_tv[t]])
                        P.op("sp", lambda e, t=t, rows=rows: e.dma_start(out=SC["vaugM"][rows], in_=tv[t][:]),
                             reads=[b_tv[t]], writes=[DB("vaugM")[n]], dma=True)
                    elif kind == "fv":
                        t = rot("tf", 2)
                        P.op("act", lambda e, t=t, j=j: e.activation(
                            out=tf[t][:, :, 1:65], in_=pk[j][:].rearrange("p (h d) -> p h d", h=8), func=AF.Copy),
                            reads=[b_pk[j]], writes=[b_tf[t]])
                        P.op("dve", lambda e, t=t: e.memset(tf[t][:, :, 0:1], 1.0), reads=[b_tf[t]], writes=[b_tf[t]])
                        P.op("sp", lambda e, t=t, rows=rows: e.dma_start(out=SC["vaugF"][rows], in_=tf[t][:]),
                             reads=[b_tf[t]], writes=[DB("vaugF")[n]], dma=True)
                    elif kind == "mo":
                        o = rot("to", 3)
                        P.op("act", lambda e, o=o, j=j: e.activation(out=to[o][:], in_=pk[j][:], func=AF.Sigmoid),
                             reads=[b_pk[j]], writes=[b_to[o]])
                        P.op("sp", lambda e, o=o, rows=rows: e.dma_start(out=SC["so"][rows], in_=to[o][:]),
                             reads=[b_to[o]], writes=[DB("so")[n]], dma=True)
                    else:
                        g = rot("tg", 2)
                        boff = c0 - 3088
                        P.op("dve", lambda e, g=g, j=j, boff=boff: e.tensor_tensor(
                            out=tg[g][:], in0=pk[j][:], in1=gbias[:, boff:boff + 512], op=ALU.add),
                            reads=[b_pk[j], b_par], writes=[b_tg[g]])
                        o = rot("to", 3)
                        P.op("act", lambda e, o=o, g=g: e.activation(out=to[o][:], in_=tg[g][:], func=AF.Sigmoid),
                             reads=[b_tg[g]], writes=[b_to[o]])
                        dcol = boff % 1024
                        dst = SC["ga"] if kind == "ga" else SC["gb"]
                        P.op("sp", lambda e, o=o, rows=rows, dst=dst, dcol=dcol: e.dma_start(
                            out=dst[rows, dcol:dcol + 512], in_=to[o][:]),
                            reads=[b_to[o]], writes=[DB("gab")[n]], dma=True)
        P.flush()


def mix_pass(P, nc, C, I, SC, DB):
    with contextlib.ExitStack() as st:
        sb = lambda name, shape, dt: st.enter_context(nc.sbuf_tensor("mx" + name, shape, dt))
        ps = lambda name, shape, dt: st.enter_context(nc.psum_tensor("mx" + name, shape, dt))
        mask01 = sb("mask01", [128, 128], F32)
        trim = sb("trim", [128, 128], BF16)
        trimf = sb("trimf", [128, 128], F32)
        onesr = sb("onesr", [1, 65], F32)
        gln = sb("gln", [128, 512], F32)
        b_c = P.buf("mxconst")
        P.op("pool", lambda e: e.memset(mask01[:], 1.0), writes=[b_c])
        P.op("pool", lambda e: e.affine_select(out=mask01[:], in_=mask01[:], pattern=[[1, 128]], compare_op=ALU.is_ge,
                                                 fill=0.0, base=0, channel_multiplier=-1), reads=[b_c], writes=[b_c])
        P.op("pool", lambda e: e.memset(trimf[:], 0.0), reads=[b_c], writes=[b_c])
        P.op("pool", lambda e: e.affine_select(out=trimf[:], in_=trimf[:], pattern=[[1, 128]], compare_op=ALU.is_ge,
                                                 fill=-30000.0, base=0, channel_multiplier=-1), reads=[b_c], writes=[b_c])
        P.op("dve", lambda e: e.tensor_copy(out=trim[:], in_=trimf[:]), reads=[b_c], writes=[b_c])
        P.op("dve", lambda e: e.memset(onesr[:], 1.0), reads=[b_c], writes=[b_c])
        P.op("sp", lambda e: e.dma_start(out=gln[:], in_=I["mlstm_norm_g"].partition_broadcast(128)),
             reads=[b_c], writes=[b_c], dma=True)
        mqz = [sb("mqz%d" % i, [128, S], BF16) for i in range(4)]
        mk = [sb("mk%d" % i, [128, S], BF16) for i in range(2)]
        b_mqk = P.buf("mqk")
        for h in range(4):
            P.op("pool", lambda e, h=h: e.memset(mqz[h][:], 0.0), writes=[b_mqk])
        for h in range(4):
            R = slice((h % 2) * 64, (h % 2) * 64 + 64)
            P.op("sp", lambda e, h=h, R=R: e.dma_start(out=mqz[h][R, :], in_=SC["mqkT"][h // 2, R, :]), reads=DB("mqkT"),
                 writes=[b_mqk], dma=True)
        for hp in range(2):
            P.op("sp", lambda e, hp=hp: e.dma_start(out=mk[hp][:], in_=SC["mqkT"][2 + hp]), reads=DB("mqkT"),
                 writes=[b_mqk], dma=True)
        Cst = [sb("Cst%d" % i, [128, 129], F32) for i in range(2)]
        Cb = [sb("Cb%d" % i, [128, 129], BF16) for i in range(2)]
        b_Cst = P.bufs_n("Cst", 2)
        b_Cb = P.bufs_n("Cb", 2)
        va = [sb("va%d" % i, [128, 4, 129], BF16) for i in range(2)]
        b_va = P.bufs_n("va", 2)
        sgo = [sb("sgo%d" % i, [128, 512], BF16) for i in range(2)]
        b_sgo = P.bufs_n("sgo", 2)
        Sm = [sb("Sm%d" % i, [128, 2, 128], BF16) for i in range(2)]
        b_Sm = P.bufs_n("Sm", 2)
        ktm = [sb("ktm%d" % i, [128, 128], BF16) for i in range(2)]
        b_ktm = P.bufs_n("ktm", 2)
        bst = [sb("bst%d" % i, [128, 2, 6], F32) for i in range(2)]
        bag = [sb("bag%d" % i, [128, 2, 2], F32) for i in range(2)]
        sm = [sb("sm%d" % i, [128, 2, 4], F32) for i in range(2)]
        b_sm = P.bufs_n("msm", 2)
        Osb = [sb("Osb%d" % i, [128, 2, 129], F32) for i in range(2)]
        b_Osb = P.bufs_n("Osb", 2)
        sq = [sb("sq%d" % i, [128, 2, 1], F32) for i in range(2)]
        b_sq = P.bufs_n("msq", 2)
        hn = [sb("hn%d" % i, [128, 512], F32) for i in range(2)]
        b_hn = P.bufs_n("hn", 2)
        ya = [sb("ya%d" % i, [128, 512], BF16) for i in range(2)]
        b_ya = P.bufs_n("ya", 2)
        yaT = [sb("yaT%d" % i, [128, 4, 128], BF16) for i in range(2)]
        b_yaT = P.bufs_n("yaTs", 2)
        pSm = ps("pSm", [128, 2, 128], F32)
        pOm = ps("pOm", [128, 2, 129], F32)
        pU = ps("pU", [128, 2, 129], F32)
        pT5 = ps("pT5", [128, 5, 128], BF16)
        pkt = pT5[:, 4, :]
        pyT = pT5[:, 0:4, :]
        b_pSm, b_pOm, b_pU, b_pkt, b_pyT = [P.buf(n) for n in ("pSm", "pOm", "pU", "pkt", "pyT")]

        def mlstm_chunk(c):
            cols = slice(c * 128, (c + 1) * 128)
            rows = slice(c * 128, (c + 1) * 128)
            vi = c % 2
            P.op("sp", lambda e: e.dma_start(out=va[vi][:], in_=SC["vaugM"][rows]), reads=[DB("vaugM")[c]],
                 writes=[b_va[vi]], dma=True)
            P.op("sp", lambda e: e.dma_start(out=sgo[vi][:], in_=SC["so"][rows]), reads=[DB("so")[c]],
                 writes=[b_sgo[vi]], dma=True)
            hnb = hn[vi]
            for hp in range(2):
                si = hp
                for hh in range(2):
                    P.op("pe", lambda e, hp=hp, hh=hh: e.matmul(
                        pSm[:, hh, :], lhsT=mk[hp][:, cols], rhs=mqz[2 * hp + hh][:, cols], start=True, stop=True),
                        reads=[b_mqk], writes=[b_pSm])
                P.op("pe", lambda e, hp=hp: e.transpose(out=pkt, in_=mk[hp][:, cols], identity=C.ident[:]),
                     reads=[b_mqk, C.b_const], writes=[b_pkt])
                P.op("dve", lambda e, si=si: e.scalar_tensor_tensor(
                    out=Sm[si][:], in0=pSm[:], scalar=0.125,
                    in1=mask01[:].unsqueeze(1).to_broadcast([128, 2, 128]), op0=ALU.mult, op1=ALU.mult),
                    reads=[b_pSm, b_c], writes=[b_Sm[si]])
                P.op("act", lambda e, si=si: e.activation(out=ktm[si][:], in_=pkt, func=AF.Copy, scale=0.125),
                     reads=[b_pkt], writes=[b_ktm[si]])
                yield
            for hp in range(2):
                si = hp
                for hh in range(2):
                    h = 2 * hp + hh
                    P.op("pe", lambda e, si=si, hh=hh, h=h: e.matmul(
                        pOm[:, hh, :], lhsT=Sm[si][:, hh, :], rhs=va[vi][:, h, :], start=True, stop=(c == 0)),
                        reads=[b_Sm[si], b_va[vi]], writes=[b_pOm])
                    if c > 0:
                        P.op("pe", lambda e, hp=hp, hh=hh, h=h: e.matmul(
                            pOm[:, hh, :], lhsT=mqz[h][:, cols], rhs=Cb[hp][:, :], start=False, stop=True),
                            reads=[b_mqk, b_Cb[hp]], writes=[b_pOm])
                osp, bos = Osb[hp], b_Osb[hp]
                P.op("act", lambda e, osp=osp: e.activation(out=osp[:], in_=pOm[:], func=AF.Copy),
                     reads=[b_pOm], writes=[bos])
                for hh in range(2):
                    h = 2 * hp + hh
                    P.op("pe", lambda e, si=si, hh=hh, h=h: e.matmul(
                        pU[:, hh, :], lhsT=ktm[si][:], rhs=va[vi][:, h, :], start=True, stop=True),
                        reads=[b_ktm[si], b_va[vi]], writes=[b_pU])
                for hh in range(2):
                    h = 2 * hp + hh
                    R = slice(hh * 64, (hh + 1) * 64)
                    if c == 0:
                        P.op("dve", lambda e, hp=hp, hh=hh, R=R: e.tensor_copy(out=Cst[hp][R, :], in_=pU[R, hh, :]),
                             reads=[b_pU], writes=[b_Cst[hp]])
                    else:
                        P.op("dve", lambda e, hp=hp, hh=hh, R=R, h=h: e.scalar_tensor_tensor(
                            out=Cst[hp][R, :], in0=Cst[hp][R, :], scalar=C.decbc[R, h, c:c + 1], in1=pU[R, hh, :],
                            op0=ALU.mult, op1=ALU.add), reads=[b_pU, b_Cst[hp], C.b_decbc], writes=[b_Cst[hp]])
                    if c < 31:
                        P.op("dve", lambda e, hp=hp, R=R, h=h: e.tensor_scalar(
                            out=Cb[hp][R, :], in0=Cst[hp][R, :], scalar1=C.decbc[R, h, c + 1:c + 2], scalar2=None,
                            op0=ALU.mult), reads=[b_Cst[hp], C.b_decbc], writes=[b_Cb[hp]])
                smp, bstp, bagp, bsm = sm[hp], bst[hp], bag[hp], b_sm[hp]
                for hh in range(2):
                    P.op("dve", lambda e, hh=hh, bstp=bstp, hp=hp: e.bn_stats(out=bstp[:, hh, :], in_=Osb[hp][:, hh, 0:128]),
                         reads=[bos], writes=[bsm])
                    P.op("dve", lambda e, hh=hh, bstp=bstp, bagp=bagp: e.bn_aggr(out=bagp[:, hh, :], in_=bstp[:, hh, :]),
                         reads=[bsm], writes=[bsm])
                sqp, bsq = sq[hp], b_sq[hp]
                P.op("act", lambda e, sqp=sqp, osp=osp: e.activation(out=sqp[:], in_=osp[:, :, 128:129], func=AF.Square),
                     reads=[bos], writes=[bsq])
                P.op("dve", lambda e, hp=hp, smp=smp, sqp=sqp: e.tensor_tensor(
                    out=smp[:, :, 0:1], in0=sqp[:], in1=C.wthr[:, c, 4 + 2 * hp:6 + 2 * hp].unsqueeze(2),
                    op=ALU.max), reads=[C.b_wthr, bsm, bsq], writes=[bsm])
                P.op("dve", lambda e, smp=smp, bagp=bagp: e.scalar_tensor_tensor(
                    out=smp[:, :, 1:2], in0=smp[:, :, 0:1], scalar=EPS, in1=bagp[:, :, 1:2], op0=ALU.mult, op1=ALU.add),
                    reads=[bsm], writes=[bsm])
                P.op("pool", lambda e, smp=smp: e.tensor_tensor(
                    out=smp[:, :, 2:3], in0=smp[:, :, 1:2],
                    in1=C.neghalf[:].unsqueeze(1).to_broadcast([128, 2, 1]), op=ALU.pow),
                    reads=[bsm, C.b_const], writes=[bsm])
                for hh in range(2):
                    h = 2 * hp + hh
                    P.op("dve", lambda e, hh=hh, h=h, smp=smp, bagp=bagp, osp=osp: e.tensor_scalar(
                        out=hnb[:, h * 128:(h + 1) * 128], in0=osp[:, hh, 0:128], scalar1=bagp[:, hh, 0:1],
                        scalar2=smp[:, hh, 2:3], op0=ALU.subtract, op1=ALU.mult),
                        reads=[bos, bsm], writes=[b_hn[vi]])
                yield
            yi = c % 2
            P.op("pool", lambda e: e.tensor_tensor(out=hnb[:], in0=hnb[:], in1=gln[:], op=ALU.mult),
                 reads=[b_hn[vi], b_c], writes=[b_hn[vi]])
            P.op("dve", lambda e: e.tensor_tensor(out=ya[yi][:], in0=hnb[:], in1=sgo[vi][:], op=ALU.mult),
                 reads=[b_hn[vi], b_sgo[vi]], writes=[b_ya[yi]])
            yield
            for k in range(4):
                P.op("pe", lambda e, k=k: e.transpose(out=pyT[:, k, :], in_=ya[yi][:, k * 128:(k + 1) * 128],
                                                      identity=C.ident[:]),
                     reads=[b_ya[yi], C.b_const], writes=[b_pyT])
            P.op("act", lambda e: e.activation(out=yaT[yi][:], in_=pyT, func=AF.Copy), reads=[b_pyT],
                 writes=[b_yaT[yi]])
            P.op("sp", lambda e: e.dma_start(out=SC["yaT"][:, :, cols].rearrange("k p t -> p k t"), in_=yaT[yi][:]),
                 reads=[b_yaT[yi]], writes=[DB("yaT")[c]], dma=True)
            yield

        def mlstm_gen():
            for c in range(32):
                yield from mlstm_chunk(c)

        VF = sb("VF", [128, 32, 8 * 65], BF16)
        b_VF = P.buf("VF")
        P.op("sp", lambda e: e.dma_start(out=VF[:], in_=SC["vaugF"].rearrange("(j p) h e -> p j (h e)", p=128)),
             reads=DB("vaugF"), writes=[b_VF], dma=True)
        QA = [sb("QA%d" % i, [70, S], BF16) for i in range(2)]
        KA = [sb("KA%d" % i, [70, S], BF16) for i in range(2)]
        b_QA = P.bufs_n("QA", 2)
        b_KA = P.bufs_n("KA", 2)
        PT = [sb("PT%d" % i, [128, 512], BF16) for i in range(3)]
        b_PT = P.bufs_n("PT", 3)
        rec = [sb("rec%d" % i, [1, 512], F32) for i in range(2)]
        b_rec = P.bufs_n("rec", 2)
        b_recd = P.bufs_n("recd", 2)
        osb = [sb("osb%d" % i, [65, 512], F32) for i in range(2)]
        b_osb = P.bufs_n("osb", 2)
        bcs = [sb("bcs%d" % i, [65, 512], F32) for i in range(2)]
        b_bcs = P.bufs_n("bcs", 2)
        ybt = [sb("ybt%d" % i, [65, 512], BF16) for i in range(2)]
        b_ybt = P.bufs_n("ybt", 2)
        pS = [ps("pS%d" % i, [128, 512], F32) for i in range(3)]
        b_pS = P.bufs_n("pS", 3)
        slot_of = {}
        pO = ps("pO", [128, 512], F32)
        b_pO = P.buf("pO")
        cnt = {"s": 0, "y": 0}

        def fox_load(h):
            hb = h % 2
            P.op("sp", lambda e: e.dma_start(out=QA[hb][:], in_=SC["qaug"][h]), reads=DB("aug"), writes=[b_QA[hb]], dma=True)
            P.op("sp", lambda e: e.dma_start(out=KA[hb][:], in_=SC["kaug"][h]), reads=DB("aug"), writes=[b_KA[hb]], dma=True)

        seq = [(h, i, j) for h in range(8) for i in range(8) for j in range(4 * i + 4)]

        def emit_S(idx):
            h, i, j = seq[idx]
            hb = h % 2
            sj = cnt["s"] % 3
            cnt["s"] += 1
            slot_of[idx] = sj
            jj = j - 4 * i
            kc = slice(j * 128, (j + 1) * 128)
            rd = [b_KA[hb], b_QA[hb]]
            if jj < 0:
                P.op("pe", lambda e: e.matmul(
                    pS[sj][:, 0:512], lhsT=KA[hb][:, kc], rhs=QA[hb][:, i * 512:(i + 1) * 512], start=True, stop=True),
                    reads=rd, writes=[b_pS[sj]])
            else:
                qs = jj * 128
                wq = 512 - qs
                q0 = i * 512 + qs
                P.op("pe", lambda e: e.matmul(pS[sj][:, 0:128], lhsT=C.ident[:], rhs=trim[:], start=True, stop=False),
                     reads=[C.b_const, b_c], writes=[b_pS[sj]])
                P.op("pe", lambda e: e.matmul(
                    pS[sj][:, 0:128], lhsT=KA[hb][:, kc], rhs=QA[hb][:, q0:q0 + 128], start=False, stop=True),
                    reads=rd, writes=[b_pS[sj]])
                if wq > 128:
                    P.op("pe", lambda e: e.matmul(
                        pS[sj][:, 128:wq], lhsT=KA[hb][:, kc], rhs=QA[hb][:, q0 + 128:q0 + wq], start=True, stop=True),
                        reads=rd, writes=[b_pS[sj]])

        def emit_rest(idx):
            h, i, j = seq[idx]
            sj = slot_of[idx]
            nkb = 4 * i + 4
            jj = j - 4 * i
            qs = max(jj, 0) * 128
            wq = 512 - qs
            tj = idx % 3
            P.op("act", lambda e: e.activation(out=PT[tj][:, 0:wq], in_=pS[sj][:, 0:wq], func=AF.Exp),
                 reads=[b_pS[sj]], writes=[b_PT[tj]])
            P.op("pe", lambda e: e.matmul(
                pO[0:65, qs:512], lhsT=VF[:, j, h * 65:(h + 1) * 65], rhs=PT[tj][:, 0:wq],
                start=(j == 0), stop=(j == nkb - 1)),
                reads=[b_VF, b_PT[tj]], writes=[b_pO])
            if j < nkb - 1:
                return
            yi = cnt["y"] % 2
            cnt["y"] += 1
            u = h * 8 + i
            P.op("act", lambda e: e.activation(out=osb[yi][:], in_=pO[0:65, :], func=AF.Copy), reads=[b_pO],
                 writes=[b_osb[yi]])
            P.op("dve", lambda e: e.reciprocal(out=rec[yi][0:1, :], in_=osb[yi][0:1, :]), reads=[b_osb[yi]],
                 writes=[b_rec[yi]])
            P.op("sp", lambda e: e.dma_start(out=SC["recd"][u:u + 1, :], in_=rec[yi][0:1, :]), reads=[b_rec[yi]],
                 writes=[b_recd[yi]], dma=True)
            P.op("sp", lambda e: e.dma_start(out=bcs[yi][:], in_=SC["recd"][u:u + 1, :].partition_broadcast(65)),
                 reads=[b_recd[yi]], writes=[b_bcs[yi]], dma=True)
            P.op("dve", lambda e: e.tensor_tensor(out=ybt[yi][:], in0=osb[yi][:], in1=bcs[yi][:], op=ALU.mult),
                 reads=[b_osb[yi], b_bcs[yi]], writes=[b_ybt[yi]])
            P.op("sp", lambda e: e.dma_start(
                out=SC["ybT"][h // 2, (h % 2) * 64:(h % 2) * 64 + 64, i * 512:(i + 1) * 512], in_=ybt[yi][1:65, :]),
                reads=[b_ybt[yi]], writes=[DB("ybT")[(h * 8 + i) % 32]], dma=True)

        gen = mlstm_gen()
        fox_load(0)
        emit_S(0)
        emit_S(1)
        for idx, (h, i, j) in enumerate(seq):
            if i == 0 and j == 0 and h + 1 < 8:
                fox_load(h + 1)
            if idx + 2 < len(seq):
                emit_S(idx + 2)
            emit_rest(idx)
            if idx % 6 == 5:
                next(gen, None)
        for _ in gen:
            pass
        P.flush()


def merge_pass(P, nc, C, I, SC, DB, h1, h1_b, h2, h2_b):
    with contextlib.ExitStack() as st:
        sb = lambda name, shape, dt: st.enter_context(nc.sbuf_tensor("mg" + name, shape, dt))
        ps = lambda name, shape, dt: st.enter_context(nc.psum_tensor("mg" + name, shape, dt))
        Wa = sb("Wa", [128, 4, D], BF16)
        Wb = sb("Wb", [128, 4, D], BF16)
        Wo = sb("Wo", [128, 8, D], BF16)
        b_W = P.buf("mgW")
        P.op("pool", lambda e: e.dma_start(out=Wa[:], in_=I["w_branch_a"].rearrange("(k p) d -> p k d", p=128)),
             writes=[b_W], dma=True)
        P.op("pool", lambda e: e.dma_start(out=Wb[:], in_=I["w_branch_b"].rearrange("(k p) d -> p k d", p=128)),
             writes=[b_W], dma=True)
        P.op("pool", lambda e: e.dma_start(out=Wo[:], in_=I["w_out"].rearrange("(k p) d -> p k d", p=128)),
             writes=[b_W], dma=True)
        gpost = sb("gpost", [128, D], F32)
        P.op("sp", lambda e: e.dma_start(out=gpost[:], in_=I["mix_post_g"].partition_broadcast(128)),
             writes=[b_W], dma=True)
        yaT = [sb("yaT%d" % i, [128, 4, 128], BF16) for i in range(2)]
        ybT = [sb("ybT%d" % i, [128, 4, 128], BF16) for i in range(2)]
        gab = [sb("gab%d" % i, [128, 2, D], BF16) for i in range(2)]
        hin = [sb("hin%d" % i, [128, D], F32) for i in range(4)]
        b_in = P.bufs_n("mgin", 2)
        b_inb = P.bufs_n("mginb", 2)
        b_ga = P.bufs_n("mgga", 2)
        b_gb = P.bufs_n("mggb", 2)
        b_hin = P.bufs_n("mghin", 4)
        t1 = [sb("t1%d" % i, [128, D], F32) for i in range(2)]
        t2 = [sb("t2%d" % i, [128, D], F32) for i in range(2)]
        mb = [sb("mb%d" % i, [128, D], BF16) for i in range(2)]
        mT = [sb("mT%d" % i, [128, 8, 128], BF16) for i in range(2)]
        junk = sb("junk", [128, D], BF16)
        hout = [sb("hout%d" % i, [128, D], F32) for i in range(2)]
        stat = sb("stat", [128, 6], F32)
        b_t1, b_t2, b_mb, b_mT = [P.bufs_n(n, 2) for n in ("t1", "t2", "mb", "mT")]
        b_junk = P.buf("mgjunk")
        b_hout = P.bufs_n("hout", 2)
        b_stat = P.bufs_n("mgstat", 2)
        pA = ps("pA", [128, D], F32)
        pB = ps("pB", [128, D], F32)
        pO = ps("pO", [128, D], F32)
        pt = ps("pt", [128, 8, 128], BF16)
        b_pA, b_pB, b_pO, b_pt = [P.buf(n) for n in ("pA", "pB", "mgpO", "mgpt")]
        h1v = h1.rearrange("(n p) d -> n p d", p=128)
        h2v = h2.rearrange("(n p) d -> n p d", p=128)

        def s1(n):
            ib = n % 2
            rows = slice(n * 128, (n + 1) * 128)
            cols = rows
            P.op("sp", lambda e: e.dma_start(out=yaT[ib][:], in_=SC["yaT"][:, :, cols].rearrange("k p t -> p k t")),
                 reads=[DB("yaT")[n]], writes=[b_in[ib]], dma=True)
            P.op("sp", lambda e: e.dma_start(out=ybT[ib][:], in_=SC["ybT"][:, :, cols].rearrange("k p t -> p k t")),
                 reads=DB("ybT"), writes=[b_inb[ib]], dma=True)
            P.op("sp", lambda e: e.dma_start(out=gab[ib][:, 0, :], in_=SC["ga"][rows]),
                 reads=[DB("gab")[n]], writes=[b_ga[ib]], dma=True)
            P.op("sp", lambda e: e.dma_start(out=gab[ib][:, 1, :], in_=SC["gb"][rows]),
                 reads=[DB("gab")[n]], writes=[b_gb[ib]], dma=True)
            P.op("sp", lambda e: e.dma_start(out=hin[n % 4][:], in_=h1v[n]),
                 reads=[h1_b[n]], writes=[b_hin[n % 4]], dma=True)
            for hf in range(2):
                hs = slice(hf * 512, (hf + 1) * 512)
                for k in range(4):
                    P.op("pe", lambda e, k=k, hs=hs: e.matmul(pA[:, hs], lhsT=yaT[ib][:, k, :], rhs=Wa[:, k, hs],
                                                              start=(k == 0), stop=(k == 3)),
                         reads=[b_in[ib], b_W], writes=[b_pA])
            for hf in range(2):
                hs = slice(hf * 512, (hf + 1) * 512)
                for k in range(4):
                    P.op("pe", lambda e, k=k, hs=hs: e.matmul(pB[:, hs], lhsT=ybT[ib][:, k, :], rhs=Wb[:, k, hs],
                                                              start=(k == 0), stop=(k == 3)),
                         reads=[b_inb[ib], b_W], writes=[b_pB])
            P.op("dve", lambda e: e.tensor_tensor(out=t1[ib][:], in0=pA[:], in1=gab[ib][:, 0, :], op=ALU.mult),
                 reads=[b_pA, b_ga[ib]], writes=[b_t1[ib]])
            P.op("dve", lambda e: e.tensor_tensor(out=t2[ib][:], in0=pB[:], in1=gab[ib][:, 1, :], op=ALU.mult),
                 reads=[b_pB, b_gb[ib]], writes=[b_t2[ib]])
            P.op("pool", lambda e: e.tensor_tensor(out=mb[ib][:], in0=t1[ib][:], in1=t2[ib][:], op=ALU.add),
                 reads=[b_t1[ib], b_t2[ib]], writes=[b_mb[ib]])

        def s2(n):
            ib = n % 2
            for k in range(8):
                P.op("pe", lambda e, k=k: e.transpose(out=pt[:, k, :], in_=mb[ib][:, k * 128:(k + 1) * 128],
                                                      identity=C.ident[:]),
                     reads=[b_mb[ib], C.b_const], writes=[b_pt])
            P.op("act", lambda e: e.activation(out=mT[ib][:], in_=pt[:], func=AF.Copy), reads=[b_pt], writes=[b_mT[ib]])
            for hf in range(2):
                hs = slice(hf * 512, (hf + 1) * 512)
                for k in range(8):
                    P.op("pe", lambda e, k=k, hs=hs: e.matmul(pO[:, hs], lhsT=mT[ib][:, k, :], rhs=Wo[:, k, hs],
                                                              start=(k == 0), stop=(k == 7)),
                         reads=[b_mT[ib], b_W], writes=[b_pO])
            si = n % 2
            ss, var, rstd = (stat[:, 3 * si + j:3 * si + j + 1] for j in range(3))
            rms_stats(P, C, pO[:], b_pO, junk[:], b_junk, ss, var, rstd, b_stat[si])
            P.op("dve", lambda e: e.scalar_tensor_tensor(
                out=hout[ib][:], in0=pO[:], scalar=rstd, in1=gpost[:], op0=ALU.mult, op1=ALU.mult),
                reads=[b_pO, b_stat[si], b_W], writes=[b_hout[ib]])
            P.op("pool", lambda e: e.tensor_tensor(out=hout[ib][:], in0=hout[ib][:], in1=hin[n % 4][:], op=ALU.add),
                 reads=[b_hout[ib], b_hin[n % 4]], writes=[b_hout[ib]])
            P.op("pool", lambda e: e.dma_start(out=h2v[n], in_=hout[ib][:]),
                 reads=[b_hout[ib]], writes=[h2_b[n]], dma=True)

        s1(0)
        for n in range(32):
            if n + 1 < 32:
                s1(n + 1)
            s2(n)
        P.flush()


def ple_pass(P, nc, C, I, uT3, uT3_b, h3, h3_b, out):
    with contextlib.ExitStack() as st:
        sb = lambda name, shape, dt: st.enter_context(nc.sbuf_tensor("pl" + name, shape, dt))
        ps = lambda name, shape, dt: st.enter_context(nc.psum_tensor("pl" + name, shape, dt))
        Wg = sb("Wg", [128, 8, D], BF16)
        Wp = sb("Wp", [128, 2, D], BF16)
        b_W = P.buf("plW")
        P.op("pool", lambda e: e.dma_start(out=Wg[:], in_=I["ple_w_gate"].rearrange("(k p) d -> p k d", p=128)),
             writes=[b_W], dma=True)
        P.op("pool", lambda e: e.dma_start(out=Wp[:], in_=I["ple_w_proj"].rearrange("(k p) d -> p k d", p=128)),
             writes=[b_W], dma=True)
        gpost = sb("gpost", [128, D], F32)
        bg = sb("bg", [128, D], F32)
        P.op("sp", lambda e: e.dma_start(out=gpost[:], in_=I["ple_post_g"].partition_broadcast(128)),
             writes=[b_W], dma=True)
        P.op("sp", lambda e: e.dma_start(out=bg[:], in_=I["ple_b_gate"].partition_broadcast(128)),
             writes=[b_W], dma=True)
        uT = [sb("uT%d" % i, [128, 8, 128], BF16) for i in range(2)]
        pin = [sb("pin%d" % i, [128, 256], F32) for i in range(2)]
        hin = [sb("hin%d" % i, [128, D], F32) for i in range(4)]
        b_in = P.bufs_n("plin", 2)
        b_pin = P.bufs_n("plpin", 2)
        b_hin = P.bufs_n("plhin", 4)
        pb = [sb("pb%d" % i, [128, 256], BF16) for i in range(2)]
        pT = [sb("pT%d" % i, [128, 2, 128], BF16) for i in range(2)]
        gt = [sb("gt%d" % i, [128, D], F32) for i in range(2)]
        ge = [sb("ge%d" % i, [128, D], F32) for i in range(2)]
        junk = sb("junk", [128, D], BF16)
        hout = [sb("hout%d" % i, [128, D], F32) for i in range(2)]
        stat = sb("stat", [128, 6], F32)
        b_pb, b_pT, b_gt, b_ge = [P.bufs_n(n, 2) for n in ("pb", "pT", "gt", "ge")]
        b_junk = P.buf("pljunk")
        b_hout = P.bufs_n("plhout", 2)
        b_stat = P.bufs_n("plstat", 2)
        pG = [ps("pG%d" % i, [128, D], F32) for i in range(2)]
        pE = ps("pE", [128, D], F32)
        ptp = ps("ptp", [128, 2, 128], BF16)
        b_pG = P.bufs_n("pG", 2)
        b_pE, b_ptp = P.buf("pE"), P.buf("ptp")
        pv = I["p"].rearrange("(n p) d -> n p d", p=128)
        h3v = h3.rearrange("(n p) d -> n p d", p=128)
        ov = out.rearrange("(n p) d -> n p d", p=128)

        def s1(n):
            ib = n % 2
            cols = slice(n * 128, (n + 1) * 128)
            P.op("sp", lambda e: e.dma_start(out=uT[ib][:], in_=uT3[:, :, cols].rearrange("k p t -> p k t")),
                 reads=[uT3_b[n]], writes=[b_in[ib]], dma=True)
            P.op("sp", lambda e: e.dma_start(out=pin[ib][:], in_=pv[n]), writes=[b_pin[ib]], dma=True)
            P.op("sp", lambda e: e.dma_start(out=hin[n % 4][:], in_=h3v[n]), reads=[h3_b[n]],
                 writes=[b_hin[n % 4]], dma=True)
            P.op("act", lambda e: e.activation(out=pb[ib][:], in_=pin[ib][:], func=AF.Copy), reads=[b_pin[ib]],
                 writes=[b_pb[ib]])
            for k in range(2):
                P.op("pe", lambda e, k=k: e.transpose(out=ptp[:, k, :], in_=pb[ib][:, k * 128:(k + 1) * 128],
                                                      identity=C.ident[:]),
                     reads=[b_pb[ib], C.b_const], writes=[b_ptp])
            P.op("dve", lambda e: e.tensor_copy(out=pT[ib][:], in_=ptp[:]), reads=[b_ptp], writes=[b_pT[ib]])
            for hf in range(2):
                hs = slice(hf * 512, (hf + 1) * 512)
                for k in range(8):
                    P.op("pe", lambda e, k=k, hs=hs: e.matmul(pG[ib][:, hs], lhsT=uT[ib][:, k, :], rhs=Wg[:, k, hs],
                                                              start=(k == 0), stop=(k == 7)),
                         reads=[b_in[ib], b_W], writes=[b_pG[ib]])

        def s2(n):
            ib = n % 2
            for hf in range(2):
                hs = slice(hf * 512, (hf + 1) * 512)
                for k in range(2):
                    P.op("pe", lambda e, k=k, hs=hs: e.matmul(pE[:, hs], lhsT=pT[ib][:, k, :], rhs=Wp[:, k, hs],
                                                              start=(k == 0), stop=(k == 1)),
                         reads=[b_pT[ib], b_W], writes=[b_pE])
            P.op("dve", lambda e: e.tensor_tensor(out=gt[ib][:], in0=pG[ib][:], in1=bg[:], op=ALU.add),
                 reads=[b_pG[ib], b_W], writes=[b_gt[ib]])
            P.op("act", lambda e: e.activation(out=gt[ib][:], in_=gt[ib][:], func=AF.Sigmoid), reads=[b_gt[ib]],
                 writes=[b_gt[ib]])
            P.op("dve", lambda e: e.tensor_tensor(out=ge[ib][:], in0=gt[ib][:], in1=pE[:], op=ALU.mult),
                 reads=[b_gt[ib], b_pE], writes=[b_ge[ib]])
            si = n % 2
            ss, var, rstd = (stat[:, 3 * si + j:3 * si + j + 1] for j in range(3))
            rms_stats(P, C, ge[ib][:], b_ge[ib], junk[:], b_junk, ss, var, rstd, b_stat[si])
            P.op("dve", lambda e: e.scalar_tensor_tensor(
                out=hout[ib][:], in0=ge[ib][:], scalar=rstd, in1=gpost[:], op0=ALU.mult, op1=ALU.mult),
                reads=[b_ge[ib], b_stat[si], b_W], writes=[b_hout[ib]])
            P.op("pool", lambda e: e.tensor_tensor(out=hout[ib][:], in0=hout[ib][:], in1=hin[n % 4][:], op=ALU.add),
                 reads=[b_hout[ib], b_hin[n % 4]], writes=[b_hout[ib]])
            P.op("pool", lambda e: e.dma_start(out=ov[n], in_=hout[ib][:]), reads=[b_hout[ib]], dma=True)

        s1(0)
        for n in range(32):
            if n + 1 < 32:
                s1(n + 1)
            s2(n)
        P.flush()


def build_program(debug=False, stage=99, only=None):
    nc = bass.Bass("TRN2", target_bir_lowering=False)
    I = {}

    def din(name, shape):
        I[name] = nc.dram_tensor(name, shape, F32, kind="ExternalInput").ap()
        return I[name]

    din("x", [S, D])
    din("p", [S, 256])
    for nm in ("ffn1", "ffn2"):
        din(nm + "_pre_g", [1, D])
        din(nm + "_w_gate", [D, DFF])
        din(nm + "_w_up", [D, DFF])
        din(nm + "_w_down", [DFF, D])
        din(nm + "_post_g", [1, D])
    din("mix_pre_g", [1, D])
    din("w_in", [D, INW])
    din("conv_w", [4, 512])
    din("conv_b", [1, 512])
    din("mlstm_i_bias", [1, 4])
    din("mlstm_f_bias", [1, 4])
    din("mlstm_norm_g", [1, 512])
    din("fox_f_bias", [1, 8])
    din("branch_gate_bias", [1, 2048])
    din("w_branch_a", [512, D])
    din("w_branch_b", [512, D])
    din("w_out", [D, D])
    din("mix_post_g", [1, D])
    din("ple_pre_g", [1, D])
    din("ple_w_gate", [D, D])
    din("ple_b_gate", [1, D])
    din("ple_w_proj", [256, D])
    din("ple_post_g", [1, D])

    skind = "ExternalOutput" if debug else "Internal"

    def dscr(name, shape, dt):
        return nc.dram_tensor(name, shape, dt, kind=skind).ap()

    out = nc.dram_tensor("out", [S, D], F32, kind="ExternalOutput").ap()
    h1 = dscr("h1", [S, D], F32)
    uT1 = dscr("uT1", [8, 128, S], BF16)
    gpre = dscr("gpre", [72, S], F32)
    SC = {
        "mqkT": dscr("mqkT", [4, 128, S], BF16),
        "vaugM": dscr("vaugM", [S, 4, 129], BF16),
        "so": dscr("so", [S, 512], BF16),
        "qaug": dscr("qaug", [8, 70, S], BF16),
        "kaug": dscr("kaug", [8, 70, S], BF16),
        "vaugF": dscr("vaugF", [S, 8, 65], BF16),
        "ga": dscr("ga", [S, D], BF16),
        "gb": dscr("gb", [S, D], BF16),
        "yaT": dscr("yaT", [4, 128, S], BF16),
        "ybT": dscr("ybT", [4, 128, S], BF16),
        "recd": dscr("recd", [64, 512], F32),
    }
    h2 = dscr("h2", [S, D], F32)
    h3 = dscr("h3", [S, D], F32)
    uT3 = dscr("uT3", [8, 128, S], BF16)

    with contextlib.ExitStack() as st:
        P = Prog(nc, st)
        C = Ctx()
        C.db = {}

        def db(name):
            if name not in C.db:
                C.db[name] = P.bufs_n("D" + name, 32)
            return C.db[name]

        setup_consts(P, nc, st, C)
        C.wthr = st.enter_context(nc.sbuf_tensor("wthr", [128, 32, 8], F32))
        C.decbc = st.enter_context(nc.sbuf_tensor("decbc", [128, 4, 32], F32))
        C.b_wthr = P.buf("wthr")
        C.b_decbc = P.buf("decbc")
        def want(name, st_no):
            return (name in only) if only is not None else (stage >= st_no)

        if want("ffn1", 1):
            ffn_pass(P, nc, C, "f1", I["x"], I["ffn1_w_gate"], I["ffn1_w_up"], I["ffn1_w_down"],
                     I["ffn1_pre_g"], I["ffn1_post_g"], h1, I["mix_pre_g"], uT1,
                     db("x"), db("h1"), db("uT1"), gate_w=I["w_in"], gate_dst=gpre, gate_b=db("gpre"))
        if want("gp", 2):
            aug_b = P.buf("augrows")
            gp_stage(P, nc, C, I, gpre, db("gpre"), SC["qaug"], SC["kaug"], aug_b)
            db("aug").append(aug_b)
        if want("win", 2):
            win_pass(P, nc, C, I, uT1, db("uT1"), SC, db)
        if want("mix", 3):
            mix_pass(P, nc, C, I, SC, db)
        if want("merge", 4):
            merge_pass(P, nc, C, I, SC, db, h1, db("h1"), h2, db("h2"))
        if want("ffn2", 5):
            ffn_pass(P, nc, C, "f2", h2, I["ffn2_w_gate"], I["ffn2_w_up"], I["ffn2_w_down"],
                     I["ffn2_pre_g"], I["ffn2_post_g"], h3, I["ple_pre_g"], uT3,
                     db("h2"), db("h3"), db("uT3"))
        if want("ple", 6):
            ple_pass(P, nc, C, I, uT3, db("uT3"), h3, db("h3"), out)
        P.flush(final=True)
    return nc

IN_NAMES = ["x", "p", "ffn1_pre_g", "ffn1_w_gate", "ffn1_w_up", "ffn1_w_down", "ffn1_post_g",
            "mix_pre_g", "w_in", "conv_w", "conv_b", "mlstm_i_bias", "mlstm_f_bias", "mlstm_norm_g",
            "fox_f_bias", "branch_gate_bias", "w_branch_a", "w_branch_b", "w_out", "mix_post_g",
            "ffn2_pre_g", "ffn2_w_gate", "ffn2_w_up", "ffn2_w_down", "ffn2_post_g",
            "ple_pre_g", "ple_w_gate", "ple_b_gate", "ple_w_proj", "ple_post_g"]


def make_in_maps(inputs, cores):
    maps = []
    shared = {}
    for k in IN_NAMES:
        if k in ("x", "p"):
            continue
        shared[k] = np.ascontiguousarray(np.asarray(inputs[k])[0], dtype=np.float32)
    x = np.asarray(inputs["x"])
    p = np.asarray(inputs["p"])
    for b in cores:
        m = dict(shared)
        m["x"] = np.ascontiguousarray(x[b], dtype=np.float32)
        m["p"] = np.ascontiguousarray(p[0, b], dtype=np.float32)
        maps.append(m)
    return maps


def kernel(**inputs):
    nc = build_program()
    maps = make_in_maps(inputs, list(range(8)))
    res = run_bass_kernel_spmd(nc, maps, core_ids=list(range(8)))
    return np.stack([np.asarray(r["out"], dtype=np.float32) for r in res.results], axis=0)
```

```python
import contextlib
import numpy as np
import concourse.bass as bass
import concourse.mybir as mybir
from concourse.bass_utils import run_bass_kernel_spmd

F32 = mybir.dt.float32
BF16 = mybir.dt.bfloat16
AF = mybir.ActivationFunctionType
ALU = mybir.AluOpType
AX = mybir.AxisListType

S = 4096
D = 1024
DFF = 2816
NFC = DFF // 128
NT = 8
TT = 512
EPS = 1e-6
INW = 5136

ENGS = ("pe", "act", "dve", "pool", "sp")


class Buf:
    __slots__ = ("name", "w", "r")

    def __init__(self, name=""):
        self.name = name
        self.w = None
        self.r = []


class Op:
    __slots__ = ("eng", "fn", "deps", "inc", "cnt", "dma", "sem", "emitted")

    def __init__(self, eng, fn, dma=False):
        self.eng = eng
        self.fn = fn
        self.deps = []
        self.inc = False
        self.cnt = 0
        self.dma = dma
        self.sem = None
        self.emitted = False


class Prog:
    def __init__(self, nc, st, n_dma_sems=20):
        self.nc = nc
        self.pending = {e: [] for e in ENGS}
        self.bufs = []
        self.nd = n_dma_sems
        self.esem = {e: st.enter_context(nc.semaphore("s_" + e)) for e in ENGS}
        self.dsem = {}
        for e in ("sp", "pool"):
            for s in range(n_dma_sems):
                self.dsem[(e, s)] = st.enter_context(nc.semaphore("d_%s_%d" % (e, s)))
        self.ecnt = {e: 0 for e in ENGS}
        self.dcnt = {e: 0 for e in ENGS}
        self.waited = {e: {} for e in ENGS}
        self.n_ops = 0

    def buf(self, name=""):
        b = Buf(name)
        self.bufs.append(b)
        return b

    def bufs_n(self, name, n):
        return [self.buf("%s%d" % (name, i)) for i in range(n)]

    def op(self, eng, fn, reads=(), writes=(), dma=False):
        o = Op(eng, fn, dma)
        seen = set()
        cand = []
        for b in reads:
            if b.w is not None:
                cand.append(b.w)
        for b in writes:
            if b.w is not None:
                cand.append(b.w)
            cand.extend(b.r)
        for d in cand:
            if d is o or id(d) in seen:
                continue
            seen.add(id(d))
            if d.eng == "pe" and eng == "pe" and not d.dma and not dma:
                continue
            o.deps.append(d)
            if not d.emitted:
                d.inc = True
        for b in reads:
            b.r.append(o)
        for b in writes:
            b.w = o
            b.r = []
        self.pending[eng].append(o)
        self.n_ops += 1
        return o

    def flush(self, final=False):
        nc = self.nc
        for b in self.bufs:
            if b.w is not None and not b.w.emitted:
                b.w.inc = True
            for r in b.r:
                if not r.emitted:
                    r.inc = True
        for e in ENGS:
            for o in self.pending[e]:
                if o.dma:
                    k = self.dcnt[e]
                    o.sem = (e, k % self.nd)
                    o.cnt = 16 * (k // self.nd + 1)
                    self.dcnt[e] = k + 1
                elif o.inc:
                    self.ecnt[e] += 1
                    o.cnt = self.ecnt[e]
        pending = self.pending
        self.pending = {e: [] for e in ENGS}

        def run(ename, eng):
            waited = self.waited[ename]
            for o in pending[ename]:
                for d in o.deps:
                    key = d.sem if d.dma else d.eng
                    if waited.get(key, 0) >= d.cnt:
                        continue
                    assert d.cnt > 0, (d.eng, ename)
                    eng.wait_ge(self.dsem[key] if d.dma else self.esem[key], d.cnt)
                    waited[key] = d.cnt
                if o.dma:
                    if o.cnt > 16 and waited.get(o.sem, 0) < o.cnt - 16:
                        eng.wait_ge(self.dsem[o.sem], o.cnt - 16)
                        waited[o.sem] = o.cnt - 16
                    o.fn(eng).then_inc(self.dsem[o.sem], 16)
                else:
                    ins = o.fn(eng)
                    if o.inc:
                        ins.then_inc(self.esem[o.eng], 1)
                o.emitted = True
            if ename == "sp":
                for q in ("sp", "pool"):
                    k = self.dcnt[q]
                    for sl in range(min(self.nd, k)):
                        last = 16 * ((k - 1 - sl) // self.nd + 1)
                        eng.wait_ge(self.dsem[(q, sl)], last)

        with nc.Block() as block:
            @block.tensor
            def _(eng):
                run("pe", eng)

            @block.scalar
            def _(eng):
                run("act", eng)

            @block.vector
            def _(eng):
                run("dve", eng)

            @block.gpsimd
            def _(eng):
                run("pool", eng)

            @block.sync
            def _(eng):
                run("sp", eng)


def bcast_last(ap2d, n):
    return ap2d.unsqueeze(2).to_broadcast([ap2d.shape[0], ap2d.shape[1], n])


class Ctx:
    pass


def load_w_kmajor(P, nc, dst, src2d, n_kc, ncols, bufs, col_chunk=1408):
    v = src2d.rearrange("(kc p) n -> kc p n", p=128)
    mdl = 4 * col_chunk
    for k in range(n_kc):
        P.op("pool", lambda e, k=k: e.dma_start(out=dst[:, k, :], in_=v[k], max_dma_last_dim=mdl),
             writes=[bufs[k]], dma=True)


def setup_consts(P, nc, st, C):
    sb = lambda name, shape, dt: st.enter_context(nc.sbuf_tensor(name, shape, dt))
    C.identf = sb("identf", [128, 128], F32)
    C.ident = sb("ident", [128, 128], BF16)
    C.neghalf = sb("neghalf", [128, 1], F32)
    C.b_const = P.buf("const")
    identf, ident = C.identf, C.ident
    P.op("pool", lambda e: e.memset(identf[:], 0.0), writes=[C.b_const])
    P.op("pool", lambda e: e.affine_select(out=identf[:], in_=identf[:], pattern=[[-1, 128]],
                                             compare_op=ALU.not_equal, fill=1.0, base=0,
                                             channel_multiplier=1),
         reads=[C.b_const], writes=[C.b_const])
    P.op("dve", lambda e: e.tensor_copy(out=ident[:], in_=identf[:]), reads=[C.b_const], writes=[C.b_const])
    P.op("pool", lambda e: e.memset(C.neghalf[:], -0.5), reads=[C.b_const], writes=[C.b_const])


def rms_stats(P, C, src_ap, src_buf, junk, b_junk, ss, var, rstd, b_stat, n_feat=D):
    P.op("act", lambda e: e.activation(out=junk, in_=src_ap, func=AF.Square, accum_out=ss),
         reads=[src_buf], writes=[b_junk, b_stat])
    P.op("dve", lambda e: e.tensor_scalar(out=var, in0=ss, scalar1=1.0 / n_feat, scalar2=EPS,
                                          op0=ALU.mult, op1=ALU.add),
         reads=[b_stat], writes=[b_stat])
    P.op("pool", lambda e: e.tensor_tensor(out=rstd, in0=var, in1=C.neghalf[:], op=ALU.pow),
         reads=[b_stat, C.b_const], writes=[b_stat])


def ffn_pass(P, nc, C, tag, src_h, w_gate, w_up, w_down, pre_g, post_g, dst_h, next_g, dst_uT,
             src_b, dst_b, uT_b, gate_w=None, gate_dst=None, gate_b=None):
    with contextlib.ExitStack() as st:
        sb = lambda name, shape, dt: st.enter_context(nc.sbuf_tensor(tag + name, shape, dt))
        ps = lambda name, shape, dt: st.enter_context(nc.psum_tensor(tag + name, shape, dt))
        Wg = sb("Wg", [128, 8, DFF], BF16)
        Wu = sb("Wu", [128, 8, DFF], BF16)
        Wd = sb("Wd", [128, NFC, D], BF16)
        b_Wg = P.bufs_n("Wg", 16)
        b_Wu = P.bufs_n("Wu", 16)
        b_Wd = P.bufs_n("Wd", 2)
        HC = DFF // 2
        for half in range(2):
            cs = slice(half * HC, (half + 1) * HC)
            for (dst, src, bb) in ((Wg, w_gate, b_Wg), (Wu, w_up, b_Wu)):
                v = src.rearrange("(kc p) n -> kc p n", p=128)
                for k in range(8):
                    P.op("pool", lambda e, k=k, dst=dst, v=v, cs=cs: e.dma_start(out=dst[:, k, cs], in_=v[k][:, cs]),
                         writes=[bb[2 * k + half]], dma=True)
        wdv = w_down.rearrange("(fc p) d -> p fc d", p=128)
        for hh in range(2):
            P.op("pool", lambda e, hh=hh: e.dma_start(out=Wd[:, hh * 11:(hh + 1) * 11, :],
                                                       in_=wdv[:, hh * 11:(hh + 1) * 11, :]),
                 writes=[b_Wd[hh]], dma=True)
        gpre = sb("gpre", [128, 8], F32)
        gnext = sb("gnext", [128, 8], F32)
        gpost = sb("gpost", [128, D], F32)
        b_par = P.buf("par")
        P.op("sp", lambda e: e.dma_start(out=gpre[:], in_=pre_g.rearrange("o (k p) -> p (o k)", p=128),
                                         allow_slow_non_contiguous=True),
             writes=[b_par], dma=True)
        P.op("sp", lambda e: e.dma_start(out=gnext[:], in_=next_g.rearrange("o (k p) -> p (o k)", p=128),
                                         allow_slow_non_contiguous=True),
             writes=[b_par], dma=True)
        P.op("sp", lambda e: e.dma_start(out=gpost[:], in_=post_g.partition_broadcast(128)),
             writes=[b_par], dma=True)
        if gate_w is not None:
            Wgt = sb("Wgt", [128, 8, 72], BF16)
            b_Wgt = P.buf("Wgt")
            P.op("pool", lambda e: e.memset(Wgt[:], 0.0), writes=[b_Wgt])
            gv = gate_w.rearrange("(kc p) n -> p kc n", p=128)
            for (c0, n, d0) in ((1540, 4, 0), (1536, 4, 32), (3080, 8, 64)):
                P.op("pool", lambda e, c0=c0, n=n, d0=d0: e.dma_start(
                    out=Wgt[:, :, d0:d0 + n], in_=gv[:, :, c0:c0 + n]),
                    reads=[], writes=[b_Wgt], dma=True)
            gsb = [sb("gsb%d" % i, [72, 128], F32) for i in range(2)]
            b_gsb = P.bufs_n("gsb", 2)

        NXB = 3
        xb = [sb("xb%d" % i, [128, D], F32) for i in range(NXB)]
        b_xb = P.bufs_n("xb", NXB)
        ubf = [sb("ubf%d" % i, [128, D], BF16) for i in range(2)]
        b_ubf = P.bufs_n("ubf", 2)
        junk = sb("junk", [128, D], BF16)
        b_junk = P.buf("junk")
        uT = sb("uT", [128, 8, TT], BF16)
        b_uT = P.bufs_n("uT", 4)
        aT = sb("aT", [128, NFC, TT], BF16)
        b_aT = P.bufs_n("aT", NFC)
        sg = [sb("sg%d" % i, [128, TT], F32) for i in range(2)]
        b_sg = P.bufs_n("sg", 2)
        hst = [sb("hst%d" % i, [128, D], F32) for i in range(2)]
        b_hst = P.bufs_n("hst", 2)
        u2T = [sb("u2T%d" % i, [128, 8, 128], BF16) for i in range(2)]
        b_u2T = P.bufs_n("u2T", 2)
        NST = 6
        stat = sb("stat", [128, 3 * NST], F32)
        b_stat = P.bufs_n("stat", NST)

        pt = ps("pt", [128, 8, 128], BF16)
        b_pt = P.buf("pt")
        pg = [ps("pg%d" % i, [128, TT], F32) for i in range(2)]
        pu = [ps("pu%d" % i, [128, TT], F32) for i in range(2)]
        b_pg = P.bufs_n("pg", 2)
        b_pu = P.bufs_n("pu", 2)
        pys = [ps("py%d" % i, [128, 512], F32) for i in range(3)]
        b_pys = P.bufs_n("py", 3)
        if gate_w is not None:
            pgt = pu[1][0:72, 0:128]
            b_pgt = b_pu[1]

        src_v = src_h.rearrange("(n p) d -> n p d", p=128)
        dst_v = dst_h.rearrange("(n p) d -> n p d", p=128)
        cnt = {"x": 0, "u": 0, "st": 0, "h": 0, "u2": 0, "sg": 0, "gs": 0, "py": 0}
        pend = []

        def norm_T(h_ap, h_buf, gcol, out_ap, out_bufs, defer=False):
            si = cnt["st"] % NST
            cnt["st"] += 1
            ss, var, rstd = (stat[:, 3 * si + j:3 * si + j + 1] for j in range(3))
            rms_stats(P, C, h_ap, h_buf, junk[:], b_junk, ss, var, rstd, b_stat[si])
            ui = cnt["u"] % 2
            cnt["u"] += 1
            u = ubf[ui]
            P.op("dve", lambda e: e.tensor_scalar(out=u[:], in0=h_ap, scalar1=rstd, scalar2=None, op0=ALU.mult),
                 reads=[h_buf, b_stat[si]], writes=[b_ubf[ui]])
            def pe_part():
                for k in range(8):
                    P.op("pe", lambda e, k=k: e.transpose(out=pt[:, k, :], in_=u[:, k * 128:(k + 1) * 128],
                                                          identity=C.ident[:]),
                         reads=[b_ubf[ui], C.b_const], writes=[b_pt])
                P.op("dve", lambda e: e.tensor_tensor(out=out_ap, in0=pt[:], in1=bcast_last(gcol[:], 128), op=ALU.mult),
                     reads=[b_pt, b_par], writes=out_bufs)
            if defer:
                return pe_part
            pe_part()

        def pre(i):
            for s in range(4):
                n = i * 4 + s
                xi = cnt["x"] % NXB
                cnt["x"] += 1
                P.op("sp", lambda e, n=n, xi=xi: e.dma_start(out=xb[xi][:], in_=src_v[n]),
                     reads=[src_b[n]], writes=[b_xb[xi]], dma=True)
                norm_T(xb[xi][:], b_xb[xi], gpre, uT[:, :, s * 128:(s + 1) * 128], [b_uT[s]])

        def gateup(i):
            for f in range(NFC):
                j = f % 2
                for k in range(8):
                    P.op("pe", lambda e, k=k, f=f, j=j: e.matmul(
                        pg[j][:], lhsT=Wg[:, k, f * 128:(f + 1) * 128], rhs=uT[:, k, :],
                        start=(k == 0), stop=(k == 7)),
                        reads=[b_Wg[2 * k + (f // 11)]] + b_uT, writes=[b_pg[j]])
                for k in range(8):
                    P.op("pe", lambda e, k=k, f=f, j=j: e.matmul(
                        pu[j][:], lhsT=Wu[:, k, f * 128:(f + 1) * 128], rhs=uT[:, k, :],
                        start=(k == 0), stop=(k == 7)),
                        reads=[b_Wu[2 * k + (f // 11)]] + b_uT, writes=[b_pu[j]])
                if f == 0:
                    while pend:
                        pend.pop(0)()
                si = cnt["sg"] % 2
                cnt["sg"] += 1
                P.op("act", lambda e, j=j, si=si: e.activation(out=sg[si][:], in_=pg[j][:], func=AF.Silu),
                     reads=[b_pg[j]], writes=[b_sg[si]])
                P.op("dve", lambda e, j=j, si=si, f=f: e.tensor_tensor(out=aT[:, f, :], in0=sg[si][:], in1=pu[j][:],
                                                                   op=ALU.mult),
                     reads=[b_sg[si], b_pu[j]], writes=[b_aT[f]])

        def down_post(i):
            for s in range(4):
                n = i * 4 + s
                pyh = []
                for hf in range(2):
                    pi = cnt["py"] % 3
                    cnt["py"] += 1
                    pyh.append((pys[pi], b_pys[pi]))
                    for f in range(NFC):
                        P.op("pe", lambda e, f=f, s=s, hf=hf, pi=pi: e.matmul(
                            pys[pi][:], lhsT=aT[:, f, s * 128:(s + 1) * 128],
                            rhs=Wd[:, f, hf * 512:(hf + 1) * 512], start=(f == 0), stop=(f == NFC - 1)),
                            reads=[b_aT[f], b_Wd[f // 11]], writes=[b_pys[pi]])
                while pend:
                    pend.pop(0)()
                xi = cnt["x"] % NXB
                cnt["x"] += 1
                P.op("sp", lambda e, n=n, xi=xi: e.dma_start(out=xb[xi][:], in_=src_v[n]),
                     reads=[src_b[n]], writes=[b_xb[xi]], dma=True)
                si = cnt["st"] % NST
                cnt["st"] += 1
                ss, var, rstd = (stat[:, 3 * si + j:3 * si + j + 1] for j in range(3))
                P.op("act", lambda e, ss=ss, t=pyh[0][0]: e.activation(out=junk[:, 0:512], in_=t[:], func=AF.Square,
                                                                      accum_out=ss),
                     reads=[pyh[0][1]], writes=[b_junk, b_stat[si]])
                P.op("act", lambda e, var=var, t=pyh[1][0]: e.activation(out=junk[:, 512:1024], in_=t[:], func=AF.Square,
                                                                        accum_out=var),
                     reads=[pyh[1][1]], writes=[b_junk, b_stat[si]])
                P.op("dve", lambda e, ss=ss, var=var: e.tensor_tensor(out=var, in0=ss, in1=var, op=ALU.add),
                     reads=[b_stat[si]], writes=[b_stat[si]])
                P.op("dve", lambda e, var=var: e.tensor_scalar(out=var, in0=var, scalar1=1.0 / D, scalar2=EPS,
                                                              op0=ALU.mult, op1=ALU.add),
                     reads=[b_stat[si]], writes=[b_stat[si]])
                P.op("pool", lambda e, var=var, rstd=rstd: e.tensor_tensor(out=rstd, in0=var, in1=C.neghalf[:], op=ALU.pow),
                     reads=[b_stat[si], C.b_const], writes=[b_stat[si]])
                hi = cnt["h"] % 2
                cnt["h"] += 1
                hb = hst[hi]
                for hf in range(2):
                    hs = slice(hf * 512, (hf + 1) * 512)
                    P.op("dve", lambda e, hb=hb, rstd=rstd, t=pyh[hf][0], hs=hs: e.scalar_tensor_tensor(
                        out=hb[:, hs], in0=t[:], scalar=rstd, in1=gpost[:, hs], op0=ALU.mult, op1=ALU.mult),
                        reads=[pyh[hf][1], b_stat[si], b_par], writes=[b_hst[hi]])
                P.op("dve", lambda e, hb=hb, xi=xi: e.scalar_tensor_tensor(
                    out=hb[:], in0=hb[:], scalar=0.5, in1=xb[xi][:], op0=ALU.mult, op1=ALU.add),
                    reads=[b_hst[hi], b_xb[xi]], writes=[b_hst[hi]])
                P.op("sp", lambda e, hb=hb, n=n: e.dma_start(out=dst_v[n], in_=hb[:]),
                     reads=[b_hst[hi]], writes=[dst_b[n]], dma=True)
                ui2 = cnt["u2"] % 2
                cnt["u2"] += 1
                pe_part = norm_T(hb[:], b_hst[hi], gnext, u2T[ui2][:], [b_u2T[ui2]], defer=True)

                def tail(pe_part=pe_part, ui2=ui2, n=n):
                    pe_part()
                    P.op("sp", lambda e: e.dma_start(
                        out=dst_uT[:, :, n * 128:(n + 1) * 128].rearrange("k p t -> p k t"), in_=u2T[ui2][:]),
                        reads=[b_u2T[ui2]], writes=[uT_b[n]], dma=True)
                    if gate_w is not None:
                        for k in range(8):
                            P.op("pe", lambda e, k=k: e.matmul(
                                pgt, lhsT=Wgt[:, k, :], rhs=u2T[ui2][:, k, :], start=(k == 0), stop=(k == 7)),
                                reads=[b_Wgt, b_u2T[ui2]], writes=[b_pgt])
                        gi = cnt["gs"] % 2
                        cnt["gs"] += 1
                        P.op("act", lambda e: e.activation(out=gsb[gi][:], in_=pgt, func=AF.Copy),
                             reads=[b_pgt], writes=[b_gsb[gi]])
                        P.op("sp", lambda e: e.dma_start(out=gate_dst[:, n * 128:(n + 1) * 128], in_=gsb[gi][:]),
                             reads=[b_gsb[gi]], writes=[gate_b[n]], dma=True)
                pend.append(tail)

        pre(0)
        for i in range(NT):
            gateup(i)
            if i + 1 < NT:
                pre(i + 1)
            down_post(i)
        while pend:
            pend.pop(0)()
        P.flush()


def gp_stage(P, nc, C, I, gpre, gpre_b, qaug, kaug, aug_b):
    with contextlib.ExitStack() as st:
        sb = lambda name, shape, dt: st.enter_context(nc.sbuf_tensor("gp" + name, shape, dt))
        ps = lambda name, shape, dt: st.enter_context(nc.psum_tensor("gp" + name, shape, dt))
        T0 = sb("T0", [72, S], F32)
        T1 = sb("T1", [72, S], F32)
        T2 = sb("T2", [72, S], F32)
        T3 = sb("T3", [72, S], F32)
        QR = sb("QR", [72, 3, S], BF16)
        ONE = sb("ONE", [72, S], BF16)
        bcol = sb("bcol", [72, 1], F32)
        negb = sb("negb", [72, 1], F32)
        bicol = sb("bicol", [72, 1], F32)
        onec = sb("onec", [72, 1], F32)
        cm = sb("cm", [72, 32], F32)
        mce = sb("mce", [72, 32], F32)
        mprev = sb("mprev", [72, 32], F32)
        dec = sb("dec", [72, 32], F32)
        esel = sb("esel", [72, 4, 128], F32)
        bT0, bT1, bT2, bT3, bQR, bONE, bsm = [P.buf(n) for n in
                                              ("T0", "T1", "T2", "T3", "QR", "ONE", "gsm")]
        ptm = ps("ptm", [128, 32, 8], F32)
        pdc = ps("pdc", [128, 4, 32], F32)
        b_ptm, b_pdc = P.buf("ptm"), P.buf("pdc")

        P.op("sp", lambda e: e.dma_start(out=T0[:], in_=gpre), reads=gpre_b, writes=[bT0], dma=True)
        P.op("sp", lambda e: e.dma_start(out=T3[0:4, :], in_=gpre[32:36, :]), reads=gpre_b, writes=[bT3], dma=True)
        P.op("dve", lambda e: e.memset(bcol[:], 0.0), writes=[bsm])
        P.op("dve", lambda e: e.memset(bicol[:], 0.0), reads=[bsm], writes=[bsm])
        P.op("dve", lambda e: e.memset(onec[:], 1.0), reads=[bsm], writes=[bsm])
        P.op("pool", lambda e: e.memset(ONE[:], 1.0), writes=[bONE])
        P.op("sp", lambda e: e.dma_start(out=bcol[0:4, :], in_=I["mlstm_f_bias"].rearrange("o n -> n o"),
                                         allow_slow_non_contiguous=True), reads=[bsm], writes=[bsm], dma=True)
        P.op("sp", lambda e: e.dma_start(out=bcol[64:72, :], in_=I["fox_f_bias"].rearrange("o n -> n o"),
                                         allow_slow_non_contiguous=True), reads=[bsm], writes=[bsm], dma=True)
        P.op("sp", lambda e: e.dma_start(out=bicol[0:4, :], in_=I["mlstm_i_bias"].rearrange("o n -> n o"),
                                         allow_slow_non_contiguous=True), reads=[bsm], writes=[bsm], dma=True)
        P.op("dve", lambda e: e.tensor_scalar(out=negb[0:72, :], in0=bcol[0:72, :], scalar1=-1.0, scalar2=None,
                                              op0=ALU.mult), reads=[bsm], writes=[bsm])
        R = slice(0, 72)
        P.op("act", lambda e: e.activation(out=T1[R, :], in_=T0[R, :], func=AF.Exp, scale=-1.0, bias=negb[R, :]),
             reads=[bT0, bsm], writes=[bT1])
        P.op("act", lambda e: e.activation(out=T1[R, :], in_=T1[R, :], func=AF.Ln, scale=1.0, bias=onec[R, :]),
             reads=[bT1, bsm], writes=[bT1])
        P.op("dve", lambda e: e.tensor_tensor_scan(out=T2[R, :], data0=T1[R, :], data1=T1[R, :], initial=0.0,
                                                   op0=ALU.add, op1=ALU.max), reads=[bT1], writes=[bT2])
        M = slice(0, 4)
        P.op("dve", lambda e: e.scalar_tensor_tensor(out=T3[M, :], in0=T3[M, :], scalar=bicol[M, :], in1=T2[M, :],
                                                     op0=ALU.add, op1=ALU.add), reads=[bT3, bT2, bsm], writes=[bT3])
        P.op("dve", lambda e: e.tensor_reduce(out=cm[M, :], in_=T3[M, :].rearrange("p (c l) -> p c l", l=128),
                                              axis=AX.X, op=ALU.max), reads=[bT3], writes=[bsm])
        P.op("dve", lambda e: e.tensor_tensor_scan(out=mce[M, :], data0=cm[M, :], data1=cm[M, :], initial=0.0,
                                                   op0=ALU.max, op1=ALU.max), reads=[bsm], writes=[bsm])
        P.op("dve", lambda e: e.tensor_tensor(out=T3[M, :].rearrange("p (c l) -> p c l", l=128),
                                              in0=T3[M, :].rearrange("p (c l) -> p c l", l=128),
                                              in1=bcast_last(mce[M, :], 128), op=ALU.subtract),
             reads=[bT3, bsm], writes=[bT3])
        P.op("act", lambda e: e.activation(out=T3[M, :], in_=T3[M, :], func=AF.Exp), reads=[bT3], writes=[bT3])
        P.op("dve", lambda e: e.tensor_tensor(out=T1[M, :].rearrange("p (c l) -> p c l", l=128),
                                              in0=T2[M, :].rearrange("p (c l) -> p c l", l=128),
                                              in1=bcast_last(mce[M, :], 128), op=ALU.subtract),
             reads=[bT2, bsm, bT1], writes=[bT1])
        P.op("act", lambda e: e.activation(out=T1[M, :], in_=T1[M, :], func=AF.Exp, scale=2.0), reads=[bT1], writes=[bT1])
        P.op("dve", lambda e: e.memset(mprev[M, :], 0.0), reads=[bsm], writes=[bsm])
        P.op("dve", lambda e: e.tensor_copy(out=mprev[M, 1:32], in_=mce[M, 0:31]), reads=[bsm], writes=[bsm])
        P.op("dve", lambda e: e.tensor_tensor(out=dec[M, :], in0=mprev[M, :], in1=mce[M, :], op=ALU.subtract),
             reads=[bsm], writes=[bsm])
        P.op("act", lambda e: e.activation(out=dec[M, :], in_=dec[M, :], func=AF.Exp), reads=[bsm], writes=[bsm])
        for c in range(32):
            P.op("pe", lambda e, c=c: e.transpose(out=ptm[:, c, 0:4], in_=T3[M, c * 128:(c + 1) * 128],
                                                  identity=C.identf[M, 0:4]),
                 reads=[bT3, C.b_const], writes=[b_ptm])
            P.op("pe", lambda e, c=c: e.transpose(out=ptm[:, c, 4:8], in_=T1[M, c * 128:(c + 1) * 128],
                                                  identity=C.identf[M, 0:4]),
                 reads=[bT1, C.b_const], writes=[b_ptm])
        P.op("dve", lambda e: e.tensor_copy(out=C.wthr[:], in_=ptm[:]), reads=[b_ptm], writes=[C.b_wthr])
        for h in range(4):
            P.op("dve", lambda e, h=h: e.tensor_copy(out=esel[M, h, :],
                                                     in_=C.identf[M, h:h + 1].to_broadcast([4, 128])),
                 reads=[C.b_const, bsm], writes=[bsm])
        for h in range(4):
            P.op("pe", lambda e, h=h: e.matmul(pdc[:, h, :], lhsT=esel[M, h, :], rhs=dec[M, :], start=True, stop=True),
                 reads=[bsm], writes=[b_pdc])
        P.op("dve", lambda e: e.tensor_copy(out=C.decbc[:], in_=pdc[:]), reads=[b_pdc], writes=[C.b_decbc])
        Fx = slice(64, 72)
        Fd = slice(64, 72)
        P.op("dve", lambda e: e.tensor_scalar(out=T0[Fx, :], in0=T2[Fx, :], scalar1=-1.0, scalar2=None, op0=ALU.mult),
             reads=[bT2, bT0], writes=[bT0])
        for part in range(3):
            P.op("dve", lambda e, part=part: e.tensor_copy(out=QR[Fx, part, :], in_=T0[Fx, :]),
                 reads=[bT0], writes=[bQR])
            if part < 2:
                P.op("dve", lambda e, part=part: e.tensor_tensor(out=T0[Fx, :], in0=T0[Fx, :], in1=QR[Fx, part, :],
                                                                 op=ALU.subtract), reads=[bT0, bQR], writes=[bT0])
        P.op("sp", lambda e: e.dma_start(out=qaug[:, 64:67, :], in_=QR[Fd, :, :]), reads=[bQR], writes=[aug_b], dma=True)
        P.op("dve", lambda e: e.tensor_scalar(out=QR[Fx, :, :], in0=QR[Fx, :, :], scalar1=-1.0, scalar2=None,
                                              op0=ALU.mult), reads=[bQR], writes=[bQR])
        P.op("sp", lambda e: e.dma_start(out=kaug[:, 67:70, :], in_=QR[Fd, :, :]), reads=[bQR], writes=[aug_b], dma=True)
        for r in range(3):
            P.op("sp", lambda e, r=r: e.dma_start(out=qaug[:, 67 + r, :], in_=ONE[Fd, :]), reads=[bONE],
                 writes=[aug_b], dma=True)
            P.op("sp", lambda e, r=r: e.dma_start(out=kaug[:, 64 + r, :], in_=ONE[Fd, :]), reads=[bONE],
                 writes=[aug_b], dma=True)
        P.flush()


def win_pass(P, nc, C, I, uT1, uT_b, SC, DB, Wpre):
    w_in = I["w_in"]
    with contextlib.ExitStack() as st:
        sb = lambda name, shape, dt: st.enter_context(nc.sbuf_tensor("wi" + name, shape, dt))
        ps = lambda name, shape, dt: st.enter_context(nc.psum_tensor("wi" + name, shape, dt))
        W, b_W = Wpre
        cw = sb("cw", [128, 4, 4], F32)
        cb = sb("cb", [128, 4], F32)
        gbias = sb("gbias", [128, 2048], F32)
        b_par = P.buf("wipar")
        for tap in range(4):
            P.op("sp", lambda e, tap=tap: e.dma_start(
                out=cw[:, :, tap], in_=I["conv_w"][tap:tap + 1, :].rearrange("o (c p) -> p (o c)", p=128),
                allow_slow_non_contiguous=True), writes=[b_par], dma=True)
        P.op("sp", lambda e: e.dma_start(out=cb[:], in_=I["conv_b"].rearrange("o (c p) -> p (o c)", p=128),
                                         allow_slow_non_contiguous=True), writes=[b_par], dma=True)
        P.op("sp", lambda e: e.dma_start(out=gbias[:], in_=I["branch_gate_bias"].partition_broadcast(128)),
             writes=[b_par], dma=True)
        uT = [sb("uT%d" % i, [128, 8, TT], BF16) for i in range(2)]
        b_uT = P.bufs_n("wiuT", 2)
        zq = sb("zq", [128, 4, 3 + TT], F32)
        b_zq = P.bufs_n("zq", 4)
        acc = [sb("acc%d" % i, [128, TT], F32) for i in range(2)]
        b_acc = P.bufs_n("acc", 2)
        fo = [sb("fo%d" % i, [128, TT], BF16) for i in range(3)]
        b_fo = P.bufs_n("fo", 3)
        tv = [sb("tv%d" % i, [128, 4, 129], BF16) for i in range(2)]
        b_tv = P.bufs_n("tv", 2)
        tf = [sb("tf%d" % i, [128, 8, 65], BF16) for i in range(2)]
        b_tf = P.bufs_n("tf", 2)
        tg = [sb("tg%d" % i, [128, 512], F32) for i in range(2)]
        b_tg = P.bufs_n("tg", 2)
        to = [sb("to%d" % i, [128, 512], BF16) for i in range(3)]
        b_to = P.bufs_n("to", 3)
        pf = [ps("pf%d" % i, [128, TT], F32) for i in range(2)]
        b_pf = P.bufs_n("pf", 2)
        pk = [ps("pk%d" % i, [128, 512], F32) for i in range(2)]
        b_pk = P.bufs_n("pk", 2)
        cnt = {"pf": 0, "pk": 0, "acc": 0, "fo": 0, "tv": 0, "tf": 0, "tg": 0, "to": 0}

        def rot(key, n):
            v = cnt[key] % n
            cnt[key] += 1
            return v

        for ch in range(4):
            P.op("dve", lambda e, ch=ch: e.memset(zq[:, ch, 0:3], 0.0), writes=[b_zq[ch]])
        for i in range(NT):
            ub = i % 2
            tcols = slice(i * TT, (i + 1) * TT)
            if i == 0:
                P.op("sp", lambda e: e.dma_start(out=uT[0][:], in_=uT1[:, :, 0:TT].rearrange("k p t -> p k t")),
                     reads=uT_b[0:4], writes=[b_uT[0]], dma=True)
            if i + 1 < NT:
                ncols = slice((i + 1) * TT, (i + 2) * TT)
                P.op("sp", lambda e, ub=ub, ncols=ncols: e.dma_start(
                    out=uT[1 - ub][:], in_=uT1[:, :, ncols].rearrange("k p t -> p k t")),
                    reads=uT_b[4 * i + 4:4 * i + 8], writes=[b_uT[1 - ub]], dma=True)
            fm = [("mqk", ch, ch * 128) for ch in range(4)] + \
                 [("fq", ch, 1544 + ch * 128) for ch in range(4)] + \
                 [("fk", ch, 2056 + ch * 128) for ch in range(4)]
            for (kind, ch, c0) in fm:
                j = rot("pf", 2)
                for k in range(8):
                    P.op("pe", lambda e, k=k, c0=c0, j=j, ub=ub: e.matmul(
                        pf[j][:], lhsT=W[:, k, c0:c0 + 128], rhs=uT[ub][:, k, :], start=(k == 0), stop=(k == 7)),
                        reads=[b_W[k], b_uT[ub]], writes=[b_pf[j]])
                if kind == "mqk":
                    P.op("act", lambda e, ch=ch, j=j: e.activation(out=zq[:, ch, 3:3 + TT], in_=pf[j][:], func=AF.Copy),
                         reads=[b_pf[j]], writes=[b_zq[ch]])
                    a = rot("acc", 2)
                    P.op("dve", lambda e, ch=ch, a=a: e.tensor_scalar(
                        out=acc[a][:], in0=zq[:, ch, 0:TT], scalar1=cw[:, ch, 0:1], scalar2=cb[:, ch:ch + 1],
                        op0=ALU.mult, op1=ALU.add), reads=[b_zq[ch], b_par], writes=[b_acc[a]])
                    for tap in range(1, 4):
                        P.op("dve", lambda e, ch=ch, a=a, tap=tap: e.scalar_tensor_tensor(
                            out=acc[a][:], in0=zq[:, ch, tap:tap + TT], scalar=cw[:, ch, tap:tap + 1], in1=acc[a][:],
                            op0=ALU.mult, op1=ALU.add), reads=[b_zq[ch], b_par, b_acc[a]], writes=[b_acc[a]])
                    P.op("dve", lambda e, ch=ch: e.tensor_copy(out=zq[:, ch, 0:3], in_=zq[:, ch, TT:TT + 3]),
                         reads=[b_zq[ch]], writes=[b_zq[ch]])
                    o = rot("fo", 3)
                    P.op("act", lambda e, a=a, o=o: e.activation(out=fo[o][:], in_=acc[a][:], func=AF.Silu),
                         reads=[b_acc[a]], writes=[b_fo[o]])
                    P.op("sp", lambda e, o=o, ch=ch, tcols=tcols: e.dma_start(out=SC["mqkT"][ch, :, tcols], in_=fo[o][:]),
                         reads=[b_fo[o]], writes=[DB("mqkT")[i]], dma=True)
                else:
                    o = rot("fo", 3)
                    sc = 0.125 if kind == "fq" else 1.0
                    P.op("act", lambda e, o=o, j=j, sc=sc: e.activation(out=fo[o][:], in_=pf[j][:], func=AF.Copy, scale=sc),
                         reads=[b_pf[j]], writes=[b_fo[o]])
                    dst = SC["qaug"] if kind == "fq" else SC["kaug"]
                    for hh in range(2):
                        P.op("sp", lambda e, o=o, ch=ch, dst=dst, tcols=tcols, hh=hh: e.dma_start(
                            out=dst[2 * ch + hh, 0:64, tcols], in_=fo[o][hh * 64:(hh + 1) * 64, :]),
                            reads=[b_fo[o]], writes=[DB("aug")[i]], dma=True)
            for s in range(4):
                n = i * 4 + s
                rows = slice(n * 128, (n + 1) * 128)
                groups = [("mv", 512), ("mo", 1024), ("fv", 2568), ("ga", 3088), ("ga", 3600), ("gb", 4112), ("gb", 4624)]
                for gi, (kind, c0) in enumerate(groups):
                    j = rot("pk", 2)
                    for k in range(8):
                        P.op("pe", lambda e, k=k, c0=c0, j=j, ub=ub, s=s: e.matmul(
                            pk[j][:], lhsT=uT[ub][:, k, s * 128:(s + 1) * 128], rhs=W[:, k, c0:c0 + 512],
                            start=(k == 0), stop=(k == 7)),
                            reads=[b_W[k], b_uT[ub]], writes=[b_pk[j]])
                    if kind == "mv":
                        t = rot("tv", 2)
                        c = n
                        P.op("dve", lambda e, t=t, j=j, c=c: e.tensor_tensor(
                            out=tv[t][:, :, 0:128], in0=pk[j][:].rearrange("p (h d) -> p h d", h=4),
                            in1=bcast_last(C.wthr[:, c, 0:4], 128), op=ALU.mult),
                            reads=[b_pk[j], C.b_wthr], writes=[b_tv[t]])
                        P.op("dve", lambda e, t=t, c=c: e.tensor_copy(out=tv[t][:, :, 128:129],
                                                                     in_=C.wthr[:, c, 0:4].unsqueeze(2)),
                             reads=[C.b_wthr, b_tv[t]], writes=[b_tv[t]])
                        P.op("sp", lambda e, t=t, rows=rows: e.dma_start(out=SC["vaugM"][rows], in_=tv[t][:]),
                             reads=[b_tv[t]], writes=[DB("vaugM")[n]], dma=True)
                    elif kind == "fv":
                        t = rot("tf", 2)
                        P.op("act", lambda e, t=t, j=j: e.activation(
                            out=tf[t][:, :, 1:65], in_=pk[j][:].rearrange("p (h d) -> p h d", h=8), func=AF.Copy),
                            reads=[b_pk[j]], writes=[b_tf[t]])
                        P.op("dve", lambda e, t=t: e.memset(tf[t][:, :, 0:1], 1.0), reads=[b_tf[t]], writes=[b_tf[t]])
                        P.op("sp", lambda e, t=t, rows=rows: e.dma_start(out=SC["vaugF"][rows], in_=tf[t][:]),
                             reads=[b_tf[t]], writes=[DB("vaugF")[n]], dma=True)
                    elif kind == "mo":
                        o = rot("to", 3)
                        P.op("act", lambda e, o=o, j=j: e.activation(out=to[o][:], in_=pk[j][:], func=AF.Sigmoid),
                             reads=[b_pk[j]], writes=[b_to[o]])
                        P.op("sp", lambda e, o=o, rows=rows: e.dma_start(out=SC["so"][rows], in_=to[o][:]),
                             reads=[b_to[o]], writes=[DB("so")[n]], dma=True)
                    else:
                        g = rot("tg", 2)
                        boff = c0 - 3088
                        P.op("dve", lambda e, g=g, j=j, boff=boff: e.tensor_tensor(
                            out=tg[g][:], in0=pk[j][:], in1=gbias[:, boff:boff + 512], op=ALU.add),
                            reads=[b_pk[j], b_par], writes=[b_tg[g]])
                        o = rot("to", 3)
                        P.op("act", lambda e, o=o, g=g: e.activation(out=to[o][:], in_=tg[g][:], func=AF.Sigmoid),
                             reads=[b_tg[g]], writes=[b_to[o]])
                        dcol = boff % 1024
                        dst = SC["ga"] if kind == "ga" else SC["gb"]
                        P.op("sp", lambda e, o=o, rows=rows, dst=dst, dcol=dcol: e.dma_start(
                            out=dst[rows, dcol:dcol + 512], in_=to[o][:]),
                            reads=[b_to[o]], writes=[DB("gab")[n]], dma=True)
        P.flush()


def mix_pass(P, nc, C, I, SC, DB):
    with contextlib.ExitStack() as st:
        sb = lambda name, shape, dt: st.enter_context(nc.sbuf_tensor("mx" + name, shape, dt))
        ps = lambda name, shape, dt: st.enter_context(nc.psum_tensor("mx" + name, shape, dt))
        mask01 = sb("mask01", [128, 128], F32)
        trim = sb("trim", [128, 128], BF16)
        trimf = sb("trimf", [128, 128], F32)
        onesr = sb("onesr", [1, 65], F32)
        gln = sb("gln", [128, 512], F32)
        b_c = P.buf("mxconst")
        P.op("pool", lambda e: e.memset(mask01[:], 1.0), writes=[b_c])
        P.op("pool", lambda e: e.affine_select(out=mask01[:], in_=mask01[:], pattern=[[1, 128]], compare_op=ALU.is_ge,
                                                 fill=0.0, base=0, channel_multiplier=-1), reads=[b_c], writes=[b_c])
        P.op("pool", lambda e: e.memset(trimf[:], 0.0), reads=[b_c], writes=[b_c])
        P.op("pool", lambda e: e.affine_select(out=trimf[:], in_=trimf[:], pattern=[[1, 128]], compare_op=ALU.is_ge,
                                                 fill=-30000.0, base=0, channel_multiplier=-1), reads=[b_c], writes=[b_c])
        P.op("dve", lambda e: e.tensor_copy(out=trim[:], in_=trimf[:]), reads=[b_c], writes=[b_c])
        P.op("dve", lambda e: e.memset(onesr[:], 1.0), reads=[b_c], writes=[b_c])
        P.op("sp", lambda e: e.dma_start(out=gln[:], in_=I["mlstm_norm_g"].partition_broadcast(128)),
             reads=[b_c], writes=[b_c], dma=True)
        mqz = [sb("mqz%d" % i, [128, S], BF16) for i in range(4)]
        mk = [sb("mk%d" % i, [128, S], BF16) for i in range(2)]
        b_mqk = P.buf("mqk")
        for h in range(4):
            P.op("pool", lambda e, h=h: e.memset(mqz[h][:], 0.0), writes=[b_mqk])
        for h in range(4):
            R = slice((h % 2) * 64, (h % 2) * 64 + 64)
            P.op("sp", lambda e, h=h, R=R: e.dma_start(out=mqz[h][R, :], in_=SC["mqkT"][h // 2, R, :]), reads=DB("mqkT"),
                 writes=[b_mqk], dma=True)
        for hp in range(2):
            P.op("sp", lambda e, hp=hp: e.dma_start(out=mk[hp][:], in_=SC["mqkT"][2 + hp]), reads=DB("mqkT"),
                 writes=[b_mqk], dma=True)
        Cst = [sb("Cst%d" % i, [128, 129], F32) for i in range(2)]
        Cb = [sb("Cb%d" % i, [128, 129], BF16) for i in range(2)]
        b_Cst = P.bufs_n("Cst", 2)
        b_Cb = P.bufs_n("Cb", 2)
        va = [sb("va%d" % i, [128, 4, 129], BF16) for i in range(2)]
        b_va = P.bufs_n("va", 2)
        sgo = [sb("sgo%d" % i, [128, 512], BF16) for i in range(2)]
        b_sgo = P.bufs_n("sgo", 2)
        Sm = [sb("Sm%d" % i, [128, 2, 128], BF16) for i in range(2)]
        b_Sm = P.bufs_n("Sm", 2)
        ktm = [sb("ktm%d" % i, [128, 128], BF16) for i in range(2)]
        b_ktm = P.bufs_n("ktm", 2)
        bst = [sb("bst%d" % i, [128, 2, 6], F32) for i in range(2)]
        bag = [sb("bag%d" % i, [128, 2, 2], F32) for i in range(2)]
        sm = [sb("sm%d" % i, [128, 2, 4], F32) for i in range(2)]
        b_sm = P.bufs_n("msm", 2)
        Osb = [sb("Osb%d" % i, [128, 2, 129], F32) for i in range(2)]
        b_Osb = P.bufs_n("Osb", 2)
        sq = [sb("sq%d" % i, [128, 2, 1], F32) for i in range(2)]
        b_sq = P.bufs_n("msq", 2)
        hn = [sb("hn%d" % i, [128, 512], F32) for i in range(2)]
        b_hn = P.bufs_n("hn", 2)
        ya = [sb("ya%d" % i, [128, 512], BF16) for i in range(2)]
        b_ya = P.bufs_n("ya", 2)
        yaT = [sb("yaT%d" % i, [128, 4, 128], BF16) for i in range(2)]
        b_yaT = P.bufs_n("yaTs", 2)
        pSm = ps("pSm", [128, 2, 128], F32)
        pOm = ps("pOm", [128, 2, 129], F32)
        pU = ps("pU", [128, 2, 129], F32)
        pT5 = ps("pT5", [128, 5, 128], BF16)
        pkt = pT5[:, 4, :]
        pyT = pT5[:, 0:4, :]
        b_pSm, b_pOm, b_pU, b_pkt, b_pyT = [P.buf(n) for n in ("pSm", "pOm", "pU", "pkt", "pyT")]

        def mlstm_chunk(c):
            cols = slice(c * 128, (c + 1) * 128)
            rows = slice(c * 128, (c + 1) * 128)
            vi = c % 2
            P.op("sp", lambda e: e.dma_start(out=va[vi][:], in_=SC["vaugM"][rows]), reads=[DB("vaugM")[c]],
                 writes=[b_va[vi]], dma=True)
            P.op("sp", lambda e: e.dma_start(out=sgo[vi][:], in_=SC["so"][rows]), reads=[DB("so")[c]],
                 writes=[b_sgo[vi]], dma=True)
            hnb = hn[vi]
            for hp in range(2):
                si = hp
                for hh in range(2):
                    P.op("pe", lambda e, hp=hp, hh=hh: e.matmul(
                        pSm[:, hh, :], lhsT=mk[hp][:, cols], rhs=mqz[2 * hp + hh][:, cols], start=True, stop=True),
                        reads=[b_mqk], writes=[b_pSm])
                P.op("pe", lambda e, hp=hp: e.transpose(out=pkt, in_=mk[hp][:, cols], identity=C.ident[:]),
                     reads=[b_mqk, C.b_const], writes=[b_pkt])
                P.op("dve", lambda e, si=si: e.scalar_tensor_tensor(
                    out=Sm[si][:], in0=pSm[:], scalar=0.125,
                    in1=mask01[:].unsqueeze(1).to_broadcast([128, 2, 128]), op0=ALU.mult, op1=ALU.mult),
                    reads=[b_pSm, b_c], writes=[b_Sm[si]])
                P.op("act", lambda e, si=si: e.activation(out=ktm[si][:], in_=pkt, func=AF.Copy, scale=0.125),
                     reads=[b_pkt], writes=[b_ktm[si]])
                yield
            for hp in range(2):
                si = hp
                for hh in range(2):
                    h = 2 * hp + hh
                    P.op("pe", lambda e, si=si, hh=hh, h=h: e.matmul(
                        pOm[:, hh, :], lhsT=Sm[si][:, hh, :], rhs=va[vi][:, h, :], start=True, stop=(c == 0)),
                        reads=[b_Sm[si], b_va[vi]], writes=[b_pOm])
                    if c > 0:
                        P.op("pe", lambda e, hp=hp, hh=hh, h=h: e.matmul(
                            pOm[:, hh, :], lhsT=mqz[h][:, cols], rhs=Cb[hp][:, :], start=False, stop=True),
                            reads=[b_mqk, b_Cb[hp]], writes=[b_pOm])
                osp, bos = Osb[hp], b_Osb[hp]
                P.op("act", lambda e, osp=osp: e.activation(out=osp[:], in_=pOm[:], func=AF.Copy),
                     reads=[b_pOm], writes=[bos])
                for hh in range(2):
                    h = 2 * hp + hh
                    P.op("pe", lambda e, si=si, hh=hh, h=h: e.matmul(
                        pU[:, hh, :], lhsT=ktm[si][:], rhs=va[vi][:, h, :], start=True, stop=True),
                        reads=[b_ktm[si], b_va[vi]], writes=[b_pU])
                for hh in range(2):
                    h = 2 * hp + hh
                    R = slice(hh * 64, (hh + 1) * 64)
                    if c == 0:
                        P.op("dve", lambda e, hp=hp, hh=hh, R=R: e.tensor_copy(out=Cst[hp][R, :], in_=pU[R, hh, :]),
                             reads=[b_pU], writes=[b_Cst[hp]])
                    else:
                        P.op("dve", lambda e, hp=hp, hh=hh, R=R, h=h: e.scalar_tensor_tensor(
                            out=Cst[hp][R, :], in0=Cst[hp][R, :], scalar=C.decbc[R, h, c:c + 1], in1=pU[R, hh, :],
                            op0=ALU.mult, op1=ALU.add), reads=[b_pU, b_Cst[hp], C.b_decbc], writes=[b_Cst[hp]])
                    if c < 31:
                        P.op("dve", lambda e, hp=hp, R=R, h=h: e.tensor_scalar(
                            out=Cb[hp][R, :], in0=Cst[hp][R, :], scalar1=C.decbc[R, h, c + 1:c + 2], scalar2=None,
                            op0=ALU.mult), reads=[b_Cst[hp], C.b_decbc], writes=[b_Cb[hp]])
                smp, bstp, bagp, bsm = sm[hp], bst[hp], bag[hp], b_sm[hp]
                for hh in range(2):
                    P.op("dve", lambda e, hh=hh, bstp=bstp, hp=hp: e.bn_stats(out=bstp[:, hh, :], in_=Osb[hp][:, hh, 0:128]),
                         reads=[bos], writes=[bsm])
                    P.op("dve", lambda e, hh=hh, bstp=bstp, bagp=bagp: e.bn_aggr(out=bagp[:, hh, :], in_=bstp[:, hh, :]),
                         reads=[bsm], writes=[bsm])
                sqp, bsq = sq[hp], b_sq[hp]
                P.op("act", lambda e, sqp=sqp, osp=osp: e.activation(out=sqp[:], in_=osp[:, :, 128:129], func=AF.Square),
                     reads=[bos], writes=[bsq])
                P.op("dve", lambda e, hp=hp, smp=smp, sqp=sqp: e.tensor_tensor(
                    out=smp[:, :, 0:1], in0=sqp[:], in1=C.wthr[:, c, 4 + 2 * hp:6 + 2 * hp].unsqueeze(2),
                    op=ALU.max), reads=[C.b_wthr, bsm, bsq], writes=[bsm])
                P.op("dve", lambda e, smp=smp, bagp=bagp: e.scalar_tensor_tensor(
                    out=smp[:, :, 1:2], in0=smp[:, :, 0:1], scalar=EPS, in1=bagp[:, :, 1:2], op0=ALU.mult, op1=ALU.add),
                    reads=[bsm], writes=[bsm])
                P.op("pool", lambda e, smp=smp: e.tensor_tensor(
                    out=smp[:, :, 2:3], in0=smp[:, :, 1:2],
                    in1=C.neghalf[:].unsqueeze(1).to_broadcast([128, 2, 1]), op=ALU.pow),
                    reads=[bsm, C.b_const], writes=[bsm])
                for hh in range(2):
                    h = 2 * hp + hh
                    P.op("dve", lambda e, hh=hh, h=h, smp=smp, bagp=bagp, osp=osp: e.tensor_scalar(
                        out=hnb[:, h * 128:(h + 1) * 128], in0=osp[:, hh, 0:128], scalar1=bagp[:, hh, 0:1],
                        scalar2=smp[:, hh, 2:3], op0=ALU.subtract, op1=ALU.mult),
                        reads=[bos, bsm], writes=[b_hn[vi]])
                yield
            yi = c % 2
            P.op("pool", lambda e: e.tensor_tensor(out=hnb[:], in0=hnb[:], in1=gln[:], op=ALU.mult),
                 reads=[b_hn[vi], b_c], writes=[b_hn[vi]])
            P.op("dve", lambda e: e.tensor_tensor(out=ya[yi][:], in0=hnb[:], in1=sgo[vi][:], op=ALU.mult),
                 reads=[b_hn[vi], b_sgo[vi]], writes=[b_ya[yi]])
            yield
            for k in range(4):
                P.op("pe", lambda e, k=k: e.transpose(out=pyT[:, k, :], in_=ya[yi][:, k * 128:(k + 1) * 128],
                                                      identity=C.ident[:]),
                     reads=[b_ya[yi], C.b_const], writes=[b_pyT])
            P.op("act", lambda e: e.activation(out=yaT[yi][:], in_=pyT, func=AF.Copy), reads=[b_pyT],
                 writes=[b_yaT[yi]])
            P.op("sp", lambda e: e.dma_start(out=SC["yaT"][:, :, cols].rearrange("k p t -> p k t"), in_=yaT[yi][:]),
                 reads=[b_yaT[yi]], writes=[DB("yaT")[c]], dma=True)
            yield

        def mlstm_gen():
            for c in range(32):
                yield from mlstm_chunk(c)

        VFW = 8 * 65 + 64
        VF = sb("VF", [128, 32, VFW], BF16)
        b_VF = P.buf("VF")
        P.op("pool", lambda e: e.memset(VF[:, :, 8 * 65:VFW], 0.0), writes=[b_VF])
        P.op("sp", lambda e: e.dma_start(out=VF[:, :, 0:8 * 65], in_=SC["vaugF"].rearrange("(j p) h e -> p j (h e)", p=128)),
             reads=DB("vaugF"), writes=[b_VF], dma=True)
        QA = [sb("QA%d" % i, [128, S], BF16) for i in range(2)]
        KA = [sb("KA%d" % i, [128, S], BF16) for i in range(2)]
        b_QA = P.bufs_n("QA", 2)
        b_KA = P.bufs_n("KA", 2)
        for i2 in range(2):
            P.op("pool", lambda e, i2=i2: e.memset(QA[i2][64:128, :], 0.0), writes=[b_QA[i2]])
            P.op("pool", lambda e, i2=i2: e.memset(KA[i2][64:128, :], 0.0), writes=[b_KA[i2]])
        PT = [sb("PT%d" % i, [128, 512], BF16) for i in range(3)]
        b_PT = P.bufs_n("PT", 3)
        rec = [sb("rec%d" % i, [1, 512], F32) for i in range(2)]
        b_rec = P.bufs_n("rec", 2)
        b_recd = P.bufs_n("recd", 2)
        osb = [sb("osb%d" % i, [65, 512], F32) for i in range(2)]
        b_osb = P.bufs_n("osb", 2)
        bcs = [sb("bcs%d" % i, [65, 512], F32) for i in range(2)]
        b_bcs = P.bufs_n("bcs", 2)
        ybt = [sb("ybt%d" % i, [65, 512], BF16) for i in range(2)]
        b_ybt = P.bufs_n("ybt", 2)
        pS = [ps("pS%d" % i, [128, 512], F32) for i in range(3)]
        b_pS = P.bufs_n("pS", 3)
        slot_of = {}
        pO = ps("pO", [128, 512], F32)
        b_pO = P.buf("pO")
        cnt = {"s": 0, "y": 0}

        def fox_load(h):
            hb = h % 2
            P.op("sp", lambda e: e.dma_start(out=QA[hb][0:70, :], in_=SC["qaug"][h]), reads=DB("aug"), writes=[b_QA[hb]], dma=True)
            P.op("sp", lambda e: e.dma_start(out=KA[hb][0:70, :], in_=SC["kaug"][h]), reads=DB("aug"), writes=[b_KA[hb]], dma=True)

        seq = [(h, i, j) for h in range(8) for i in range(8) for j in range(4 * i + 4)]

        def emit_S(idx):
            h, i, j = seq[idx]
            hb = h % 2
            sj = cnt["s"] % 3
            cnt["s"] += 1
            slot_of[idx] = sj
            jj = j - 4 * i
            kc = slice(j * 128, (j + 1) * 128)
            rd = [b_KA[hb], b_QA[hb]]
            if jj < 0:
                P.op("pe", lambda e: e.matmul(
                    pS[sj][:, 0:512], lhsT=KA[hb][:, kc], rhs=QA[hb][:, i * 512:(i + 1) * 512], start=True, stop=True),
                    reads=rd, writes=[b_pS[sj]])
            else:
                qs = jj * 128
                wq = 512 - qs
                q0 = i * 512 + qs
                P.op("pe", lambda e: e.matmul(pS[sj][:, 0:128], lhsT=C.ident[:], rhs=trim[:], start=True, stop=False),
                     reads=[C.b_const, b_c], writes=[b_pS[sj]])
                P.op("pe", lambda e: e.matmul(
                    pS[sj][:, 0:128], lhsT=KA[hb][:, kc], rhs=QA[hb][:, q0:q0 + 128], start=False, stop=True),
                    reads=rd, writes=[b_pS[sj]])
                if wq > 128:
                    P.op("pe", lambda e: e.matmul(
                        pS[sj][:, 128:wq], lhsT=KA[hb][:, kc], rhs=QA[hb][:, q0 + 128:q0 + wq], start=True, stop=True),
                        reads=rd, writes=[b_pS[sj]])

        def emit_rest(idx):
            h, i, j = seq[idx]
            sj = slot_of[idx]
            nkb = 4 * i + 4
            jj = j - 4 * i
            qs = max(jj, 0) * 128
            wq = 512 - qs
            tj = idx % 3
            P.op("act", lambda e: e.activation(out=PT[tj][:, 0:wq], in_=pS[sj][:, 0:wq], func=AF.Exp),
                 reads=[b_pS[sj]], writes=[b_PT[tj]])
            P.op("pe", lambda e: e.matmul(
                pO[:, qs:512], lhsT=VF[:, j, h * 65:h * 65 + 128], rhs=PT[tj][:, 0:wq],
                start=(j == 0), stop=(j == nkb - 1)),
                reads=[b_VF, b_PT[tj]], writes=[b_pO])
            if j < nkb - 1:
                return
            yi = cnt["y"] % 2
            cnt["y"] += 1
            u = h * 8 + i
            P.op("act", lambda e: e.activation(out=osb[yi][:], in_=pO[0:65, :], func=AF.Copy), reads=[b_pO],
                 writes=[b_osb[yi]])
            P.op("dve", lambda e: e.reciprocal(out=rec[yi][0:1, :], in_=osb[yi][0:1, :]), reads=[b_osb[yi]],
                 writes=[b_rec[yi]])
            P.op("sp", lambda e: e.dma_start(out=SC["recd"][u:u + 1, :], in_=rec[yi][0:1, :]), reads=[b_rec[yi]],
                 writes=[b_recd[yi]], dma=True)
            P.op("sp", lambda e: e.dma_start(out=bcs[yi][:], in_=SC["recd"][u:u + 1, :].partition_broadcast(65)),
                 reads=[b_recd[yi]], writes=[b_bcs[yi]], dma=True)
            P.op("dve", lambda e: e.tensor_tensor(out=ybt[yi][:], in0=osb[yi][:], in1=bcs[yi][:], op=ALU.mult),
                 reads=[b_osb[yi], b_bcs[yi]], writes=[b_ybt[yi]])
            P.op("sp", lambda e: e.dma_start(
                out=SC["ybT"][h // 2, (h % 2) * 64:(h % 2) * 64 + 64, i * 512:(i + 1) * 512], in_=ybt[yi][1:65, :]),
                reads=[b_ybt[yi]], writes=[DB("ybT")[(h * 8 + i) % 32]], dma=True)

        gen = mlstm_gen()
        fox_load(0)
        emit_S(0)
        emit_S(1)
        for idx, (h, i, j) in enumerate(seq):
            if i == 0 and j == 0 and h + 1 < 8:
                fox_load(h + 1)
            if idx + 2 < len(seq):
                emit_S(idx + 2)
            emit_rest(idx)
            if idx % 6 == 5:
                next(gen, None)
        for _ in gen:
            pass
        P.flush()


def merge_pass(P, nc, C, I, SC, DB, h1, h1_b, h2, h2_b):
    with contextlib.ExitStack() as st:
        sb = lambda name, shape, dt: st.enter_context(nc.sbuf_tensor("mg" + name, shape, dt))
        ps = lambda name, shape, dt: st.enter_context(nc.psum_tensor("mg" + name, shape, dt))
        Wa = sb("Wa", [128, 4, D], BF16)
        Wb = sb("Wb", [128, 4, D], BF16)
        Wo = sb("Wo", [128, 8, D], BF16)
        b_W = P.buf("mgW")
        P.op("pool", lambda e: e.dma_start(out=Wa[:], in_=I["w_branch_a"].rearrange("(k p) d -> p k d", p=128)),
             writes=[b_W], dma=True)
        P.op("pool", lambda e: e.dma_start(out=Wb[:], in_=I["w_branch_b"].rearrange("(k p) d -> p k d", p=128)),
             writes=[b_W], dma=True)
        P.op("pool", lambda e: e.dma_start(out=Wo[:], in_=I["w_out"].rearrange("(k p) d -> p k d", p=128)),
             writes=[b_W], dma=True)
        gpost = sb("gpost", [128, D], F32)
        P.op("sp", lambda e: e.dma_start(out=gpost[:], in_=I["mix_post_g"].partition_broadcast(128)),
             writes=[b_W], dma=True)
        yaT = [sb("yaT%d" % i, [128, 4, 128], BF16) for i in range(2)]
        ybT = [sb("ybT%d" % i, [128, 4, 128], BF16) for i in range(2)]
        gab = [sb("gab%d" % i, [128, 2, D], BF16) for i in range(2)]
        hin = [sb("hin%d" % i, [128, D], F32) for i in range(4)]
        b_in = P.bufs_n("mgin", 2)
        b_inb = P.bufs_n("mginb", 2)
        b_ga = P.bufs_n("mgga", 2)
        b_gb = P.bufs_n("mggb", 2)
        b_hin = P.bufs_n("mghin", 4)
        t1 = [sb("t1%d" % i, [128, D], F32) for i in range(2)]
        t2 = [sb("t2%d" % i, [128, D], F32) for i in range(2)]
        mb = [sb("mb%d" % i, [128, D], BF16) for i in range(2)]
        mT = [sb("mT%d" % i, [128, 8, 128], BF16) for i in range(2)]
        junk = sb("junk", [128, D], BF16)
        hout = [sb("hout%d" % i, [128, D], F32) for i in range(2)]
        stat = sb("stat", [128, 6], F32)
        b_t1, b_t2, b_mb, b_mT = [P.bufs_n(n, 2) for n in ("t1", "t2", "mb", "mT")]
        b_junk = P.buf("mgjunk")
        b_hout = P.bufs_n("hout", 2)
        b_stat = P.bufs_n("mgstat", 2)
        pA = ps("pA", [128, D], F32)
        pB = ps("pB", [128, D], F32)
        pO = ps("pO", [128, D], F32)
        pt = ps("pt", [128, 8, 128], BF16)
        b_pA, b_pB, b_pO, b_pt = [P.buf(n) for n in ("pA", "pB", "mgpO", "mgpt")]
        h1v = h1.rearrange("(n p) d -> n p d", p=128)
        h2v = h2.rearrange("(n p) d -> n p d", p=128)

        def s1(n):
            ib = n % 2
            rows = slice(n * 128, (n + 1) * 128)
            cols = rows
            P.op("sp", lambda e: e.dma_start(out=yaT[ib][:], in_=SC["yaT"][:, :, cols].rearrange("k p t -> p k t")),
                 reads=[DB("yaT")[n]], writes=[b_in[ib]], dma=True)
            P.op("sp", lambda e: e.dma_start(out=ybT[ib][:], in_=SC["ybT"][:, :, cols].rearrange("k p t -> p k t")),
                 reads=DB("ybT"), writes=[b_inb[ib]], dma=True)
            P.op("sp", lambda e: e.dma_start(out=gab[ib][:, 0, :], in_=SC["ga"][rows]),
                 reads=[DB("gab")[n]], writes=[b_ga[ib]], dma=True)
            P.op("sp", lambda e: e.dma_start(out=gab[ib][:, 1, :], in_=SC["gb"][rows]),
                 reads=[DB("gab")[n]], writes=[b_gb[ib]], dma=True)
            P.op("sp", lambda e: e.dma_start(out=hin[n % 4][:], in_=h1v[n]),
                 reads=[h1_b[n]], writes=[b_hin[n % 4]], dma=True)
            for hf in range(2):
                hs = slice(hf * 512, (hf + 1) * 512)
                for k in range(4):
                    P.op("pe", lambda e, k=k, hs=hs: e.matmul(pA[:, hs], lhsT=yaT[ib][:, k, :], rhs=Wa[:, k, hs],
                                                              start=(k == 0), stop=(k == 3)),
                         reads=[b_in[ib], b_W], writes=[b_pA])
            for hf in range(2):
                hs = slice(hf * 512, (hf + 1) * 512)
                for k in range(4):
                    P.op("pe", lambda e, k=k, hs=hs: e.matmul(pB[:, hs], lhsT=ybT[ib][:, k, :], rhs=Wb[:, k, hs],
                                                              start=(k == 0), stop=(k == 3)),
                         reads=[b_inb[ib], b_W], writes=[b_pB])
            P.op("dve", lambda e: e.tensor_tensor(out=t1[ib][:], in0=pA[:], in1=gab[ib][:, 0, :], op=ALU.mult),
                 reads=[b_pA, b_ga[ib]], writes=[b_t1[ib]])
            P.op("dve", lambda e: e.tensor_tensor(out=t2[ib][:], in0=pB[:], in1=gab[ib][:, 1, :], op=ALU.mult),
                 reads=[b_pB, b_gb[ib]], writes=[b_t2[ib]])
            P.op("pool", lambda e: e.tensor_tensor(out=mb[ib][:], in0=t1[ib][:], in1=t2[ib][:], op=ALU.add),
                 reads=[b_t1[ib], b_t2[ib]], writes=[b_mb[ib]])

        def s2(n):
            ib = n % 2
            for k in range(8):
                P.op("pe", lambda e, k=k: e.transpose(out=pt[:, k, :], in_=mb[ib][:, k * 128:(k + 1) * 128],
                                                      identity=C.ident[:]),
                     reads=[b_mb[ib], C.b_const], writes=[b_pt])
            P.op("act", lambda e: e.activation(out=mT[ib][:], in_=pt[:], func=AF.Copy), reads=[b_pt], writes=[b_mT[ib]])
            for hf in range(2):
                hs = slice(hf * 512, (hf + 1) * 512)
                for k in range(8):
                    P.op("pe", lambda e, k=k, hs=hs: e.matmul(pO[:, hs], lhsT=mT[ib][:, k, :], rhs=Wo[:, k, hs],
                                                              start=(k == 0), stop=(k == 7)),
                         reads=[b_mT[ib], b_W], writes=[b_pO])
            si = n % 2
            ss, var, rstd = (stat[:, 3 * si + j:3 * si + j + 1] for j in range(3))
            rms_stats(P, C, pO[:], b_pO, junk[:], b_junk, ss, var, rstd, b_stat[si])
            P.op("dve", lambda e: e.scalar_tensor_tensor(
                out=hout[ib][:], in0=pO[:], scalar=rstd, in1=gpost[:], op0=ALU.mult, op1=ALU.mult),
                reads=[b_pO, b_stat[si], b_W], writes=[b_hout[ib]])
            P.op("pool", lambda e: e.tensor_tensor(out=hout[ib][:], in0=hout[ib][:], in1=hin[n % 4][:], op=ALU.add),
                 reads=[b_hout[ib], b_hin[n % 4]], writes=[b_hout[ib]])
            P.op("pool", lambda e: e.dma_start(out=h2v[n], in_=hout[ib][:]),
                 reads=[b_hout[ib]], writes=[h2_b[n]], dma=True)

        s1(0)
        for n in range(32):
            if n + 1 < 32:
                s1(n + 1)
            s2(n)
        P.flush()


def ple_pass(P, nc, C, I, uT3, uT3_b, h3, h3_b, out):
    with contextlib.ExitStack() as st:
        sb = lambda name, shape, dt: st.enter_context(nc.sbuf_tensor("pl" + name, shape, dt))
        ps = lambda name, shape, dt: st.enter_context(nc.psum_tensor("pl" + name, shape, dt))
        Wg = sb("Wg", [128, 8, D], BF16)
        Wp = sb("Wp", [128, 2, D], BF16)
        b_W = P.buf("plW")
        P.op("pool", lambda e: e.dma_start(out=Wg[:], in_=I["ple_w_gate"].rearrange("(k p) d -> p k d", p=128)),
             writes=[b_W], dma=True)
        P.op("pool", lambda e: e.dma_start(out=Wp[:], in_=I["ple_w_proj"].rearrange("(k p) d -> p k d", p=128)),
             writes=[b_W], dma=True)
        gpost = sb("gpost", [128, D], F32)
        bg = sb("bg", [128, D], F32)
        P.op("sp", lambda e: e.dma_start(out=gpost[:], in_=I["ple_post_g"].partition_broadcast(128)),
             writes=[b_W], dma=True)
        P.op("sp", lambda e: e.dma_start(out=bg[:], in_=I["ple_b_gate"].partition_broadcast(128)),
             writes=[b_W], dma=True)
        uT = [sb("uT%d" % i, [128, 8, 128], BF16) for i in range(2)]
        pin = [sb("pin%d" % i, [128, 256], F32) for i in range(2)]
        hin = [sb("hin%d" % i, [128, D], F32) for i in range(4)]
        b_in = P.bufs_n("plin", 2)
        b_pin = P.bufs_n("plpin", 2)
        b_hin = P.bufs_n("plhin", 4)
        pb = [sb("pb%d" % i, [128, 256], BF16) for i in range(2)]
        pT = [sb("pT%d" % i, [128, 2, 128], BF16) for i in range(2)]
        gt = [sb("gt%d" % i, [128, D], F32) for i in range(2)]
        ge = [sb("ge%d" % i, [128, D], F32) for i in range(2)]
        junk = sb("junk", [128, D], BF16)
        hout = [sb("hout%d" % i, [128, D], F32) for i in range(2)]
        stat = sb("stat", [128, 6], F32)
        b_pb, b_pT, b_gt, b_ge = [P.bufs_n(n, 2) for n in ("pb", "pT", "gt", "ge")]
        b_junk = P.buf("pljunk")
        b_hout = P.bufs_n("plhout", 2)
        b_stat = P.bufs_n("plstat", 2)
        pG = [ps("pG%d" % i, [128, D], F32) for i in range(2)]
        pE = ps("pE", [128, D], F32)
        ptp = ps("ptp", [128, 2, 128], BF16)
        b_pG = P.bufs_n("pG", 2)
        b_pE, b_ptp = P.buf("pE"), P.buf("ptp")
        pv = I["p"].rearrange("(n p) d -> n p d", p=128)
        h3v = h3.rearrange("(n p) d -> n p d", p=128)
        ov = out.rearrange("(n p) d -> n p d", p=128)

        def s1(n):
            ib = n % 2
            cols = slice(n * 128, (n + 1) * 128)
            P.op("sp", lambda e: e.dma_start(out=uT[ib][:], in_=uT3[:, :, cols].rearrange("k p t -> p k t")),
                 reads=[uT3_b[n]], writes=[b_in[ib]], dma=True)
            P.op("sp", lambda e: e.dma_start(out=pin[ib][:], in_=pv[n]), writes=[b_pin[ib]], dma=True)
            P.op("sp", lambda e: e.dma_start(out=hin[n % 4][:], in_=h3v[n]), reads=[h3_b[n]],
                 writes=[b_hin[n % 4]], dma=True)
            P.op("act", lambda e: e.activation(out=pb[ib][:], in_=pin[ib][:], func=AF.Copy), reads=[b_pin[ib]],
                 writes=[b_pb[ib]])
            for k in range(2):
                P.op("pe", lambda e, k=k: e.transpose(out=ptp[:, k, :], in_=pb[ib][:, k * 128:(k + 1) * 128],
                                                      identity=C.ident[:]),
                     reads=[b_pb[ib], C.b_const], writes=[b_ptp])
            P.op("dve", lambda e: e.tensor_copy(out=pT[ib][:], in_=ptp[:]), reads=[b_ptp], writes=[b_pT[ib]])
            for hf in range(2):
                hs = slice(hf * 512, (hf + 1) * 512)
                for k in range(8):
                    P.op("pe", lambda e, k=k, hs=hs: e.matmul(pG[ib][:, hs], lhsT=uT[ib][:, k, :], rhs=Wg[:, k, hs],
                                                              start=(k == 0), stop=(k == 7)),
                         reads=[b_in[ib], b_W], writes=[b_pG[ib]])

        def s2(n):
            ib = n % 2
            for hf in range(2):
                hs = slice(hf * 512, (hf + 1) * 512)
                for k in range(2):
                    P.op("pe", lambda e, k=k, hs=hs: e.matmul(pE[:, hs], lhsT=pT[ib][:, k, :], rhs=Wp[:, k, hs],
                                                              start=(k == 0), stop=(k == 1)),
                         reads=[b_pT[ib], b_W], writes=[b_pE])
            P.op("dve", lambda e: e.tensor_tensor(out=gt[ib][:], in0=pG[ib][:], in1=bg[:], op=ALU.add),
                 reads=[b_pG[ib], b_W], writes=[b_gt[ib]])
            P.op("act", lambda e: e.activation(out=gt[ib][:], in_=gt[ib][:], func=AF.Sigmoid), reads=[b_gt[ib]],
                 writes=[b_gt[ib]])
            P.op("dve", lambda e: e.tensor_tensor(out=ge[ib][:], in0=gt[ib][:], in1=pE[:], op=ALU.mult),
                 reads=[b_gt[ib], b_pE], writes=[b_ge[ib]])
            si = n % 2
            ss, var, rstd = (stat[:, 3 * si + j:3 * si + j + 1] for j in range(3))
            rms_stats(P, C, ge[ib][:], b_ge[ib], junk[:], b_junk, ss, var, rstd, b_stat[si])
            P.op("dve", lambda e: e.scalar_tensor_tensor(
                out=hout[ib][:], in0=ge[ib][:], scalar=rstd, in1=gpost[:], op0=ALU.mult, op1=ALU.mult),
                reads=[b_ge[ib], b_stat[si], b_W], writes=[b_hout[ib]])
            P.op("pool", lambda e: e.tensor_tensor(out=hout[ib][:], in0=hout[ib][:], in1=hin[n % 4][:], op=ALU.add),
                 reads=[b_hout[ib], b_hin[n % 4]], writes=[b_hout[ib]])
            P.op("pool", lambda e: e.dma_start(out=ov[n], in_=hout[ib][:]), reads=[b_hout[ib]], dma=True)

        s1(0)
        for n in range(32):
            if n + 1 < 32:
                s1(n + 1)
            s2(n)
        P.flush()


def build_program(debug=False, stage=99, only=None):
    nc = bass.Bass("TRN2", target_bir_lowering=False)
    I = {}

    def din(name, shape):
        I[name] = nc.dram_tensor(name, shape, F32, kind="ExternalInput").ap()
        return I[name]

    din("x", [S, D])
    din("p", [S, 256])
    for nm in ("ffn1", "ffn2"):
        din(nm + "_pre_g", [1, D])
        din(nm + "_w_gate", [D, DFF])
        din(nm + "_w_up", [D, DFF])
        din(nm + "_w_down", [DFF, D])
        din(nm + "_post_g", [1, D])
    din("mix_pre_g", [1, D])
    din("w_in", [D, INW])
    din("conv_w", [4, 512])
    din("conv_b", [1, 512])
    din("mlstm_i_bias", [1, 4])
    din("mlstm_f_bias", [1, 4])
    din("mlstm_norm_g", [1, 512])
    din("fox_f_bias", [1, 8])
    din("branch_gate_bias", [1, 2048])
    din("w_branch_a", [512, D])
    din("w_branch_b", [512, D])
    din("w_out", [D, D])
    din("mix_post_g", [1, D])
    din("ple_pre_g", [1, D])
    din("ple_w_gate", [D, D])
    din("ple_b_gate", [1, D])
    din("ple_w_proj", [256, D])
    din("ple_post_g", [1, D])

    skind = "ExternalOutput" if debug else "Internal"

    def dscr(name, shape, dt):
        return nc.dram_tensor(name, shape, dt, kind=skind).ap()

    out = nc.dram_tensor("out", [S, D], F32, kind="ExternalOutput").ap()
    h1 = dscr("h1", [S, D], F32)
    uT1 = dscr("uT1", [8, 128, S], BF16)
    gpre = dscr("gpre", [72, S], F32)
    SC = {
        "mqkT": dscr("mqkT", [4, 128, S], BF16),
        "vaugM": dscr("vaugM", [S, 4, 129], BF16),
        "so": dscr("so", [S, 512], BF16),
        "qaug": dscr("qaug", [8, 70, S], BF16),
        "kaug": dscr("kaug", [8, 70, S], BF16),
        "vaugF": dscr("vaugF", [S, 8, 65], BF16),
        "ga": dscr("ga", [S, D], BF16),
        "gb": dscr("gb", [S, D], BF16),
        "yaT": dscr("yaT", [4, 128, S], BF16),
        "ybT": dscr("ybT", [4, 128, S], BF16),
        "recd": dscr("recd", [64, 512], F32),
    }
    h2 = dscr("h2", [S, D], F32)
    h3 = dscr("h3", [S, D], F32)
    uT3 = dscr("uT3", [8, 128, S], BF16)

    with contextlib.ExitStack() as st:
        P = Prog(nc, st)
        C = Ctx()
        C.db = {}

        def db(name):
            if name not in C.db:
                C.db[name] = P.bufs_n("D" + name, 32)
            return C.db[name]

        setup_consts(P, nc, st, C)
        C.wthr = st.enter_context(nc.sbuf_tensor("wthr", [128, 32, 8], F32))
        C.decbc = st.enter_context(nc.sbuf_tensor("decbc", [128, 4, 32], F32))
        C.b_wthr = P.buf("wthr")
        C.b_decbc = P.buf("decbc")
        def want(name, st_no):
            return (name in only) if only is not None else (stage >= st_no)

        if want("ffn1", 1):
            ffn_pass(P, nc, C, "f1", I["x"], I["ffn1_w_gate"], I["ffn1_w_up"], I["ffn1_w_down"],
                     I["ffn1_pre_g"], I["ffn1_post_g"], h1, I["mix_pre_g"], uT1,
                     db("x"), db("h1"), db("uT1"), gate_w=I["w_in"], gate_dst=gpre, gate_b=db("gpre"))
        with contextlib.ExitStack() as stW:
            if want("win", 2):
                Wt = stW.enter_context(nc.sbuf_tensor("wiW", [128, 8, INW], BF16))
                b_Wt = P.bufs_n("Win", 8)
                wv = I["w_in"].rearrange("(kc p) n -> kc p n", p=128)
                for k in range(8):
                    P.op("pool", lambda e, k=k: e.dma_start(out=Wt[:, k, :], in_=wv[k], max_dma_last_dim=4 * 1284),
                         writes=[b_Wt[k]], dma=True)
            if want("gp", 2):
                aug_b = P.buf("augrows")
                gp_stage(P, nc, C, I, gpre, db("gpre"), SC["qaug"], SC["kaug"], aug_b)
                db("aug").append(aug_b)
            if want("win", 2):
                win_pass(P, nc, C, I, uT1, db("uT1"), SC, db, (Wt, b_Wt))
        if want("mix", 3):
            mix_pass(P, nc, C, I, SC, db)
        if want("merge", 4):
            merge_pass(P, nc, C, I, SC, db, h1, db("h1"), h2, db("h2"))
        if want("ffn2", 5):
            ffn_pass(P, nc, C, "f2", h2, I["ffn2_w_gate"], I["ffn2_w_up"], I["ffn2_w_down"],
                     I["ffn2_pre_g"], I["ffn2_post_g"], h3, I["ple_pre_g"], uT3,
                     db("h2"), db("h3"), db("uT3"))
        if want("ple", 6):
            ple_pass(P, nc, C, I, uT3, db("uT3"), h3, db("h3"), out)
        P.flush(final=True)
    return nc

IN_NAMES = ["x", "p", "ffn1_pre_g", "ffn1_w_gate", "ffn1_w_up", "ffn1_w_down", "ffn1_post_g",
            "mix_pre_g", "w_in", "conv_w", "conv_b", "mlstm_i_bias", "mlstm_f_bias", "mlstm_norm_g",
            "fox_f_bias", "branch_gate_bias", "w_branch_a", "w_branch_b", "w_out", "mix_post_g",
            "ffn2_pre_g", "ffn2_w_gate", "ffn2_w_up", "ffn2_w_down", "ffn2_post_g",
            "ple_pre_g", "ple_w_gate", "ple_b_gate", "ple_w_proj", "ple_post_g"]


def make_in_maps(inputs, cores):
    maps = []
    shared = {}
    for k in IN_NAMES:
        if k in ("x", "p"):
            continue
        shared[k] = np.ascontiguousarray(np.asarray(inputs[k])[0], dtype=np.float32)
    x = np.asarray(inputs["x"])
    p = np.asarray(inputs["p"])
    for b in cores:
        m = dict(shared)
        m["x"] = np.ascontiguousarray(x[b], dtype=np.float32)
        m["p"] = np.ascontiguousarray(p[0, b], dtype=np.float32)
        maps.append(m)
    return maps


def kernel(**inputs):
    nc = build_program()
    maps = make_in_maps(inputs, list(range(8)))
    res = run_bass_kernel_spmd(nc, maps, core_ids=list(range(8)))
    return np.stack([np.asarray(r["out"], dtype=np.float32) for r in res.results], axis=0)
```
